# Optimizing a Trainium2 kernel written in Bass

```python
import jax
import jax.numpy as jnp
from jax import lax
import numpy as np

D_MODEL = 1024
BATCH = 1
SEQ = 16384
DEPTH = 4

HEAD_DIM = 64
RWKV_HEADS = (3 * D_MODEL) // (8 * HEAD_DIM)
DSA_HEADS = D_MODEL // (4 * HEAD_DIM)
SWA_HEADS = D_MODEL // HEAD_DIM - RWKV_HEADS - DSA_HEADS
SWA_KV_HEADS = 2
RWKV_W = RWKV_HEADS * HEAD_DIM
DSA_W = DSA_HEADS * HEAD_DIM
SWA_W = SWA_HEADS * HEAD_DIM
DECAY_LORA = 64
AAA_LORA = 64
GATE_LORA = 128
GN_EPS = 64e-5
KV_LORA = 128
IDX_HEADS = 4
IDX_DIM = 64
TOPK_MAX = 256
Q_BLOCK = 128
WINDOW = 128
N_EXPERTS = 64
TOP_K = 8
N_GROUPS = 8
TOPK_GROUPS = 4
D_EXPERT = 256
ROUTED_SCALE = 2.5
MOE_BLOCK = 128
ALPHA = (2 * DEPTH) ** 0.25
BETA = (8 * DEPTH) ** -0.25
LN_EPS = 1e-5
NEG = -1e30

RWKV_SIZES = (RWKV_W, RWKV_W, RWKV_W, DECAY_LORA, AAA_LORA, GATE_LORA)
DSA_SIZES = (DSA_W, KV_LORA, IDX_HEADS * IDX_DIM, IDX_DIM, IDX_HEADS)
SWA_SIZES = (SWA_W, SWA_KV_HEADS * HEAD_DIM, SWA_KV_HEADS * HEAD_DIM)
N_RWKV_COLS = sum(RWKV_SIZES)
N_DSA_COLS = sum(DSA_SIZES)
N_SWA_COLS = sum(SWA_SIZES)
P_IN = N_RWKV_COLS + N_DSA_COLS + N_SWA_COLS

kernel_name = 'hybrid_rwkv7_dsa_swa_moe_deepnorm'


def split_cols(t, sizes):
    return jnp.split(t, [int(s) for s in np.cumsum(sizes)[:-1]], axis=-1)


def layer_norm(x, g, b):
    xf = x.astype(jnp.float32)
    mu = xf.mean(-1, keepdims=True)
    var = jnp.square(xf - mu).mean(-1, keepdims=True)
    return ((xf - mu) * lax.rsqrt(var + LN_EPS)).astype(x.dtype) * g + b


def rms_norm(x, g):
    xf = x.astype(jnp.float32)
    return (xf * lax.rsqrt(jnp.mean(xf * xf, -1, keepdims=True) + 1e-6)).astype(x.dtype) * g


def token_shift(y):
    return jnp.pad(y, ((0, 0), (1, 0), (0, 0)))[:, :-1]


def alibi_slopes(n):
    return 2.0 ** (-8.0 * (jnp.arange(n, dtype=jnp.float32) + 1.0) / n)


def swiglu(t, w1, w3, w2):
    return (jax.nn.silu(t @ w1) * (t @ w3)) @ w2


def rwkv7_mix(cols, mu, w0, w2, a0, a2, g2, k_k, k_a, r_k, ln_g, ln_b):
    B, L, _ = cols.shape
    f32 = jnp.float32
    cols = cols + (token_shift(cols) - cols) * mu
    r, k, v, wl, al, gl = split_cols(cols, RWKV_SIZES)
    w = -jax.nn.softplus(-(w0 + jnp.tanh(wl) @ w2)) - 0.5
    a = jax.nn.sigmoid(a0 + al @ a2)
    g = jax.nn.sigmoid(gl) @ g2
    heads = lambda t: t.reshape(B, L, RWKV_HEADS, HEAD_DIM)
    kk = heads(k * k_k).astype(f32)
    kk = kk / jnp.maximum(jnp.sqrt(jnp.sum(kk * kk, -1, keepdims=True)), 1e-12)
    k = k * (1 + (a - 1) * k_a)
    rh, kh, vh, ah = heads(r), heads(k), heads(v), heads(a)
    decay = jnp.exp(-jnp.exp(heads(w).astype(f32)))
    tmaj = lambda t: jnp.moveaxis(t.astype(f32), 1, 0)

    def step(S, inp):
        r_t, w_t, k_t, v_t, kk_t, a_t = inp
        sa = jnp.einsum('bhvk,bhk->bhv', S, -kk_t)
        S = (S * w_t[:, :, None, :] + sa[..., None] * (kk_t * a_t)[:, :, None, :]
             + v_t[..., None] * k_t[:, :, None, :])
        return S, jnp.einsum('bhvk,bhk->bhv', S, r_t)

    S0 = jnp.zeros((B, RWKV_HEADS, HEAD_DIM, HEAD_DIM), f32)
    _, o = lax.scan(step, S0, (tmaj(rh), tmaj(decay), tmaj(kh), tmaj(vh), tmaj(kk), tmaj(ah)))
    o = jnp.moveaxis(o, 0, 1)
    mean = o.mean(-1, keepdims=True)
    var = jnp.square(o - mean).mean(-1, keepdims=True)
    o = ((o - mean) * lax.rsqrt(var + GN_EPS)).reshape(B, L, RWKV_W).astype(cols.dtype) * ln_g + ln_b
    bonus = jnp.sum(rh * kh * r_k, -1, keepdims=True) * vh
    return (o + bonus.reshape(B, L, RWKV_W)) * g


def dsa_mix(cols, kv_norm, w_uk, w_uv, ik_g, ik_b, slopes):
    B, L, _ = cols.shape
    f32 = jnp.float32
    topk = min(TOPK_MAX, L // 4)
    q, c_kv, iq, ik, iw = split_cols(cols, DSA_SIZES)
    c_kv = rms_norm(c_kv, kv_norm)
    q = q.reshape(B, L, DSA_HEADS, HEAD_DIM)
    q_lat = jnp.einsum('blhd,hdr->blhr', q, w_uk) * HEAD_DIM ** -0.5
    iq = iq.reshape(B, L, IDX_HEADS, IDX_DIM).astype(f32)
    ik = layer_norm(ik, ik_g, ik_b).astype(f32)
    iw = iw.astype(f32) * (IDX_HEADS ** -0.5 * IDX_DIM ** -0.5)
    nb = L // Q_BLOCK
    blk = lambda t: jnp.moveaxis(t.reshape(B, nb, Q_BLOCK, *t.shape[2:]), 1, 0)
    key_pos = jnp.arange(L)
    gather = jax.vmap(lambda c, i: c[i])

    def one_block(args):
        q_b, iq_b, iw_b, pos_b = args
        s = jnp.einsum('bqhd,bkd->bqhk', iq_b, ik)
        score = jnp.einsum('bqhk,bqh->bqk', jax.nn.relu(s), iw_b)
        causal = key_pos[None, :] <= pos_b[:, None]
        score = jnp.where(causal[None], score, NEG)
        _, idx = lax.top_k(score, topk)
        c_sel = gather(c_kv, idx)
        valid = idx <= pos_b[None, :, None]
        dist = (pos_b[None, :, None] - idx).astype(f32)
        logits = jnp.einsum('bqhr,bqkr->bhqk', q_b, c_sel).astype(f32)
        logits = logits - slopes[:, None, None] * dist[:, None]
        logits = jnp.where(valid[:, None], logits, NEG)
        p = jax.nn.softmax(logits, axis=-1).astype(c_sel.dtype)
        return jnp.einsum('bhqk,bqkr->bqhr', p, c_sel)

    pos = jnp.arange(L).reshape(nb, Q_BLOCK)
    o_lat = lax.map(one_block, (blk(q_lat), blk(iq), blk(iw), pos))
    o_lat = jnp.moveaxis(o_lat, 0, 1).reshape(B, L, DSA_HEADS, KV_LORA)
    o = jnp.einsum('blhr,hrd->blhd', o_lat, w_uv)
    return o.reshape(B, L, DSA_W)


def swa_mix(cols, sinks, slopes):
    B, L, _ = cols.shape
    f32 = jnp.float32
    nb = L // WINDOW
    G = SWA_HEADS // SWA_KV_HEADS
    q, k, v = split_cols(cols, SWA_SIZES)
    q = q.reshape(B, nb, WINDOW, SWA_KV_HEADS, G, HEAD_DIM)
    k = k.reshape(B, nb, WINDOW, SWA_KV_HEADS, HEAD_DIM)
    v = v.reshape(B, nb, WINDOW, SWA_KV_HEADS, HEAD_DIM)
    prev = lambda t: jnp.pad(t, ((0, 0), (1, 0), (0, 0), (0, 0), (0, 0)))[:, :-1]
    k2 = jnp.concatenate([prev(k), k], axis=2)
    v2 = jnp.concatenate([prev(v), v], axis=2)
    s = jnp.einsum('bnqgrd,bnkgd->bngrqk', q, k2).astype(f32) * HEAD_DIM ** -0.5
    qi = jnp.arange(WINDOW)
    kj = jnp.arange(2 * WINDOW)
    dist = qi[:, None] + WINDOW - kj[None, :]
    key_abs = jnp.arange(nb)[:, None] * WINDOW - WINDOW + kj[None, :]
    valid = ((dist >= 0) & (dist < WINDOW))[None] & (key_abs >= 0)[:, None, :]
    s = s - slopes.reshape(SWA_KV_HEADS, G)[:, :, None, None] * dist.astype(f32)
    s = jnp.where(valid[None, :, None, None], s, NEG)
    sink = sinks.astype(f32).reshape(SWA_KV_HEADS, G)[None, None, :, :, None, None]
    m = jnp.maximum(s.max(-1, keepdims=True), sink)
    e = jnp.exp(s - m)
    p = e / (e.sum(-1, keepdims=True) + jnp.exp(sink - m))
    o = jnp.einsum('bngrqk,bnkgd->bnqgrd', p.astype(v2.dtype), v2)
    return o.reshape(B, L, SWA_W)


def grouped_experts(t, idx, gate, w1, w3, w2):
    T, D = t.shape
    A = T * TOP_K
    n_blocks = -(-A // MOE_BLOCK) + N_EXPERTS
    cap = n_blocks * MOE_BLOCK
    flat_e = idx.reshape(A)
    order = jnp.argsort(flat_e)
    e_sorted = flat_e[order]
    tok_sorted = (order // TOP_K).astype(jnp.int32)
    gate_sorted = gate.reshape(A)[order]
    counts = jnp.bincount(flat_e, length=N_EXPERTS)
    padded = (counts + MOE_BLOCK - 1) // MOE_BLOCK * MOE_BLOCK
    pad_end = jnp.cumsum(padded)
    pad_start = pad_end - padded
    start = jnp.cumsum(counts) - counts
    slot = pad_start[e_sorted] + jnp.arange(A) - start[e_sorted]
    slot_tok = jnp.full((cap,), T, jnp.int32).at[slot].set(tok_sorted)
    slot_gate = jnp.zeros((cap,), t.dtype).at[slot].set(gate_sorted)
    block_e = jnp.minimum(jnp.searchsorted(pad_end, jnp.arange(n_blocks) * MOE_BLOCK, side='right'), N_EXPERTS - 1)
    t_pad = jnp.concatenate([t, jnp.zeros((1, D), t.dtype)], axis=0)

    def one_block(args):
        tok_b, g_b, e = args
        xb = t_pad[tok_b]
        return swiglu(xb, w1[e], w3[e], w2[e]) * g_b[:, None]

    ys = lax.map(one_block, (slot_tok.reshape(n_blocks, MOE_BLOCK), slot_gate.reshape(n_blocks, MOE_BLOCK), block_e))
    return jnp.zeros((T + 1, D), t.dtype).at[slot_tok].add(ys.reshape(cap, D))[:T]


def moe_ffn(h, router_w, router_bias, w1, w3, w2, sw1, sw3, sw2):
    B, L, D = h.shape
    T = B * L
    t = h.reshape(T, D)
    scores = jax.nn.sigmoid((t @ router_w).astype(jnp.float32))
    sel = scores + router_bias
    grp_score = lax.top_k(sel.reshape(T, N_GROUPS, N_EXPERTS // N_GROUPS), 2)[0].sum(-1)
    _, top_g = lax.top_k(grp_score, TOPK_GROUPS)
    gmask = jax.nn.one_hot(top_g, N_GROUPS, dtype=jnp.float32).sum(1) > 0
    sel = jnp.where(jnp.repeat(gmask, N_EXPERTS // N_GROUPS, axis=1), sel, NEG)
    _, idx = lax.top_k(sel, TOP_K)
    gate = jnp.take_along_axis(scores, idx, axis=1)
    gate = (gate / gate.sum(-1, keepdims=True) * ROUTED_SCALE).astype(t.dtype)
    out = grouped_experts(t, idx, gate, w1, w3, w2) + swiglu(t, sw1, sw3, sw2)
    return out.reshape(B, L, D)


def setup_inputs(seed: int = 0) -> dict:
    key = jax.random.key(seed)
    ks = iter(jax.random.split(key, 64))
    nrm = lambda shape, s: jax.random.normal(next(ks), shape, jnp.float32) * s
    uni = lambda shape, lo, hi: jax.random.uniform(next(ks), shape, jnp.float32, lo, hi)
    Lr, D = DEPTH, D_MODEL
    return {
        'x': nrm((BATCH, SEQ, D), 1.0),
        'c': nrm((BATCH, D), 1.0),
        'w_mod': nrm((Lr, D, 6 * D), 0.5 * D ** -0.5),
        'b_mod': nrm((Lr, 6 * D), 0.01),
        'w_in': nrm((Lr, D, P_IN), D ** -0.5),
        'rwkv_mu': uni((Lr, N_RWKV_COLS), 0.0, 1.0),
        'rwkv_w0': uni((Lr, RWKV_W), -6.0, 0.0),
        'rwkv_w2': nrm((Lr, DECAY_LORA, RWKV_W), 0.5 * DECAY_LORA ** -0.5),
        'rwkv_a0': nrm((Lr, RWKV_W), 0.1),
        'rwkv_a2': nrm((Lr, AAA_LORA, RWKV_W), AAA_LORA ** -0.5),
        'rwkv_g2': nrm((Lr, GATE_LORA, RWKV_W), GATE_LORA ** -0.5),
        'rwkv_k_k': 0.85 + nrm((Lr, RWKV_W), 0.05),
        'rwkv_k_a': 1.0 + nrm((Lr, RWKV_W), 0.05),
        'rwkv_r_k': nrm((Lr, RWKV_HEADS, HEAD_DIM), 0.1),
        'rwkv_ln_g': 1.0 + nrm((Lr, RWKV_W), 0.02),
        'rwkv_ln_b': nrm((Lr, RWKV_W), 0.02),
        'dsa_kv_norm': 1.0 + nrm((Lr, KV_LORA), 0.02),
        'dsa_w_uk': nrm((Lr, DSA_HEADS, HEAD_DIM, KV_LORA), HEAD_DIM ** -0.5),
        'dsa_w_uv': nrm((Lr, DSA_HEADS, KV_LORA, HEAD_DIM), KV_LORA ** -0.5),
        'dsa_ik_g': 1.0 + nrm((Lr, IDX_DIM), 0.02),
        'dsa_ik_b': nrm((Lr, IDX_DIM), 0.02),
        'swa_sinks': nrm((Lr, SWA_HEADS), 1.0),
        'w_out': nrm((Lr, D, D), BETA * D ** -0.5),
        'ln_mix_g': 1.0 + nrm((Lr, D), 0.02),
        'ln_mix_b': nrm((Lr, D), 0.02),
        'router_w': nrm((Lr, D, N_EXPERTS), D ** -0.5),
        'router_bias': nrm((Lr, N_EXPERTS), 0.01),
        'exp_w1': nrm((Lr, N_EXPERTS, D, D_EXPERT), D ** -0.5),
        'exp_w3': nrm((Lr, N_EXPERTS, D, D_EXPERT), D ** -0.5),
        'exp_w2': nrm((Lr, N_EXPERTS, D_EXPERT, D), BETA * D_EXPERT ** -0.5),
        'sh_w1': nrm((Lr, D, D_EXPERT), D ** -0.5),
        'sh_w3': nrm((Lr, D, D_EXPERT), D ** -0.5),
        'sh_w2': nrm((Lr, D_EXPERT, D), BETA * D_EXPERT ** -0.5),
        'ln_ffn_g': 1.0 + nrm((Lr, D), 0.02),
        'ln_ffn_b': nrm((Lr, D), 0.02),
    }


def reference(x, c, w_mod, b_mod, w_in, rwkv_mu, rwkv_w0, rwkv_w2, rwkv_a0, rwkv_a2, rwkv_g2,
              rwkv_k_k, rwkv_k_a, rwkv_r_k, rwkv_ln_g, rwkv_ln_b, dsa_kv_norm, dsa_w_uk, dsa_w_uv,
              dsa_ik_g, dsa_ik_b, swa_sinks, w_out, ln_mix_g, ln_mix_b, router_w, router_bias,
              exp_w1, exp_w3, exp_w2, sh_w1, sh_w3, sh_w2, ln_ffn_g, ln_ffn_b):
    slopes = alibi_slopes(SWA_HEADS + DSA_HEADS)
    swa_slopes = slopes[:SWA_HEADS]
    dsa_slopes = slopes[SWA_HEADS:]
    cond = jax.nn.silu(c)
    for l in range(DEPTH):
        mod = cond @ w_mod[l] + b_mod[l]
        sh1, sc1, g1, sh2, sc2, g2 = [m[:, None, :] for m in jnp.split(mod, 6, axis=-1)]
        h = x * (1 + sc1) + sh1
        proj = h @ w_in[l]
        p_rwkv, p_dsa, p_swa = split_cols(proj, (N_RWKV_COLS, N_DSA_COLS, N_SWA_COLS))
        mixed = jnp.concatenate([
            rwkv7_mix(p_rwkv, rwkv_mu[l], rwkv_w0[l], rwkv_w2[l], rwkv_a0[l], rwkv_a2[l], rwkv_g2[l],
                      rwkv_k_k[l], rwkv_k_a[l], rwkv_r_k[l], rwkv_ln_g[l], rwkv_ln_b[l]),
            dsa_mix(p_dsa, dsa_kv_norm[l], dsa_w_uk[l], dsa_w_uv[l], dsa_ik_g[l], dsa_ik_b[l], dsa_slopes),
            swa_mix(p_swa, swa_sinks[l], swa_slopes),
        ], axis=-1)
        x = layer_norm(ALPHA * x + g1 * (mixed @ w_out[l]), ln_mix_g[l], ln_mix_b[l])
        h = x * (1 + sc2) + sh2
        y = moe_ffn(h, router_w[l], router_bias[l], exp_w1[l], exp_w3[l], exp_w2[l],
                    sh_w1[l], sh_w3[l], sh_w2[l])
        x = layer_norm(ALPHA * x + g2 * y, ln_ffn_g[l], ln_ffn_b[l])
    return x
```

```python
import numpy as np
import ml_dtypes


from contextlib import ExitStack
import concourse.bass as bass
import concourse.mybir as mybir
from concourse.bass_utils import run_bass_kernel_spmd

F32 = mybir.dt.float32
BF16 = mybir.dt.bfloat16
AF = mybir.ActivationFunctionType
ALU = mybir.AluOpType
AX = mybir.AxisListType


SKIP_SELF = False


class TB:
    __slots__ = ("name", "w", "r")

    def __init__(self, name="?"):
        self.name = name
        self.w = None
        self.r = {}


class Prog:
    ENGS = ("pe", "dve", "act", "pool", "sp")

    def __init__(self, nc, es, ring=8):
        self.nc = nc
        self.es = es
        self.ops = {e: [] for e in self.ENGS}
        self.cnt = {e: 0 for e in self.ENGS}
        self.waited = {e: {} for e in self.ENGS}
        self.sems = {}
        for e in self.ENGS:
            self.sems["c_" + e] = es.enter_context(nc.semaphore("c_" + e))
        self.ring = {}
        for e in ("sp", "pool", "act"):
            names = [f"d_{e}{i}" for i in range(ring)]
            for n in names:
                self.sems[n] = es.enter_context(nc.semaphore(n))
            self.ring[e] = {"names": names, "uses": [0] * ring, "next": 0}
        self.nbuf = 0

    def tb(self, name=None):
        self.nbuf += 1
        return TB(name or f"b{self.nbuf}")

    def sbuf(self, name, shape, dtype):
        return self.es.enter_context(self.nc.sbuf_tensor("sb_" + name, list(shape), dtype))

    def psum(self, name, shape, dtype=F32):
        return self.es.enter_context(self.nc.psum_tensor("ps_" + name, list(shape), dtype))

    def _need(self, eng, evs):
        need = {}
        for ev in evs:
            if ev is None:
                continue
            k, v = ev
            if need.get(k, 0) < v:
                need[k] = v
        w = self.waited[eng]
        for k, v in need.items():
            if SKIP_SELF and k == "c_" + eng:
                continue
            if w.get(k, 0) < v:
                self.ops[eng].append(("wait", k, v))
                w[k] = v

    def op(self, eng, fn, reads=(), writes=(), dma=False, acc=False):
        evs = []
        for b in reads:
            evs.append(b.w)
        for b in writes:
            if not (acc and eng == "pe" and b.w is not None and b.w[0] == "c_pe"):
                evs.append(b.w)
            for k, v in b.r.items():
                evs.append((k, v))
        if dma:
            rg = self.ring[eng]
            i = rg["next"]
            rg["next"] = (i + 1) % len(rg["names"])
            k = rg["names"][i]
            evs.append((k, 16 * rg["uses"][i]))
            rg["uses"][i] += 1
            ev = (k, 16 * rg["uses"][i])
            inc = 16
        else:
            self.cnt[eng] += 1
            k = "c_" + eng
            ev = (k, self.cnt[eng])
            inc = 1
        self._need(eng, evs)
        self.ops[eng].append(("op", fn, k, inc))
        for b in reads:
            if b.r.get(ev[0], 0) < ev[1]:
                b.r[ev[0]] = ev[1]
        for b in writes:
            b.w = ev
            b.r = {}
        return ev

    def barrier(self):
        evs = [("c_" + x, self.cnt[x]) for x in self.ENGS]
        for e, rg in self.ring.items():
            for n, u in zip(rg["names"], rg["uses"]):
                evs.append((n, 16 * u))
        for e in self.ENGS:
            self._need(e, evs)

    def scope(self):
        prog = self

        class _Scope:
            def __enter__(self_):
                self_.old = prog.es
                self_.st = ExitStack()
                self_.st.__enter__()
                prog.es = self_.st
                return prog

            def __exit__(self_, *a):
                prog.barrier()
                prog.es = self_.old
                return self_.st.__exit__(*a)
        return _Scope()

    def finish(self, out_bufs):
        self._need("sp", [b.w for b in out_bufs])
        nc = self.nc
        sems = self.sems
        ops = self.ops

        def replay(engobj, lst):
            for it in lst:
                if it[0] == "wait":
                    engobj.wait_ge(sems[it[1]], it[2])
                else:
                    ins = it[1](engobj)
                    ins.then_inc(sems[it[2]], it[3])

        with nc.Block() as block:
            @block.tensor
            def _(e):
                replay(e, ops["pe"])

            @block.vector
            def _(e):
                replay(e, ops["dve"])

            @block.scalar
            def _(e):
                replay(e, ops["act"])

            @block.gpsimd
            def _(e):
                replay(e, ops["pool"])

            @block.sync
            def _(e):
                replay(e, ops["sp"])


D = 1024
ALPHA = (2 * 4) ** 0.25
LN_EPS = 1e-5
NE = 64
DE = 256


def bcast_last(ap, n):
    shp = list(ap.shape)
    return ap.unsqueeze(len(shp)).to_broadcast(shp + [n])


class Consts:
    pass


def emit_layernorm(P, z, z_tb, out, out_tb, gb, bb, par_tb, sc, eps_t, tagn):
    st, mv, rs, xn = sc["st"], sc["mv"], sc["rs"], sc["xn"]
    stb = sc["tb"]
    P.op("dve", lambda e: e.bn_stats(out=st[:, 0, :], in_=z[:, 0:512]), reads=[z_tb], writes=[stb])
    P.op("dve", lambda e: e.bn_stats(out=st[:, 1, :], in_=z[:, 512:1024]), reads=[z_tb, stb], writes=[stb])
    P.op("dve", lambda e: e.bn_aggr(out=mv[:], in_=st[:].rearrange("p a b -> p (a b)")), reads=[stb], writes=[stb])
    P.op("act", lambda e: e.activation(out=rs[:], in_=mv[:, 1:2], func=AF.Sqrt, bias=eps_t[:, 0:1], scale=1.0),
         reads=[stb], writes=[stb])
    P.op("dve", lambda e: e.reciprocal(out=rs[:], in_=rs[:]), reads=[stb], writes=[stb])
    P.op("dve", lambda e: e.tensor_scalar(out=xn[:], in0=z, scalar1=mv[:, 0:1], scalar2=rs[:, 0:1],
                                          op0=ALU.subtract, op1=ALU.mult), reads=[z_tb, stb], writes=[sc["xn_tb"]])
    P.op("pool", lambda e: e.tensor_tensor(out=xn[:], in0=xn[:], in1=gb, op=ALU.mult),
         reads=[sc["xn_tb"]] + list(par_tb), writes=[sc["xn_tb"]])
    P.op("pool", lambda e: e.tensor_tensor(out=out, in0=xn[:], in1=bb, op=ALU.add),
         reads=[sc["xn_tb"]] + list(par_tb), writes=[out_tb])


NRW = 1408
C_DQ, C_CKV, C_IQ, C_IK, C_IW = 1408, 1664, 1792, 2048, 2112
C_SQ, C_SK, C_SV = 2116, 2500, 2628
PIN = 2756
DECAY_C = -0.6065306597126334


STOP = 99


class StopEmit(Exception):
    pass


def ck(n):
    if STOP == n:
        raise StopEmit()


class Slots:
    def __init__(self, P, aps, tbs=None):
        self.aps = aps
        self.tbs = tbs if tbs is not None else [P.tb() for _ in aps]
        self.i = 0

    def next(self):
        i = self.i
        self.i = (i + 1) % len(self.aps)
        return self.aps[i], self.tbs[i]


def emit_k1(P, nc, NBLK, dr):
    cst = P.sbuf("cst", [128, 5, 128], F32)
    cst_tb = P.tb("cst")
    P.op("sp", lambda e: e.dma_start(out=cst[:], in_=dr["cst"]), writes=[cst_tb], dma=True)
    ident, BO, LS, US, UI = (cst[:, i, :] for i in range(5))
    identb = P.sbuf("identb", [128, 128], BF16)
    P.op("dve", lambda e: e.tensor_copy(out=identb[:], in_=ident), reads=[cst_tb], writes=[cst_tb])
    par = P.sbuf("par", [128, 64], F32)
    par_tb = P.tb("par")
    P.op("sp", lambda e: e.dma_start(out=par[:], in_=dr["par"]), writes=[par_tb], dma=True)
    P.op("dve", lambda e: e.tensor_scalar(out=par[:, 37:45], in0=par[:, 37:45], scalar1=1.0, scalar2=None, op0=ALU.add),
         reads=[par_tb], writes=[par_tb])
    P.op("dve", lambda e: e.tensor_scalar(out=par[:, 23:26], in0=par[:, 20:23], scalar1=-1.0, scalar2=1.0, op0=ALU.mult, op1=ALU.add),
         reads=[par_tb], writes=[par_tb])
    bcp = P.sbuf("bcp", [128, 320], F32)
    P.op("sp", lambda e: e.dma_start(out=bcp[:], in_=dr["bcp"]), writes=[par_tb], dma=True)
    eps6 = P.sbuf("eps6", [128, 2], F32)
    P.op("pool", lambda e: e.memset(eps6[:, 0:1], 1e-6), writes=[par_tb])
    P.op("pool", lambda e: e.memset(eps6[:, 1:2], 1e-5), writes=[par_tb])
    lora = P.sbuf("lora", [128, 384], F32)
    g2w = P.sbuf("g2w", [128, 384], F32)
    P.op("sp", lambda e: e.dma_start(out=lora[:], in_=dr["lora"]), writes=[par_tb], dma=True)
    P.op("sp", lambda e: e.dma_start(out=g2w[:], in_=dr["g2w"]), writes=[par_tb], dma=True)
    wuk_f = P.sbuf("wuk_f", [128, 2, 128], F32)
    wuk = P.sbuf("wuk", [128, 2, 128], BF16)
    P.op("sp", lambda e: e.dma_start(out=wuk_f[:], in_=dr["wuk"]), writes=[par_tb], dma=True)
    P.op("dve", lambda e: e.tensor_copy(out=wuk[:], in_=wuk_f[:]), reads=[par_tb], writes=[par_tb])
    win = P.sbuf("win", [128, 8, PIN], BF16)
    win_tb = P.tb("win")
    wst = [P.sbuf(f"wst{i}", [128, PIN], F32) for i in range(2)]
    wst_tb = [P.tb() for _ in range(2)]
    for kc in range(8):
        s = kc % 2
        P.op("sp", lambda e, kc=kc, s=s: e.dma_start(out=wst[s][:], in_=dr["w_in"][kc * 128:(kc + 1) * 128, :]),
             writes=[wst_tb[s]], dma=True)
        P.op("pool", lambda e, kc=kc, s=s: e.tensor_copy(out=win[:, kc, :], in_=wst[s][:]), reads=[wst_tb[s]], writes=[win_tb])
    pbk = [P.psum(f"k1pb{i}", [128, 512], F32) for i in range(8)]
    bank_tb = [P.tb(f"bank{i}") for i in range(8)]
    q128 = Slots(P, [pbk[b][:, q * 128:(q + 1) * 128] for q in range(4) for b in (2, 3, 4, 5)],
                 [bank_tb[b] for q in range(4) for b in (2, 3, 4, 5)])
    h256 = Slots(P, [pbk[b][:, q * 256:(q + 1) * 256] for q in range(2) for b in (6, 7)],
                 [bank_tb[b] for q in range(2) for b in (6, 7)])
    pT_tb = [bank_tb[0], bank_tb[1]]
    xb = [P.sbuf(f"xb{i}", [128, D], F32) for i in range(2)]
    xb_tb = [P.tb(), P.tb()]
    hT = P.sbuf("hT", [128, 8, 128], BF16)
    hT_tb = P.tb("hT")
    hh = P.sbuf("hh", [128, 8, 1], BF16)
    pr = P.sbuf("pr", [128, 11, 129], F32)
    pr_tb = P.tb("pr")
    prev = P.sbuf("prevc", [128, 11, 1], F32)
    prev_tb = P.tb("prev")
    X = P.sbuf("X", [128, 11, 128], F32)
    X_tb = P.tb("X")
    dif = P.sbuf("dif", [128, 11, 128], F32)
    fm = {n: P.sbuf("fm_" + n, [128, 3, 128], F32) for n in
          ["lw", "a", "kk", "kp", "t1", "cum", "einc", "eexc", "einv", "eend", "at", "bh", "kh", "rt", "bc", "kc", "g", "bon", "beta"]}
    fa = P.tb("fmall")
    fm_tb = {n: fa for n in fm}
    LA = P.sbuf("LA", [128, 128], F32)
    SG = P.sbuf("SG", [128, 128], F32)
    gC = P.sbuf("gC", [128, 3], F32)
    GCe = P.sbuf("GCe", [128, 3], F32)
    ones = P.sbuf("ones128", [128, 128], F32)
    P.op("pool", lambda e: e.memset(ones[:], 1.0), writes=[par_tb])
    tokm = {n: P.sbuf("tok_" + n, [128, 384], F32) for n in ["at", "bc", "kc", "v"]}
    tok_tb = {n: P.tb("tok_" + n) for n in tokm}
    tk = P.sbuf("tk", [128, 580], F32)
    tk_tb = P.tb("tk")
    HW = [{"MX": P.sbuf(f"MX{i}", [128, 256], F32), "MT": P.sbuf(f"MT{i}", [128, 128], F32), "MKT": P.sbuf(f"MKT{i}", [128, 128], F32),
           "NBT": P.sbuf(f"NBT{i}", [128, 128], F32), "NKT": P.sbuf(f"NKT{i}", [128, 128], F32), "DG": P.sbuf(f"DG{i}", [128, 128], F32)} for i in range(2)]
    HW_tb = [{n: P.tb(f"{n}{i}") for n in ("MX", "MT", "MKT", "NBT", "NKT", "DG")} for i in range(2)]
    ATb = P.sbuf("ATb", [64, 6, 64], F32)
    Db = P.sbuf("Db", [64, 6, 64], F32)
    AD_tb = [P.tb() for _ in range(6)]
    PQ = P.sbuf("PQ", [64, 6, 128], F32)
    PTt = P.sbuf("PTt", [64, 6, 64], F32)
    PQ_tb = [P.tb() for _ in range(6)]
    ost = [P.sbuf(f"ost{i}", [128, 128], F32) for i in range(4)]
    ost_s = Slots(P, [o[:] for o in ost])
    obf = [P.sbuf(f"obf{i}", [128, 512], BF16) for i in range(4)]
    obf_s = Slots(P, [o[:] for o in obf])
    nsc = {"st": P.sbuf("n_st", [128, 6], F32), "mv": P.sbuf("n_mv", [128, 2], F32), "rs": P.sbuf("n_rs", [128, 2], F32),
           "aiw": P.sbuf("n_aiw", [128, 4], F32), "sg": P.sbuf("n_sg", [128, 4], F32), "t": P.sbuf("n_t", [128, 256], F32)}
    nsc_tb = P.tb("nsc")
    out_tb = dr["out_tb"]

    def out_dma(dst, src, rtb):
        P.op("sp", lambda e: e.dma_start(out=dst, in_=src), reads=[rtb], writes=[out_tb], dma=True)

    for hd in range(6):
        P.op("pool", lambda e, hd=hd: e.memset(PQ[:, hd, 64:128], 0.0), writes=[PQ_tb[hd]])
        P.op("pool", lambda e, hd=hd: e.tensor_copy(out=PQ[:, hd, 0:64], in_=ident[0:64, 0:64]), reads=[cst_tb], writes=[PQ_tb[hd]])
        P.op("pool", lambda e, hd=hd: e.tensor_copy(out=PTt[:, hd, :], in_=ident[0:64, 0:64]), reads=[cst_tb], writes=[PQ_tb[hd]])

    def emit_pq_out(b):
        for hd in range(6):
            out_dma(dr["o_PT"][b, hd], PTt[:, hd, :], PQ_tb[hd])
            out_dma(dr["o_Q"][b, hd], PQ[:, hd, 64:128], PQ_tb[hd])

    ck(1)
    P.op("sp", lambda e: e.dma_start(out=xb[1][0:1, :], in_=dr["xh"]), writes=[xb_tb[1]], dma=True)
    for kc in range(8):
        P.op("pe", lambda e, kc=kc: e.transpose(out=pbk[0][:, kc:kc + 1], in_=xb[1][0:1, kc * 128:(kc + 1) * 128],
                                                identity=ident[0:1, 0:1]), reads=[xb_tb[1], cst_tb], writes=[pT_tb[0]], acc=True)
    for kc in range(8):
        P.op("act", lambda e, kc=kc: e.activation(out=hh[:, kc, :], in_=pbk[0][:, kc:kc + 1], func=AF.Identity,
                                                  bias=par[:, 29 + kc:30 + kc], scale=par[:, 37 + kc:38 + kc]),
             reads=[pT_tb[0], par_tb], writes=[hT_tb])
    for c in range(11):
        pa, ptb = q128.next()
        for kc in range(8):
            P.op("pe", lambda e, c=c, kc=kc, pa=pa: e.matmul(pa[:, 0:1], lhsT=win[:, kc, c * 128:(c + 1) * 128], rhs=hh[:, kc, :],
                                                             start=(kc == 0), stop=(kc == 7)), reads=[win_tb, hT_tb], writes=[ptb], acc=True)
        P.op("dve", lambda e, c=c, pa=pa: e.tensor_scalar(out=prev[:, c, :], in0=pa[:, 0:1], scalar1=par[:, 45:46], scalar2=None, op0=ALU.mult),
             reads=[ptb, par_tb], writes=[prev_tb])

    ck(2)
    emit_pq_out(0)
    for b in range(NBLK):
        t0 = b * 128
        xi = b % 2
        P.op("sp", lambda e, xi=xi, t0=t0: e.dma_start(out=xb[xi][:], in_=dr["x"][t0:t0 + 128, :]), writes=[xb_tb[xi]], dma=True)
        for kc in range(8):
            bank = kc // 4
            P.op("pe", lambda e, kc=kc, bank=bank, xi=xi: e.transpose(out=pbk[bank][:, (kc % 4) * 128:(kc % 4 + 1) * 128],
                                                                      in_=xb[xi][:, kc * 128:(kc + 1) * 128], identity=ident),
                 reads=[xb_tb[xi], cst_tb], writes=[pT_tb[bank]], acc=True)
        for kc in range(8):
            bank = kc // 4
            P.op("act", lambda e, kc=kc, bank=bank: e.activation(out=hT[:, kc, :], in_=pbk[bank][:, (kc % 4) * 128:(kc % 4 + 1) * 128],
                                                                 func=AF.Identity, bias=par[:, 29 + kc:30 + kc], scale=par[:, 37 + kc:38 + kc]),
                 reads=[pT_tb[bank], par_tb], writes=[hT_tb])

        ck(3)

        def projT(col0, evac):
            pa, ptb = q128.next()
            for kc in range(8):
                P.op("pe", lambda e, kc=kc, pa=pa: e.matmul(pa, lhsT=win[:, kc, col0:col0 + 128], rhs=hT[:, kc, :],
                                                            start=(kc == 0), stop=(kc == 7)), reads=[win_tb, hT_tb], writes=[ptb], acc=True)
            evac(pa, ptb)

        P.op("pool", lambda e: e.tensor_copy(out=pr[:, :, 0:1], in_=prev[:]), reads=[prev_tb], writes=[pr_tb])
        for c in range(11):
            projT(c * 128, lambda pa, ptb, c=c: P.op("act", lambda e: e.activation(out=pr[:, c, 1:129], in_=pa, func=AF.Identity),
                                                     reads=[ptb], writes=[pr_tb]))
        P.op("pool", lambda e: e.tensor_copy(out=prev[:], in_=pr[:, :, 128:129]), reads=[pr_tb], writes=[prev_tb])
        P.op("dve", lambda e: e.tensor_tensor(out=dif[:], in0=pr[:, :, 0:128], in1=pr[:, :, 1:129], op=ALU.subtract), reads=[pr_tb], writes=[X_tb])
        P.op("dve", lambda e: e.tensor_tensor(out=dif[:], in0=dif[:], in1=bcast_last(par[:, 0:11], 128), op=ALU.mult), reads=[X_tb, par_tb], writes=[X_tb])
        P.op("dve", lambda e: e.tensor_tensor(out=X[:], in0=dif[:], in1=pr[:, :, 1:129], op=ALU.add), reads=[X_tb, pr_tb], writes=[X_tb])
        ck(4)
        r_, k_, v_ = X[:, 0:3, :], X[:, 3:6, :], X[:, 6:9, :]
        P.op("act", lambda e: e.activation(out=LA[0:64, :], in_=X[0:64, 9, :], func=AF.Tanh), reads=[X_tb], writes=[fm_tb["lw"]])
        P.op("act", lambda e: e.activation(out=LA[64:128, :], in_=X[64:128, 9, :], func=AF.Identity), reads=[X_tb], writes=[fm_tb["lw"]])
        P.op("act", lambda e: e.activation(out=SG[:], in_=X[:, 10, :], func=AF.Sigmoid), reads=[X_tb], writes=[fm_tb["g"]])
        for cc in range(3):
            pa, ptb = q128.next()
            P.op("pe", lambda e, cc=cc, pa=pa: e.matmul(pa, lhsT=lora[0:64, cc * 128:(cc + 1) * 128], rhs=LA[0:64, :], start=True, stop=True),
                 reads=[par_tb, fm_tb["lw"]], writes=[ptb])
            P.op("act", lambda e, cc=cc, pa=pa: e.activation(out=fm["lw"][:, cc, :], in_=pa, func=AF.Sigmoid, bias=par[:, 11 + cc:12 + cc], scale=1.0),
                 reads=[ptb, par_tb], writes=[fm_tb["cum"]])
            pa2, ptb2 = q128.next()
            P.op("pe", lambda e, cc=cc, pa2=pa2: e.matmul(pa2, lhsT=lora[64:128, cc * 128:(cc + 1) * 128], rhs=LA[64:128, :], start=True, stop=True),
                 reads=[par_tb, fm_tb["lw"]], writes=[ptb2])
            P.op("act", lambda e, cc=cc, pa2=pa2: e.activation(out=fm["a"][:, cc, :], in_=pa2, func=AF.Sigmoid, bias=par[:, 14 + cc:15 + cc], scale=1.0),
                 reads=[ptb2, par_tb], writes=[fm_tb["a"]])
            pa3, ptb3 = q128.next()
            P.op("pe", lambda e, cc=cc, pa3=pa3: e.matmul(pa3, lhsT=g2w[:, cc * 128:(cc + 1) * 128], rhs=SG[:], start=True, stop=True),
                 reads=[par_tb, fm_tb["g"]], writes=[ptb3])
            P.op("act", lambda e, cc=cc, pa3=pa3: e.activation(out=fm["g"][:, cc, :], in_=pa3, func=AF.Identity), reads=[ptb3], writes=[fm_tb["bon"]])
        dv = lambda fn, rd, wr: P.op("dve", fn, reads=[fm_tb[n] if isinstance(n, str) else n for n in rd],
                                     writes=[fm_tb[n] if isinstance(n, str) else n for n in wr])
        F = fm
        pcol = lambda c0: bcast_last(par[:, c0:c0 + 3], 128)
        dv(lambda e: e.tensor_scalar(out=F["lw"][:], in0=F["lw"][:], scalar1=DECAY_C, scalar2=None, op0=ALU.mult), ["cum"], ["cum"])
        out_dma(dr["o_g"].rearrange("(c p) t -> p c t", p=128)[:, :, t0:t0 + 128], F["g"][:], fm_tb["bon"])
        dv(lambda e: e.tensor_tensor(out=F["kk"][:], in0=k_, in1=pcol(17), op=ALU.mult), [X_tb, par_tb], ["kk"])
        dv(lambda e: e.tensor_tensor(out=F["t1"][:], in0=F["kk"][:], in1=F["kk"][:], op=ALU.mult), ["kk"], ["t1"])
        for cc in range(3):
            pa, ptb = q128.next()
            P.op("pe", lambda e, cc=cc, pa=pa: e.matmul(pa, lhsT=BO, rhs=F["t1"][:, cc, :], start=True, stop=True), reads=[cst_tb, fm_tb["t1"]], writes=[ptb])
            P.op("act", lambda e, cc=cc, pa=pa: e.activation(out=F["kp"][:, cc, :], in_=pa, func=AF.Sqrt), reads=[ptb], writes=[fm_tb["kp"]])
        dv(lambda e: e.tensor_scalar(out=F["kp"][:], in0=F["kp"][:], scalar1=1e-12, scalar2=None, op0=ALU.max), ["kp"], ["kp"])
        dv(lambda e: e.reciprocal(out=F["kp"][:], in_=F["kp"][:]), ["kp"], ["kp"])
        dv(lambda e: e.tensor_tensor(out=F["kk"][:], in0=F["kk"][:], in1=F["kp"][:], op=ALU.mult), ["kk", "kp"], ["kk"])
        dv(lambda e: e.tensor_tensor(out=F["t1"][:], in0=F["a"][:], in1=pcol(20), op=ALU.mult), ["a", par_tb], ["t1"])
        dv(lambda e: e.tensor_tensor(out=F["t1"][:], in0=F["t1"][:], in1=pcol(23), op=ALU.add), ["t1", par_tb], ["t1"])
        dv(lambda e: e.tensor_tensor(out=F["kp"][:], in0=k_, in1=F["t1"][:], op=ALU.mult), [X_tb, "t1", "kp"], ["kp"])
        dv(lambda e: e.tensor_tensor(out=F["t1"][:], in0=r_, in1=pcol(26), op=ALU.mult), [X_tb, par_tb], ["t1"])
        dv(lambda e: e.tensor_tensor(out=F["t1"][:], in0=F["t1"][:], in1=F["kp"][:], op=ALU.mult), ["t1", "kp"], ["t1"])
        for cc in range(3):
            pa, ptb = q128.next()
            P.op("pe", lambda e, cc=cc, pa=pa: e.matmul(pa, lhsT=BO, rhs=F["t1"][:, cc, :], start=True, stop=True), reads=[cst_tb, fm_tb["t1"]], writes=[ptb])
            P.op("dve", lambda e, cc=cc, pa=pa: e.tensor_tensor(out=F["bon"][:, cc, :], in0=pa, in1=X[:, 6 + cc, :], op=ALU.mult),
                 reads=[ptb, X_tb], writes=[fm_tb["g"]])
        out_dma(dr["o_bon"].rearrange("(c p) t -> p c t", p=128)[:, :, t0:t0 + 128], F["bon"][:], fm_tb["g"])
        dv(lambda e: e.tensor_tensor(out=F["beta"][:], in0=F["a"][:], in1=F["kk"][:], op=ALU.mult), ["a", "kk"], ["beta"])
        for cc in range(3):
            dv(lambda e, cc=cc: e.tensor_tensor_scan(out=F["cum"][:, cc, :], data0=ones[:], data1=F["lw"][:, cc, :], initial=0.0,
                                                     op0=ALU.mult, op1=ALU.add), ["cum", par_tb], ["einc"])
        dv(lambda e: e.tensor_copy(out=gC[:], in_=F["cum"][:, :, 127]), ["einc"], ["einc"])
        dv(lambda e: e.tensor_tensor(out=F["t1"][:], in0=F["cum"][:], in1=F["lw"][:], op=ALU.subtract), ["einc", "t1"], ["t1"])
        ac = lambda fn, rd, wr: P.op("act", fn, reads=[fm_tb[n] for n in rd], writes=[fm_tb[n] for n in wr])
        ac(lambda e: e.activation(out=F["einc"][:], in_=F["cum"][:], func=AF.Exp), ["einc"], ["eexc"])
        ac(lambda e: e.activation(out=F["eexc"][:], in_=F["t1"][:], func=AF.Exp), ["t1"], ["einv"])
        ac(lambda e: e.activation(out=F["einv"][:], in_=F["cum"][:], func=AF.Exp, scale=-1.0), ["einc"], ["eend"])
        for cc in range(3):
            ac(lambda e, cc=cc: e.activation(out=F["eend"][:, cc, :], in_=F["cum"][:, cc, :], func=AF.Exp, scale=-1.0, bias=gC[:, cc:cc + 1]),
               ["einc"], ["at"])
        ac(lambda e: e.activation(out=GCe[:], in_=gC[:], func=AF.Exp), ["einc"], ["at"])
        dv(lambda e: e.scalar_tensor_tensor(out=F["at"][:], in0=F["kk"][:], scalar=-1.0, in1=F["eexc"][:], op0=ALU.mult, op1=ALU.mult),
           ["kk", "einv", "at"], ["bh"])
        dv(lambda e: e.tensor_tensor(out=F["bh"][:], in0=F["beta"][:], in1=F["einv"][:], op=ALU.mult), ["beta", "eend"], ["kh"])
        dv(lambda e: e.tensor_tensor(out=F["kh"][:], in0=F["kp"][:], in1=F["einv"][:], op=ALU.mult), ["kp", "eend"], ["rt"])
        dv(lambda e: e.tensor_tensor(out=F["rt"][:], in0=r_, in1=F["einc"][:], op=ALU.mult), [X_tb, "eexc"], ["bc"])
        dv(lambda e: e.tensor_tensor(out=F["bc"][:], in0=F["beta"][:], in1=F["eend"][:], op=ALU.mult), ["beta", "at"], ["kc"])
        dv(lambda e: e.tensor_tensor(out=F["kc"][:], in0=F["kp"][:], in1=F["eend"][:], op=ALU.mult), ["kp", "at"], ["lw"])
        allf = [fm_tb[n] for n in ("bh", "kh", "rt", "bc", "kc", "lw")]
        ck(5)
        for nm, src, stb in (("at", F["at"], fm_tb["bh"]), ("bc", F["bc"], fm_tb["kc"]), ("kc", F["kc"], fm_tb["lw"]), ("v", None, X_tb)):
            for cc in range(3):
                pa, ptb = q128.next()
                s_ap = X[:, 6 + cc, :] if src is None else src[:, cc, :]
                P.op("pe", lambda e, pa=pa, s_ap=s_ap: e.transpose(out=pa, in_=s_ap, identity=ident), reads=[stb, cst_tb], writes=[ptb])
                P.op("act", lambda e, pa=pa, nm=nm, cc=cc: e.activation(out=tokm[nm][:, cc * 128:(cc + 1) * 128], in_=pa, func=AF.Identity),
                     reads=[ptb], writes=[tok_tb[nm]])
        ck(6)
        def _head(hd, b=b):
            cc, p0 = hd // 2, (hd % 2) * 64
            sl = slice(p0, p0 + 64)
            aT, bT, kT, rT = F["at"][sl, cc, :], F["bh"][sl, cc, :], F["kh"][sl, cc, :], F["rt"][sl, cc, :]
            tcol = slice(hd * 64, hd * 64 + 64)
            MX, MT, MKT, NBT, NKT, DG = (HW[hd % 2][n] for n in ("MX", "MT", "MKT", "NBT", "NKT", "DG"))
            MX_tb, MT_tb, MKT_tb, NBT_tb, NKT_tb, DG_tb = (HW_tb[hd % 2][n] for n in ("MX", "MT", "MKT", "NBT", "NKT", "DG"))

            def mm_mask(lhsT, rhs, mask, dst, dtb):
                pa, ptb = q128.next()
                P.op("pe", lambda e: e.matmul(pa, lhsT=lhsT, rhs=rhs, start=True, stop=True), reads=allf, writes=[ptb])
                P.op("dve", lambda e: e.tensor_tensor(out=dst, in0=pa, in1=mask, op=ALU.mult), reads=[ptb, cst_tb], writes=[dtb])

            mm_mask(aT, bT, LS, MX[:, 0:128], MX_tb)
            mm_mask(bT, aT, US, MT[:], MT_tb)
            mm_mask(kT, aT, US, MKT[:], MKT_tb)
            mm_mask(bT, rT, UI, NBT[:], NBT_tb)
            mm_mask(kT, rT, UI, NKT[:], NKT_tb)
            P.op("pool", lambda e, tcol=tcol: e.tensor_copy(out=MX[:, 128:192], in_=tokm["at"][:, tcol]), reads=[tok_tb["at"]], writes=[MX_tb])
            pa, ptb = q128.next()
            P.op("pe", lambda e, pa=pa, tcol=tcol: e.matmul(pa[:, 0:64], lhsT=MKT[:], rhs=tokm["v"][:, tcol], start=True, stop=True),
                 reads=[MKT_tb, tok_tb["v"]], writes=[ptb])
            P.op("act", lambda e, pa=pa: e.activation(out=MX[:, 192:256], in_=pa[:, 0:64], func=AF.Identity), reads=[ptb], writes=[MX_tb])
            for it in range(7):
                last = it == 6
                ph, phtb = h256.next()
                if not last:
                    P.op("pe", lambda e, ph=ph: e.matmul(ph, lhsT=MT[:], rhs=MX[:], start=True, stop=True), reads=[MT_tb, MX_tb], writes=[phtb])
                    pa, ptb = q128.next()
                    P.op("pe", lambda e, pa=pa: e.matmul(pa, lhsT=MX[:, 0:128], rhs=MT[:], start=True, stop=True), reads=[MT_tb, MX_tb], writes=[ptb])
                    P.op("act", lambda e, ph=ph: e.activation(out=MX[:, 0:128], in_=ph[:, 0:128], func=AF.Identity), reads=[phtb], writes=[MX_tb])
                    P.op("dve", lambda e, ph=ph: e.tensor_tensor(out=MX[:, 128:256], in0=MX[:, 128:256], in1=ph[:, 128:256], op=ALU.add),
                         reads=[phtb, MX_tb], writes=[MX_tb])
                    P.op("act", lambda e, pa=pa: e.activation(out=MT[:], in_=pa, func=AF.Identity), reads=[ptb], writes=[MT_tb])
                else:
                    P.op("pe", lambda e, ph=ph: e.matmul(ph[:, 128:256], lhsT=MT[:], rhs=MX[:, 128:256], start=True, stop=True),
                         reads=[MT_tb, MX_tb], writes=[phtb])
                    P.op("dve", lambda e, ph=ph: e.tensor_tensor(out=MX[:, 128:256], in0=MX[:, 128:256], in1=ph[:, 128:256], op=ALU.add),
                         reads=[phtb, MX_tb], writes=[MX_tb])
            W0, U0 = MX[:, 128:192], MX[:, 192:256]
            P.op("dve", lambda e, p0=p0, cc=cc: e.tensor_scalar(out=DG[:, 0:64], in0=cst[:, 0, p0:p0 + 64], scalar1=GCe[:, cc:cc + 1], scalar2=None, op0=ALU.mult),
                 reads=[cst_tb, fm_tb["at"]], writes=[DG_tb])
            pa, ptb = q128.next()
            P.op("pe", lambda e, pa=pa, tcol=tcol: e.matmul(pa[0:64, 0:64], lhsT=W0, rhs=tokm["bc"][:, tcol], start=True, stop=False),
                 reads=[MX_tb, tok_tb["bc"]], writes=[ptb])
            P.op("pe", lambda e, pa=pa, p0=p0: e.matmul(pa[0:64, 0:64], lhsT=cst[:, 0, p0:p0 + 64], rhs=DG[:, 0:64], start=False, stop=True),
                 reads=[DG_tb, cst_tb], writes=[ptb], acc=True)
            P.op("act", lambda e, pa=pa, hd=hd: e.activation(out=ATb[:, hd, :], in_=pa[0:64, 0:64], func=AF.Identity), reads=[ptb], writes=[AD_tb[hd]])
            pa, ptb = q128.next()
            P.op("pe", lambda e, pa=pa, tcol=tcol: e.matmul(pa[0:64, 0:64], lhsT=tokm["bc"][:, tcol], rhs=U0, start=True, stop=False),
                 reads=[MX_tb, tok_tb["bc"]], writes=[ptb])
            P.op("pe", lambda e, pa=pa, tcol=tcol: e.matmul(pa[0:64, 0:64], lhsT=tokm["kc"][:, tcol], rhs=tokm["v"][:, tcol], start=False, stop=True),
                 reads=[tok_tb["kc"], tok_tb["v"]], writes=[ptb], acc=True)
            P.op("act", lambda e, pa=pa, hd=hd: e.activation(out=Db[:, hd, :], in_=pa[0:64, 0:64], func=AF.Identity), reads=[ptb], writes=[AD_tb[hd]])
            pa, ptb = q128.next()
            P.op("pe", lambda e, pa=pa: e.matmul(pa[0:64, :], lhsT=W0, rhs=NBT[:], start=True, stop=False), reads=[MX_tb, NBT_tb], writes=[ptb])
            P.op("pe", lambda e, pa=pa, p0=p0, cc=cc: e.matmul(pa[0:64, :], lhsT=cst[:, 0, p0:p0 + 64], rhs=F["rt"][:, cc, :], start=False, stop=True),
                 reads=[cst_tb] + allf, writes=[ptb], acc=True)
            oa, otb = ost_s.next()
            P.op("act", lambda e, pa=pa, oa=oa: e.activation(out=oa[0:64, :], in_=pa[0:64, :], func=AF.Identity), reads=[ptb], writes=[otb])
            out_dma(dr["o_YwT"][b, hd], oa[0:64, :], otb)
            pa, ptb = q128.next()
            P.op("pe", lambda e, pa=pa: e.matmul(pa[0:64, :], lhsT=U0, rhs=NBT[:], start=True, stop=False), reads=[MX_tb, NBT_tb], writes=[ptb])
            P.op("pe", lambda e, pa=pa, tcol=tcol: e.matmul(pa[0:64, :], lhsT=tokm["v"][:, tcol], rhs=NKT[:], start=False, stop=True),
                 reads=[tok_tb["v"], NKT_tb], writes=[ptb], acc=True)
            oa, otb = ost_s.next()
            P.op("act", lambda e, pa=pa, oa=oa: e.activation(out=oa[0:64, :], in_=pa[0:64, :], func=AF.Identity), reads=[ptb], writes=[otb])
            out_dma(dr["o_Y0T"][b, hd], oa[0:64, :], otb)
            pa, ptb = q128.next()
            P.op("pe", lambda e, pa=pa, hd=hd: e.matmul(pa[0:64, :], lhsT=ATb[:, hd, :], rhs=PQ[:, hd, :], start=True, stop=True),
                 reads=[AD_tb[hd], PQ_tb[hd]], writes=[ptb])
            pa2, ptb2 = q128.next()
            P.op("pe", lambda e, pa2=pa2, hd=hd: e.matmul(pa2[0:64, 0:64], lhsT=PQ[:, hd, 0:64], rhs=ATb[:, hd, :], start=True, stop=True),
                 reads=[AD_tb[hd], PQ_tb[hd]], writes=[ptb2])
            P.op("act", lambda e, pa=pa, hd=hd: e.activation(out=PQ[:, hd, 0:64], in_=pa[0:64, 0:64], func=AF.Identity), reads=[ptb], writes=[PQ_tb[hd]])
            P.op("dve", lambda e, pa=pa, hd=hd: e.tensor_tensor(out=PQ[:, hd, 64:128], in0=pa[0:64, 64:128], in1=Db[:, hd, :], op=ALU.add),
                 reads=[ptb, AD_tb[hd]], writes=[PQ_tb[hd]])
            P.op("act", lambda e, pa2=pa2, hd=hd: e.activation(out=PTt[:, hd, :], in_=pa2[0:64, 0:64], func=AF.Identity), reads=[ptb2], writes=[PQ_tb[hd]])
        for hd in range(6):
            _head(hd)
        ck(7)
        emit_pq_out(b + 1)

        for m in range(2):
            def ev(pa, ptb, m=m):
                oa, otb = obf_s.next()
                P.op("act", lambda e: e.activation(out=oa[:, 0:128], in_=pa, func=AF.Identity), reads=[ptb], writes=[otb])
                for hh_ in range(2):
                    h = 2 * m + hh_
                    s2 = slice(hh_ * 64, hh_ * 64 + 64)
                    pq, pqtb = q128.next()
                    P.op("pe", lambda e, pq=pq, s2=s2: e.matmul(pq, lhsT=wuk[s2, m, :], rhs=oa[s2, 0:128], start=True, stop=True), reads=[otb, par_tb], writes=[pqtb])
                    ob, obtb = obf_s.next()
                    P.op("act", lambda e, pq=pq, ob=ob: e.activation(out=ob[:, 0:128], in_=pq, func=AF.Identity, scale=0.125), reads=[pqtb], writes=[obtb])
                    out_dma(dr["o_qlat"][:, h, t0:t0 + 128], ob[:, 0:128], obtb)
            projT(C_DQ + m * 128, ev)
        for m in range(3):
            def ev(pa, ptb, m=m):
                oa, otb = obf_s.next()
                P.op("act", lambda e: e.activation(out=oa[:, 0:128], in_=pa, func=AF.Identity), reads=[ptb], writes=[otb])
                out_dma(dr["o_sq"][m * 128:(m + 1) * 128, t0:t0 + 128], oa[:, 0:128], otb)
            projT(C_SQ + m * 128, ev)

        def ev(pa, ptb):
            oa, otb = obf_s.next()
            P.op("act", lambda e: e.activation(out=oa[:, 0:128], in_=pa, func=AF.Identity), reads=[ptb], writes=[otb])
            out_dma(dr["o_sk"][:, t0:t0 + 128], oa[:, 0:128], otb)
        projT(C_SK, ev)
        ck(8)
        for (col0, ncol, off, bank) in ((C_CKV, 452, 0, 0), (C_SV, 128, 452, 1)):
            for kc in range(8):
                P.op("pe", lambda e, kc=kc, col0=col0, ncol=ncol, bank=bank: e.matmul(pbk[bank][:, 0:ncol], lhsT=hT[:, kc, :], rhs=win[:, kc, col0:col0 + ncol],
                                                                                      start=(kc == 0), stop=(kc == 7)),
                     reads=[win_tb, hT_tb], writes=[pT_tb[bank]], acc=True)
            P.op("act", lambda e, ncol=ncol, off=off, bank=bank: e.activation(out=tk[:, off:off + ncol], in_=pbk[bank][:, 0:ncol], func=AF.Identity),
                 reads=[pT_tb[bank]], writes=[tk_tb])
        N = nsc
        nv = lambda fn: P.op("dve", fn, reads=[tk_tb, nsc_tb, par_tb], writes=[nsc_tb])
        nv(lambda e: e.tensor_tensor(out=N["t"][:, 0:128], in0=tk[:, 0:128], in1=tk[:, 0:128], op=ALU.mult))
        nv(lambda e: e.tensor_reduce(out=N["rs"][:, 0:1], in_=N["t"][:, 0:128], axis=AX.X, op=ALU.add))
        P.op("act", lambda e: e.activation(out=N["rs"][:, 0:1], in_=N["rs"][:, 0:1], func=AF.Sqrt, scale=1.0 / 128, bias=eps6[:, 0:1]),
             reads=[nsc_tb, par_tb], writes=[nsc_tb])
        nv(lambda e: e.reciprocal(out=N["rs"][:, 0:1], in_=N["rs"][:, 0:1]))
        nv(lambda e: e.scalar_tensor_tensor(out=N["t"][:, 0:128], in0=tk[:, 0:128], scalar=N["rs"][:, 0:1], in1=bcp[:, 0:128], op0=ALU.mult, op1=ALU.mult))
        oa, otb = obf_s.next()
        P.op("dve", lambda e, oa=oa: e.tensor_copy(out=oa[:, 0:128], in_=N["t"][:, 0:128]), reads=[nsc_tb], writes=[otb])
        out_dma(dr["o_ckv"][t0:t0 + 128, :], oa[:, 0:128], otb)
        pa, ptb = q128.next()
        pab = pa.bitcast(BF16)
        P.op("pe", lambda e, pab=pab, oa=oa: e.transpose(out=pab[:, 0:128], in_=oa[:, 0:128], identity=identb[:]), reads=[otb, cst_tb], writes=[ptb])
        P.op("act", lambda e, pab=pab, oa=oa: e.activation(out=oa[:, 128:256], in_=pab[:, 0:128], func=AF.Identity), reads=[ptb], writes=[otb])
        out_dma(dr["o_ckvT"][:, t0:t0 + 128], oa[:, 128:256], otb)
        nv(lambda e: e.bn_stats(out=N["st"][:], in_=tk[:, 384:448]))
        nv(lambda e: e.bn_aggr(out=N["mv"][:], in_=N["st"][:]))
        P.op("act", lambda e: e.activation(out=N["rs"][:, 1:2], in_=N["mv"][:, 1:2], func=AF.Sqrt, scale=1.0, bias=eps6[:, 1:2]),
             reads=[nsc_tb, par_tb], writes=[nsc_tb])
        nv(lambda e: e.reciprocal(out=N["rs"][:, 1:2], in_=N["rs"][:, 1:2]))
        nv(lambda e: e.tensor_scalar(out=N["t"][:, 128:192], in0=tk[:, 384:448], scalar1=N["mv"][:, 0:1], scalar2=N["rs"][:, 1:2], op0=ALU.subtract, op1=ALU.mult))
        nv(lambda e: e.tensor_tensor(out=N["t"][:, 128:192], in0=N["t"][:, 128:192], in1=bcp[:, 128:192], op=ALU.mult))
        oa, otb = obf_s.next()
        P.op("dve", lambda e, oa=oa: e.tensor_tensor(out=oa[:, 0:64], in0=N["t"][:, 128:192], in1=bcp[:, 192:256], op=ALU.add), reads=[nsc_tb, par_tb], writes=[otb])
        pa, ptb = q128.next()
        pab = pa.bitcast(BF16)
        P.op("pe", lambda e, pab=pab, oa=oa: e.transpose(out=pab[0:64, 0:128], in_=oa[:, 0:64], identity=identb[:]), reads=[otb, cst_tb], writes=[ptb])
        P.op("act", lambda e, pab=pab, oa=oa: e.activation(out=oa[0:64, 128:256], in_=pab[0:64, 0:128], func=AF.Identity), reads=[ptb], writes=[otb])
        out_dma(dr["o_ikT"][:, t0:t0 + 128], oa[0:64, 128:256], otb)
        P.op("act", lambda e: e.activation(out=N["sg"][:], in_=tk[:, 448:452], func=AF.Sign), reads=[tk_tb], writes=[nsc_tb])
        nv(lambda e: e.tensor_tensor(out=N["aiw"][:], in0=tk[:, 448:452], in1=N["sg"][:], op=ALU.mult))
        oa, otb = ost_s.next()
        P.op("dve", lambda e, oa=oa: e.tensor_copy(out=oa[:, 0:4], in_=N["sg"][:]), reads=[nsc_tb], writes=[otb])
        out_dma(dr["o_sgn"][t0:t0 + 128, :], oa[:, 0:4], otb)
        ob, obtb = obf_s.next()
        for h in range(4):
            P.op("dve", lambda e, h=h, ob=ob: e.tensor_scalar(out=ob[:, h * 64:(h + 1) * 64], in0=tk[:, 128 + h * 64:128 + (h + 1) * 64],
                                                              scalar1=N["aiw"][:, h:h + 1], scalar2=1.0 / 16, op0=ALU.mult, op1=ALU.mult),
                 reads=[tk_tb, nsc_tb], writes=[obtb])
        oc, octb = obf_s.next()
        for h in range(4):
            pa, ptb = q128.next()
            pab = pa.bitcast(BF16)
            P.op("pe", lambda e, pab=pab, ob=ob, h=h: e.transpose(out=pab[0:64, 0:128], in_=ob[:, h * 64:(h + 1) * 64], identity=identb[:]),
                 reads=[obtb, cst_tb], writes=[ptb])
            P.op("act", lambda e, pab=pab, oc=oc, h=h: e.activation(out=oc[0:64, h * 128:(h + 1) * 128], in_=pab[0:64, 0:128], func=AF.Identity),
                 reads=[ptb], writes=[octb])
        out_dma(dr["o_iqs"][:, :, t0:t0 + 128], oc[0:64, :].rearrange("p (h t) -> p h t", h=4), octb)
        od, odtb = obf_s.next()
        P.op("dve", lambda e, od=od: e.tensor_copy(out=od[:, 0:128], in_=tk[:, 452:580]), reads=[tk_tb], writes=[odtb])
        out_dma(dr["o_sv"][t0:t0 + 128, :], od[:, 0:128], odtb)


GN_EPS = 64e-5
NITER = 21
TOPK = 256


def emit_k2(P, nc, NBQ, dr, parts=("rwkv", "swa", "dsa")):
    NT = NBQ * 128
    NKT = 8 * NBQ
    seg_of = lambda j: (8 * j) // NBQ
    out_tb = dr["out_tb"]
    pb = [P.psum(f"k2pb{i}", [128, 512], F32) for i in range(8)]
    bank = [P.tb(f"k2bank{i}") for i in range(8)]
    cst = P.sbuf("cst2", [128, 128], F32)
    cst_tb = P.tb("cst2")
    P.op("sp", lambda e: e.dma_start(out=cst[:], in_=dr["ident"]), writes=[cst_tb], dma=True)
    identb = P.sbuf("identb2", [128, 128], BF16)
    onesb = P.sbuf("onesb", [128, 64], BF16)
    o64 = P.sbuf("o64", [64, 64], F32)
    P.op("dve", lambda e: e.tensor_copy(out=identb[:], in_=cst[:]), reads=[cst_tb], writes=[cst_tb])
    P.op("pool", lambda e: e.memset(onesb[:], 1.0), writes=[cst_tb])
    P.op("pool", lambda e: e.memset(o64[:], 1.0 / 64), writes=[cst_tb])
    obuf = [P.sbuf(f"k2o{i}", [64, 512], BF16) for i in range(4)]
    obs = Slots(P, [o[:] for o in obuf])

    def out_mix(head, j, src, stb):
        P.op("sp", lambda e: e.dma_start(out=dr["mixT"][head, :, j * 128:(j + 1) * 128], in_=src), reads=[stb], writes=[out_tb], dma=True)

    if "rwkv" in parts:
      with P.scope():
        rp = P.sbuf("rp", [64, 16], F32)
        rp_tb = P.tb("rp")
        P.op("sp", lambda e: e.dma_start(out=rp[:], in_=dr["rwkv_ln"]), writes=[rp_tb], dma=True)
        epsg = P.sbuf("epsg", [64, 1], F32)
        P.op("pool", lambda e: e.memset(epsg[:], GN_EPS), writes=[rp_tb])
        segP = P.sbuf("segP", [64, 8, 6, 64], F32)
        segQ = P.sbuf("segQ", [64, 8, 6, 64], F32)
        seg_tb = P.tb("seg")
        P.op("sp", lambda e: e.dma_start(out=segP[:], in_=dr["segPT"].rearrange("s h a b -> a s h b")), writes=[seg_tb], dma=True)
        P.op("sp", lambda e: e.dma_start(out=segQ[:], in_=dr["segQ"].rearrange("s h a b -> a s h b")), writes=[seg_tb], dma=True)
        St = P.sbuf("St", [64, 8, 6, 64], F32)
        St_tb = [P.tb(f"St{s}") for s in range(8)]
        P.op("pool", lambda e: e.memset(St[:, 0, :, :], 0.0), writes=[St_tb[0]])
        for s in range(7):
            for hd in range(6):
                P.op("pe", lambda e, s=s, hd=hd: e.matmul(pb[0][0:64, hd * 64:(hd + 1) * 64], lhsT=segP[:, s, hd, :], rhs=St[:, s, hd, :],
                                                          start=True, stop=True), reads=[seg_tb, St_tb[s]], writes=[bank[0]], acc=True)
            P.op("dve", lambda e, s=s: e.tensor_tensor(out=St[:, s + 1, :, :].rearrange("p h v -> p (h v)"), in0=pb[0][0:64, 0:384],
                                                       in1=segQ[:, s, :, :].rearrange("p h v -> p (h v)"), op=ALU.add),
                 reads=[bank[0], seg_tb], writes=[St_tb[s + 1]])
        rin = [{n: P.sbuf(f"r_{n}{i}", [64, 6, w], F32) for n, w in (("yw", 128), ("y0", 128), ("pt", 64), ("q", 64), ("bon", 128), ("g", 128))} for i in range(2)]
        rin_tb = [P.tb(f"rin{i}") for i in range(2)]
        Sb = P.sbuf("Sb", [64, 6, 64], F32)
        Sb_tb = P.tb("Sb")
        yy = P.sbuf("yy", [64, 6, 128], F32)
        cen = P.sbuf("cen", [64, 6, 128], F32)
        sq = P.sbuf("sqr", [64, 6, 128], F32)
        rsd = P.sbuf("rsd", [64, 6, 128], F32)
        ww_tb = P.tb("rwkvwork")
        for j in range(NBQ):
            I = rin[j % 2]
            itb = rin_tb[j % 2]
            for n, src in (("yw", dr["YwT"][j]), ("y0", dr["Y0T"][j]), ("pt", dr["PbT"][j]), ("q", dr["Qb"][j])):
                P.op("sp", lambda e, n=n, src=src, I=I: e.dma_start(out=I[n][:], in_=src.rearrange("h a b -> a h b")), writes=[itb], dma=True)
            P.op("sp", lambda e, I=I, j=j: e.dma_start(out=I["bon"][:], in_=dr["bon"][:, :, j * 128:(j + 1) * 128]), writes=[itb], dma=True)
            P.op("sp", lambda e, I=I, j=j: e.dma_start(out=I["g"][:], in_=dr["g"][:, :, j * 128:(j + 1) * 128]), writes=[itb], dma=True)
            s = seg_of(j)
            for hd in range(6):
                P.op("pe", lambda e, hd=hd, I=I, s=s: e.matmul(pb[0][0:64, hd * 64:(hd + 1) * 64], lhsT=I["pt"][:, hd, :], rhs=St[:, s, hd, :], start=True, stop=True),
                     reads=[itb, St_tb[s]], writes=[bank[0]], acc=True)
            P.op("dve", lambda e, I=I: e.tensor_tensor(out=Sb[:].rearrange("p h v -> p (h v)"), in0=pb[0][0:64, 0:384],
                                                       in1=I["q"][:].rearrange("p h v -> p (h v)"), op=ALU.add), reads=[bank[0], itb], writes=[Sb_tb])
            for half in range(2):
                bk = 1 + half
                for q in range(3):
                    hd = half * 3 + q
                    P.op("pe", lambda e, hd=hd, q=q, bk=bk, I=I: e.matmul(pb[bk][0:64, q * 128:(q + 1) * 128], lhsT=Sb[:, hd, :], rhs=I["yw"][:, hd, :], start=True, stop=True),
                         reads=[Sb_tb, itb], writes=[bank[bk]], acc=True)
                hs = slice(half * 3, half * 3 + 3)
                f3 = lambda t, hs=hs: t[:, hs, :].rearrange("p h t -> p (h t)")
                P.op("dve", lambda e, bk=bk, I=I, f3=f3: e.tensor_tensor(out=f3(yy), in0=pb[bk][0:64, 0:384], in1=f3(I["y0"]), op=ALU.add),
                     reads=[bank[bk], itb], writes=[ww_tb])
                P.op("pe", lambda e, bk=bk, f3=f3: e.matmul(pb[bk][0:64, 0:384], lhsT=o64[:], rhs=f3(yy), start=True, stop=True), reads=[ww_tb, cst_tb], writes=[bank[bk]])
                P.op("dve", lambda e, bk=bk, f3=f3: e.tensor_tensor(out=f3(cen), in0=f3(yy), in1=pb[bk][0:64, 0:384], op=ALU.subtract), reads=[bank[bk], ww_tb], writes=[ww_tb])
                P.op("pool", lambda e, f3=f3: e.tensor_tensor(out=f3(sq), in0=f3(cen), in1=f3(cen), op=ALU.mult), reads=[ww_tb], writes=[ww_tb])
                P.op("pe", lambda e, bk=bk, f3=f3: e.matmul(pb[bk][0:64, 0:384], lhsT=o64[:], rhs=f3(sq), start=True, stop=True), reads=[ww_tb, cst_tb], writes=[bank[bk]])
                P.op("act", lambda e, bk=bk, f3=f3: e.activation(out=f3(rsd), in_=pb[bk][0:64, 0:384], func=AF.Sqrt, bias=epsg[:, 0:1], scale=1.0),
                     reads=[bank[bk], rp_tb], writes=[ww_tb])
                P.op("dve", lambda e, f3=f3: e.reciprocal(out=f3(rsd), in_=f3(rsd)), reads=[ww_tb], writes=[ww_tb])
                P.op("dve", lambda e, f3=f3: e.tensor_tensor(out=f3(cen), in0=f3(cen), in1=f3(rsd), op=ALU.mult), reads=[ww_tb], writes=[ww_tb])
                ob, obtb = obs.next()
                for q in range(3):
                    hd = half * 3 + q
                    P.op("dve", lambda e, hd=hd: e.tensor_scalar(out=cen[:, hd, :], in0=cen[:, hd, :], scalar1=rp[:, hd:hd + 1], scalar2=rp[:, 6 + hd:7 + hd],
                                                                 op0=ALU.mult, op1=ALU.add), reads=[ww_tb, rp_tb], writes=[ww_tb])
                P.op("pool", lambda e, f3=f3, I=I: e.tensor_tensor(out=f3(cen), in0=f3(cen), in1=f3(I["bon"]), op=ALU.add), reads=[ww_tb, itb], writes=[ww_tb])
                P.op("pool", lambda e, f3=f3, I=I, ob=ob: e.tensor_tensor(out=ob[:, 0:384], in0=f3(cen), in1=f3(I["g"]), op=ALU.mult), reads=[ww_tb, itb], writes=[obtb])
                P.op("sp", lambda e, ob=ob, j=j, half=half: e.dma_start(out=dr["mixT"][half * 3:half * 3 + 3, :, j * 128:(j + 1) * 128].rearrange("h p t -> p h t"),
                                                                          in_=ob[:, 0:384].rearrange("p (h t) -> p h t", h=3)), reads=[obtb], writes=[out_tb], dma=True)

    if "swa" in parts:
      with P.scope():
        swb = P.sbuf("swb", [128, 2, 6, 128], F32)
        swbf = P.sbuf("swbf", [128, 6, 128], F32)
        sw_tb = P.tb("swc")
        P.op("sp", lambda e: e.dma_start(out=swb[:], in_=dr["swb"]), writes=[sw_tb], dma=True)
        P.op("sp", lambda e: e.dma_start(out=swbf[:], in_=dr["swb_first"]), writes=[sw_tb], dma=True)
        es = P.sbuf("esink", [64, 6], F32)
        P.op("sp", lambda e: e.dma_start(out=es[:], in_=dr["sink_b"]), writes=[sw_tb], dma=True)
        P.op("act", lambda e: e.activation(out=es[:], in_=es[:], func=AF.Exp), reads=[sw_tb], writes=[sw_tb])
        sin = [{"q": P.sbuf(f"s_q{i}", [64, 6, 128], BF16), "k": P.sbuf(f"s_k{i}", [64, 2, 256], BF16), "v": P.sbuf(f"s_v{i}", [128, 2, 128], BF16)} for i in range(2)]
        sin_tb = [P.tb(f"sin{i}") for i in range(2)]
        stmp = P.sbuf("stmp", [128, 384], F32)
        sE = [P.sbuf(f"sE{i}", [128, 384], BF16) for i in range(2)]
        sE_tb = [P.tb(), P.tb()]
        st_tb = P.tb("stmp")
        srec = P.sbuf("srec", [64, 384], F32)
        for j in range(NBQ):
            I = sin[j % 2]
            itb = sin_tb[j % 2]
            P.op("sp", lambda e, I=I, j=j: e.dma_start(out=I["q"][:], in_=dr["sq"][:, :, j * 128:(j + 1) * 128]), writes=[itb], dma=True)
            P.op("sp", lambda e, I=I, j=j: e.dma_start(out=I["k"][:], in_=dr["sk2"][:, :, j, :]), writes=[itb], dma=True)
            P.op("sp", lambda e, I=I, j=j: e.dma_start(out=I["v"][:], in_=dr["sv2"][j].rearrange("k p c -> p k c")), writes=[itb], dma=True)
            for g in range(2):
                bn, bd = 5, 6
                for kt2 in range(2):
                    bs = 3 + kt2
                    P.op("pe", lambda e, g=g, kt2=kt2, bs=bs, I=I: e.matmul(pb[bs][:, 0:384], lhsT=I["k"][:, g, kt2 * 128:(kt2 + 1) * 128],
                                                                            rhs=I["q"][:, 3 * g:3 * g + 3, :], start=True, stop=True),
                         reads=[itb], writes=[bank[bs]])
                    btab = swbf[:, 3 * g:3 * g + 3, :] if (j == 0 and kt2 == 0) else swb[:, kt2, 3 * g:3 * g + 3, :]
                    P.op("dve", lambda e, bs=bs, btab=btab: e.scalar_tensor_tensor(out=stmp[:].rearrange("p (h t) -> p h t", h=3), in0=pb[bs][:, 0:384].rearrange("p (h t) -> p h t", h=3),
                                                                                  scalar=0.125, in1=btab, op0=ALU.mult, op1=ALU.add),
                         reads=[bank[bs], sw_tb], writes=[st_tb])
                    E, etb = sE[kt2], sE_tb[kt2]
                    P.op("act", lambda e, E=E: e.activation(out=E[:], in_=stmp[:], func=AF.Exp), reads=[st_tb], writes=[etb])
                    P.op("pe", lambda e, E=E, g=g, kt2=kt2, I=I: e.matmul(pb[bn][0:64, 0:384], lhsT=I["v"][:, kt2, g * 64:(g + 1) * 64], rhs=E[:],
                                                                          start=(kt2 == 0), stop=(kt2 == 1)), reads=[etb, itb], writes=[bank[bn]], acc=True)
                    P.op("pe", lambda e, E=E, kt2=kt2: e.matmul(pb[bd][0:64, 0:384], lhsT=onesb[:], rhs=E[:], start=(kt2 == 0), stop=(kt2 == 1)),
                         reads=[etb, cst_tb], writes=[bank[bd]], acc=True)
                for q in range(3):
                    P.op("dve", lambda e, q=q, g=g: e.tensor_scalar(out=srec[:, q * 128:(q + 1) * 128], in0=pb[bd][0:64, q * 128:(q + 1) * 128],
                                                                    scalar1=es[:, 3 * g + q:3 * g + q + 1], scalar2=None, op0=ALU.add),
                         reads=[bank[bd], sw_tb], writes=[st_tb])
                P.op("dve", lambda e: e.reciprocal(out=srec[:], in_=srec[:]), reads=[st_tb], writes=[st_tb])
                ob, obtb = obs.next()
                P.op("dve", lambda e, ob=ob: e.tensor_tensor(out=ob[:, 0:384], in0=pb[bn][0:64, 0:384], in1=srec[:], op=ALU.mult), reads=[bank[bn], st_tb], writes=[obtb])
                P.op("sp", lambda e, ob=ob, j=j, g=g: e.dma_start(out=dr["mixT"][10 + 3 * g:13 + 3 * g, :, j * 128:(j + 1) * 128].rearrange("h p t -> p h t"),
                                                                    in_=ob[:, 0:384].rearrange("p (h t) -> p h t", h=3)), reads=[obtb], writes=[out_tb], dma=True)

    if "dsa" in parts:
      with P.scope():
        ckvT = P.sbuf("ckvT", [128, NKT * 128], BF16)
        ckv = P.sbuf("ckv", [128, NKT, 128], BF16)
        key_tb = P.tb("keys")
        for c in range(0, NKT, 16):
            P.op("sp", lambda e, c=c: e.dma_start(out=ckvT[:, c * 128:(c + 16) * 128], in_=dr["ckvT"][:, c * 128:(c + 16) * 128]), writes=[key_tb], dma=True)
            P.op("sp", lambda e, c=c: e.dma_start(out=ckv[:, c:c + 16, :], in_=dr["ckv"][c * 128:(c + 16) * 128, :].rearrange("(k p) r -> p k r", p=128)),
                 writes=[key_tb], dma=True)
        ikc = [P.sbuf(f"ikc{i}", [64, 512], BF16) for i in range(3)]
        ikc_tb = [P.tb() for _ in range(3)]
        wuv_f = P.sbuf("wuv_f", [128, 4, 64], F32)
        wuv = P.sbuf("wuv", [128, 4, 64], BF16)
        dpar_tb = P.tb("dpar")
        P.op("sp", lambda e: e.dma_start(out=wuv_f[:], in_=dr["wuv"].rearrange("h r d -> r h d")), writes=[dpar_tb], dma=True)
        P.op("dve", lambda e: e.tensor_copy(out=wuv[:], in_=wuv_f[:]), reads=[dpar_tb], writes=[dpar_tb])
        dmask = P.sbuf("dmask", [128, 1024], F32)
        ab = P.sbuf("ab", [128, 4, 128], F32)
        P.op("sp", lambda e: e.dma_start(out=dmask[:], in_=dr["dmask"]), writes=[dpar_tb], dma=True)
        P.op("sp", lambda e: e.dma_start(out=ab[:], in_=dr["ab"]), writes=[dpar_tb], dma=True)
        Isc = P.sbuf("Isc", [128, NKT * 128], F32)
        Isc_tb = P.tb("Isc")
        msk = P.sbuf("msk", [128, NKT * 128], BF16)
        msk_tb = P.tb("msk")
        qin = [{"ql": P.sbuf(f"d_ql{i}", [128, 4, 128], BF16), "iq": P.sbuf(f"d_iq{i}", [64, 4, 128], BF16), "sg": P.sbuf(f"d_sg{i}", [128, 4], F32)} for i in range(2)]
        qin_tb = [P.tb(), P.tb()]
        rl = [P.sbuf(f"rl{i}", [128, 512], F32) for i in range(2)]
        rl_tb = [P.tb(), P.tb()]
        bs_ = {n: P.sbuf("bs_" + n, [128, 1], F32) for n in ("lo", "hi", "mid", "cnt", "ge", "d")}
        bs_tb = P.tb("bisect")
        Eb = [P.sbuf(f"Eb{i}", [128, 512], F32) for i in range(2)]
        Eb_tb = [[P.tb() for _ in range(4)] for _ in range(2)]
        PTb = [P.sbuf(f"PTb{i}", [128, 512], BF16) for i in range(2)]
        PT_tb = [P.tb(), P.tb()]
        olat = P.sbuf("olat", [128, 512], BF16)
        drec = P.sbuf("drec", [64, 512], F32)
        fin_tb = P.tb("dsafin")
        for j in range(NBQ):
            nkt = 8 * (j + 1)
            n = nkt * 128
            Q = qin[j % 2]
            qtb = qin_tb[j % 2]
            P.op("sp", lambda e, Q=Q, j=j: e.dma_start(out=Q["ql"][:], in_=dr["qlat"][:, :, j * 128:(j + 1) * 128]), writes=[qtb], dma=True)
            P.op("sp", lambda e, Q=Q, j=j: e.dma_start(out=Q["iq"][:], in_=dr["iqs"][:, :, j * 128:(j + 1) * 128]), writes=[qtb], dma=True)
            P.op("sp", lambda e, Q=Q, j=j: e.dma_start(out=Q["sg"][:], in_=dr["sgn"][j * 128:(j + 1) * 128, :]), writes=[qtb], dma=True)
            for c4 in range(nkt // 4):
                ii = c4 % 3
                P.op("sp", lambda e, c4=c4, ii=ii: e.dma_start(out=ikc[ii][:], in_=dr["ikT"][:, c4 * 512:(c4 + 1) * 512]), writes=[ikc_tb[ii]], dma=True)
                for h in range(4):
                    bk = (c4 * 4 + h) % 2
                    P.op("pe", lambda e, h=h, bk=bk, ii=ii, Q=Q: e.matmul(pb[bk][:], lhsT=Q["iq"][:, h, :], rhs=ikc[ii][:], start=True, stop=True),
                         reads=[qtb, ikc_tb[ii]], writes=[bank[bk]])
                    P.op("act", lambda e, bk=bk: e.activation(out=rl[bk][:], in_=pb[bk][:], func=AF.Relu), reads=[bank[bk]], writes=[rl_tb[bk]])
                    dst = Isc[:, c4 * 512:(c4 + 1) * 512]
                    if h == 0:
                        P.op("dve", lambda e, bk=bk, dst=dst, Q=Q: e.tensor_scalar(out=dst, in0=rl[bk][:], scalar1=Q["sg"][:, 0:1], scalar2=None, op0=ALU.mult),
                             reads=[rl_tb[bk], qtb], writes=[Isc_tb])
                    else:
                        P.op("dve", lambda e, bk=bk, dst=dst, h=h, Q=Q: e.scalar_tensor_tensor(out=dst, in0=rl[bk][:], scalar=Q["sg"][:, h:h + 1], in1=dst,
                                                                                              op0=ALU.mult, op1=ALU.add), reads=[rl_tb[bk], qtb, Isc_tb], writes=[Isc_tb])
            P.op("dve", lambda e, n=n: e.tensor_tensor(out=Isc[:, n - 1024:n], in0=Isc[:, n - 1024:n], in1=dmask[:], op=ALU.add), reads=[Isc_tb, dpar_tb], writes=[Isc_tb])
            B = bs_
            bv = lambda fn: P.op("dve", fn, reads=[bs_tb, Isc_tb], writes=[bs_tb])
            bv(lambda e: e.memset(B["lo"][:], -64.0))
            bv(lambda e, n=n: e.tensor_reduce(out=B["hi"][:], in_=Isc[:, 0:n], axis=AX.X, op=ALU.max))
            bv(lambda e: e.tensor_scalar(out=B["d"][:], in0=B["hi"][:], scalar1=64.0, scalar2=None, op0=ALU.add))
            for it in range(NITER):
                ck_ = 0.5 ** (it + 1)
                bv(lambda e, ck_=ck_: e.scalar_tensor_tensor(out=B["mid"][:], in0=B["d"][:], scalar=ck_, in1=B["lo"][:], op0=ALU.mult, op1=ALU.add))
                P.op("dve", lambda e, n=n: e.tensor_scalar(out=msk[:, 0:n], in0=Isc[:, 0:n], scalar1=B["mid"][:, 0:1], scalar2=0.0, op0=ALU.is_ge, op1=ALU.add,
                                                          accum_out=B["cnt"][:]), reads=[bs_tb, Isc_tb], writes=[bs_tb, msk_tb])
                bv(lambda e, ck_=ck_: e.tensor_scalar(out=B["ge"][:], in0=B["cnt"][:], scalar1=float(TOPK) - 0.5, scalar2=ck_, op0=ALU.is_ge, op1=ALU.mult))
                bv(lambda e: e.scalar_tensor_tensor(out=B["lo"][:], in0=B["ge"][:], scalar=B["d"][:, 0:1], in1=B["lo"][:], op0=ALU.mult, op1=ALU.add))
            P.op("dve", lambda e, n=n: e.tensor_scalar(out=msk[:, 0:n], in0=Isc[:, 0:n], scalar1=B["lo"][:, 0:1], scalar2=None, op0=ALU.is_ge),
                 reads=[bs_tb, Isc_tb], writes=[msk_tb])
            for kt in range(nkt):
                i2 = kt % 2
                dl = nkt - 1 - kt
                pmT = pb[2].bitcast(BF16)
                P.op("pe", lambda e, kt=kt, pmT=pmT: e.transpose(out=pmT[:, 0:128], in_=msk[:, kt * 128:(kt + 1) * 128], identity=identb[:]),
                     reads=[msk_tb, cst_tb], writes=[bank[2]])
                bS = 3 + i2
                P.op("pe", lambda e, kt=kt, bS=bS, Q=Q: e.matmul(pb[bS][:], lhsT=ckvT[:, kt * 128:(kt + 1) * 128], rhs=Q["ql"][:], start=True, stop=True),
                     reads=[key_tb, qtb], writes=[bank[bS]])
                for h in range(4):
                    P.op("act", lambda e, h=h, bS=bS, i2=i2, dl=dl: e.activation(out=Eb[i2][:, h * 128:(h + 1) * 128], in_=pb[bS][:, h * 128:(h + 1) * 128], func=AF.Exp,
                                                                               bias=ab[:, h, dl:dl + 1], scale=1.0), reads=[bank[bS], dpar_tb], writes=[Eb_tb[i2][h]])
                P.op("dve", lambda e, i2=i2, pmT=pmT: e.tensor_tensor(out=PTb[i2][:].rearrange("p (h t) -> p h t", h=4), in0=Eb[i2][:].rearrange("p (h t) -> p h t", h=4),
                                                                      in1=pmT[:, 0:128].unsqueeze(1).to_broadcast([128, 4, 128]), op=ALU.mult),
                     reads=Eb_tb[i2] + [bank[2]], writes=[PT_tb[i2]])
                P.op("pe", lambda e, kt=kt, i2=i2, nkt=nkt: e.matmul(pb[5][:], lhsT=ckv[:, kt, :], rhs=PTb[i2][:], start=(kt == 0), stop=(kt == nkt - 1)),
                     reads=[key_tb, PT_tb[i2]], writes=[bank[5]], acc=True)
                P.op("pe", lambda e, kt=kt, i2=i2, nkt=nkt: e.matmul(pb[6][0:64, :], lhsT=onesb[:], rhs=PTb[i2][:], start=(kt == 0), stop=(kt == nkt - 1)),
                     reads=[cst_tb, PT_tb[i2]], writes=[bank[6]], acc=True)
            P.op("act", lambda e: e.activation(out=olat[:], in_=pb[5][:], func=AF.Identity), reads=[bank[5]], writes=[fin_tb])
            P.op("dve", lambda e: e.reciprocal(out=drec[:], in_=pb[6][0:64, :]), reads=[bank[6]], writes=[fin_tb])
            for h in range(4):
                P.op("pe", lambda e, h=h: e.matmul(pb[7][0:64, h * 128:(h + 1) * 128], lhsT=wuv[:, h, :], rhs=olat[:, h * 128:(h + 1) * 128], start=True, stop=True),
                     reads=[fin_tb, dpar_tb], writes=[bank[7]], acc=True)
            ob, obtb = obs.next()
            P.op("dve", lambda e, ob=ob: e.tensor_tensor(out=ob[:, 0:512], in0=pb[7][0:64, :], in1=drec[:], op=ALU.mult), reads=[bank[7], fin_tb], writes=[obtb])
            P.op("sp", lambda e, ob=ob, j=j: e.dma_start(out=dr["mixT"][6:10, :, j * 128:(j + 1) * 128].rearrange("h p t -> p h t"),
                                                           in_=ob[:, 0:512].rearrange("p (h t) -> p h t", h=4)), reads=[obtb], writes=[out_tb], dma=True)


def emit_ffn(P, nc, NT, exp_ids, dr, ident, ident_tb):
    PT = min(1024, NT)
    NB = PT // 128
    npass = NT // PT
    NX = len(exp_ids)
    TW = min(512, PT)
    NTT = PT // TW
    BPT = TW // 128
    es = P.es
    wo_tb = P.tb("wo")
    stg = [P.sbuf(f"stg{i}", [128, 2048], F32) for i in range(3)]
    stg_tb = [P.tb(f"stg{i}") for i in range(3)]
    rw = P.sbuf("rw", [128, 8, NE], F32)
    rw_tb = P.tb("rw")
    rbias = P.sbuf("rbias", [128, NE], F32)
    bc = [P.sbuf(f"bc{i}", [128, D], F32) for i in range(3)]
    bc_tb = [P.tb(f"bc{i}") for i in range(3)]
    modT = P.sbuf("modT", [128, 48], F32)
    modT_tb = P.tb("modT")
    eps_t = P.sbuf("eps_t", [128, 1], F32)
    xm = P.sbuf("xm", [128, NB, D], F32)
    xm_tb = [P.tb(f"xm{b}") for b in range(NB)]
    yacc = P.sbuf("yacc", [128, NB, D], F32)
    ya_tb = [P.tb(f"ya{b}") for b in range(NB)]
    h2T = P.sbuf("h2T", [128, 8, PT], BF16)
    h2_tb = [P.tb(f"h2{b}") for b in range(NB)]
    h2f_tb = P.tb("h2f")
    gateT = P.sbuf("gateT", [65, PT], F32)
    gT_tb = [P.tb(f"gT{b}") for b in range(NB)]
    xr = [P.sbuf(f"xr{i}", [128, D], F32) for i in range(2)]
    xr_tb = [P.tb(f"xr{i}") for i in range(2)]
    zt = P.sbuf("zt", [128, D], F32)
    zt_tb = P.tb("zt")
    lnsc = {"st": P.sbuf("ln_st", [128, 2, 6], F32), "mv": P.sbuf("ln_mv", [128, 2], F32),
            "rs": P.sbuf("ln_rs", [128, 1], F32), "xn": P.sbuf("ln_xn", [128, D], F32),
            "tb": P.tb("ln_s"), "xn_tb": P.tb("ln_xn")}
    rt_tb = P.tb("rt")
    wb_tb = [{k: P.tb(f"{k}b{i}") for k in ("w1", "w3", "w2")} for i in range(2)]
    selt_tb = P.tb("selt")
    Gb_tb = P.tb("Gb")
    ssb_tb = [P.tb(f"ssb{i}") for i in range(2)]
    gg_tb = [[P.tb(f"gg{fc}_{tt}") for tt in range(NTT)] for fc in range(2)]
    pb = [P.psum(f"pb{i}", [128, 512], F32) for i in range(8)]
    pb_tb = [P.tb(f"pb{i}") for i in range(8)]

    P.op("pool", lambda e: e.memset(eps_t[:], LN_EPS), writes=[modT_tb])
    P.op("sp", lambda e: e.dma_start(out=modT[:], in_=dr["modT"]), writes=[modT_tb], dma=True)
    P.op("dve", lambda e: e.tensor_scalar(out=modT[:, 32:40], in0=modT[:, 32:40], scalar1=1.0, scalar2=None,
                                          op0=ALU.add), reads=[modT_tb], writes=[modT_tb])
    P.op("sp", lambda e: e.dma_start(out=rw[:], in_=dr["router_w"].rearrange("(kc p) n -> p kc n", p=128)),
         writes=[rw_tb], dma=True)
    P.op("sp", lambda e: e.dma_start(out=rbias[:], in_=dr["rbias_b"]), writes=[rw_tb], dma=True)
    def load_bc(i, src):
        P.op("sp", lambda e: e.dma_start(out=bc[i][:], in_=src), writes=[bc_tb[i]], dma=True)

    def _pass(ps):
        t0 = ps * PT
        load_bc(0, dr["modb"][:, 2 * D:3 * D])
        load_bc(1, dr["lnp"][:, 0, :])
        load_bc(2, dr["lnp"][:, 1, :])
        P.op("pool", lambda e: e.memset(gateT[64:65, :], 1.0), writes=gT_tb)
        _sc1 = P.scope()
        _sc1.__enter__()
        wo = P.sbuf(f"p{ps}_wo", [64, 16, D], BF16)
        mixb = [P.sbuf(f"p{ps}_mixb{i}", [64, 16, 128], BF16) for i in range(2)]
        mixb_tb = [P.tb(f"mixb{i}") for i in range(2)]
        h2f = P.sbuf(f"p{ps}_h2f", [128, 8, 128], F32)
        rt = {k: P.sbuf(f"p{ps}_rt_" + k, [128, n], F32) for k, n in
              [("sc", 64), ("sel", 64), ("eq", 64), ("sel2", 64), ("m1", 8), ("m2", 8), ("grp", 8), ("t8", 8),
               ("gm", 8), ("g4", 8), ("selm", 64), ("em", 64), ("gt", 64), ("den", 1), ("gate", 64)]}
        for c in range(16):
            s_ = c % 3
            P.op("sp", lambda e, c=c, s_=s_: e.dma_start(out=stg[s_][0:64, 0:D], in_=dr["w_out"][c * 64:(c + 1) * 64, :]),
                 writes=[stg_tb[s_]], dma=True)
            P.op("pool", lambda e, c=c, s_=s_: e.tensor_copy(out=wo[:, c, :], in_=stg[s_][0:64, 0:D]),
                 reads=[stg_tb[s_]], writes=[wo_tb])


        for b in range(NB):
            tk = t0 + b * 128
            xi = b % 2
            P.op("sp", lambda e, xi=xi, tk=tk: e.dma_start(out=xr[xi][:], in_=dr["xres"][tk:tk + 128, :]),
                 writes=[xr_tb[xi]], dma=True)
            P.op("sp", lambda e, xi=xi, tk=tk: e.dma_start(out=mixb[xi][:], in_=dr["mixT"][:, :, tk:tk + 128].rearrange("h p t -> p h t")),
                 writes=[mixb_tb[xi]], dma=True)
            for hlf in range(2):
                for c in range(16):
                    P.op("pe", lambda e, c=c, hlf=hlf, xi=xi: e.matmul(
                        pb[hlf][:], lhsT=mixb[xi][:, c, :], rhs=wo[:, c, hlf * 512:(hlf + 1) * 512],
                        start=(c == 0), stop=(c == 15)), reads=[mixb_tb[xi], wo_tb], writes=[pb_tb[hlf]], acc=True)
            for hlf in range(2):
                P.op("dve", lambda e, hlf=hlf: e.tensor_tensor(out=zt[:, hlf * 512:(hlf + 1) * 512], in0=pb[hlf][:],
                                                               in1=bc[0][:, hlf * 512:(hlf + 1) * 512], op=ALU.mult),
                     reads=[pb_tb[hlf], bc_tb[0]], writes=[zt_tb])
            P.op("dve", lambda e, xi=xi: e.scalar_tensor_tensor(out=zt[:], in0=xr[xi][:], scalar=ALPHA, in1=zt[:],
                                                                op0=ALU.mult, op1=ALU.add),
                 reads=[xr_tb[xi], zt_tb], writes=[zt_tb])
            emit_layernorm(P, zt[:], zt_tb, xm[:, b, :], xm_tb[b], bc[1][:], bc[2][:], [bc_tb[1], bc_tb[2]], lnsc, eps_t, "m")
            for kc in range(8):
                bank = 2 + kc // 4
                P.op("pe", lambda e, kc=kc, bank=bank, b=b: e.transpose(
                    out=pb[bank][:, (kc % 4) * 128:(kc % 4 + 1) * 128], in_=xm[:, b, kc * 128:(kc + 1) * 128],
                    identity=ident[:]), reads=[xm_tb[b], ident_tb], writes=[pb_tb[bank]], acc=True)
            for kc in range(8):
                bank = 2 + kc // 4
                src = pb[bank][:, (kc % 4) * 128:(kc % 4 + 1) * 128]
                P.op("act", lambda e, kc=kc, src=src, b=b: e.activation(
                    out=h2T[:, kc, b * 128:(b + 1) * 128], in_=src, func=AF.Identity,
                    bias=modT[:, 24 + kc:25 + kc], scale=modT[:, 32 + kc:33 + kc]),
                    reads=[pb_tb[bank], modT_tb], writes=[h2_tb[b]])
                P.op("act", lambda e, kc=kc, src=src: e.activation(
                    out=h2f[:, kc, :], in_=src, func=AF.Identity,
                    bias=modT[:, 24 + kc:25 + kc], scale=modT[:, 32 + kc:33 + kc]),
                    reads=[pb_tb[bank], modT_tb], writes=[h2f_tb])
            for kc in range(8):
                P.op("pe", lambda e, kc=kc: e.matmul(pb[4][:, 0:NE], lhsT=h2f[:, kc, :], rhs=rw[:, kc, :],
                                                     start=(kc == 0), stop=(kc == 7)),
                     reads=[h2f_tb, rw_tb], writes=[pb_tb[4]], acc=True)
            R = rt
            P.op("act", lambda e: e.activation(out=R["sc"][:], in_=pb[4][:, 0:NE], func=AF.Sigmoid),
                 reads=[pb_tb[4]], writes=[rt_tb])
            dv = lambda fn: P.op("dve", fn, reads=[rt_tb, rw_tb], writes=[rt_tb])
            g3 = lambda t: t[:].rearrange("p (g j) -> p g j", j=8)
            dv(lambda e: e.tensor_tensor(out=R["sel"][:], in0=R["sc"][:], in1=rbias[:], op=ALU.add))
            dv(lambda e: e.tensor_reduce(out=R["m1"][:], in_=g3(R["sel"]), axis=AX.X, op=ALU.max))
            dv(lambda e: e.tensor_tensor(out=g3(R["eq"]), in0=g3(R["sel"]), in1=bcast_last(R["m1"][:], 8), op=ALU.is_equal))
            dv(lambda e: e.scalar_tensor_tensor(out=R["sel2"][:], in0=R["eq"][:], scalar=-4.0, in1=R["sel"][:],
                                                op0=ALU.mult, op1=ALU.add))
            dv(lambda e: e.tensor_reduce(out=R["m2"][:], in_=g3(R["sel2"]), axis=AX.X, op=ALU.max))
            dv(lambda e: e.tensor_tensor(out=R["grp"][:], in0=R["m1"][:], in1=R["m2"][:], op=ALU.add))
            dv(lambda e: e.max(out=R["t8"][:], in_=R["grp"][:]))
            dv(lambda e: e.tensor_scalar(out=R["gm"][:], in0=R["grp"][:], scalar1=R["t8"][:, 3:4], scalar2=None, op0=ALU.is_ge))
            dv(lambda e: e.tensor_scalar(out=R["g4"][:], in0=R["gm"][:], scalar1=4.0, scalar2=-4.0, op0=ALU.mult, op1=ALU.add))
            dv(lambda e: e.tensor_tensor(out=g3(R["selm"]), in0=g3(R["sel"]), in1=bcast_last(R["gm"][:], 8), op=ALU.mult))
            dv(lambda e: e.tensor_tensor(out=g3(R["selm"]), in0=g3(R["selm"]), in1=bcast_last(R["g4"][:], 8), op=ALU.add))
            dv(lambda e: e.max(out=R["t8"][:], in_=R["selm"][:]))
            dv(lambda e: e.tensor_scalar(out=R["em"][:], in0=R["selm"][:], scalar1=R["t8"][:, 7:8], scalar2=None, op0=ALU.is_ge))
            dv(lambda e: e.tensor_tensor(out=R["gt"][:], in0=R["sc"][:], in1=R["em"][:], op=ALU.mult))
            dv(lambda e: e.tensor_reduce(out=R["den"][:], in_=R["gt"][:], axis=AX.X, op=ALU.add))
            dv(lambda e: e.reciprocal(out=R["den"][:], in_=R["den"][:]))
            dv(lambda e: e.tensor_scalar(out=R["gate"][:], in0=R["gt"][:], scalar1=R["den"][:, 0:1], scalar2=2.5,
                                         op0=ALU.mult, op1=ALU.mult))
            P.op("pe", lambda e: e.transpose(out=pb[5][0:64, 0:128], in_=R["gate"][:], identity=ident[:]),
                 reads=[rt_tb, ident_tb], writes=[pb_tb[5]])
            P.op("act", lambda e, b=b: e.activation(out=gateT[0:64, b * 128:(b + 1) * 128], in_=pb[5][0:64, 0:128],
                                                    func=AF.Identity), reads=[pb_tb[5]], writes=[gT_tb[b]])
        _sc1.__exit__(None, None, None)
        _sc2 = P.scope()
        _sc2.__enter__()
        wb = [{"w1": P.sbuf(f"p{ps}_w1b{i}", [128, 8, DE], BF16), "w3": P.sbuf(f"p{ps}_w3b{i}", [128, 8, DE], BF16),
               "w2": P.sbuf(f"p{ps}_w2b{i}", [128, 2, D], BF16)} for i in range(2)]
        selt = P.sbuf(f"p{ps}_selt", [65, 128], F32)
        Gb = P.sbuf(f"p{ps}_Gb", [128, PT], BF16)
        ssb = [P.sbuf(f"p{ps}_ssb{i}", [128, TW], F32) for i in range(2)]
        ggT = P.sbuf(f"p{ps}_ggT", [128, 2, PT], BF16)

        for xi_, eid in enumerate(exp_ids):
            par = xi_ % 2
            W = wb[par]
            Wt = wb_tb[par]
            P.op("sp", lambda e, xi_=xi_: e.dma_start(out=stg[0][:].rearrange("p (kc f) -> p kc f", f=DE),
                                                     in_=dr["ew1"][xi_].rearrange("(kc p) f -> p kc f", p=128)),
                 writes=[stg_tb[0]], dma=True)
            P.op("pool", lambda e, W=W: e.tensor_copy(out=W["w1"][:].rearrange("p kc f -> p (kc f)"), in_=stg[0][:]),
                 reads=[stg_tb[0]], writes=[Wt["w1"]])
            P.op("sp", lambda e, xi_=xi_: e.dma_start(out=stg[1][:].rearrange("p (kc f) -> p kc f", f=DE),
                                                     in_=dr["ew3"][xi_].rearrange("(kc p) f -> p kc f", p=128)),
                 writes=[stg_tb[1]], dma=True)
            P.op("pool", lambda e, W=W: e.tensor_copy(out=W["w3"][:].rearrange("p kc f -> p (kc f)"), in_=stg[1][:]),
                 reads=[stg_tb[1]], writes=[Wt["w3"]])
            P.op("sp", lambda e, xi_=xi_: e.dma_start(out=stg[2][:].rearrange("p (fc d) -> p fc d", d=D),
                                                     in_=dr["ew2"][xi_].rearrange("(fc p) d -> p fc d", p=128)),
                 writes=[stg_tb[2]], dma=True)
            P.op("pool", lambda e, W=W: e.tensor_copy(out=W["w2"][:].rearrange("p fc d -> p (fc d)"), in_=stg[2][:]),
                 reads=[stg_tb[2]], writes=[Wt["w2"]])
            P.op("pool", lambda e, eid=eid: e.tensor_copy(out=selt[:], in_=ident[0:65, eid:eid + 1].to_broadcast([65, 128])),
                 reads=[ident_tb], writes=[selt_tb])
            for tt in range(NTT):
                P.op("pe", lambda e, tt=tt: e.matmul(pb[6][:, 0:TW], lhsT=selt[:], rhs=gateT[:, tt * TW:(tt + 1) * TW],
                                                     start=True, stop=True),
                     reads=[selt_tb] + gT_tb, writes=[pb_tb[6]])
                P.op("act", lambda e, tt=tt: e.activation(out=Gb[:, tt * TW:(tt + 1) * TW], in_=pb[6][:, 0:TW], func=AF.Identity),
                     reads=[pb_tb[6]], writes=[Gb_tb])
            for tt in range(NTT):
                for fc in range(2):
                    i2 = (tt * 2 + fc) % 2
                    b1, b3 = i2, 2 + i2
                    h_tbs = h2_tb[tt * BPT:(tt + 1) * BPT]
                    for kc in range(8):
                        P.op("pe", lambda e, kc=kc, fc=fc, tt=tt, b1=b1, W=W: e.matmul(
                            pb[b1][:, 0:TW], lhsT=W["w1"][:, kc, fc * 128:(fc + 1) * 128], rhs=h2T[:, kc, tt * TW:(tt + 1) * TW],
                            start=(kc == 0), stop=(kc == 7)), reads=[Wt["w1"]] + h_tbs, writes=[pb_tb[b1]], acc=True)
                    for kc in range(8):
                        P.op("pe", lambda e, kc=kc, fc=fc, tt=tt, b3=b3, W=W: e.matmul(
                            pb[b3][:, 0:TW], lhsT=W["w3"][:, kc, fc * 128:(fc + 1) * 128], rhs=h2T[:, kc, tt * TW:(tt + 1) * TW],
                            start=(kc == 0), stop=(kc == 7)), reads=[Wt["w3"]] + h_tbs, writes=[pb_tb[b3]], acc=True)
                    P.op("act", lambda e, i2=i2, b1=b1: e.activation(out=ssb[i2][:], in_=pb[b1][:, 0:TW], func=AF.Silu),
                         reads=[pb_tb[b1]], writes=[ssb_tb[i2]])
                    P.op("dve", lambda e, i2=i2, b3=b3: e.tensor_tensor(out=ssb[i2][:], in0=ssb[i2][:], in1=pb[b3][:, 0:TW], op=ALU.mult),
                         reads=[ssb_tb[i2], pb_tb[b3]], writes=[ssb_tb[i2]])
                    P.op("dve", lambda e, i2=i2, fc=fc, tt=tt: e.tensor_tensor(
                        out=ggT[:, fc, tt * TW:(tt + 1) * TW], in0=ssb[i2][:], in1=Gb[:, tt * TW:(tt + 1) * TW], op=ALU.mult),
                        reads=[ssb_tb[i2], Gb_tb], writes=[gg_tb[fc][tt]])
            for b in range(NB):
                tt = b // BPT
                for dh in range(2):
                    bk = 4 + (b * 2 + dh) % 2
                    for fc in range(2):
                        P.op("pe", lambda e, b=b, dh=dh, fc=fc, bk=bk, W=W: e.matmul(
                            pb[bk][:], lhsT=ggT[:, fc, b * 128:(b + 1) * 128], rhs=W["w2"][:, fc, dh * 512:(dh + 1) * 512],
                            start=(fc == 0), stop=(fc == 1)), reads=[gg_tb[fc][tt], Wt["w2"]], writes=[pb_tb[bk]], acc=True)
                    if xi_ == 0:
                        P.op("dve", lambda e, b=b, dh=dh, bk=bk: e.tensor_copy(out=yacc[:, b, dh * 512:(dh + 1) * 512], in_=pb[bk][:]),
                             reads=[pb_tb[bk]], writes=[ya_tb[b]])
                    else:
                        P.op("dve", lambda e, b=b, dh=dh, bk=bk: e.tensor_tensor(
                            out=yacc[:, b, dh * 512:(dh + 1) * 512], in0=yacc[:, b, dh * 512:(dh + 1) * 512], in1=pb[bk][:], op=ALU.add),
                            reads=[pb_tb[bk], ya_tb[b]], writes=[ya_tb[b]])
        _sc2.__exit__(None, None, None)
        load_bc(0, dr["modb"][:, 5 * D:6 * D])
        load_bc(1, dr["lnp"][:, 2, :])
        load_bc(2, dr["lnp"][:, 3, :])
        for b in range(NB):
            tk = t0 + b * 128
            xi = b % 2
            P.op("dve", lambda e, b=b: e.tensor_tensor(out=zt[:], in0=yacc[:, b, :], in1=bc[0][:], op=ALU.mult),
                 reads=[ya_tb[b], bc_tb[0]], writes=[zt_tb])
            P.op("dve", lambda e, b=b: e.scalar_tensor_tensor(out=zt[:], in0=xm[:, b, :], scalar=ALPHA, in1=zt[:],
                                                              op0=ALU.mult, op1=ALU.add),
                 reads=[xm_tb[b], zt_tb], writes=[zt_tb])
            emit_layernorm(P, zt[:], zt_tb, xr[xi][:], xr_tb[xi], bc[1][:], bc[2][:], [bc_tb[1], bc_tb[2]], lnsc, eps_t, "f")
            P.op("sp", lambda e, xi=xi, tk=tk: e.dma_start(out=dr["out"][tk:tk + 128, :], in_=xr[xi][:]),
                 reads=[xr_tb[xi]], writes=[dr["out_tb"]], dma=True)

    for ps in range(npass):
        _pass(ps)


def emit_k0(P, nc, dr):
    cT = P.sbuf("cT", [128, 8], F32)
    c_tb = P.tb("cT")
    P.op("sp", lambda e: e.dma_start(out=cT[:], in_=dr["cT"]), writes=[c_tb], dma=True)
    P.op("act", lambda e: e.activation(out=cT[:], in_=cT[:], func=AF.Silu), reads=[c_tb], writes=[c_tb])
    wm = [P.sbuf(f"wm{i}", [128, 8, 768], F32) for i in range(2)]
    wm_tb = [P.tb(), P.tb()]
    bm = P.sbuf("bm", [1, 4, 768], F32)
    P.op("sp", lambda e: e.dma_start(out=bm[:], in_=dr["b_mod"].unsqueeze(0)), writes=[c_tb], dma=True)
    ps = [P.psum(f"k0ps{i}", [128, 512], F32) for i in range(2)]
    ps_tb = [P.tb(), P.tb()]
    ot = P.sbuf("k0o", [1, 4, 768], F32)
    o_tb = P.tb("k0o")
    for l in range(4):
        W, wtb = wm[l % 2], wm_tb[l % 2]
        P.op("sp", lambda e, l=l, W=W: e.dma_start(out=W[:], in_=dr["w_mod"][l].rearrange("(kc p) n -> p kc n", p=128)), writes=[wtb], dma=True)
        for hf in range(2):
            for kc in range(8):
                P.op("pe", lambda e, kc=kc, hf=hf, W=W: e.matmul(ps[hf][0:1, 0:384], lhsT=cT[:, kc:kc + 1], rhs=W[:, kc, hf * 384:(hf + 1) * 384],
                                                                 start=(kc == 0), stop=(kc == 7)), reads=[c_tb, wtb], writes=[ps_tb[hf]], acc=True)
            P.op("dve", lambda e, l=l, hf=hf: e.tensor_tensor(out=ot[0:1, l, hf * 384:(hf + 1) * 384], in0=ps[hf][0:1, 0:384],
                                                              in1=bm[0:1, l, hf * 384:(hf + 1) * 384], op=ALU.add), reads=[ps_tb[hf], c_tb], writes=[o_tb])
    P.op("sp", lambda e: e.dma_start(out=dr["mod"].unsqueeze(0), in_=ot[:]), reads=[o_tb], writes=[dr["out_tb"]], dma=True)


D = 1024
def consts128():
    i = np.arange(128)
    ident = np.eye(128, dtype=np.float32)
    bo = (i[:, None] // 64 == i[None, :] // 64).astype(np.float32)
    ls = (i[None, :] < i[:, None]).astype(np.float32)
    us = (i[None, :] > i[:, None]).astype(np.float32)
    ui = (i[None, :] >= i[:, None]).astype(np.float32)
    return np.ascontiguousarray(np.stack([ident, bo, ls, us, ui], 1))

def k1_common(inp, l, mod):
    col = lambda v: np.ascontiguousarray(v.reshape(-1, 128).T)
    par = np.zeros((128, 64), np.float32)
    par[:, 0:11] = col(inp['rwkv_mu'][l])
    par[:, 11:14] = col(inp['rwkv_w0'][l]); par[:, 14:17] = col(inp['rwkv_a0'][l])
    par[:, 17:20] = col(inp['rwkv_k_k'][l]); par[:, 20:23] = col(inp['rwkv_k_a'][l])
    par[:, 26:29] = col(inp['rwkv_r_k'][l].reshape(-1))
    par[:, 29:37] = col(mod[0:D]); par[:, 37:45] = col(mod[D:2 * D])
    bcp = np.zeros((128, 320), np.float32)
    bcp[:, 0:128] = inp['dsa_kv_norm'][l][None]; bcp[:, 128:192] = inp['dsa_ik_g'][l][None]; bcp[:, 192:256] = inp['dsa_ik_b'][l][None]
    lora = np.ascontiguousarray(np.concatenate([inp['rwkv_w2'][l], inp['rwkv_a2'][l]], 0))
    wuk = np.ascontiguousarray(inp['dsa_w_uk'][l].reshape(2, 128, 128).transpose(1, 0, 2))
    return {"par": par, "bcp": bcp, "lora": lora, "g2w": np.ascontiguousarray(inp['rwkv_g2'][l]), "wuk": wuk,
            "cst": consts128(), "w_in": np.ascontiguousarray(inp['w_in'][l])}


def alibi_slopes(n):
    return (2.0 ** (-8.0 * (np.arange(n, dtype=np.float32) + 1.0) / n)).astype(np.float32)

K1_CAT = {"o_bon": 1, "o_g": 1, "o_qlat": 2, "o_iqs": 2, "o_sgn": 0, "o_ckv": 0, "o_ckvT": 1, "o_ikT": 1, "o_sq": 1, "o_sk": 1, "o_sv": 0,
          "o_YwT": 0, "o_Y0T": 0}

def k2_tables(i):
    sl = alibi_slopes(10)
    swa_sl, dsa_sl = sl[:6], sl[6:]
    p = np.arange(128)
    r = np.arange(8)[None, :, None]; pq = p[:, None, None]; pk = p[None, None, :]
    valid = (r < i) | ((r == i) & (pk <= pq))
    dmask = np.where(valid, 0.0, -1e30).astype(np.float32).reshape(128, 1024)
    dl = np.arange(128)[None, None, :]
    ab = (dsa_sl[None, :, None] * (128.0 * (7 - dl - i) + p[:, None, None] - 127.0)).astype(np.float32)
    q = p[None, None, None, :]; pk4 = p[:, None, None, None]; tile = np.arange(2)[None, :, None, None]
    dist = q - pk4 + np.where(tile == 0, 128, 0)
    ok = (dist >= 0) & (dist < 128)
    swb = np.where(ok, -swa_sl[None, None, :, None] * dist, -30000.0).astype(np.float32)
    swb_first = swb[:, 0].copy()
    if i == 0:
        swb_first[:] = -30000.0
    return {"dmask": dmask, "ab": np.ascontiguousarray(ab), "swb": np.ascontiguousarray(swb), "swb_first": np.ascontiguousarray(swb_first),
            "ident": np.eye(128, dtype=np.float32)}


def k2_inputs(K1, inp, l, NBQ):
    G = {n: np.concatenate([K1[c][n] for c in range(8)], ax) for n, ax in K1_CAT.items()}
    PT = np.stack([K1[c]["o_PT"] for c in range(8)]); Qa = np.stack([K1[c]["o_Q"] for c in range(8)])
    T = 8 * NBQ * 128
    bf = G["o_sk"].dtype
    maps = []
    for i in range(8):
        gbs = 8 * np.arange(NBQ) + i
        tok = (gbs[:, None] * 128 + np.arange(128)[None, :]).reshape(-1)
        prev = ((gbs - 1)[:, None] * 128 + np.arange(128)[None, :])
        own = (gbs[:, None] * 128 + np.arange(128)[None, :])
        m = {}
        m["YwT"] = np.ascontiguousarray(G["o_YwT"][gbs]); m["Y0T"] = np.ascontiguousarray(G["o_Y0T"][gbs])
        m["PbT"] = np.ascontiguousarray(PT[gbs // NBQ, gbs % NBQ]); m["Qb"] = np.ascontiguousarray(Qa[gbs // NBQ, gbs % NBQ])
        m["segPT"] = np.ascontiguousarray(PT[:, NBQ]); m["segQ"] = np.ascontiguousarray(Qa[:, NBQ])
        hm = lambda a: np.ascontiguousarray(a.reshape(6, 64, T)[:, :, tok].transpose(1, 0, 2))
        m["bon"] = hm(G["o_bon"]); m["g"] = hm(G["o_g"]); m["sq"] = hm(G["o_sq"])
        m["qlat"] = np.ascontiguousarray(G["o_qlat"][:, :, tok]); m["iqs"] = np.ascontiguousarray(G["o_iqs"][:, :, tok]); m["sgn"] = np.ascontiguousarray(G["o_sgn"][tok])
        sk = G["o_sk"].reshape(2, 64, T); sv = G["o_sv"]
        sk2 = np.zeros((64, 2, NBQ, 256), bf); sv2 = np.zeros((NBQ, 2, 128, 128), bf)
        for j in range(NBQ):
            if gbs[j] > 0:
                sk2[:, :, j, 0:128] = sk[:, :, prev[j]].transpose(1, 0, 2); sv2[j, 0] = sv[prev[j]]
            sk2[:, :, j, 128:256] = sk[:, :, own[j]].transpose(1, 0, 2); sv2[j, 1] = sv[own[j]]
        m["sk2"] = sk2; m["sv2"] = sv2
        m["ckv"] = G["o_ckv"]; m["ckvT"] = G["o_ckvT"]; m["ikT"] = G["o_ikT"]
        m["wuv"] = np.ascontiguousarray(inp['dsa_w_uv'][l])
        m["sink_b"] = np.ascontiguousarray(np.broadcast_to(inp['swa_sinks'][l][None], (64, 6)))
        rl = np.zeros((64, 16), np.float32)
        rl[:, 0:6] = inp['rwkv_ln_g'][l].reshape(6, 64).T; rl[:, 6:12] = inp['rwkv_ln_b'][l].reshape(6, 64).T
        m["rwkv_ln"] = rl
        m.update(k2_tables(i))
        maps.append(m)
    return maps


NPBF = ml_dtypes.bfloat16
NBQ_FULL = 16
_PROGS = {}


def _k1_spec(NBLK):
    NTk = NBLK * 128
    return {"o_YwT": ([NBLK, 6, 64, 128], F32), "o_Y0T": ([NBLK, 6, 64, 128], F32), "o_PT": ([NBLK + 1, 6, 64, 64], F32), "o_Q": ([NBLK + 1, 6, 64, 64], F32),
            "o_bon": ([384, NTk], F32), "o_g": ([384, NTk], F32), "o_qlat": ([128, 4, NTk], BF16), "o_iqs": ([64, 4, NTk], BF16), "o_sgn": ([NTk, 4], F32),
            "o_ckv": ([NTk, 128], BF16), "o_ckvT": ([128, NTk], BF16), "o_ikT": ([64, NTk], BF16), "o_sq": ([384, NTk], BF16), "o_sk": ([128, NTk], BF16),
            "o_sv": ([NTk, 128], BF16)}


def _build_k0():
    nc = bass.Bass("TRN2", target_bir_lowering=False)
    di = lambda n, s: nc.dram_tensor(n, list(s), F32, kind="ExternalInput").ap()
    dr = {"cT": di("cT", [128, 8]), "w_mod": di("w_mod", [4, 1024, 768]), "b_mod": di("b_mod", [4, 768])}
    dr["mod"] = nc.dram_tensor("mod", [4, 768], F32, kind="ExternalOutput").ap()
    with ExitStack() as es:
        P = Prog(nc, es)
        dr["out_tb"] = P.tb("out")
        emit_k0(P, nc, dr)
        P.finish([dr["out_tb"]])
    return nc


def _build_k1(NBLK):
    nc = bass.Bass("TRN2", target_bir_lowering=False)
    NTk = NBLK * 128
    di = lambda n, s: nc.dram_tensor(n, list(s), F32, kind="ExternalInput").ap()
    dr = {"x": di("x", [NTk, D]), "xh": di("xh", [1, D]), "par": di("par", [128, 64]), "bcp": di("bcp", [128, 320]), "lora": di("lora", [128, 384]),
          "g2w": di("g2w", [128, 384]), "wuk": di("wuk", [128, 2, 128]), "cst": di("cst", [128, 5, 128]), "w_in": di("w_in", [D, 2756])}
    for n, (s, dt) in _k1_spec(NBLK).items():
        dr[n] = nc.dram_tensor(n, s, dt, kind="ExternalOutput").ap()
    with ExitStack() as es:
        P = Prog(nc, es)
        dr["out_tb"] = P.tb("out")
        emit_k1(P, nc, NBLK, dr)
        P.finish([dr["out_tb"]])
    return nc


def _k2_in(NBQ):
    return {"YwT": ([NBQ, 6, 64, 128], F32), "Y0T": ([NBQ, 6, 64, 128], F32), "PbT": ([NBQ, 6, 64, 64], F32), "Qb": ([NBQ, 6, 64, 64], F32),
            "segPT": ([8, 6, 64, 64], F32), "segQ": ([8, 6, 64, 64], F32), "bon": ([64, 6, NBQ * 128], F32), "g": ([64, 6, NBQ * 128], F32),
            "sq": ([64, 6, NBQ * 128], BF16), "qlat": ([128, 4, NBQ * 128], BF16), "iqs": ([64, 4, NBQ * 128], BF16), "sgn": ([NBQ * 128, 4], F32),
            "sk2": ([64, 2, NBQ, 256], BF16), "sv2": ([NBQ, 2, 128, 128], BF16), "ckv": ([8 * NBQ * 128, 128], BF16), "ckvT": ([128, 8 * NBQ * 128], BF16),
            "ikT": ([64, 8 * NBQ * 128], BF16), "wuv": ([4, 128, 64], F32), "sink_b": ([64, 6], F32), "rwkv_ln": ([64, 16], F32), "dmask": ([128, 1024], F32),
            "ab": ([128, 4, 128], F32), "swb": ([128, 2, 6, 128], F32), "swb_first": ([128, 6, 128], F32), "ident": ([128, 128], F32)}


def _build_k2(NBQ):
    nc = bass.Bass("TRN2", target_bir_lowering=False)
    dr = {n: nc.dram_tensor(n, s, dt, kind="ExternalInput").ap() for n, (s, dt) in _k2_in(NBQ).items()}
    dr["mixT"] = nc.dram_tensor("mixT", [16, 64, NBQ * 128], BF16, kind="ExternalOutput").ap()
    with ExitStack() as es:
        P = Prog(nc, es)
        dr["out_tb"] = P.tb("out")
        emit_k2(P, nc, NBQ, dr)
        P.finish([dr["out_tb"]])
    return nc


def _build_k3(NT, exp_ids):
    nc = bass.Bass("TRN2", target_bir_lowering=False)
    di = lambda n, s: nc.dram_tensor(n, list(s), F32, kind="ExternalInput").ap()
    NX = len(exp_ids)
    dr = {"xres": di("xres", [NT, D]), "mixT": nc.dram_tensor("mixT", [16, 64, NT], BF16, kind="ExternalInput").ap(),
          "modb": di("modb", [128, 6 * D]), "modT": di("modT", [128, 48]), "lnp": di("lnp", [128, 4, D]), "w_out": di("w_out", [D, D]),
          "router_w": di("router_w", [D, NE]), "rbias_b": di("rbias_b", [128, NE]), "ew1": di("ew1", [NX, D, DE]), "ew3": di("ew3", [NX, D, DE]),
          "ew2": di("ew2", [NX, DE, D]), "ident": di("ident", [128, 128])}
    dr["out"] = nc.dram_tensor("out", [NT, D], F32, kind="ExternalOutput").ap()
    with ExitStack() as es:
        P = Prog(nc, es)
        dr["out_tb"] = P.tb("out")
        ident = P.sbuf("ident_sb", [128, 128], F32)
        ident_tb = P.tb("ident")
        P.op("sp", lambda e: e.dma_start(out=ident[:], in_=dr["ident"]), writes=[ident_tb], dma=True)
        emit_ffn(P, nc, NT, exp_ids, dr, ident, ident_tb)
        P.finish([dr["out_tb"]])
    return nc


def _prog(key, fn):
    if key not in _PROGS:
        _PROGS[key] = fn()
    return _PROGS[key]


def _run(nc, maps):
    res = run_bass_kernel_spmd(nc, maps, core_ids=list(range(8)))
    return [{k: np.asarray(v) for k, v in r.items()} for r in res.results]


def _forward(inp, NBQ=NBQ_FULL, n_layers=4, exp_ids=None):
    NT = NBQ * 128
    T = 8 * NT
    if exp_ids is None:
        exp_ids = list(range(NE)) + [NE]
    inp = {k: np.asarray(v) for k, v in inp.items()}
    cT = np.ascontiguousarray(inp['c'][0].reshape(8, 128).T)
    r0 = _run(_prog("k0", _build_k0), [{"cT": cT, "w_mod": np.ascontiguousarray(inp['w_mod'][:, :, i * 768:(i + 1) * 768]),
                                         "b_mod": np.ascontiguousarray(inp['b_mod'][:, i * 768:(i + 1) * 768])} for i in range(8)])
    mod = np.concatenate([r0[i]["mod"] for i in range(8)], 1)
    x = np.ascontiguousarray(inp['x'][0][:T])
    ident = np.eye(128, dtype=np.float32)
    toks = [((8 * np.arange(NBQ) + i)[:, None] * 128 + np.arange(128)[None, :]).reshape(-1) for i in range(8)]
    for l in range(n_layers):
        common = k1_common(inp, l, mod[l])
        maps = []
        for c in range(8):
            m = dict(common)
            m["par"] = common["par"].copy()
            m["par"][:, 45] = 0.0 if c == 0 else 1.0
            m["x"] = np.ascontiguousarray(x[c * NT:(c + 1) * NT])
            m["xh"] = np.ascontiguousarray(x[c * NT - 1:c * NT]) if c > 0 else np.zeros((1, D), np.float32)
            maps.append(m)
        K1 = _run(_prog(("k1", NBQ), lambda: _build_k1(NBQ)), maps)
        K2 = _run(_prog(("k2", NBQ), lambda: _build_k2(NBQ)), k2_inputs(K1, inp, l, NBQ))
        del K1
        sel = [e for e in exp_ids if e < NE]
        ew1 = np.concatenate([inp['exp_w1'][l][sel], inp['sh_w1'][l][None]], 0)
        ew3 = np.concatenate([inp['exp_w3'][l][sel], inp['sh_w3'][l][None]], 0)
        ew2 = np.concatenate([inp['exp_w2'][l][sel], inp['sh_w2'][l][None]], 0)
        lnp = np.stack([inp['ln_mix_g'][l], inp['ln_mix_b'][l], inp['ln_ffn_g'][l], inp['ln_ffn_b'][l]])
        common3 = {"modb": np.ascontiguousarray(np.broadcast_to(mod[l][None], (128, 6 * D))), "modT": np.ascontiguousarray(mod[l].reshape(48, 128).T),
                   "lnp": np.ascontiguousarray(np.broadcast_to(lnp[None], (128, 4, D))), "w_out": np.ascontiguousarray(inp['w_out'][l]),
                   "router_w": np.ascontiguousarray(inp['router_w'][l]),
                   "rbias_b": np.ascontiguousarray(np.broadcast_to(inp['router_bias'][l][None], (128, NE))), "ew1": ew1, "ew3": ew3, "ew2": ew2, "ident": ident}
        maps3 = [{**common3, "xres": np.ascontiguousarray(x[toks[i]]), "mixT": K2[i]["mixT"]} for i in range(8)]
        K3 = _run(_prog(("k3", NT, tuple(exp_ids)), lambda: _build_k3(NT, exp_ids)), maps3)
        del maps3, ew1, ew3, ew2
        xn = np.empty_like(x)
        for i in range(8):
            xn[toks[i]] = K3[i]["out"]
        x = xn
    return x


def kernel(**inputs):
    x = _forward(inputs)
    return np.ascontiguousarray(x[None].astype(np.float32))
```

```python
import numpy as np
import ml_dtypes


from contextlib import ExitStack
import concourse.bass as bass
import concourse.mybir as mybir
from concourse.bass_utils import run_bass_kernel_spmd

F32 = mybir.dt.float32
BF16 = mybir.dt.bfloat16
AF = mybir.ActivationFunctionType
ALU = mybir.AluOpType
AX = mybir.AxisListType


SKIP_SELF = False


class TB:
    __slots__ = ("name", "w", "r")

    def __init__(self, name="?"):
        self.name = name
        self.w = None
        self.r = {}


class Prog:
    ENGS = ("pe", "dve", "act", "pool", "sp")

    def __init__(self, nc, es, ring=8):
        self.nc = nc
        self.es = es
        self.ops = {e: [] for e in self.ENGS}
        self.cnt = {e: 0 for e in self.ENGS}
        self.waited = {e: {} for e in self.ENGS}
        self.sems = {}
        for e in self.ENGS:
            self.sems["c_" + e] = es.enter_context(nc.semaphore("c_" + e))
        self.ring = {}
        for e in ("sp", "pool", "act"):
            names = [f"d_{e}{i}" for i in range(ring)]
            for n in names:
                self.sems[n] = es.enter_context(nc.semaphore(n))
            self.ring[e] = {"names": names, "uses": [0] * ring, "next": 0}
        self.nbuf = 0

    def tb(self, name=None):
        self.nbuf += 1
        return TB(name or f"b{self.nbuf}")

    def sbuf(self, name, shape, dtype):
        return self.es.enter_context(self.nc.sbuf_tensor("sb_" + name, list(shape), dtype))

    def psum(self, name, shape, dtype=F32):
        return self.es.enter_context(self.nc.psum_tensor("ps_" + name, list(shape), dtype))

    def _need(self, eng, evs):
        need = {}
        for ev in evs:
            if ev is None:
                continue
            k, v = ev
            if need.get(k, 0) < v:
                need[k] = v
        w = self.waited[eng]
        for k, v in need.items():
            if SKIP_SELF and k == "c_" + eng:
                continue
            if w.get(k, 0) < v:
                self.ops[eng].append(("wait", k, v))
                w[k] = v

    def op(self, eng, fn, reads=(), writes=(), dma=False, acc=False):
        evs = []
        for b in reads:
            evs.append(b.w)
        for b in writes:
            if not (acc and eng == "pe" and b.w is not None and b.w[0] == "c_pe"):
                evs.append(b.w)
            for k, v in b.r.items():
                evs.append((k, v))
        if dma:
            rg = self.ring[eng]
            i = rg["next"]
            rg["next"] = (i + 1) % len(rg["names"])
            k = rg["names"][i]
            evs.append((k, 16 * rg["uses"][i]))
            rg["uses"][i] += 1
            ev = (k, 16 * rg["uses"][i])
            inc = 16
        else:
            self.cnt[eng] += 1
            k = "c_" + eng
            ev = (k, self.cnt[eng])
            inc = 1
        self._need(eng, evs)
        self.ops[eng].append(("op", fn, k, inc))
        for b in reads:
            if b.r.get(ev[0], 0) < ev[1]:
                b.r[ev[0]] = ev[1]
        for b in writes:
            b.w = ev
            b.r = {}
        return ev

    def barrier(self):
        evs = [("c_" + x, self.cnt[x]) for x in self.ENGS]
        for e, rg in self.ring.items():
            for n, u in zip(rg["names"], rg["uses"]):
                evs.append((n, 16 * u))
        for e in self.ENGS:
            self._need(e, evs)

    def scope(self):
        prog = self

        class _Scope:
            def __enter__(self_):
                self_.old = prog.es
                self_.st = ExitStack()
                self_.st.__enter__()
                prog.es = self_.st
                return prog

            def __exit__(self_, *a):
                prog.barrier()
                prog.es = self_.old
                return self_.st.__exit__(*a)
        return _Scope()

    def finish(self, out_bufs):
        self._need("sp", [b.w for b in out_bufs])
        nc = self.nc
        sems = self.sems
        ops = self.ops

        def replay(engobj, lst):
            for it in lst:
                if it[0] == "wait":
                    engobj.wait_ge(sems[it[1]], it[2])
                else:
                    ins = it[1](engobj)
                    ins.then_inc(sems[it[2]], it[3])

        with nc.Block() as block:
            @block.tensor
            def _(e):
                replay(e, ops["pe"])

            @block.vector
            def _(e):
                replay(e, ops["dve"])

            @block.scalar
            def _(e):
                replay(e, ops["act"])

            @block.gpsimd
            def _(e):
                replay(e, ops["pool"])

            @block.sync
            def _(e):
                replay(e, ops["sp"])


D = 1024
ALPHA = (2 * 4) ** 0.25
LN_EPS = 1e-5
NE = 64
DE = 256


def bcast_last(ap, n):
    shp = list(ap.shape)
    return ap.unsqueeze(len(shp)).to_broadcast(shp + [n])


class Consts:
    pass


def emit_layernorm(P, z, z_tb, out, out_tb, gb, bb, par_tb, sc, eps_t, tagn):
    st, mv, rs, xn = sc["st"], sc["mv"], sc["rs"], sc["xn"]
    stb = sc["tb"]
    P.op("dve", lambda e: e.bn_stats(out=st[:, 0, :], in_=z[:, 0:512]), reads=[z_tb], writes=[stb])
    P.op("dve", lambda e: e.bn_stats(out=st[:, 1, :], in_=z[:, 512:1024]), reads=[z_tb, stb], writes=[stb])
    P.op("dve", lambda e: e.bn_aggr(out=mv[:], in_=st[:].rearrange("p a b -> p (a b)")), reads=[stb], writes=[stb])
    P.op("act", lambda e: e.activation(out=rs[:], in_=mv[:, 1:2], func=AF.Sqrt, bias=eps_t[:, 0:1], scale=1.0),
         reads=[stb], writes=[stb])
    P.op("dve", lambda e: e.reciprocal(out=rs[:], in_=rs[:]), reads=[stb], writes=[stb])
    P.op("dve", lambda e: e.tensor_scalar(out=xn[:], in0=z, scalar1=mv[:, 0:1], scalar2=rs[:, 0:1],
                                          op0=ALU.subtract, op1=ALU.mult), reads=[z_tb, stb], writes=[sc["xn_tb"]])
    P.op("pool", lambda e: e.tensor_tensor(out=xn[:], in0=xn[:], in1=gb, op=ALU.mult),
         reads=[sc["xn_tb"]] + list(par_tb), writes=[sc["xn_tb"]])
    P.op("pool", lambda e: e.tensor_tensor(out=out, in0=xn[:], in1=bb, op=ALU.add),
         reads=[sc["xn_tb"]] + list(par_tb), writes=[out_tb])


NRW = 1408
C_DQ, C_CKV, C_IQ, C_IK, C_IW = 1408, 1664, 1792, 2048, 2112
C_SQ, C_SK, C_SV = 2116, 2500, 2628
PIN = 2756
DECAY_C = -0.6065306597126334


STOP = 99


class StopEmit(Exception):
    pass


def ck(n):
    if STOP == n:
        raise StopEmit()


class Slots:
    def __init__(self, P, aps, tbs=None):
        self.aps = aps
        self.tbs = tbs if tbs is not None else [P.tb() for _ in aps]
        self.i = 0

    def next(self):
        i = self.i
        self.i = (i + 1) % len(self.aps)
        return self.aps[i], self.tbs[i]


def emit_k1(P, nc, NBLK, dr):
    cst = P.sbuf("cst", [128, 5, 128], F32)
    cst_tb = P.tb("cst")
    P.op("sp", lambda e: e.dma_start(out=cst[:], in_=dr["cst"]), writes=[cst_tb], dma=True)
    ident, BO, LS, US, UI = (cst[:, i, :] for i in range(5))
    identb = P.sbuf("identb", [128, 128], BF16)
    P.op("dve", lambda e: e.tensor_copy(out=identb[:], in_=ident), reads=[cst_tb], writes=[cst_tb])
    par = P.sbuf("par", [128, 64], F32)
    par_tb = P.tb("par")
    P.op("sp", lambda e: e.dma_start(out=par[:], in_=dr["par"]), writes=[par_tb], dma=True)
    P.op("dve", lambda e: e.tensor_scalar(out=par[:, 37:45], in0=par[:, 37:45], scalar1=1.0, scalar2=None, op0=ALU.add),
         reads=[par_tb], writes=[par_tb])
    P.op("dve", lambda e: e.tensor_scalar(out=par[:, 23:26], in0=par[:, 20:23], scalar1=-1.0, scalar2=1.0, op0=ALU.mult, op1=ALU.add),
         reads=[par_tb], writes=[par_tb])
    bcp = P.sbuf("bcp", [128, 320], F32)
    P.op("sp", lambda e: e.dma_start(out=bcp[:], in_=dr["bcp"]), writes=[par_tb], dma=True)
    eps6 = P.sbuf("eps6", [128, 2], F32)
    P.op("pool", lambda e: e.memset(eps6[:, 0:1], 1e-6), writes=[par_tb])
    P.op("pool", lambda e: e.memset(eps6[:, 1:2], 1e-5), writes=[par_tb])
    lora = P.sbuf("lora", [128, 384], F32)
    g2w = P.sbuf("g2w", [128, 384], F32)
    P.op("sp", lambda e: e.dma_start(out=lora[:], in_=dr["lora"]), writes=[par_tb], dma=True)
    P.op("sp", lambda e: e.dma_start(out=g2w[:], in_=dr["g2w"]), writes=[par_tb], dma=True)
    wuk_f = P.sbuf("wuk_f", [128, 2, 128], F32)
    wuk = P.sbuf("wuk", [128, 2, 128], BF16)
    P.op("sp", lambda e: e.dma_start(out=wuk_f[:], in_=dr["wuk"]), writes=[par_tb], dma=True)
    P.op("dve", lambda e: e.tensor_copy(out=wuk[:], in_=wuk_f[:]), reads=[par_tb], writes=[par_tb])
    win = P.sbuf("win", [128, 8, PIN], BF16)
    win_tb = P.tb("win")
    wst = [P.sbuf(f"wst{i}", [128, PIN], F32) for i in range(2)]
    wst_tb = [P.tb() for _ in range(2)]
    for kc in range(8):
        s = kc % 2
        P.op("sp", lambda e, kc=kc, s=s: e.dma_start(out=wst[s][:], in_=dr["w_in"][kc * 128:(kc + 1) * 128, :]),
             writes=[wst_tb[s]], dma=True)
        P.op("pool", lambda e, kc=kc, s=s: e.tensor_copy(out=win[:, kc, :], in_=wst[s][:]), reads=[wst_tb[s]], writes=[win_tb])
    pbk = [P.psum(f"k1pb{i}", [128, 512], F32) for i in range(8)]
    bank_tb = [P.tb(f"bank{i}") for i in range(8)]
    q128 = Slots(P, [pbk[b][:, q * 128:(q + 1) * 128] for q in range(4) for b in (2, 3, 4, 5)],
                 [bank_tb[b] for q in range(4) for b in (2, 3, 4, 5)])
    h256 = Slots(P, [pbk[b][:, q * 256:(q + 1) * 256] for q in range(2) for b in (6, 7)],
                 [bank_tb[b] for q in range(2) for b in (6, 7)])
    pT_tb = [bank_tb[0], bank_tb[1]]
    xb = [P.sbuf(f"xb{i}", [128, D], F32) for i in range(2)]
    xb_tb = [P.tb(), P.tb()]
    hT = P.sbuf("hT", [128, 8, 128], BF16)
    hT_tb = P.tb("hT")
    hh = P.sbuf("hh", [128, 8, 1], BF16)
    pr = P.sbuf("pr", [128, 11, 129], F32)
    pr_tb = P.tb("pr")
    prev = P.sbuf("prevc", [128, 11, 1], F32)
    prev_tb = P.tb("prev")
    X = P.sbuf("X", [128, 11, 128], F32)
    X_tb = P.tb("X")
    dif = P.sbuf("dif", [128, 11, 128], F32)
    fm = {n: P.sbuf("fm_" + n, [128, 3, 128], F32) for n in
          ["lw", "a", "kk", "kp", "t1", "cum", "einc", "eexc", "einv", "eend", "at", "bh", "kh", "rt", "bc", "kc", "g", "bon", "beta"]}
    fa = P.tb("fmall")
    fm_tb = {n: fa for n in fm}
    LA = P.sbuf("LA", [128, 128], F32)
    SG = P.sbuf("SG", [128, 128], F32)
    gC = P.sbuf("gC", [128, 3], F32)
    GCe = P.sbuf("GCe", [128, 3], F32)
    ones = P.sbuf("ones128", [128, 128], F32)
    P.op("pool", lambda e: e.memset(ones[:], 1.0), writes=[par_tb])
    tokm = {n: P.sbuf("tok_" + n, [128, 384], F32) for n in ["at", "bc", "kc", "v"]}
    tok_tb = {n: P.tb("tok_" + n) for n in tokm}
    tk = P.sbuf("tk", [128, 580], F32)
    tk_tb = P.tb("tk")
    HW = [{"MX": P.sbuf(f"MX{i}", [128, 256], F32), "MT": P.sbuf(f"MT{i}", [128, 128], F32), "MKT": P.sbuf(f"MKT{i}", [128, 128], F32),
           "NBT": P.sbuf(f"NBT{i}", [128, 128], F32), "NKT": P.sbuf(f"NKT{i}", [128, 128], F32), "DG": P.sbuf(f"DG{i}", [128, 128], F32)} for i in range(2)]
    HW_tb = [{n: P.tb(f"{n}{i}") for n in ("MX", "MT", "MKT", "NBT", "NKT", "DG")} for i in range(2)]
    ATb = P.sbuf("ATb", [64, 6, 64], F32)
    Db = P.sbuf("Db", [64, 6, 64], F32)
    AD_tb = [P.tb() for _ in range(6)]
    PQ = P.sbuf("PQ", [64, 6, 128], F32)
    PTt = P.sbuf("PTt", [64, 6, 64], F32)
    PQ_tb = [P.tb() for _ in range(6)]
    ost = [P.sbuf(f"ost{i}", [128, 128], F32) for i in range(4)]
    ost_s = Slots(P, [o[:] for o in ost])
    obf = [P.sbuf(f"obf{i}", [128, 512], BF16) for i in range(4)]
    obf_s = Slots(P, [o[:] for o in obf])
    nsc = {"st": P.sbuf("n_st", [128, 6], F32), "mv": P.sbuf("n_mv", [128, 2], F32), "rs": P.sbuf("n_rs", [128, 2], F32),
           "aiw": P.sbuf("n_aiw", [128, 4], F32), "sg": P.sbuf("n_sg", [128, 4], F32), "t": P.sbuf("n_t", [128, 256], F32)}
    nsc_tb = P.tb("nsc")
    out_tb = dr["out_tb"]

    def out_dma(dst, src, rtb):
        P.op("sp", lambda e: e.dma_start(out=dst, in_=src), reads=[rtb], writes=[out_tb], dma=True)

    for hd in range(6):
        P.op("pool", lambda e, hd=hd: e.memset(PQ[:, hd, 64:128], 0.0), writes=[PQ_tb[hd]])
        P.op("pool", lambda e, hd=hd: e.tensor_copy(out=PQ[:, hd, 0:64], in_=ident[0:64, 0:64]), reads=[cst_tb], writes=[PQ_tb[hd]])
        P.op("pool", lambda e, hd=hd: e.tensor_copy(out=PTt[:, hd, :], in_=ident[0:64, 0:64]), reads=[cst_tb], writes=[PQ_tb[hd]])

    def emit_pq_out(b):
        for hd in range(6):
            out_dma(dr["o_PT"][b, hd], PTt[:, hd, :], PQ_tb[hd])
            out_dma(dr["o_Q"][b, hd], PQ[:, hd, 64:128], PQ_tb[hd])

    ck(1)
    P.op("sp", lambda e: e.dma_start(out=xb[1][0:1, :], in_=dr["xh"]), writes=[xb_tb[1]], dma=True)
    for kc in range(8):
        P.op("pe", lambda e, kc=kc: e.transpose(out=pbk[0][:, kc:kc + 1], in_=xb[1][0:1, kc * 128:(kc + 1) * 128],
                                                identity=ident[0:1, 0:1]), reads=[xb_tb[1], cst_tb], writes=[pT_tb[0]], acc=True)
    for kc in range(8):
        P.op("act", lambda e, kc=kc: e.activation(out=hh[:, kc, :], in_=pbk[0][:, kc:kc + 1], func=AF.Identity,
                                                  bias=par[:, 29 + kc:30 + kc], scale=par[:, 37 + kc:38 + kc]),
             reads=[pT_tb[0], par_tb], writes=[hT_tb])
    for c in range(11):
        pa, ptb = q128.next()
        for kc in range(8):
            P.op("pe", lambda e, c=c, kc=kc, pa=pa: e.matmul(pa[:, 0:1], lhsT=win[:, kc, c * 128:(c + 1) * 128], rhs=hh[:, kc, :],
                                                             start=(kc == 0), stop=(kc == 7)), reads=[win_tb, hT_tb], writes=[ptb], acc=True)
        P.op("dve", lambda e, c=c, pa=pa: e.tensor_scalar(out=prev[:, c, :], in0=pa[:, 0:1], scalar1=par[:, 45:46], scalar2=None, op0=ALU.mult),
             reads=[ptb, par_tb], writes=[prev_tb])

    ck(2)
    emit_pq_out(0)
    for b in range(NBLK):
        t0 = b * 128
        xi = b % 2
        P.op("sp", lambda e, xi=xi, t0=t0: e.dma_start(out=xb[xi][:], in_=dr["x"][t0:t0 + 128, :]), writes=[xb_tb[xi]], dma=True)
        for kc in range(8):
            bank = kc // 4
            P.op("pe", lambda e, kc=kc, bank=bank, xi=xi: e.transpose(out=pbk[bank][:, (kc % 4) * 128:(kc % 4 + 1) * 128],
                                                                      in_=xb[xi][:, kc * 128:(kc + 1) * 128], identity=ident),
                 reads=[xb_tb[xi], cst_tb], writes=[pT_tb[bank]], acc=True)
        for kc in range(8):
            bank = kc // 4
            P.op("act", lambda e, kc=kc, bank=bank: e.activation(out=hT[:, kc, :], in_=pbk[bank][:, (kc % 4) * 128:(kc % 4 + 1) * 128],
                                                                 func=AF.Identity, bias=par[:, 29 + kc:30 + kc], scale=par[:, 37 + kc:38 + kc]),
                 reads=[pT_tb[bank], par_tb], writes=[hT_tb])

        ck(3)

        def projT(col0, evac):
            pa, ptb = q128.next()
            for kc in range(8):
                P.op("pe", lambda e, kc=kc, pa=pa: e.matmul(pa, lhsT=win[:, kc, col0:col0 + 128], rhs=hT[:, kc, :],
                                                            start=(kc == 0), stop=(kc == 7)), reads=[win_tb, hT_tb], writes=[ptb], acc=True)
            evac(pa, ptb)

        P.op("pool", lambda e: e.tensor_copy(out=pr[:, :, 0:1], in_=prev[:]), reads=[prev_tb], writes=[pr_tb])
        for c in range(11):
            projT(c * 128, lambda pa, ptb, c=c: P.op("act", lambda e: e.activation(out=pr[:, c, 1:129], in_=pa, func=AF.Identity),
                                                     reads=[ptb], writes=[pr_tb]))
        P.op("pool", lambda e: e.tensor_copy(out=prev[:], in_=pr[:, :, 128:129]), reads=[pr_tb], writes=[prev_tb])
        P.op("dve", lambda e: e.tensor_tensor(out=dif[:], in0=pr[:, :, 0:128], in1=pr[:, :, 1:129], op=ALU.subtract), reads=[pr_tb], writes=[X_tb])
        P.op("dve", lambda e: e.tensor_tensor(out=dif[:], in0=dif[:], in1=bcast_last(par[:, 0:11], 128), op=ALU.mult), reads=[X_tb, par_tb], writes=[X_tb])
        P.op("dve", lambda e: e.tensor_tensor(out=X[:], in0=dif[:], in1=pr[:, :, 1:129], op=ALU.add), reads=[X_tb, pr_tb], writes=[X_tb])
        ck(4)
        r_, k_, v_ = X[:, 0:3, :], X[:, 3:6, :], X[:, 6:9, :]
        P.op("act", lambda e: e.activation(out=LA[0:64, :], in_=X[0:64, 9, :], func=AF.Tanh), reads=[X_tb], writes=[fm_tb["lw"]])
        P.op("act", lambda e: e.activation(out=LA[64:128, :], in_=X[64:128, 9, :], func=AF.Identity), reads=[X_tb], writes=[fm_tb["lw"]])
        P.op("act", lambda e: e.activation(out=SG[:], in_=X[:, 10, :], func=AF.Sigmoid), reads=[X_tb], writes=[fm_tb["g"]])
        for cc in range(3):
            pa, ptb = q128.next()
            P.op("pe", lambda e, cc=cc, pa=pa: e.matmul(pa, lhsT=lora[0:64, cc * 128:(cc + 1) * 128], rhs=LA[0:64, :], start=True, stop=True),
                 reads=[par_tb, fm_tb["lw"]], writes=[ptb])
            P.op("act", lambda e, cc=cc, pa=pa: e.activation(out=fm["lw"][:, cc, :], in_=pa, func=AF.Sigmoid, bias=par[:, 11 + cc:12 + cc], scale=1.0),
                 reads=[ptb, par_tb], writes=[fm_tb["cum"]])
            pa2, ptb2 = q128.next()
            P.op("pe", lambda e, cc=cc, pa2=pa2: e.matmul(pa2, lhsT=lora[64:128, cc * 128:(cc + 1) * 128], rhs=LA[64:128, :], start=True, stop=True),
                 reads=[par_tb, fm_tb["lw"]], writes=[ptb2])
            P.op("act", lambda e, cc=cc, pa2=pa2: e.activation(out=fm["a"][:, cc, :], in_=pa2, func=AF.Sigmoid, bias=par[:, 14 + cc:15 + cc], scale=1.0),
                 reads=[ptb2, par_tb], writes=[fm_tb["a"]])
            pa3, ptb3 = q128.next()
            P.op("pe", lambda e, cc=cc, pa3=pa3: e.matmul(pa3, lhsT=g2w[:, cc * 128:(cc + 1) * 128], rhs=SG[:], start=True, stop=True),
                 reads=[par_tb, fm_tb["g"]], writes=[ptb3])
            P.op("act", lambda e, cc=cc, pa3=pa3: e.activation(out=fm["g"][:, cc, :], in_=pa3, func=AF.Identity), reads=[ptb3], writes=[fm_tb["bon"]])
        dv = lambda fn, rd, wr: P.op("dve", fn, reads=[fm_tb[n] if isinstance(n, str) else n for n in rd],
                                     writes=[fm_tb[n] if isinstance(n, str) else n for n in wr])
        F = fm
        pcol = lambda c0: bcast_last(par[:, c0:c0 + 3], 128)
        dv(lambda e: e.tensor_scalar(out=F["lw"][:], in0=F["lw"][:], scalar1=DECAY_C, scalar2=None, op0=ALU.mult), ["cum"], ["cum"])
        out_dma(dr["o_g"].rearrange("(c p) t -> p c t", p=128)[:, :, t0:t0 + 128], F["g"][:], fm_tb["bon"])
        dv(lambda e: e.tensor_tensor(out=F["kk"][:], in0=k_, in1=pcol(17), op=ALU.mult), [X_tb, par_tb], ["kk"])
        dv(lambda e: e.tensor_tensor(out=F["t1"][:], in0=F["kk"][:], in1=F["kk"][:], op=ALU.mult), ["kk"], ["t1"])
        for cc in range(3):
            pa, ptb = q128.next()
            P.op("pe", lambda e, cc=cc, pa=pa: e.matmul(pa, lhsT=BO, rhs=F["t1"][:, cc, :], start=True, stop=True), reads=[cst_tb, fm_tb["t1"]], writes=[ptb])
            P.op("act", lambda e, cc=cc, pa=pa: e.activation(out=F["kp"][:, cc, :], in_=pa, func=AF.Sqrt), reads=[ptb], writes=[fm_tb["kp"]])
        dv(lambda e: e.tensor_scalar(out=F["kp"][:], in0=F["kp"][:], scalar1=1e-12, scalar2=None, op0=ALU.max), ["kp"], ["kp"])
        dv(lambda e: e.reciprocal(out=F["kp"][:], in_=F["kp"][:]), ["kp"], ["kp"])
        dv(lambda e: e.tensor_tensor(out=F["kk"][:], in0=F["kk"][:], in1=F["kp"][:], op=ALU.mult), ["kk", "kp"], ["kk"])
        dv(lambda e: e.tensor_tensor(out=F["t1"][:], in0=F["a"][:], in1=pcol(20), op=ALU.mult), ["a", par_tb], ["t1"])
        dv(lambda e: e.tensor_tensor(out=F["t1"][:], in0=F["t1"][:], in1=pcol(23), op=ALU.add), ["t1", par_tb], ["t1"])
        dv(lambda e: e.tensor_tensor(out=F["kp"][:], in0=k_, in1=F["t1"][:], op=ALU.mult), [X_tb, "t1", "kp"], ["kp"])
        dv(lambda e: e.tensor_tensor(out=F["t1"][:], in0=r_, in1=pcol(26), op=ALU.mult), [X_tb, par_tb], ["t1"])
        dv(lambda e: e.tensor_tensor(out=F["t1"][:], in0=F["t1"][:], in1=F["kp"][:], op=ALU.mult), ["t1", "kp"], ["t1"])
        for cc in range(3):
            pa, ptb = q128.next()
            P.op("pe", lambda e, cc=cc, pa=pa: e.matmul(pa, lhsT=BO, rhs=F["t1"][:, cc, :], start=True, stop=True), reads=[cst_tb, fm_tb["t1"]], writes=[ptb])
            P.op("dve", lambda e, cc=cc, pa=pa: e.tensor_tensor(out=F["bon"][:, cc, :], in0=pa, in1=X[:, 6 + cc, :], op=ALU.mult),
                 reads=[ptb, X_tb], writes=[fm_tb["g"]])
        out_dma(dr["o_bon"].rearrange("(c p) t -> p c t", p=128)[:, :, t0:t0 + 128], F["bon"][:], fm_tb["g"])
        dv(lambda e: e.tensor_tensor(out=F["beta"][:], in0=F["a"][:], in1=F["kk"][:], op=ALU.mult), ["a", "kk"], ["beta"])
        for cc in range(3):
            dv(lambda e, cc=cc: e.tensor_tensor_scan(out=F["cum"][:, cc, :], data0=ones[:], data1=F["lw"][:, cc, :], initial=0.0,
                                                     op0=ALU.mult, op1=ALU.add), ["cum", par_tb], ["einc"])
        dv(lambda e: e.tensor_copy(out=gC[:], in_=F["cum"][:, :, 127]), ["einc"], ["einc"])
        dv(lambda e: e.tensor_tensor(out=F["t1"][:], in0=F["cum"][:], in1=F["lw"][:], op=ALU.subtract), ["einc", "t1"], ["t1"])
        ac = lambda fn, rd, wr: P.op("act", fn, reads=[fm_tb[n] for n in rd], writes=[fm_tb[n] for n in wr])
        ac(lambda e: e.activation(out=F["einc"][:], in_=F["cum"][:], func=AF.Exp), ["einc"], ["eexc"])
        ac(lambda e: e.activation(out=F["eexc"][:], in_=F["t1"][:], func=AF.Exp), ["t1"], ["einv"])
        ac(lambda e: e.activation(out=F["einv"][:], in_=F["cum"][:], func=AF.Exp, scale=-1.0), ["einc"], ["eend"])
        for cc in range(3):
            ac(lambda e, cc=cc: e.activation(out=F["eend"][:, cc, :], in_=F["cum"][:, cc, :], func=AF.Exp, scale=-1.0, bias=gC[:, cc:cc + 1]),
               ["einc"], ["at"])
        ac(lambda e: e.activation(out=GCe[:], in_=gC[:], func=AF.Exp), ["einc"], ["at"])
        dv(lambda e: e.scalar_tensor_tensor(out=F["at"][:], in0=F["kk"][:], scalar=-1.0, in1=F["eexc"][:], op0=ALU.mult, op1=ALU.mult),
           ["kk", "einv", "at"], ["bh"])
        dv(lambda e: e.tensor_tensor(out=F["bh"][:], in0=F["beta"][:], in1=F["einv"][:], op=ALU.mult), ["beta", "eend"], ["kh"])
        dv(lambda e: e.tensor_tensor(out=F["kh"][:], in0=F["kp"][:], in1=F["einv"][:], op=ALU.mult), ["kp", "eend"], ["rt"])
        dv(lambda e: e.tensor_tensor(out=F["rt"][:], in0=r_, in1=F["einc"][:], op=ALU.mult), [X_tb, "eexc"], ["bc"])
        dv(lambda e: e.tensor_tensor(out=F["bc"][:], in0=F["beta"][:], in1=F["eend"][:], op=ALU.mult), ["beta", "at"], ["kc"])
        dv(lambda e: e.tensor_tensor(out=F["kc"][:], in0=F["kp"][:], in1=F["eend"][:], op=ALU.mult), ["kp", "at"], ["lw"])
        allf = [fm_tb[n] for n in ("bh", "kh", "rt", "bc", "kc", "lw")]
        ck(5)
        for nm, src, stb in (("at", F["at"], fm_tb["bh"]), ("bc", F["bc"], fm_tb["kc"]), ("kc", F["kc"], fm_tb["lw"]), ("v", None, X_tb)):
            for cc in range(3):
                pa, ptb = q128.next()
                s_ap = X[:, 6 + cc, :] if src is None else src[:, cc, :]
                P.op("pe", lambda e, pa=pa, s_ap=s_ap: e.transpose(out=pa, in_=s_ap, identity=ident), reads=[stb, cst_tb], writes=[ptb])
                P.op("act", lambda e, pa=pa, nm=nm, cc=cc: e.activation(out=tokm[nm][:, cc * 128:(cc + 1) * 128], in_=pa, func=AF.Identity),
                     reads=[ptb], writes=[tok_tb[nm]])
        ck(6)
        def _head(hd, b=b):
            cc, p0 = hd // 2, (hd % 2) * 64
            sl = slice(p0, p0 + 64)
            aT, bT, kT, rT = F["at"][sl, cc, :], F["bh"][sl, cc, :], F["kh"][sl, cc, :], F["rt"][sl, cc, :]
            tcol = slice(hd * 64, hd * 64 + 64)
            MX, MT, MKT, NBT, NKT, DG = (HW[hd % 2][n] for n in ("MX", "MT", "MKT", "NBT", "NKT", "DG"))
            MX_tb, MT_tb, MKT_tb, NBT_tb, NKT_tb, DG_tb = (HW_tb[hd % 2][n] for n in ("MX", "MT", "MKT", "NBT", "NKT", "DG"))

            def mm_mask(lhsT, rhs, mask, dst, dtb):
                pa, ptb = q128.next()
                P.op("pe", lambda e: e.matmul(pa, lhsT=lhsT, rhs=rhs, start=True, stop=True), reads=allf, writes=[ptb])
                P.op("dve", lambda e: e.tensor_tensor(out=dst, in0=pa, in1=mask, op=ALU.mult), reads=[ptb, cst_tb], writes=[dtb])

            mm_mask(aT, bT, LS, MX[:, 0:128], MX_tb)
            mm_mask(bT, aT, US, MT[:], MT_tb)
            mm_mask(kT, aT, US, MKT[:], MKT_tb)
            mm_mask(bT, rT, UI, NBT[:], NBT_tb)
            mm_mask(kT, rT, UI, NKT[:], NKT_tb)
            P.op("pool", lambda e, tcol=tcol: e.tensor_copy(out=MX[:, 128:192], in_=tokm["at"][:, tcol]), reads=[tok_tb["at"]], writes=[MX_tb])
            pa, ptb = q128.next()
            P.op("pe", lambda e, pa=pa, tcol=tcol: e.matmul(pa[:, 0:64], lhsT=MKT[:], rhs=tokm["v"][:, tcol], start=True, stop=True),
                 reads=[MKT_tb, tok_tb["v"]], writes=[ptb])
            P.op("act", lambda e, pa=pa: e.activation(out=MX[:, 192:256], in_=pa[:, 0:64], func=AF.Identity), reads=[ptb], writes=[MX_tb])
            for it in range(7):
                last = it == 6
                ph, phtb = h256.next()
                if not last:
                    P.op("pe", lambda e, ph=ph: e.matmul(ph, lhsT=MT[:], rhs=MX[:], start=True, stop=True), reads=[MT_tb, MX_tb], writes=[phtb])
                    pa, ptb = q128.next()
                    P.op("pe", lambda e, pa=pa: e.matmul(pa, lhsT=MX[:, 0:128], rhs=MT[:], start=True, stop=True), reads=[MT_tb, MX_tb], writes=[ptb])
                    P.op("act", lambda e, ph=ph: e.activation(out=MX[:, 0:128], in_=ph[:, 0:128], func=AF.Identity), reads=[phtb], writes=[MX_tb])
                    P.op("dve", lambda e, ph=ph: e.tensor_tensor(out=MX[:, 128:256], in0=MX[:, 128:256], in1=ph[:, 128:256], op=ALU.add),
                         reads=[phtb, MX_tb], writes=[MX_tb])
                    P.op("act", lambda e, pa=pa: e.activation(out=MT[:], in_=pa, func=AF.Identity), reads=[ptb], writes=[MT_tb])
                else:
                    P.op("pe", lambda e, ph=ph: e.matmul(ph[:, 128:256], lhsT=MT[:], rhs=MX[:, 128:256], start=True, stop=True),
                         reads=[MT_tb, MX_tb], writes=[phtb])
                    P.op("dve", lambda e, ph=ph: e.tensor_tensor(out=MX[:, 128:256], in0=MX[:, 128:256], in1=ph[:, 128:256], op=ALU.add),
                         reads=[phtb, MX_tb], writes=[MX_tb])
            W0, U0 = MX[:, 128:192], MX[:, 192:256]
            P.op("dve", lambda e, p0=p0, cc=cc: e.tensor_scalar(out=DG[:, 0:64], in0=cst[:, 0, p0:p0 + 64], scalar1=GCe[:, cc:cc + 1], scalar2=None, op0=ALU.mult),
                 reads=[cst_tb, fm_tb["at"]], writes=[DG_tb])
            pa, ptb = q128.next()
            P.op("pe", lambda e, pa=pa, tcol=tcol: e.matmul(pa[0:64, 0:64], lhsT=W0, rhs=tokm["bc"][:, tcol], start=True, stop=False),
                 reads=[MX_tb, tok_tb["bc"]], writes=[ptb])
            P.op("pe", lambda e, pa=pa, p0=p0: e.matmul(pa[0:64, 0:64], lhsT=cst[:, 0, p0:p0 + 64], rhs=DG[:, 0:64], start=False, stop=True),
                 reads=[DG_tb, cst_tb], writes=[ptb], acc=True)
            P.op("act", lambda e, pa=pa, hd=hd: e.activation(out=ATb[:, hd, :], in_=pa[0:64, 0:64], func=AF.Identity), reads=[ptb], writes=[AD_tb[hd]])
            pa, ptb = q128.next()
            P.op("pe", lambda e, pa=pa, tcol=tcol: e.matmul(pa[0:64, 0:64], lhsT=tokm["bc"][:, tcol], rhs=U0, start=True, stop=False),
                 reads=[MX_tb, tok_tb["bc"]], writes=[ptb])
            P.op("pe", lambda e, pa=pa, tcol=tcol: e.matmul(pa[0:64, 0:64], lhsT=tokm["kc"][:, tcol], rhs=tokm["v"][:, tcol], start=False, stop=True),
                 reads=[tok_tb["kc"], tok_tb["v"]], writes=[ptb], acc=True)
            P.op("act", lambda e, pa=pa, hd=hd: e.activation(out=Db[:, hd, :], in_=pa[0:64, 0:64], func=AF.Identity), reads=[ptb], writes=[AD_tb[hd]])
            pa, ptb = q128.next()
            P.op("pe", lambda e, pa=pa: e.matmul(pa[0:64, :], lhsT=W0, rhs=NBT[:], start=True, stop=False), reads=[MX_tb, NBT_tb], writes=[ptb])
            P.op("pe", lambda e, pa=pa, p0=p0, cc=cc: e.matmul(pa[0:64, :], lhsT=cst[:, 0, p0:p0 + 64], rhs=F["rt"][:, cc, :], start=False, stop=True),
                 reads=[cst_tb] + allf, writes=[ptb], acc=True)
            oa, otb = ost_s.next()
            P.op("act", lambda e, pa=pa, oa=oa: e.activation(out=oa[0:64, :], in_=pa[0:64, :], func=AF.Identity), reads=[ptb], writes=[otb])
            out_dma(dr["o_YwT"][b, hd], oa[0:64, :], otb)
            pa, ptb = q128.next()
            P.op("pe", lambda e, pa=pa: e.matmul(pa[0:64, :], lhsT=U0, rhs=NBT[:], start=True, stop=False), reads=[MX_tb, NBT_tb], writes=[ptb])
            P.op("pe", lambda e, pa=pa, tcol=tcol: e.matmul(pa[0:64, :], lhsT=tokm["v"][:, tcol], rhs=NKT[:], start=False, stop=True),
                 reads=[tok_tb["v"], NKT_tb], writes=[ptb], acc=True)
            oa, otb = ost_s.next()
            P.op("act", lambda e, pa=pa, oa=oa: e.activation(out=oa[0:64, :], in_=pa[0:64, :], func=AF.Identity), reads=[ptb], writes=[otb])
            out_dma(dr["o_Y0T"][b, hd], oa[0:64, :], otb)
            pa, ptb = q128.next()
            P.op("pe", lambda e, pa=pa, hd=hd: e.matmul(pa[0:64, :], lhsT=ATb[:, hd, :], rhs=PQ[:, hd, :], start=True, stop=True),
                 reads=[AD_tb[hd], PQ_tb[hd]], writes=[ptb])
            pa2, ptb2 = q128.next()
            P.op("pe", lambda e, pa2=pa2, hd=hd: e.matmul(pa2[0:64, 0:64], lhsT=PQ[:, hd, 0:64], rhs=ATb[:, hd, :], start=True, stop=True),
                 reads=[AD_tb[hd], PQ_tb[hd]], writes=[ptb2])
            P.op("act", lambda e, pa=pa, hd=hd: e.activation(out=PQ[:, hd, 0:64], in_=pa[0:64, 0:64], func=AF.Identity), reads=[ptb], writes=[PQ_tb[hd]])
            P.op("dve", lambda e, pa=pa, hd=hd: e.tensor_tensor(out=PQ[:, hd, 64:128], in0=pa[0:64, 64:128], in1=Db[:, hd, :], op=ALU.add),
                 reads=[ptb, AD_tb[hd]], writes=[PQ_tb[hd]])
            P.op("act", lambda e, pa2=pa2, hd=hd: e.activation(out=PTt[:, hd, :], in_=pa2[0:64, 0:64], func=AF.Identity), reads=[ptb2], writes=[PQ_tb[hd]])
        for hd in range(6):
            _head(hd)
        ck(7)
        emit_pq_out(b + 1)

        for m in range(2):
            def ev(pa, ptb, m=m):
                oa, otb = obf_s.next()
                P.op("act", lambda e: e.activation(out=oa[:, 0:128], in_=pa, func=AF.Identity), reads=[ptb], writes=[otb])
                for hh_ in range(2):
                    h = 2 * m + hh_
                    s2 = slice(hh_ * 64, hh_ * 64 + 64)
                    pq, pqtb = q128.next()
                    P.op("pe", lambda e, pq=pq, s2=s2: e.matmul(pq, lhsT=wuk[s2, m, :], rhs=oa[s2, 0:128], start=True, stop=True), reads=[otb, par_tb], writes=[pqtb])
                    ob, obtb = obf_s.next()
                    P.op("act", lambda e, pq=pq, ob=ob: e.activation(out=ob[:, 0:128], in_=pq, func=AF.Identity, scale=0.125), reads=[pqtb], writes=[obtb])
                    out_dma(dr["o_qlat"][:, h, t0:t0 + 128], ob[:, 0:128], obtb)
            projT(C_DQ + m * 128, ev)
        for m in range(3):
            def ev(pa, ptb, m=m):
                oa, otb = obf_s.next()
                P.op("act", lambda e: e.activation(out=oa[:, 0:128], in_=pa, func=AF.Identity), reads=[ptb], writes=[otb])
                out_dma(dr["o_sq"][m * 128:(m + 1) * 128, t0:t0 + 128], oa[:, 0:128], otb)
            projT(C_SQ + m * 128, ev)

        def ev(pa, ptb):
            oa, otb = obf_s.next()
            P.op("act", lambda e: e.activation(out=oa[:, 0:128], in_=pa, func=AF.Identity), reads=[ptb], writes=[otb])
            out_dma(dr["o_sk"][:, t0:t0 + 128], oa[:, 0:128], otb)
        projT(C_SK, ev)
        ck(8)
        for (col0, ncol, off, bank) in ((C_CKV, 452, 0, 0), (C_SV, 128, 452, 1)):
            for kc in range(8):
                P.op("pe", lambda e, kc=kc, col0=col0, ncol=ncol, bank=bank: e.matmul(pbk[bank][:, 0:ncol], lhsT=hT[:, kc, :], rhs=win[:, kc, col0:col0 + ncol],
                                                                                      start=(kc == 0), stop=(kc == 7)),
                     reads=[win_tb, hT_tb], writes=[pT_tb[bank]], acc=True)
            P.op("act", lambda e, ncol=ncol, off=off, bank=bank: e.activation(out=tk[:, off:off + ncol], in_=pbk[bank][:, 0:ncol], func=AF.Identity),
                 reads=[pT_tb[bank]], writes=[tk_tb])
        N = nsc
        nv = lambda fn: P.op("dve", fn, reads=[tk_tb, nsc_tb, par_tb], writes=[nsc_tb])
        nv(lambda e: e.tensor_tensor(out=N["t"][:, 0:128], in0=tk[:, 0:128], in1=tk[:, 0:128], op=ALU.mult))
        nv(lambda e: e.tensor_reduce(out=N["rs"][:, 0:1], in_=N["t"][:, 0:128], axis=AX.X, op=ALU.add))
        P.op("act", lambda e: e.activation(out=N["rs"][:, 0:1], in_=N["rs"][:, 0:1], func=AF.Sqrt, scale=1.0 / 128, bias=eps6[:, 0:1]),
             reads=[nsc_tb, par_tb], writes=[nsc_tb])
        nv(lambda e: e.reciprocal(out=N["rs"][:, 0:1], in_=N["rs"][:, 0:1]))
        nv(lambda e: e.scalar_tensor_tensor(out=N["t"][:, 0:128], in0=tk[:, 0:128], scalar=N["rs"][:, 0:1], in1=bcp[:, 0:128], op0=ALU.mult, op1=ALU.mult))
        oa, otb = obf_s.next()
        P.op("dve", lambda e, oa=oa: e.tensor_copy(out=oa[:, 0:128], in_=N["t"][:, 0:128]), reads=[nsc_tb], writes=[otb])
        out_dma(dr["o_ckv"][t0:t0 + 128, :], oa[:, 0:128], otb)
        pa, ptb = q128.next()
        pab = pa.bitcast(BF16)
        P.op("pe", lambda e, pab=pab, oa=oa: e.transpose(out=pab[:, 0:128], in_=oa[:, 0:128], identity=identb[:]), reads=[otb, cst_tb], writes=[ptb])
        P.op("act", lambda e, pab=pab, oa=oa: e.activation(out=oa[:, 128:256], in_=pab[:, 0:128], func=AF.Identity), reads=[ptb], writes=[otb])
        out_dma(dr["o_ckvT"][:, t0:t0 + 128], oa[:, 128:256], otb)
        nv(lambda e: e.bn_stats(out=N["st"][:], in_=tk[:, 384:448]))
        nv(lambda e: e.bn_aggr(out=N["mv"][:], in_=N["st"][:]))
        P.op("act", lambda e: e.activation(out=N["rs"][:, 1:2], in_=N["mv"][:, 1:2], func=AF.Sqrt, scale=1.0, bias=eps6[:, 1:2]),
             reads=[nsc_tb, par_tb], writes=[nsc_tb])
        nv(lambda e: e.reciprocal(out=N["rs"][:, 1:2], in_=N["rs"][:, 1:2]))
        nv(lambda e: e.tensor_scalar(out=N["t"][:, 128:192], in0=tk[:, 384:448], scalar1=N["mv"][:, 0:1], scalar2=N["rs"][:, 1:2], op0=ALU.subtract, op1=ALU.mult))
        nv(lambda e: e.tensor_tensor(out=N["t"][:, 128:192], in0=N["t"][:, 128:192], in1=bcp[:, 128:192], op=ALU.mult))
        oa, otb = obf_s.next()
        P.op("dve", lambda e, oa=oa: e.tensor_tensor(out=oa[:, 0:64], in0=N["t"][:, 128:192], in1=bcp[:, 192:256], op=ALU.add), reads=[nsc_tb, par_tb], writes=[otb])
        pa, ptb = q128.next()
        pab = pa.bitcast(BF16)
        P.op("pe", lambda e, pab=pab, oa=oa: e.transpose(out=pab[0:64, 0:128], in_=oa[:, 0:64], identity=identb[:]), reads=[otb, cst_tb], writes=[ptb])
        P.op("act", lambda e, pab=pab, oa=oa: e.activation(out=oa[0:64, 128:256], in_=pab[0:64, 0:128], func=AF.Identity), reads=[ptb], writes=[otb])
        out_dma(dr["o_ikT"][:, t0:t0 + 128], oa[0:64, 128:256], otb)
        P.op("act", lambda e: e.activation(out=N["sg"][:], in_=tk[:, 448:452], func=AF.Sign), reads=[tk_tb], writes=[nsc_tb])
        nv(lambda e: e.tensor_tensor(out=N["aiw"][:], in0=tk[:, 448:452], in1=N["sg"][:], op=ALU.mult))
        oa, otb = ost_s.next()
        P.op("dve", lambda e, oa=oa: e.tensor_copy(out=oa[:, 0:4], in_=N["sg"][:]), reads=[nsc_tb], writes=[otb])
        out_dma(dr["o_sgn"][t0:t0 + 128, :], oa[:, 0:4], otb)
        ob, obtb = obf_s.next()
        for h in range(4):
            P.op("dve", lambda e, h=h, ob=ob: e.tensor_scalar(out=ob[:, h * 64:(h + 1) * 64], in0=tk[:, 128 + h * 64:128 + (h + 1) * 64],
                                                              scalar1=N["aiw"][:, h:h + 1], scalar2=1.0 / 16, op0=ALU.mult, op1=ALU.mult),
                 reads=[tk_tb, nsc_tb], writes=[obtb])
        oc, octb = obf_s.next()
        for h in range(4):
            pa, ptb = q128.next()
            pab = pa.bitcast(BF16)
            P.op("pe", lambda e, pab=pab, ob=ob, h=h: e.transpose(out=pab[0:64, 0:128], in_=ob[:, h * 64:(h + 1) * 64], identity=identb[:]),
                 reads=[obtb, cst_tb], writes=[ptb])
            P.op("act", lambda e, pab=pab, oc=oc, h=h: e.activation(out=oc[0:64, h * 128:(h + 1) * 128], in_=pab[0:64, 0:128], func=AF.Identity),
                 reads=[ptb], writes=[octb])
        out_dma(dr["o_iqs"][:, :, t0:t0 + 128], oc[0:64, :].rearrange("p (h t) -> p h t", h=4), octb)
        od, odtb = obf_s.next()
        P.op("dve", lambda e, od=od: e.tensor_copy(out=od[:, 0:128], in_=tk[:, 452:580]), reads=[tk_tb], writes=[odtb])
        out_dma(dr["o_sv"][t0:t0 + 128, :], od[:, 0:128], odtb)


GN_EPS = 64e-5
NITER = 16
TOPK = 256


def emit_k2(P, nc, NBQ, dr, parts=("rwkv", "swa", "dsa")):
    NT = NBQ * 128
    NKT = 8 * NBQ
    seg_of = lambda j: (8 * j) // NBQ
    out_tb = dr["out_tb"]
    pb = [P.psum(f"k2pb{i}", [128, 512], F32) for i in range(8)]
    bank = [P.tb(f"k2bank{i}") for i in range(8)]
    cst = P.sbuf("cst2", [128, 128], F32)
    cst_tb = P.tb("cst2")
    P.op("sp", lambda e: e.dma_start(out=cst[:], in_=dr["ident"]), writes=[cst_tb], dma=True)
    identb = P.sbuf("identb2", [128, 128], BF16)
    onesb = P.sbuf("onesb", [128, 64], BF16)
    o64 = P.sbuf("o64", [64, 64], F32)
    P.op("dve", lambda e: e.tensor_copy(out=identb[:], in_=cst[:]), reads=[cst_tb], writes=[cst_tb])
    P.op("pool", lambda e: e.memset(onesb[:], 1.0), writes=[cst_tb])
    P.op("pool", lambda e: e.memset(o64[:], 1.0 / 64), writes=[cst_tb])
    obuf = [P.sbuf(f"k2o{i}", [64, 512], BF16) for i in range(4)]
    obs = Slots(P, [o[:] for o in obuf])

    def out_mix(head, j, src, stb):
        P.op("sp", lambda e: e.dma_start(out=dr["mixT"][head, :, j * 128:(j + 1) * 128], in_=src), reads=[stb], writes=[out_tb], dma=True)

    if "rwkv" in parts:
      with P.scope():
        rp = P.sbuf("rp", [64, 16], F32)
        rp_tb = P.tb("rp")
        P.op("sp", lambda e: e.dma_start(out=rp[:], in_=dr["rwkv_ln"]), writes=[rp_tb], dma=True)
        epsg = P.sbuf("epsg", [64, 1], F32)
        P.op("pool", lambda e: e.memset(epsg[:], GN_EPS), writes=[rp_tb])
        segP = P.sbuf("segP", [64, 8, 6, 64], F32)
        segQ = P.sbuf("segQ", [64, 8, 6, 64], F32)
        seg_tb = P.tb("seg")
        P.op("sp", lambda e: e.dma_start(out=segP[:], in_=dr["segPT"].rearrange("s h a b -> a s h b")), writes=[seg_tb], dma=True)
        P.op("sp", lambda e: e.dma_start(out=segQ[:], in_=dr["segQ"].rearrange("s h a b -> a s h b")), writes=[seg_tb], dma=True)
        St = P.sbuf("St", [64, 8, 6, 64], F32)
        St_tb = [P.tb(f"St{s}") for s in range(8)]
        P.op("pool", lambda e: e.memset(St[:, 0, :, :], 0.0), writes=[St_tb[0]])
        for s in range(7):
            for hd in range(6):
                P.op("pe", lambda e, s=s, hd=hd: e.matmul(pb[0][0:64, hd * 64:(hd + 1) * 64], lhsT=segP[:, s, hd, :], rhs=St[:, s, hd, :],
                                                          start=True, stop=True), reads=[seg_tb, St_tb[s]], writes=[bank[0]], acc=True)
            P.op("dve", lambda e, s=s: e.tensor_tensor(out=St[:, s + 1, :, :].rearrange("p h v -> p (h v)"), in0=pb[0][0:64, 0:384],
                                                       in1=segQ[:, s, :, :].rearrange("p h v -> p (h v)"), op=ALU.add),
                 reads=[bank[0], seg_tb], writes=[St_tb[s + 1]])
        rin = [{n: P.sbuf(f"r_{n}{i}", [64, 6, w], F32) for n, w in (("yw", 128), ("y0", 128), ("pt", 64), ("q", 64), ("bon", 128), ("g", 128))} for i in range(2)]
        rin_tb = [P.tb(f"rin{i}") for i in range(2)]
        Sb = P.sbuf("Sb", [64, 6, 64], F32)
        Sb_tb = P.tb("Sb")
        yy = P.sbuf("yy", [64, 6, 128], F32)
        cen = P.sbuf("cen", [64, 6, 128], F32)
        sq = P.sbuf("sqr", [64, 6, 128], F32)
        rsd = P.sbuf("rsd", [64, 6, 128], F32)
        ww_tb = P.tb("rwkvwork")
        for j in range(NBQ):
            I = rin[j % 2]
            itb = rin_tb[j % 2]
            for n, src in (("yw", dr["YwT"][j]), ("y0", dr["Y0T"][j]), ("pt", dr["PbT"][j]), ("q", dr["Qb"][j])):
                P.op("sp", lambda e, n=n, src=src, I=I: e.dma_start(out=I[n][:], in_=src.rearrange("h a b -> a h b")), writes=[itb], dma=True)
            P.op("sp", lambda e, I=I, j=j: e.dma_start(out=I["bon"][:], in_=dr["bon"][:, :, j * 128:(j + 1) * 128]), writes=[itb], dma=True)
            P.op("sp", lambda e, I=I, j=j: e.dma_start(out=I["g"][:], in_=dr["g"][:, :, j * 128:(j + 1) * 128]), writes=[itb], dma=True)
            s = seg_of(j)
            for hd in range(6):
                P.op("pe", lambda e, hd=hd, I=I, s=s: e.matmul(pb[0][0:64, hd * 64:(hd + 1) * 64], lhsT=I["pt"][:, hd, :], rhs=St[:, s, hd, :], start=True, stop=True),
                     reads=[itb, St_tb[s]], writes=[bank[0]], acc=True)
            P.op("dve", lambda e, I=I: e.tensor_tensor(out=Sb[:].rearrange("p h v -> p (h v)"), in0=pb[0][0:64, 0:384],
                                                       in1=I["q"][:].rearrange("p h v -> p (h v)"), op=ALU.add), reads=[bank[0], itb], writes=[Sb_tb])
            for half in range(2):
                bk = 1 + half
                for q in range(3):
                    hd = half * 3 + q
                    P.op("pe", lambda e, hd=hd, q=q, bk=bk, I=I: e.matmul(pb[bk][0:64, q * 128:(q + 1) * 128], lhsT=Sb[:, hd, :], rhs=I["yw"][:, hd, :], start=True, stop=True),
                         reads=[Sb_tb, itb], writes=[bank[bk]], acc=True)
                hs = slice(half * 3, half * 3 + 3)
                f3 = lambda t, hs=hs: t[:, hs, :].rearrange("p h t -> p (h t)")
                P.op("dve", lambda e, bk=bk, I=I, f3=f3: e.tensor_tensor(out=f3(yy), in0=pb[bk][0:64, 0:384], in1=f3(I["y0"]), op=ALU.add),
                     reads=[bank[bk], itb], writes=[ww_tb])
                P.op("pe", lambda e, bk=bk, f3=f3: e.matmul(pb[bk][0:64, 0:384], lhsT=o64[:], rhs=f3(yy), start=True, stop=True), reads=[ww_tb, cst_tb], writes=[bank[bk]])
                P.op("dve", lambda e, bk=bk, f3=f3: e.tensor_tensor(out=f3(cen), in0=f3(yy), in1=pb[bk][0:64, 0:384], op=ALU.subtract), reads=[bank[bk], ww_tb], writes=[ww_tb])
                P.op("pool", lambda e, f3=f3: e.tensor_tensor(out=f3(sq), in0=f3(cen), in1=f3(cen), op=ALU.mult), reads=[ww_tb], writes=[ww_tb])
                P.op("pe", lambda e, bk=bk, f3=f3: e.matmul(pb[bk][0:64, 0:384], lhsT=o64[:], rhs=f3(sq), start=True, stop=True), reads=[ww_tb, cst_tb], writes=[bank[bk]])
                P.op("act", lambda e, bk=bk, f3=f3: e.activation(out=f3(rsd), in_=pb[bk][0:64, 0:384], func=AF.Sqrt, bias=epsg[:, 0:1], scale=1.0),
                     reads=[bank[bk], rp_tb], writes=[ww_tb])
                P.op("dve", lambda e, f3=f3: e.reciprocal(out=f3(rsd), in_=f3(rsd)), reads=[ww_tb], writes=[ww_tb])
                P.op("dve", lambda e, f3=f3: e.tensor_tensor(out=f3(cen), in0=f3(cen), in1=f3(rsd), op=ALU.mult), reads=[ww_tb], writes=[ww_tb])
                ob, obtb = obs.next()
                for q in range(3):
                    hd = half * 3 + q
                    P.op("dve", lambda e, hd=hd: e.tensor_scalar(out=cen[:, hd, :], in0=cen[:, hd, :], scalar1=rp[:, hd:hd + 1], scalar2=rp[:, 6 + hd:7 + hd],
                                                                 op0=ALU.mult, op1=ALU.add), reads=[ww_tb, rp_tb], writes=[ww_tb])
                P.op("pool", lambda e, f3=f3, I=I: e.tensor_tensor(out=f3(cen), in0=f3(cen), in1=f3(I["bon"]), op=ALU.add), reads=[ww_tb, itb], writes=[ww_tb])
                P.op("pool", lambda e, f3=f3, I=I, ob=ob: e.tensor_tensor(out=ob[:, 0:384], in0=f3(cen), in1=f3(I["g"]), op=ALU.mult), reads=[ww_tb, itb], writes=[obtb])
                P.op("sp", lambda e, ob=ob, j=j, half=half: e.dma_start(out=dr["mixT"][half * 3:half * 3 + 3, :, j * 128:(j + 1) * 128].rearrange("h p t -> p h t"),
                                                                          in_=ob[:, 0:384].rearrange("p (h t) -> p h t", h=3)), reads=[obtb], writes=[out_tb], dma=True)

    if "swa" in parts:
      with P.scope():
        swb = P.sbuf("swb", [128, 2, 6, 128], F32)
        swbf = P.sbuf("swbf", [128, 6, 128], F32)
        sw_tb = P.tb("swc")
        P.op("sp", lambda e: e.dma_start(out=swb[:], in_=dr["swb"]), writes=[sw_tb], dma=True)
        P.op("sp", lambda e: e.dma_start(out=swbf[:], in_=dr["swb_first"]), writes=[sw_tb], dma=True)
        es = P.sbuf("esink", [64, 6], F32)
        P.op("sp", lambda e: e.dma_start(out=es[:], in_=dr["sink_b"]), writes=[sw_tb], dma=True)
        P.op("act", lambda e: e.activation(out=es[:], in_=es[:], func=AF.Exp), reads=[sw_tb], writes=[sw_tb])
        sin = [{"q": P.sbuf(f"s_q{i}", [64, 6, 128], BF16), "k": P.sbuf(f"s_k{i}", [64, 2, 256], BF16), "v": P.sbuf(f"s_v{i}", [128, 2, 128], BF16)} for i in range(2)]
        sin_tb = [P.tb(f"sin{i}") for i in range(2)]
        stmp = P.sbuf("stmp", [128, 384], F32)
        sE = [P.sbuf(f"sE{i}", [128, 384], BF16) for i in range(2)]
        sE_tb = [P.tb(), P.tb()]
        st_tb = P.tb("stmp")
        srec = P.sbuf("srec", [64, 384], F32)
        for j in range(NBQ):
            I = sin[j % 2]
            itb = sin_tb[j % 2]
            P.op("sp", lambda e, I=I, j=j: e.dma_start(out=I["q"][:], in_=dr["sq"][:, :, j * 128:(j + 1) * 128]), writes=[itb], dma=True)
            P.op("sp", lambda e, I=I, j=j: e.dma_start(out=I["k"][:], in_=dr["sk2"][:, :, j, :]), writes=[itb], dma=True)
            P.op("sp", lambda e, I=I, j=j: e.dma_start(out=I["v"][:], in_=dr["sv2"][j].rearrange("k p c -> p k c")), writes=[itb], dma=True)
            for g in range(2):
                bn, bd = 5, 6
                for kt2 in range(2):
                    bs = 3 + kt2
                    P.op("pe", lambda e, g=g, kt2=kt2, bs=bs, I=I: e.matmul(pb[bs][:, 0:384], lhsT=I["k"][:, g, kt2 * 128:(kt2 + 1) * 128],
                                                                            rhs=I["q"][:, 3 * g:3 * g + 3, :], start=True, stop=True),
                         reads=[itb], writes=[bank[bs]])
                    btab = swbf[:, 3 * g:3 * g + 3, :] if (j == 0 and kt2 == 0) else swb[:, kt2, 3 * g:3 * g + 3, :]
                    P.op("dve", lambda e, bs=bs, btab=btab: e.scalar_tensor_tensor(out=stmp[:].rearrange("p (h t) -> p h t", h=3), in0=pb[bs][:, 0:384].rearrange("p (h t) -> p h t", h=3),
                                                                                  scalar=0.125, in1=btab, op0=ALU.mult, op1=ALU.add),
                         reads=[bank[bs], sw_tb], writes=[st_tb])
                    E, etb = sE[kt2], sE_tb[kt2]
                    P.op("act", lambda e, E=E: e.activation(out=E[:], in_=stmp[:], func=AF.Exp), reads=[st_tb], writes=[etb])
                    P.op("pe", lambda e, E=E, g=g, kt2=kt2, I=I: e.matmul(pb[bn][0:64, 0:384], lhsT=I["v"][:, kt2, g * 64:(g + 1) * 64], rhs=E[:],
                                                                          start=(kt2 == 0), stop=(kt2 == 1)), reads=[etb, itb], writes=[bank[bn]], acc=True)
                    P.op("pe", lambda e, E=E, kt2=kt2: e.matmul(pb[bd][0:64, 0:384], lhsT=onesb[:], rhs=E[:], start=(kt2 == 0), stop=(kt2 == 1)),
                         reads=[etb, cst_tb], writes=[bank[bd]], acc=True)
                for q in range(3):
                    P.op("dve", lambda e, q=q, g=g: e.tensor_scalar(out=srec[:, q * 128:(q + 1) * 128], in0=pb[bd][0:64, q * 128:(q + 1) * 128],
                                                                    scalar1=es[:, 3 * g + q:3 * g + q + 1], scalar2=None, op0=ALU.add),
                         reads=[bank[bd], sw_tb], writes=[st_tb])
                P.op("dve", lambda e: e.reciprocal(out=srec[:], in_=srec[:]), reads=[st_tb], writes=[st_tb])
                ob, obtb = obs.next()
                P.op("dve", lambda e, ob=ob: e.tensor_tensor(out=ob[:, 0:384], in0=pb[bn][0:64, 0:384], in1=srec[:], op=ALU.mult), reads=[bank[bn], st_tb], writes=[obtb])
                P.op("sp", lambda e, ob=ob, j=j, g=g: e.dma_start(out=dr["mixT"][10 + 3 * g:13 + 3 * g, :, j * 128:(j + 1) * 128].rearrange("h p t -> p h t"),
                                                                    in_=ob[:, 0:384].rearrange("p (h t) -> p h t", h=3)), reads=[obtb], writes=[out_tb], dma=True)

    if "dsa" in parts:
      with P.scope():
        ckvT = P.sbuf("ckvT", [128, NKT * 128], BF16)
        ckv = P.sbuf("ckv", [128, NKT, 128], BF16)
        key_tb = P.tb("keys")
        for c in range(0, NKT, 16):
            P.op("sp", lambda e, c=c: e.dma_start(out=ckvT[:, c * 128:(c + 16) * 128], in_=dr["ckvT"][:, c * 128:(c + 16) * 128]), writes=[key_tb], dma=True)
            P.op("sp", lambda e, c=c: e.dma_start(out=ckv[:, c:c + 16, :], in_=dr["ckv"][c * 128:(c + 16) * 128, :].rearrange("(k p) r -> p k r", p=128)),
                 writes=[key_tb], dma=True)
        ikc = [P.sbuf(f"ikc{i}", [64, 512], BF16) for i in range(3)]
        ikc_tb = [P.tb() for _ in range(3)]
        wuv_f = P.sbuf("wuv_f", [128, 4, 64], F32)
        wuv = P.sbuf("wuv", [128, 4, 64], BF16)
        dpar_tb = P.tb("dpar")
        P.op("sp", lambda e: e.dma_start(out=wuv_f[:], in_=dr["wuv"].rearrange("h r d -> r h d")), writes=[dpar_tb], dma=True)
        P.op("dve", lambda e: e.tensor_copy(out=wuv[:], in_=wuv_f[:]), reads=[dpar_tb], writes=[dpar_tb])
        dmask = P.sbuf("dmask", [128, 1024], F32)
        ab = P.sbuf("ab", [128, 4, 128], F32)
        P.op("sp", lambda e: e.dma_start(out=dmask[:], in_=dr["dmask"]), writes=[dpar_tb], dma=True)
        P.op("sp", lambda e: e.dma_start(out=ab[:], in_=dr["ab"]), writes=[dpar_tb], dma=True)
        Isc = P.sbuf("Isc", [128, NKT * 128], F32)
        Isc_tb = P.tb("Isc")
        msk = P.sbuf("msk", [128, NKT * 128], BF16)
        msk_tb = P.tb("msk")
        qin = [{"ql": P.sbuf(f"d_ql{i}", [128, 4, 128], BF16), "iq": P.sbuf(f"d_iq{i}", [64, 4, 128], BF16), "sg": P.sbuf(f"d_sg{i}", [128, 4], F32)} for i in range(2)]
        qin_tb = [P.tb(), P.tb()]
        rl = [P.sbuf(f"rl{i}", [128, 512], F32) for i in range(2)]
        rl_tb = [P.tb(), P.tb()]
        bs_ = {n: P.sbuf("bs_" + n, [128, 1], F32) for n in ("lo", "hi", "mid", "cnt", "ge", "d")}
        bs_tb = P.tb("bisect")
        Eb = [P.sbuf(f"Eb{i}", [128, 512], F32) for i in range(2)]
        Eb_tb = [[P.tb() for _ in range(4)] for _ in range(2)]
        PTb = [P.sbuf(f"PTb{i}", [128, 512], BF16) for i in range(2)]
        PT_tb = [P.tb(), P.tb()]
        olat = P.sbuf("olat", [128, 512], BF16)
        drec = P.sbuf("drec", [64, 512], F32)
        fin_tb = P.tb("dsafin")
        for j in range(NBQ):
            nkt = 8 * (j + 1)
            n = nkt * 128
            Q = qin[j % 2]
            qtb = qin_tb[j % 2]
            P.op("sp", lambda e, Q=Q, j=j: e.dma_start(out=Q["ql"][:], in_=dr["qlat"][:, :, j * 128:(j + 1) * 128]), writes=[qtb], dma=True)
            P.op("sp", lambda e, Q=Q, j=j: e.dma_start(out=Q["iq"][:], in_=dr["iqs"][:, :, j * 128:(j + 1) * 128]), writes=[qtb], dma=True)
            P.op("sp", lambda e, Q=Q, j=j: e.dma_start(out=Q["sg"][:], in_=dr["sgn"][j * 128:(j + 1) * 128, :]), writes=[qtb], dma=True)
            for c4 in range(nkt // 4):
                ii = c4 % 3
                P.op("sp", lambda e, c4=c4, ii=ii: e.dma_start(out=ikc[ii][:], in_=dr["ikT"][:, c4 * 512:(c4 + 1) * 512]), writes=[ikc_tb[ii]], dma=True)
                for h in range(4):
                    bk = (c4 * 4 + h) % 2
                    P.op("pe", lambda e, h=h, bk=bk, ii=ii, Q=Q: e.matmul(pb[bk][:], lhsT=Q["iq"][:, h, :], rhs=ikc[ii][:], start=True, stop=True),
                         reads=[qtb, ikc_tb[ii]], writes=[bank[bk]])
                    P.op("act", lambda e, bk=bk: e.activation(out=rl[bk][:], in_=pb[bk][:], func=AF.Relu), reads=[bank[bk]], writes=[rl_tb[bk]])
                    dst = Isc[:, c4 * 512:(c4 + 1) * 512]
                    if h == 0:
                        P.op("dve", lambda e, bk=bk, dst=dst, Q=Q: e.tensor_scalar(out=dst, in0=rl[bk][:], scalar1=Q["sg"][:, 0:1], scalar2=None, op0=ALU.mult),
                             reads=[rl_tb[bk], qtb], writes=[Isc_tb])
                    else:
                        P.op("dve", lambda e, bk=bk, dst=dst, h=h, Q=Q: e.scalar_tensor_tensor(out=dst, in0=rl[bk][:], scalar=Q["sg"][:, h:h + 1], in1=dst,
                                                                                              op0=ALU.mult, op1=ALU.add), reads=[rl_tb[bk], qtb, Isc_tb], writes=[Isc_tb])
            P.op("dve", lambda e, n=n: e.tensor_tensor(out=Isc[:, n - 1024:n], in0=Isc[:, n - 1024:n], in1=dmask[:], op=ALU.add), reads=[Isc_tb, dpar_tb], writes=[Isc_tb])
            B = bs_
            bv = lambda fn: P.op("dve", fn, reads=[bs_tb, Isc_tb], writes=[bs_tb])
            bv(lambda e: e.memset(B["lo"][:], -64.0))
            bv(lambda e, n=n: e.tensor_reduce(out=B["hi"][:], in_=Isc[:, 0:n], axis=AX.X, op=ALU.max))
            bv(lambda e: e.tensor_scalar(out=B["d"][:], in0=B["hi"][:], scalar1=64.0, scalar2=None, op0=ALU.add))
            for it in range(NITER):
                ck_ = 0.5 ** (it + 1)
                bv(lambda e, ck_=ck_: e.scalar_tensor_tensor(out=B["mid"][:], in0=B["d"][:], scalar=ck_, in1=B["lo"][:], op0=ALU.mult, op1=ALU.add))
                P.op("dve", lambda e, n=n: e.tensor_scalar(out=msk[:, 0:n], in0=Isc[:, 0:n], scalar1=B["mid"][:, 0:1], scalar2=0.0, op0=ALU.is_ge, op1=ALU.add,
                                                          accum_out=B["cnt"][:]), reads=[bs_tb, Isc_tb], writes=[bs_tb, msk_tb])
                bv(lambda e, ck_=ck_: e.tensor_scalar(out=B["ge"][:], in0=B["cnt"][:], scalar1=float(TOPK) - 0.5, scalar2=ck_, op0=ALU.is_ge, op1=ALU.mult))
                bv(lambda e: e.scalar_tensor_tensor(out=B["lo"][:], in0=B["ge"][:], scalar=B["d"][:, 0:1], in1=B["lo"][:], op0=ALU.mult, op1=ALU.add))
            P.op("dve", lambda e, n=n: e.tensor_scalar(out=msk[:, 0:n], in0=Isc[:, 0:n], scalar1=B["lo"][:, 0:1], scalar2=None, op0=ALU.is_ge),
                 reads=[bs_tb, Isc_tb], writes=[msk_tb])
            for kt in range(nkt):
                i2 = kt % 2
                dl = nkt - 1 - kt
                pmT = pb[2].bitcast(BF16)
                P.op("pe", lambda e, kt=kt, pmT=pmT: e.transpose(out=pmT[:, 0:128], in_=msk[:, kt * 128:(kt + 1) * 128], identity=identb[:]),
                     reads=[msk_tb, cst_tb], writes=[bank[2]])
                bS = 3 + i2
                P.op("pe", lambda e, kt=kt, bS=bS, Q=Q: e.matmul(pb[bS][:], lhsT=ckvT[:, kt * 128:(kt + 1) * 128], rhs=Q["ql"][:], start=True, stop=True),
                     reads=[key_tb, qtb], writes=[bank[bS]])
                for h in range(4):
                    P.op("act", lambda e, h=h, bS=bS, i2=i2, dl=dl: e.activation(out=Eb[i2][:, h * 128:(h + 1) * 128], in_=pb[bS][:, h * 128:(h + 1) * 128], func=AF.Exp,
                                                                               bias=ab[:, h, dl:dl + 1], scale=1.0), reads=[bank[bS], dpar_tb], writes=[Eb_tb[i2][h]])
                P.op("dve", lambda e, i2=i2, pmT=pmT: e.tensor_tensor(out=PTb[i2][:].rearrange("p (h t) -> p h t", h=4), in0=Eb[i2][:].rearrange("p (h t) -> p h t", h=4),
                                                                      in1=pmT[:, 0:128].unsqueeze(1).to_broadcast([128, 4, 128]), op=ALU.mult),
                     reads=Eb_tb[i2] + [bank[2]], writes=[PT_tb[i2]])
                P.op("pe", lambda e, kt=kt, i2=i2, nkt=nkt: e.matmul(pb[5][:], lhsT=ckv[:, kt, :], rhs=PTb[i2][:], start=(kt == 0), stop=(kt == nkt - 1)),
                     reads=[key_tb, PT_tb[i2]], writes=[bank[5]], acc=True)
                P.op("pe", lambda e, kt=kt, i2=i2, nkt=nkt: e.matmul(pb[6][0:64, :], lhsT=onesb[:], rhs=PTb[i2][:], start=(kt == 0), stop=(kt == nkt - 1)),
                     reads=[cst_tb, PT_tb[i2]], writes=[bank[6]], acc=True)
            P.op("act", lambda e: e.activation(out=olat[:], in_=pb[5][:], func=AF.Identity), reads=[bank[5]], writes=[fin_tb])
            P.op("dve", lambda e: e.reciprocal(out=drec[:], in_=pb[6][0:64, :]), reads=[bank[6]], writes=[fin_tb])
            for h in range(4):
                P.op("pe", lambda e, h=h: e.matmul(pb[7][0:64, h * 128:(h + 1) * 128], lhsT=wuv[:, h, :], rhs=olat[:, h * 128:(h + 1) * 128], start=True, stop=True),
                     reads=[fin_tb, dpar_tb], writes=[bank[7]], acc=True)
            ob, obtb = obs.next()
            P.op("dve", lambda e, ob=ob: e.tensor_tensor(out=ob[:, 0:512], in0=pb[7][0:64, :], in1=drec[:], op=ALU.mult), reads=[bank[7], fin_tb], writes=[obtb])
            P.op("sp", lambda e, ob=ob, j=j: e.dma_start(out=dr["mixT"][6:10, :, j * 128:(j + 1) * 128].rearrange("h p t -> p h t"),
                                                           in_=ob[:, 0:512].rearrange("p (h t) -> p h t", h=4)), reads=[obtb], writes=[out_tb], dma=True)


def emit_ffn(P, nc, NT, exp_ids, dr, ident, ident_tb):
    PT = min(1024, NT)
    NB = PT // 128
    npass = NT // PT
    NX = len(exp_ids)
    TW = min(512, PT)
    NTT = PT // TW
    BPT = TW // 128
    es = P.es
    wo_tb = P.tb("wo")
    stg = [P.sbuf(f"stg{i}", [128, 2048], F32) for i in range(3)]
    stg_tb = [P.tb(f"stg{i}") for i in range(3)]
    rw = P.sbuf("rw", [128, 8, NE], F32)
    rw_tb = P.tb("rw")
    rbias = P.sbuf("rbias", [128, NE], F32)
    bc = [P.sbuf(f"bc{i}", [128, D], F32) for i in range(3)]
    bc_tb = [P.tb(f"bc{i}") for i in range(3)]
    modT = P.sbuf("modT", [128, 48], F32)
    modT_tb = P.tb("modT")
    eps_t = P.sbuf("eps_t", [128, 1], F32)
    xm = P.sbuf("xm", [128, NB, D], F32)
    xm_tb = [P.tb(f"xm{b}") for b in range(NB)]
    yacc = P.sbuf("yacc", [128, NB, D], F32)
    ya_tb = [P.tb(f"ya{b}") for b in range(NB)]
    h2T = P.sbuf("h2T", [128, 8, PT], BF16)
    h2_tb = [P.tb(f"h2{b}") for b in range(NB)]
    h2f_tb = P.tb("h2f")
    gateT = P.sbuf("gateT", [65, PT], F32)
    gT_tb = [P.tb(f"gT{b}") for b in range(NB)]
    xr = [P.sbuf(f"xr{i}", [128, D], F32) for i in range(2)]
    xr_tb = [P.tb(f"xr{i}") for i in range(2)]
    zt = P.sbuf("zt", [128, D], F32)
    zt_tb = P.tb("zt")
    lnsc = {"st": P.sbuf("ln_st", [128, 2, 6], F32), "mv": P.sbuf("ln_mv", [128, 2], F32),
            "rs": P.sbuf("ln_rs", [128, 1], F32), "xn": P.sbuf("ln_xn", [128, D], F32),
            "tb": P.tb("ln_s"), "xn_tb": P.tb("ln_xn")}
    rt_tb = P.tb("rt")
    wb_tb = [{k: P.tb(f"{k}b{i}") for k in ("w1", "w3", "w2")} for i in range(2)]
    selt_tb = P.tb("selt")
    Gb_tb = P.tb("Gb")
    ssb_tb = [P.tb(f"ssb{i}") for i in range(2)]
    gg_tb = [[P.tb(f"gg{fc}_{tt}") for tt in range(NTT)] for fc in range(2)]
    pb = [P.psum(f"pb{i}", [128, 512], F32) for i in range(8)]
    pb_tb = [P.tb(f"pb{i}") for i in range(8)]

    P.op("pool", lambda e: e.memset(eps_t[:], LN_EPS), writes=[modT_tb])
    P.op("sp", lambda e: e.dma_start(out=modT[:], in_=dr["modT"]), writes=[modT_tb], dma=True)
    P.op("dve", lambda e: e.tensor_scalar(out=modT[:, 32:40], in0=modT[:, 32:40], scalar1=1.0, scalar2=None,
                                          op0=ALU.add), reads=[modT_tb], writes=[modT_tb])
    P.op("sp", lambda e: e.dma_start(out=rw[:], in_=dr["router_w"].rearrange("(kc p) n -> p kc n", p=128)),
         writes=[rw_tb], dma=True)
    P.op("sp", lambda e: e.dma_start(out=rbias[:], in_=dr["rbias_b"]), writes=[rw_tb], dma=True)
    def load_bc(i, src):
        P.op("sp", lambda e: e.dma_start(out=bc[i][:], in_=src), writes=[bc_tb[i]], dma=True)

    def _pass(ps):
        t0 = ps * PT
        load_bc(0, dr["modb"][:, 2 * D:3 * D])
        load_bc(1, dr["lnp"][:, 0, :])
        load_bc(2, dr["lnp"][:, 1, :])
        P.op("pool", lambda e: e.memset(gateT[64:65, :], 1.0), writes=gT_tb)
        _sc1 = P.scope()
        _sc1.__enter__()
        wo = P.sbuf(f"p{ps}_wo", [64, 16, D], BF16)
        mixb = [P.sbuf(f"p{ps}_mixb{i}", [64, 16, 128], BF16) for i in range(2)]
        mixb_tb = [P.tb(f"mixb{i}") for i in range(2)]
        h2f = P.sbuf(f"p{ps}_h2f", [128, 8, 128], F32)
        rt = {k: P.sbuf(f"p{ps}_rt_" + k, [128, n], F32) for k, n in
              [("sc", 64), ("sel", 64), ("eq", 64), ("sel2", 64), ("m1", 8), ("m2", 8), ("grp", 8), ("t8", 8),
               ("gm", 8), ("g4", 8), ("selm", 64), ("em", 64), ("gt", 64), ("den", 1), ("gate", 64)]}
        for c in range(16):
            s_ = c % 3
            P.op("sp", lambda e, c=c, s_=s_: e.dma_start(out=stg[s_][0:64, 0:D], in_=dr["w_out"][c * 64:(c + 1) * 64, :]),
                 writes=[stg_tb[s_]], dma=True)
            P.op("pool", lambda e, c=c, s_=s_: e.tensor_copy(out=wo[:, c, :], in_=stg[s_][0:64, 0:D]),
                 reads=[stg_tb[s_]], writes=[wo_tb])


        for b in range(NB):
            tk = t0 + b * 128
            xi = b % 2
            P.op("sp", lambda e, xi=xi, tk=tk: e.dma_start(out=xr[xi][:], in_=dr["xres"][tk:tk + 128, :]),
                 writes=[xr_tb[xi]], dma=True)
            P.op("sp", lambda e, xi=xi, tk=tk: e.dma_start(out=mixb[xi][:], in_=dr["mixT"][:, :, tk:tk + 128].rearrange("h p t -> p h t")),
                 writes=[mixb_tb[xi]], dma=True)
            for hlf in range(2):
                for c in range(16):
                    P.op("pe", lambda e, c=c, hlf=hlf, xi=xi: e.matmul(
                        pb[hlf][:], lhsT=mixb[xi][:, c, :], rhs=wo[:, c, hlf * 512:(hlf + 1) * 512],
                        start=(c == 0), stop=(c == 15)), reads=[mixb_tb[xi], wo_tb], writes=[pb_tb[hlf]], acc=True)
            for hlf in range(2):
                P.op("dve", lambda e, hlf=hlf: e.tensor_tensor(out=zt[:, hlf * 512:(hlf + 1) * 512], in0=pb[hlf][:],
                                                               in1=bc[0][:, hlf * 512:(hlf + 1) * 512], op=ALU.mult),
                     reads=[pb_tb[hlf], bc_tb[0]], writes=[zt_tb])
            P.op("dve", lambda e, xi=xi: e.scalar_tensor_tensor(out=zt[:], in0=xr[xi][:], scalar=ALPHA, in1=zt[:],
                                                                op0=ALU.mult, op1=ALU.add),
                 reads=[xr_tb[xi], zt_tb], writes=[zt_tb])
            emit_layernorm(P, zt[:], zt_tb, xm[:, b, :], xm_tb[b], bc[1][:], bc[2][:], [bc_tb[1], bc_tb[2]], lnsc, eps_t, "m")
            for kc in range(8):
                bank = 2 + kc // 4
                P.op("pe", lambda e, kc=kc, bank=bank, b=b: e.transpose(
                    out=pb[bank][:, (kc % 4) * 128:(kc % 4 + 1) * 128], in_=xm[:, b, kc * 128:(kc + 1) * 128],
                    identity=ident[:]), reads=[xm_tb[b], ident_tb], writes=[pb_tb[bank]], acc=True)
            for kc in range(8):
                bank = 2 + kc // 4
                src = pb[bank][:, (kc % 4) * 128:(kc % 4 + 1) * 128]
                P.op("act", lambda e, kc=kc, src=src, b=b: e.activation(
                    out=h2T[:, kc, b * 128:(b + 1) * 128], in_=src, func=AF.Identity,
                    bias=modT[:, 24 + kc:25 + kc], scale=modT[:, 32 + kc:33 + kc]),
                    reads=[pb_tb[bank], modT_tb], writes=[h2_tb[b]])
                P.op("act", lambda e, kc=kc, src=src: e.activation(
                    out=h2f[:, kc, :], in_=src, func=AF.Identity,
                    bias=modT[:, 24 + kc:25 + kc], scale=modT[:, 32 + kc:33 + kc]),
                    reads=[pb_tb[bank], modT_tb], writes=[h2f_tb])
            for kc in range(8):
                P.op("pe", lambda e, kc=kc: e.matmul(pb[4][:, 0:NE], lhsT=h2f[:, kc, :], rhs=rw[:, kc, :],
                                                     start=(kc == 0), stop=(kc == 7)),
                     reads=[h2f_tb, rw_tb], writes=[pb_tb[4]], acc=True)
            R = rt
            P.op("act", lambda e: e.activation(out=R["sc"][:], in_=pb[4][:, 0:NE], func=AF.Sigmoid),
                 reads=[pb_tb[4]], writes=[rt_tb])
            dv = lambda fn: P.op("dve", fn, reads=[rt_tb, rw_tb], writes=[rt_tb])
            g3 = lambda t: t[:].rearrange("p (g j) -> p g j", j=8)
            dv(lambda e: e.tensor_tensor(out=R["sel"][:], in0=R["sc"][:], in1=rbias[:], op=ALU.add))
            dv(lambda e: e.tensor_reduce(out=R["m1"][:], in_=g3(R["sel"]), axis=AX.X, op=ALU.max))
            dv(lambda e: e.tensor_tensor(out=g3(R["eq"]), in0=g3(R["sel"]), in1=bcast_last(R["m1"][:], 8), op=ALU.is_equal))
            dv(lambda e: e.scalar_tensor_tensor(out=R["sel2"][:], in0=R["eq"][:], scalar=-4.0, in1=R["sel"][:],
                                                op0=ALU.mult, op1=ALU.add))
            dv(lambda e: e.tensor_reduce(out=R["m2"][:], in_=g3(R["sel2"]), axis=AX.X, op=ALU.max))
            dv(lambda e: e.tensor_tensor(out=R["grp"][:], in0=R["m1"][:], in1=R["m2"][:], op=ALU.add))
            dv(lambda e: e.max(out=R["t8"][:], in_=R["grp"][:]))
            dv(lambda e: e.tensor_scalar(out=R["gm"][:], in0=R["grp"][:], scalar1=R["t8"][:, 3:4], scalar2=None, op0=ALU.is_ge))
            dv(lambda e: e.tensor_scalar(out=R["g4"][:], in0=R["gm"][:], scalar1=4.0, scalar2=-4.0, op0=ALU.mult, op1=ALU.add))
            dv(lambda e: e.tensor_tensor(out=g3(R["selm"]), in0=g3(R["sel"]), in1=bcast_last(R["gm"][:], 8), op=ALU.mult))
            dv(lambda e: e.tensor_tensor(out=g3(R["selm"]), in0=g3(R["selm"]), in1=bcast_last(R["g4"][:], 8), op=ALU.add))
            dv(lambda e: e.max(out=R["t8"][:], in_=R["selm"][:]))
            dv(lambda e: e.tensor_scalar(out=R["em"][:], in0=R["selm"][:], scalar1=R["t8"][:, 7:8], scalar2=None, op0=ALU.is_ge))
            dv(lambda e: e.tensor_tensor(out=R["gt"][:], in0=R["sc"][:], in1=R["em"][:], op=ALU.mult))
            dv(lambda e: e.tensor_reduce(out=R["den"][:], in_=R["gt"][:], axis=AX.X, op=ALU.add))
            dv(lambda e: e.reciprocal(out=R["den"][:], in_=R["den"][:]))
            dv(lambda e: e.tensor_scalar(out=R["gate"][:], in0=R["gt"][:], scalar1=R["den"][:, 0:1], scalar2=2.5,
                                         op0=ALU.mult, op1=ALU.mult))
            P.op("pe", lambda e: e.transpose(out=pb[5][0:64, 0:128], in_=R["gate"][:], identity=ident[:]),
                 reads=[rt_tb, ident_tb], writes=[pb_tb[5]])
            P.op("act", lambda e, b=b: e.activation(out=gateT[0:64, b * 128:(b + 1) * 128], in_=pb[5][0:64, 0:128],
                                                    func=AF.Identity), reads=[pb_tb[5]], writes=[gT_tb[b]])
        _sc1.__exit__(None, None, None)
        _sc2 = P.scope()
        _sc2.__enter__()
        wb = [{"w1": P.sbuf(f"p{ps}_w1b{i}", [128, 8, DE], BF16), "w3": P.sbuf(f"p{ps}_w3b{i}", [128, 8, DE], BF16),
               "w2": P.sbuf(f"p{ps}_w2b{i}", [128, 2, D], BF16)} for i in range(2)]
        selt = P.sbuf(f"p{ps}_selt", [65, 128], F32)
        Gb = P.sbuf(f"p{ps}_Gb", [128, PT], BF16)
        ssb = [P.sbuf(f"p{ps}_ssb{i}", [128, TW], F32) for i in range(2)]
        ggT = P.sbuf(f"p{ps}_ggT", [128, 2, PT], BF16)

        for xi_, eid in enumerate(exp_ids):
            par = xi_ % 2
            W = wb[par]
            Wt = wb_tb[par]
            P.op("sp", lambda e, xi_=xi_: e.dma_start(out=stg[0][:].rearrange("p (kc f) -> p kc f", f=DE),
                                                     in_=dr["ew1"][xi_].rearrange("(kc p) f -> p kc f", p=128)),
                 writes=[stg_tb[0]], dma=True)
            P.op("pool", lambda e, W=W: e.tensor_copy(out=W["w1"][:].rearrange("p kc f -> p (kc f)"), in_=stg[0][:]),
                 reads=[stg_tb[0]], writes=[Wt["w1"]])
            P.op("sp", lambda e, xi_=xi_: e.dma_start(out=stg[1][:].rearrange("p (kc f) -> p kc f", f=DE),
                                                     in_=dr["ew3"][xi_].rearrange("(kc p) f -> p kc f", p=128)),
                 writes=[stg_tb[1]], dma=True)
            P.op("pool", lambda e, W=W: e.tensor_copy(out=W["w3"][:].rearrange("p kc f -> p (kc f)"), in_=stg[1][:]),
                 reads=[stg_tb[1]], writes=[Wt["w3"]])
            P.op("sp", lambda e, xi_=xi_: e.dma_start(out=stg[2][:].rearrange("p (fc d) -> p fc d", d=D),
                                                     in_=dr["ew2"][xi_].rearrange("(fc p) d -> p fc d", p=128)),
                 writes=[stg_tb[2]], dma=True)
            P.op("pool", lambda e, W=W: e.tensor_copy(out=W["w2"][:].rearrange("p fc d -> p (fc d)"), in_=stg[2][:]),
                 reads=[stg_tb[2]], writes=[Wt["w2"]])
            P.op("pool", lambda e, eid=eid: e.tensor_copy(out=selt[:], in_=ident[0:65, eid:eid + 1].to_broadcast([65, 128])),
                 reads=[ident_tb], writes=[selt_tb])
            for tt in range(NTT):
                P.op("pe", lambda e, tt=tt: e.matmul(pb[6][:, 0:TW], lhsT=selt[:], rhs=gateT[:, tt * TW:(tt + 1) * TW],
                                                     start=True, stop=True),
                     reads=[selt_tb] + gT_tb, writes=[pb_tb[6]])
                P.op("act", lambda e, tt=tt: e.activation(out=Gb[:, tt * TW:(tt + 1) * TW], in_=pb[6][:, 0:TW], func=AF.Identity),
                     reads=[pb_tb[6]], writes=[Gb_tb])
            for tt in range(NTT):
                for fc in range(2):
                    i2 = (tt * 2 + fc) % 2
                    b1, b3 = i2, 2 + i2
                    h_tbs = h2_tb[tt * BPT:(tt + 1) * BPT]
                    for kc in range(8):
                        P.op("pe", lambda e, kc=kc, fc=fc, tt=tt, b1=b1, W=W: e.matmul(
                            pb[b1][:, 0:TW], lhsT=W["w1"][:, kc, fc * 128:(fc + 1) * 128], rhs=h2T[:, kc, tt * TW:(tt + 1) * TW],
                            start=(kc == 0), stop=(kc == 7)), reads=[Wt["w1"]] + h_tbs, writes=[pb_tb[b1]], acc=True)
                    for kc in range(8):
                        P.op("pe", lambda e, kc=kc, fc=fc, tt=tt, b3=b3, W=W: e.matmul(
                            pb[b3][:, 0:TW], lhsT=W["w3"][:, kc, fc * 128:(fc + 1) * 128], rhs=h2T[:, kc, tt * TW:(tt + 1) * TW],
                            start=(kc == 0), stop=(kc == 7)), reads=[Wt["w3"]] + h_tbs, writes=[pb_tb[b3]], acc=True)
                    P.op("act", lambda e, i2=i2, b1=b1: e.activation(out=ssb[i2][:], in_=pb[b1][:, 0:TW], func=AF.Silu),
                         reads=[pb_tb[b1]], writes=[ssb_tb[i2]])
                    P.op("dve", lambda e, i2=i2, b3=b3: e.tensor_tensor(out=ssb[i2][:], in0=ssb[i2][:], in1=pb[b3][:, 0:TW], op=ALU.mult),
                         reads=[ssb_tb[i2], pb_tb[b3]], writes=[ssb_tb[i2]])
                    P.op("dve", lambda e, i2=i2, fc=fc, tt=tt: e.tensor_tensor(
                        out=ggT[:, fc, tt * TW:(tt + 1) * TW], in0=ssb[i2][:], in1=Gb[:, tt * TW:(tt + 1) * TW], op=ALU.mult),
                        reads=[ssb_tb[i2], Gb_tb], writes=[gg_tb[fc][tt]])
            for b in range(NB):
                tt = b // BPT
                for dh in range(2):
                    bk = 4 + (b * 2 + dh) % 2
                    for fc in range(2):
                        P.op("pe", lambda e, b=b, dh=dh, fc=fc, bk=bk, W=W: e.matmul(
                            pb[bk][:], lhsT=ggT[:, fc, b * 128:(b + 1) * 128], rhs=W["w2"][:, fc, dh * 512:(dh + 1) * 512],
                            start=(fc == 0), stop=(fc == 1)), reads=[gg_tb[fc][tt], Wt["w2"]], writes=[pb_tb[bk]], acc=True)
                    if xi_ == 0:
                        P.op("dve", lambda e, b=b, dh=dh, bk=bk: e.tensor_copy(out=yacc[:, b, dh * 512:(dh + 1) * 512], in_=pb[bk][:]),
                             reads=[pb_tb[bk]], writes=[ya_tb[b]])
                    else:
                        P.op("dve", lambda e, b=b, dh=dh, bk=bk: e.tensor_tensor(
                            out=yacc[:, b, dh * 512:(dh + 1) * 512], in0=yacc[:, b, dh * 512:(dh + 1) * 512], in1=pb[bk][:], op=ALU.add),
                            reads=[pb_tb[bk], ya_tb[b]], writes=[ya_tb[b]])
        _sc2.__exit__(None, None, None)
        load_bc(0, dr["modb"][:, 5 * D:6 * D])
        load_bc(1, dr["lnp"][:, 2, :])
        load_bc(2, dr["lnp"][:, 3, :])
        for b in range(NB):
            tk = t0 + b * 128
            xi = b % 2
            P.op("dve", lambda e, b=b: e.tensor_tensor(out=zt[:], in0=yacc[:, b, :], in1=bc[0][:], op=ALU.mult),
                 reads=[ya_tb[b], bc_tb[0]], writes=[zt_tb])
            P.op("dve", lambda e, b=b: e.scalar_tensor_tensor(out=zt[:], in0=xm[:, b, :], scalar=ALPHA, in1=zt[:],
                                                              op0=ALU.mult, op1=ALU.add),
                 reads=[xm_tb[b], zt_tb], writes=[zt_tb])
            emit_layernorm(P, zt[:], zt_tb, xr[xi][:], xr_tb[xi], bc[1][:], bc[2][:], [bc_tb[1], bc_tb[2]], lnsc, eps_t, "f")
            P.op("sp", lambda e, xi=xi, tk=tk: e.dma_start(out=dr["out"][tk:tk + 128, :], in_=xr[xi][:]),
                 reads=[xr_tb[xi]], writes=[dr["out_tb"]], dma=True)

    for ps in range(npass):
        _pass(ps)


def emit_k0(P, nc, dr):
    cT = P.sbuf("cT", [128, 8], F32)
    c_tb = P.tb("cT")
    P.op("sp", lambda e: e.dma_start(out=cT[:], in_=dr["cT"]), writes=[c_tb], dma=True)
    P.op("act", lambda e: e.activation(out=cT[:], in_=cT[:], func=AF.Silu), reads=[c_tb], writes=[c_tb])
    wm = [P.sbuf(f"wm{i}", [128, 8, 768], F32) for i in range(2)]
    wm_tb = [P.tb(), P.tb()]
    bm = P.sbuf("bm", [1, 4, 768], F32)
    P.op("sp", lambda e: e.dma_start(out=bm[:], in_=dr["b_mod"].unsqueeze(0)), writes=[c_tb], dma=True)
    ps = [P.psum(f"k0ps{i}", [128, 512], F32) for i in range(2)]
    ps_tb = [P.tb(), P.tb()]
    ot = P.sbuf("k0o", [1, 4, 768], F32)
    o_tb = P.tb("k0o")
    for l in range(4):
        W, wtb = wm[l % 2], wm_tb[l % 2]
        P.op("sp", lambda e, l=l, W=W: e.dma_start(out=W[:], in_=dr["w_mod"][l].rearrange("(kc p) n -> p kc n", p=128)), writes=[wtb], dma=True)
        for hf in range(2):
            for kc in range(8):
                P.op("pe", lambda e, kc=kc, hf=hf, W=W: e.matmul(ps[hf][0:1, 0:384], lhsT=cT[:, kc:kc + 1], rhs=W[:, kc, hf * 384:(hf + 1) * 384],
                                                                 start=(kc == 0), stop=(kc == 7)), reads=[c_tb, wtb], writes=[ps_tb[hf]], acc=True)
            P.op("dve", lambda e, l=l, hf=hf: e.tensor_tensor(out=ot[0:1, l, hf * 384:(hf + 1) * 384], in0=ps[hf][0:1, 0:384],
                                                              in1=bm[0:1, l, hf * 384:(hf + 1) * 384], op=ALU.add), reads=[ps_tb[hf], c_tb], writes=[o_tb])
    P.op("sp", lambda e: e.dma_start(out=dr["mod"].unsqueeze(0), in_=ot[:]), reads=[o_tb], writes=[dr["out_tb"]], dma=True)


D = 1024
def consts128():
    i = np.arange(128)
    ident = np.eye(128, dtype=np.float32)
    bo = (i[:, None] // 64 == i[None, :] // 64).astype(np.float32)
    ls = (i[None, :] < i[:, None]).astype(np.float32)
    us = (i[None, :] > i[:, None]).astype(np.float32)
    ui = (i[None, :] >= i[:, None]).astype(np.float32)
    return np.ascontiguousarray(np.stack([ident, bo, ls, us, ui], 1))

def k1_common(inp, l, mod):
    col = lambda v: np.ascontiguousarray(v.reshape(-1, 128).T)
    par = np.zeros((128, 64), np.float32)
    par[:, 0:11] = col(inp['rwkv_mu'][l])
    par[:, 11:14] = col(inp['rwkv_w0'][l]); par[:, 14:17] = col(inp['rwkv_a0'][l])
    par[:, 17:20] = col(inp['rwkv_k_k'][l]); par[:, 20:23] = col(inp['rwkv_k_a'][l])
    par[:, 26:29] = col(inp['rwkv_r_k'][l].reshape(-1))
    par[:, 29:37] = col(mod[0:D]); par[:, 37:45] = col(mod[D:2 * D])
    bcp = np.zeros((128, 320), np.float32)
    bcp[:, 0:128] = inp['dsa_kv_norm'][l][None]; bcp[:, 128:192] = inp['dsa_ik_g'][l][None]; bcp[:, 192:256] = inp['dsa_ik_b'][l][None]
    lora = np.ascontiguousarray(np.concatenate([inp['rwkv_w2'][l], inp['rwkv_a2'][l]], 0))
    wuk = np.ascontiguousarray(inp['dsa_w_uk'][l].reshape(2, 128, 128).transpose(1, 0, 2))
    return {"par": par, "bcp": bcp, "lora": lora, "g2w": np.ascontiguousarray(inp['rwkv_g2'][l]), "wuk": wuk,
            "cst": consts128(), "w_in": np.ascontiguousarray(inp['w_in'][l])}


def alibi_slopes(n):
    return (2.0 ** (-8.0 * (np.arange(n, dtype=np.float32) + 1.0) / n)).astype(np.float32)

K1_CAT = {"o_bon": 1, "o_g": 1, "o_qlat": 2, "o_iqs": 2, "o_sgn": 0, "o_ckv": 0, "o_ckvT": 1, "o_ikT": 1, "o_sq": 1, "o_sk": 1, "o_sv": 0,
          "o_YwT": 0, "o_Y0T": 0}

def k2_tables(i):
    sl = alibi_slopes(10)
    swa_sl, dsa_sl = sl[:6], sl[6:]
    p = np.arange(128)
    r = np.arange(8)[None, :, None]; pq = p[:, None, None]; pk = p[None, None, :]
    valid = (r < i) | ((r == i) & (pk <= pq))
    dmask = np.where(valid, 0.0, -1e30).astype(np.float32).reshape(128, 1024)
    dl = np.arange(128)[None, None, :]
    ab = (dsa_sl[None, :, None] * (128.0 * (7 - dl - i) + p[:, None, None] - 127.0)).astype(np.float32)
    q = p[None, None, None, :]; pk4 = p[:, None, None, None]; tile = np.arange(2)[None, :, None, None]
    dist = q - pk4 + np.where(tile == 0, 128, 0)
    ok = (dist >= 0) & (dist < 128)
    swb = np.where(ok, -swa_sl[None, None, :, None] * dist, -30000.0).astype(np.float32)
    swb_first = swb[:, 0].copy()
    if i == 0:
        swb_first[:] = -30000.0
    return {"dmask": dmask, "ab": np.ascontiguousarray(ab), "swb": np.ascontiguousarray(swb), "swb_first": np.ascontiguousarray(swb_first),
            "ident": np.eye(128, dtype=np.float32)}


def k2_inputs(K1, inp, l, NBQ):
    G = {n: np.concatenate([K1[c][n] for c in range(8)], ax) for n, ax in K1_CAT.items()}
    PT = np.stack([K1[c]["o_PT"] for c in range(8)]); Qa = np.stack([K1[c]["o_Q"] for c in range(8)])
    T = 8 * NBQ * 128
    bf = G["o_sk"].dtype
    maps = []
    for i in range(8):
        gbs = 8 * np.arange(NBQ) + i
        tok = (gbs[:, None] * 128 + np.arange(128)[None, :]).reshape(-1)
        prev = ((gbs - 1)[:, None] * 128 + np.arange(128)[None, :])
        own = (gbs[:, None] * 128 + np.arange(128)[None, :])
        m = {}
        m["YwT"] = np.ascontiguousarray(G["o_YwT"][gbs]); m["Y0T"] = np.ascontiguousarray(G["o_Y0T"][gbs])
        m["PbT"] = np.ascontiguousarray(PT[gbs // NBQ, gbs % NBQ]); m["Qb"] = np.ascontiguousarray(Qa[gbs // NBQ, gbs % NBQ])
        m["segPT"] = np.ascontiguousarray(PT[:, NBQ]); m["segQ"] = np.ascontiguousarray(Qa[:, NBQ])
        hm = lambda a: np.ascontiguousarray(a.reshape(6, 64, T)[:, :, tok].transpose(1, 0, 2))
        m["bon"] = hm(G["o_bon"]); m["g"] = hm(G["o_g"]); m["sq"] = hm(G["o_sq"])
        m["qlat"] = np.ascontiguousarray(G["o_qlat"][:, :, tok]); m["iqs"] = np.ascontiguousarray(G["o_iqs"][:, :, tok]); m["sgn"] = np.ascontiguousarray(G["o_sgn"][tok])
        sk = G["o_sk"].reshape(2, 64, T); sv = G["o_sv"]
        sk2 = np.zeros((64, 2, NBQ, 256), bf); sv2 = np.zeros((NBQ, 2, 128, 128), bf)
        for j in range(NBQ):
            if gbs[j] > 0:
                sk2[:, :, j, 0:128] = sk[:, :, prev[j]].transpose(1, 0, 2); sv2[j, 0] = sv[prev[j]]
            sk2[:, :, j, 128:256] = sk[:, :, own[j]].transpose(1, 0, 2); sv2[j, 1] = sv[own[j]]
        m["sk2"] = sk2; m["sv2"] = sv2
        m["ckv"] = G["o_ckv"]; m["ckvT"] = G["o_ckvT"]; m["ikT"] = G["o_ikT"]
        m["wuv"] = np.ascontiguousarray(inp['dsa_w_uv'][l])
        m["sink_b"] = np.ascontiguousarray(np.broadcast_to(inp['swa_sinks'][l][None], (64, 6)))
        rl = np.zeros((64, 16), np.float32)
        rl[:, 0:6] = inp['rwkv_ln_g'][l].reshape(6, 64).T; rl[:, 6:12] = inp['rwkv_ln_b'][l].reshape(6, 64).T
        m["rwkv_ln"] = rl
        m.update(k2_tables(i))
        maps.append(m)
    return maps


NPBF = ml_dtypes.bfloat16
NBQ_FULL = 16
_PROGS = {}


def _k1_spec(NBLK):
    NTk = NBLK * 128
    return {"o_YwT": ([NBLK, 6, 64, 128], F32), "o_Y0T": ([NBLK, 6, 64, 128], F32), "o_PT": ([NBLK + 1, 6, 64, 64], F32), "o_Q": ([NBLK + 1, 6, 64, 64], F32),
            "o_bon": ([384, NTk], F32), "o_g": ([384, NTk], F32), "o_qlat": ([128, 4, NTk], BF16), "o_iqs": ([64, 4, NTk], BF16), "o_sgn": ([NTk, 4], F32),
            "o_ckv": ([NTk, 128], BF16), "o_ckvT": ([128, NTk], BF16), "o_ikT": ([64, NTk], BF16), "o_sq": ([384, NTk], BF16), "o_sk": ([128, NTk], BF16),
            "o_sv": ([NTk, 128], BF16)}


def _build_k0():
    nc = bass.Bass("TRN2", target_bir_lowering=False)
    di = lambda n, s: nc.dram_tensor(n, list(s), F32, kind="ExternalInput").ap()
    dr = {"cT": di("cT", [128, 8]), "w_mod": di("w_mod", [4, 1024, 768]), "b_mod": di("b_mod", [4, 768])}
    dr["mod"] = nc.dram_tensor("mod", [4, 768], F32, kind="ExternalOutput").ap()
    with ExitStack() as es:
        P = Prog(nc, es)
        dr["out_tb"] = P.tb("out")
        emit_k0(P, nc, dr)
        P.finish([dr["out_tb"]])
    return nc


def _build_k1(NBLK):
    nc = bass.Bass("TRN2", target_bir_lowering=False)
    NTk = NBLK * 128
    di = lambda n, s: nc.dram_tensor(n, list(s), F32, kind="ExternalInput").ap()
    dr = {"x": di("x", [NTk, D]), "xh": di("xh", [1, D]), "par": di("par", [128, 64]), "bcp": di("bcp", [128, 320]), "lora": di("lora", [128, 384]),
          "g2w": di("g2w", [128, 384]), "wuk": di("wuk", [128, 2, 128]), "cst": di("cst", [128, 5, 128]), "w_in": di("w_in", [D, 2756])}
    for n, (s, dt) in _k1_spec(NBLK).items():
        dr[n] = nc.dram_tensor(n, s, dt, kind="ExternalOutput").ap()
    with ExitStack() as es:
        P = Prog(nc, es)
        dr["out_tb"] = P.tb("out")
        emit_k1(P, nc, NBLK, dr)
        P.finish([dr["out_tb"]])
    return nc


def _k2_in(NBQ):
    return {"YwT": ([NBQ, 6, 64, 128], F32), "Y0T": ([NBQ, 6, 64, 128], F32), "PbT": ([NBQ, 6, 64, 64], F32), "Qb": ([NBQ, 6, 64, 64], F32),
            "segPT": ([8, 6, 64, 64], F32), "segQ": ([8, 6, 64, 64], F32), "bon": ([64, 6, NBQ * 128], F32), "g": ([64, 6, NBQ * 128], F32),
            "sq": ([64, 6, NBQ * 128], BF16), "qlat": ([128, 4, NBQ * 128], BF16), "iqs": ([64, 4, NBQ * 128], BF16), "sgn": ([NBQ * 128, 4], F32),
            "sk2": ([64, 2, NBQ, 256], BF16), "sv2": ([NBQ, 2, 128, 128], BF16), "ckv": ([8 * NBQ * 128, 128], BF16), "ckvT": ([128, 8 * NBQ * 128], BF16),
            "ikT": ([64, 8 * NBQ * 128], BF16), "wuv": ([4, 128, 64], F32), "sink_b": ([64, 6], F32), "rwkv_ln": ([64, 16], F32), "dmask": ([128, 1024], F32),
            "ab": ([128, 4, 128], F32), "swb": ([128, 2, 6, 128], F32), "swb_first": ([128, 6, 128], F32), "ident": ([128, 128], F32)}


def _build_k2(NBQ):
    nc = bass.Bass("TRN2", target_bir_lowering=False)
    dr = {n: nc.dram_tensor(n, s, dt, kind="ExternalInput").ap() for n, (s, dt) in _k2_in(NBQ).items()}
    dr["mixT"] = nc.dram_tensor("mixT", [16, 64, NBQ * 128], BF16, kind="ExternalOutput").ap()
    with ExitStack() as es:
        P = Prog(nc, es)
        dr["out_tb"] = P.tb("out")
        emit_k2(P, nc, NBQ, dr)
        P.finish([dr["out_tb"]])
    return nc


def _build_k3(NT, exp_ids):
    nc = bass.Bass("TRN2", target_bir_lowering=False)
    di = lambda n, s: nc.dram_tensor(n, list(s), F32, kind="ExternalInput").ap()
    NX = len(exp_ids)
    dr = {"xres": di("xres", [NT, D]), "mixT": nc.dram_tensor("mixT", [16, 64, NT], BF16, kind="ExternalInput").ap(),
          "modb": di("modb", [128, 6 * D]), "modT": di("modT", [128, 48]), "lnp": di("lnp", [128, 4, D]), "w_out": di("w_out", [D, D]),
          "router_w": di("router_w", [D, NE]), "rbias_b": di("rbias_b", [128, NE]), "ew1": di("ew1", [NX, D, DE]), "ew3": di("ew3", [NX, D, DE]),
          "ew2": di("ew2", [NX, DE, D]), "ident": di("ident", [128, 128])}
    dr["out"] = nc.dram_tensor("out", [NT, D], F32, kind="ExternalOutput").ap()
    with ExitStack() as es:
        P = Prog(nc, es)
        dr["out_tb"] = P.tb("out")
        ident = P.sbuf("ident_sb", [128, 128], F32)
        ident_tb = P.tb("ident")
        P.op("sp", lambda e: e.dma_start(out=ident[:], in_=dr["ident"]), writes=[ident_tb], dma=True)
        emit_ffn(P, nc, NT, exp_ids, dr, ident, ident_tb)
        P.finish([dr["out_tb"]])
    return nc


def _prog(key, fn):
    if key not in _PROGS:
        _PROGS[key] = fn()
    return _PROGS[key]


def _run(nc, maps):
    res = run_bass_kernel_spmd(nc, maps, core_ids=list(range(8)))
    return [{k: np.asarray(v) for k, v in r.items()} for r in res.results]


def _forward(inp, NBQ=NBQ_FULL, n_layers=4, exp_ids=None):
    NT = NBQ * 128
    T = 8 * NT
    if exp_ids is None:
        exp_ids = list(range(NE)) + [NE]
    inp = {k: np.asarray(v) for k, v in inp.items()}
    cT = np.ascontiguousarray(inp['c'][0].reshape(8, 128).T)
    r0 = _run(_prog("k0", _build_k0), [{"cT": cT, "w_mod": np.ascontiguousarray(inp['w_mod'][:, :, i * 768:(i + 1) * 768]),
                                         "b_mod": np.ascontiguousarray(inp['b_mod'][:, i * 768:(i + 1) * 768])} for i in range(8)])
    mod = np.concatenate([r0[i]["mod"] for i in range(8)], 1)
    x = np.ascontiguousarray(inp['x'][0][:T])
    ident = np.eye(128, dtype=np.float32)
    toks = [((8 * np.arange(NBQ) + i)[:, None] * 128 + np.arange(128)[None, :]).reshape(-1) for i in range(8)]
    for l in range(n_layers):
        common = k1_common(inp, l, mod[l])
        maps = []
        for c in range(8):
            m = dict(common)
            m["par"] = common["par"].copy()
            m["par"][:, 45] = 0.0 if c == 0 else 1.0
            m["x"] = np.ascontiguousarray(x[c * NT:(c + 1) * NT])
            m["xh"] = np.ascontiguousarray(x[c * NT - 1:c * NT]) if c > 0 else np.zeros((1, D), np.float32)
            maps.append(m)
        K1 = _run(_prog(("k1", NBQ), lambda: _build_k1(NBQ)), maps)
        K2 = _run(_prog(("k2", NBQ), lambda: _build_k2(NBQ)), k2_inputs(K1, inp, l, NBQ))
        del K1
        sel = [e for e in exp_ids if e < NE]
        ew1 = np.concatenate([inp['exp_w1'][l][sel], inp['sh_w1'][l][None]], 0)
        ew3 = np.concatenate([inp['exp_w3'][l][sel], inp['sh_w3'][l][None]], 0)
        ew2 = np.concatenate([inp['exp_w2'][l][sel], inp['sh_w2'][l][None]], 0)
        lnp = np.stack([inp['ln_mix_g'][l], inp['ln_mix_b'][l], inp['ln_ffn_g'][l], inp['ln_ffn_b'][l]])
        common3 = {"modb": np.ascontiguousarray(np.broadcast_to(mod[l][None], (128, 6 * D))), "modT": np.ascontiguousarray(mod[l].reshape(48, 128).T),
                   "lnp": np.ascontiguousarray(np.broadcast_to(lnp[None], (128, 4, D))), "w_out": np.ascontiguousarray(inp['w_out'][l]),
                   "router_w": np.ascontiguousarray(inp['router_w'][l]),
                   "rbias_b": np.ascontiguousarray(np.broadcast_to(inp['router_bias'][l][None], (128, NE))), "ew1": ew1, "ew3": ew3, "ew2": ew2, "ident": ident}
        maps3 = [{**common3, "xres": np.ascontiguousarray(x[toks[i]]), "mixT": K2[i]["mixT"]} for i in range(8)]
        K3 = _run(_prog(("k3", NT, tuple(exp_ids)), lambda: _build_k3(NT, exp_ids)), maps3)
        del maps3, ew1, ew3, ew2
        xn = np.empty_like(x)
        for i in range(8):
            xn[toks[i]] = K3[i]["out"]
        x = xn
    return x


def kernel(**inputs):
    x = _forward(inputs)
    return np.ascontiguousarray(x[None].astype(np.float32))
```

```python
import numpy as np
import ml_dtypes


from contextlib import ExitStack
import concourse.bass as bass
import concourse.mybir as mybir
from concourse.bass_utils import run_bass_kernel_spmd

F32 = mybir.dt.float32
BF16 = mybir.dt.bfloat16
AF = mybir.ActivationFunctionType
ALU = mybir.AluOpType
AX = mybir.AxisListType


SKIP_SELF = False


class TB:
    __slots__ = ("name", "w", "r")

    def __init__(self, name="?"):
        self.name = name
        self.w = None
        self.r = {}


class Prog:
    ENGS = ("pe", "dve", "act", "pool", "sp")

    def __init__(self, nc, es, ring=8):
        self.nc = nc
        self.es = es
        self.ops = {e: [] for e in self.ENGS}
        self.cnt = {e: 0 for e in self.ENGS}
        self.waited = {e: {} for e in self.ENGS}
        self.sems = {}
        for e in self.ENGS:
            self.sems["c_" + e] = es.enter_context(nc.semaphore("c_" + e))
        self.ring = {}
        for e in ("sp", "pool", "act"):
            names = [f"d_{e}{i}" for i in range(ring)]
            for n in names:
                self.sems[n] = es.enter_context(nc.semaphore(n))
            self.ring[e] = {"names": names, "uses": [0] * ring, "next": 0}
        self.nbuf = 0

    def tb(self, name=None):
        self.nbuf += 1
        return TB(name or f"b{self.nbuf}")

    def sbuf(self, name, shape, dtype):
        return self.es.enter_context(self.nc.sbuf_tensor("sb_" + name, list(shape), dtype))

    def psum(self, name, shape, dtype=F32):
        return self.es.enter_context(self.nc.psum_tensor("ps_" + name, list(shape), dtype))

    def _need(self, eng, evs):
        need = {}
        for ev in evs:
            if ev is None:
                continue
            k, v = ev
            if need.get(k, 0) < v:
                need[k] = v
        w = self.waited[eng]
        for k, v in need.items():
            if SKIP_SELF and k == "c_" + eng:
                continue
            if w.get(k, 0) < v:
                self.ops[eng].append(("wait", k, v))
                w[k] = v

    def op(self, eng, fn, reads=(), writes=(), dma=False, acc=False):
        evs = []
        for b in reads:
            evs.append(b.w)
        for b in writes:
            if not (acc and eng == "pe" and b.w is not None and b.w[0] == "c_pe"):
                evs.append(b.w)
            for k, v in b.r.items():
                evs.append((k, v))
        if dma:
            rg = self.ring[eng]
            i = rg["next"]
            rg["next"] = (i + 1) % len(rg["names"])
            k = rg["names"][i]
            evs.append((k, 16 * rg["uses"][i]))
            rg["uses"][i] += 1
            ev = (k, 16 * rg["uses"][i])
            inc = 16
        else:
            self.cnt[eng] += 1
            k = "c_" + eng
            ev = (k, self.cnt[eng])
            inc = 1
        self._need(eng, evs)
        self.ops[eng].append(("op", fn, k, inc))
        for b in reads:
            if b.r.get(ev[0], 0) < ev[1]:
                b.r[ev[0]] = ev[1]
        for b in writes:
            b.w = ev
            b.r = {}
        return ev

    def barrier(self):
        evs = [("c_" + x, self.cnt[x]) for x in self.ENGS]
        for e, rg in self.ring.items():
            for n, u in zip(rg["names"], rg["uses"]):
                evs.append((n, 16 * u))
        for e in self.ENGS:
            self._need(e, evs)

    def scope(self):
        prog = self

        class _Scope:
            def __enter__(self_):
                self_.old = prog.es
                self_.st = ExitStack()
                self_.st.__enter__()
                prog.es = self_.st
                return prog

            def __exit__(self_, *a):
                prog.barrier()
                prog.es = self_.old
                return self_.st.__exit__(*a)
        return _Scope()

    def finish(self, out_bufs):
        self._need("sp", [b.w for b in out_bufs])
        nc = self.nc
        sems = self.sems
        ops = self.ops

        def replay(engobj, lst):
            for it in lst:
                if it[0] == "wait":
                    engobj.wait_ge(sems[it[1]], it[2])
                else:
                    ins = it[1](engobj)
                    ins.then_inc(sems[it[2]], it[3])

        with nc.Block() as block:
            @block.tensor
            def _(e):
                replay(e, ops["pe"])

            @block.vector
            def _(e):
                replay(e, ops["dve"])

            @block.scalar
            def _(e):
                replay(e, ops["act"])

            @block.gpsimd
            def _(e):
                replay(e, ops["pool"])

            @block.sync
            def _(e):
                replay(e, ops["sp"])


D = 1024
ALPHA = (2 * 4) ** 0.25
LN_EPS = 1e-5
NE = 64
DE = 256


def bcast_last(ap, n):
    shp = list(ap.shape)
    return ap.unsqueeze(len(shp)).to_broadcast(shp + [n])


class Consts:
    pass


def emit_layernorm(P, z, z_tb, out, out_tb, gb, bb, par_tb, sc, eps_t, tagn):
    st, mv, rs, xn = sc["st"], sc["mv"], sc["rs"], sc["xn"]
    stb = sc["tb"]
    P.op("dve", lambda e: e.bn_stats(out=st[:, 0, :], in_=z[:, 0:512]), reads=[z_tb], writes=[stb])
    P.op("dve", lambda e: e.bn_stats(out=st[:, 1, :], in_=z[:, 512:1024]), reads=[z_tb, stb], writes=[stb])
    P.op("dve", lambda e: e.bn_aggr(out=mv[:], in_=st[:].rearrange("p a b -> p (a b)")), reads=[stb], writes=[stb])
    P.op("act", lambda e: e.activation(out=rs[:], in_=mv[:, 1:2], func=AF.Sqrt, bias=eps_t[:, 0:1], scale=1.0),
         reads=[stb], writes=[stb])
    P.op("dve", lambda e: e.reciprocal(out=rs[:], in_=rs[:]), reads=[stb], writes=[stb])
    P.op("dve", lambda e: e.tensor_scalar(out=xn[:], in0=z, scalar1=mv[:, 0:1], scalar2=rs[:, 0:1],
                                          op0=ALU.subtract, op1=ALU.mult), reads=[z_tb, stb], writes=[sc["xn_tb"]])
    P.op("pool", lambda e: e.tensor_tensor(out=xn[:], in0=xn[:], in1=gb, op=ALU.mult),
         reads=[sc["xn_tb"]] + list(par_tb), writes=[sc["xn_tb"]])
    P.op("pool", lambda e: e.tensor_tensor(out=out, in0=xn[:], in1=bb, op=ALU.add),
         reads=[sc["xn_tb"]] + list(par_tb), writes=[out_tb])


NRW = 1408
C_DQ, C_CKV, C_IQ, C_IK, C_IW = 1408, 1664, 1792, 2048, 2112
C_SQ, C_SK, C_SV = 2116, 2500, 2628
PIN = 2756
DECAY_C = -0.6065306597126334


STOP = 99


class StopEmit(Exception):
    pass


def ck(n):
    if STOP == n:
        raise StopEmit()


class Slots:
    def __init__(self, P, aps, tbs=None):
        self.aps = aps
        self.tbs = tbs if tbs is not None else [P.tb() for _ in aps]
        self.i = 0

    def next(self):
        i = self.i
        self.i = (i + 1) % len(self.aps)
        return self.aps[i], self.tbs[i]


def emit_k1(P, nc, NBLK, dr):
    cst = P.sbuf("cst", [128, 5, 128], F32)
    cst_tb = P.tb("cst")
    P.op("sp", lambda e: e.dma_start(out=cst[:], in_=dr["cst"]), writes=[cst_tb], dma=True)
    ident, BO, LS, US, UI = (cst[:, i, :] for i in range(5))
    identb = P.sbuf("identb", [128, 128], BF16)
    P.op("dve", lambda e: e.tensor_copy(out=identb[:], in_=ident), reads=[cst_tb], writes=[cst_tb])
    par = P.sbuf("par", [128, 64], F32)
    par_tb = P.tb("par")
    P.op("sp", lambda e: e.dma_start(out=par[:], in_=dr["par"]), writes=[par_tb], dma=True)
    P.op("dve", lambda e: e.tensor_scalar(out=par[:, 37:45], in0=par[:, 37:45], scalar1=1.0, scalar2=None, op0=ALU.add),
         reads=[par_tb], writes=[par_tb])
    P.op("dve", lambda e: e.tensor_scalar(out=par[:, 23:26], in0=par[:, 20:23], scalar1=-1.0, scalar2=1.0, op0=ALU.mult, op1=ALU.add),
         reads=[par_tb], writes=[par_tb])
    bcp = P.sbuf("bcp", [128, 320], F32)
    P.op("sp", lambda e: e.dma_start(out=bcp[:], in_=dr["bcp"]), writes=[par_tb], dma=True)
    eps6 = P.sbuf("eps6", [128, 2], F32)
    P.op("pool", lambda e: e.memset(eps6[:, 0:1], 1e-6), writes=[par_tb])
    P.op("pool", lambda e: e.memset(eps6[:, 1:2], 1e-5), writes=[par_tb])
    lora = P.sbuf("lora", [128, 384], F32)
    g2w = P.sbuf("g2w", [128, 384], F32)
    P.op("sp", lambda e: e.dma_start(out=lora[:], in_=dr["lora"]), writes=[par_tb], dma=True)
    P.op("sp", lambda e: e.dma_start(out=g2w[:], in_=dr["g2w"]), writes=[par_tb], dma=True)
    wuk_f = P.sbuf("wuk_f", [128, 2, 128], F32)
    wuk = P.sbuf("wuk", [128, 2, 128], BF16)
    P.op("sp", lambda e: e.dma_start(out=wuk_f[:], in_=dr["wuk"]), writes=[par_tb], dma=True)
    P.op("dve", lambda e: e.tensor_copy(out=wuk[:], in_=wuk_f[:]), reads=[par_tb], writes=[par_tb])
    win = P.sbuf("win", [128, 8, PIN], BF16)
    win_tb = P.tb("win")
    wst = [P.sbuf(f"wst{i}", [128, PIN], F32) for i in range(2)]
    wst_tb = [P.tb() for _ in range(2)]
    for kc in range(8):
        s = kc % 2
        P.op("sp", lambda e, kc=kc, s=s: e.dma_start(out=wst[s][:], in_=dr["w_in"][kc * 128:(kc + 1) * 128, :]),
             writes=[wst_tb[s]], dma=True)
        P.op("pool", lambda e, kc=kc, s=s: e.tensor_copy(out=win[:, kc, :], in_=wst[s][:]), reads=[wst_tb[s]], writes=[win_tb])
    pbk = [P.psum(f"k1pb{i}", [128, 512], F32) for i in range(8)]
    bank_tb = [P.tb(f"bank{i}") for i in range(8)]
    q128 = Slots(P, [pbk[b][:, q * 128:(q + 1) * 128] for q in range(4) for b in (2, 3, 4, 5)],
                 [bank_tb[b] for q in range(4) for b in (2, 3, 4, 5)])
    h256 = Slots(P, [pbk[b][:, q * 256:(q + 1) * 256] for q in range(2) for b in (6, 7)],
                 [bank_tb[b] for q in range(2) for b in (6, 7)])
    pT_tb = [bank_tb[0], bank_tb[1]]
    xb = [P.sbuf(f"xb{i}", [128, D], F32) for i in range(2)]
    xb_tb = [P.tb(), P.tb()]
    hT = P.sbuf("hT", [128, 8, 128], BF16)
    hT_tb = P.tb("hT")
    hh = P.sbuf("hh", [128, 8, 1], BF16)
    pr = P.sbuf("pr", [128, 11, 129], F32)
    pr_tb = P.tb("pr")
    prev = P.sbuf("prevc", [128, 11, 1], F32)
    prev_tb = P.tb("prev")
    X = P.sbuf("X", [128, 11, 128], F32)
    X_tb = P.tb("X")
    dif = P.sbuf("dif", [128, 11, 128], F32)
    fm = {n: P.sbuf("fm_" + n, [128, 3, 128], F32) for n in
          ["lw", "a", "kk", "kp", "t1", "cum", "einc", "eexc", "einv", "eend", "at", "bh", "kh", "rt", "bc", "kc", "g", "bon", "beta"]}
    fa = P.tb("fmall")
    fm_tb = {n: fa for n in fm}
    LA = P.sbuf("LA", [128, 128], F32)
    SG = P.sbuf("SG", [128, 128], F32)
    gC = P.sbuf("gC", [128, 3], F32)
    GCe = P.sbuf("GCe", [128, 3], F32)
    ones = P.sbuf("ones128", [128, 128], F32)
    P.op("pool", lambda e: e.memset(ones[:], 1.0), writes=[par_tb])
    tokm = {n: P.sbuf("tok_" + n, [128, 384], F32) for n in ["at", "bc", "kc", "v"]}
    tok_tb = {n: P.tb("tok_" + n) for n in tokm}
    tk = P.sbuf("tk", [128, 580], F32)
    tk_tb = P.tb("tk")
    HW = [{"MX": P.sbuf(f"MX{i}", [128, 256], F32), "MT": P.sbuf(f"MT{i}", [128, 128], F32), "MKT": P.sbuf(f"MKT{i}", [128, 128], F32),
           "NBT": P.sbuf(f"NBT{i}", [128, 128], F32), "NKT": P.sbuf(f"NKT{i}", [128, 128], F32), "DG": P.sbuf(f"DG{i}", [128, 128], F32)} for i in range(2)]
    HW_tb = [{n: P.tb(f"{n}{i}") for n in ("MX", "MT", "MKT", "NBT", "NKT", "DG")} for i in range(2)]
    ATb = P.sbuf("ATb", [64, 6, 64], F32)
    Db = P.sbuf("Db", [64, 6, 64], F32)
    AD_tb = [P.tb() for _ in range(6)]
    PQ = P.sbuf("PQ", [64, 6, 128], F32)
    PTt = P.sbuf("PTt", [64, 6, 64], F32)
    PQ_tb = [P.tb() for _ in range(6)]
    ost = [P.sbuf(f"ost{i}", [128, 128], F32) for i in range(4)]
    ost_s = Slots(P, [o[:] for o in ost])
    obf = [P.sbuf(f"obf{i}", [128, 512], BF16) for i in range(4)]
    obf_s = Slots(P, [o[:] for o in obf])
    nsc = {"st": P.sbuf("n_st", [128, 6], F32), "mv": P.sbuf("n_mv", [128, 2], F32), "rs": P.sbuf("n_rs", [128, 2], F32),
           "aiw": P.sbuf("n_aiw", [128, 4], F32), "sg": P.sbuf("n_sg", [128, 4], F32), "t": P.sbuf("n_t", [128, 256], F32)}
    nsc_tb = P.tb("nsc")
    out_tb = dr["out_tb"]

    def out_dma(dst, src, rtb):
        P.op("sp", lambda e: e.dma_start(out=dst, in_=src), reads=[rtb], writes=[out_tb], dma=True)

    for hd in range(6):
        P.op("pool", lambda e, hd=hd: e.memset(PQ[:, hd, 64:128], 0.0), writes=[PQ_tb[hd]])
        P.op("pool", lambda e, hd=hd: e.tensor_copy(out=PQ[:, hd, 0:64], in_=ident[0:64, 0:64]), reads=[cst_tb], writes=[PQ_tb[hd]])
        P.op("pool", lambda e, hd=hd: e.tensor_copy(out=PTt[:, hd, :], in_=ident[0:64, 0:64]), reads=[cst_tb], writes=[PQ_tb[hd]])

    def emit_pq_out(b):
        for hd in range(6):
            out_dma(dr["o_PT"][b, hd], PTt[:, hd, :], PQ_tb[hd])
            out_dma(dr["o_Q"][b, hd], PQ[:, hd, 64:128], PQ_tb[hd])

    ck(1)
    P.op("sp", lambda e: e.dma_start(out=xb[1][0:1, :], in_=dr["xh"]), writes=[xb_tb[1]], dma=True)
    for kc in range(8):
        P.op("pe", lambda e, kc=kc: e.transpose(out=pbk[0][:, kc:kc + 1], in_=xb[1][0:1, kc * 128:(kc + 1) * 128],
                                                identity=ident[0:1, 0:1]), reads=[xb_tb[1], cst_tb], writes=[pT_tb[0]], acc=True)
    for kc in range(8):
        P.op("act", lambda e, kc=kc: e.activation(out=hh[:, kc, :], in_=pbk[0][:, kc:kc + 1], func=AF.Identity,
                                                  bias=par[:, 29 + kc:30 + kc], scale=par[:, 37 + kc:38 + kc]),
             reads=[pT_tb[0], par_tb], writes=[hT_tb])
    for c in range(11):
        pa, ptb = q128.next()
        for kc in range(8):
            P.op("pe", lambda e, c=c, kc=kc, pa=pa: e.matmul(pa[:, 0:1], lhsT=win[:, kc, c * 128:(c + 1) * 128], rhs=hh[:, kc, :],
                                                             start=(kc == 0), stop=(kc == 7)), reads=[win_tb, hT_tb], writes=[ptb], acc=True)
        P.op("dve", lambda e, c=c, pa=pa: e.tensor_scalar(out=prev[:, c, :], in0=pa[:, 0:1], scalar1=par[:, 45:46], scalar2=None, op0=ALU.mult),
             reads=[ptb, par_tb], writes=[prev_tb])

    ck(2)
    emit_pq_out(0)
    for b in range(NBLK):
        t0 = b * 128
        xi = b % 2
        P.op("sp", lambda e, xi=xi, t0=t0: e.dma_start(out=xb[xi][:], in_=dr["x"][t0:t0 + 128, :]), writes=[xb_tb[xi]], dma=True)
        for kc in range(8):
            bank = kc // 4
            P.op("pe", lambda e, kc=kc, bank=bank, xi=xi: e.transpose(out=pbk[bank][:, (kc % 4) * 128:(kc % 4 + 1) * 128],
                                                                      in_=xb[xi][:, kc * 128:(kc + 1) * 128], identity=ident),
                 reads=[xb_tb[xi], cst_tb], writes=[pT_tb[bank]], acc=True)
        for kc in range(8):
            bank = kc // 4
            P.op("act", lambda e, kc=kc, bank=bank: e.activation(out=hT[:, kc, :], in_=pbk[bank][:, (kc % 4) * 128:(kc % 4 + 1) * 128],
                                                                 func=AF.Identity, bias=par[:, 29 + kc:30 + kc], scale=par[:, 37 + kc:38 + kc]),
                 reads=[pT_tb[bank], par_tb], writes=[hT_tb])

        ck(3)

        def projT(col0, evac):
            pa, ptb = q128.next()
            for kc in range(8):
                P.op("pe", lambda e, kc=kc, pa=pa: e.matmul(pa, lhsT=win[:, kc, col0:col0 + 128], rhs=hT[:, kc, :],
                                                            start=(kc == 0), stop=(kc == 7)), reads=[win_tb, hT_tb], writes=[ptb], acc=True)
            evac(pa, ptb)

        P.op("pool", lambda e: e.tensor_copy(out=pr[:, :, 0:1], in_=prev[:]), reads=[prev_tb], writes=[pr_tb])
        for c in range(11):
            projT(c * 128, lambda pa, ptb, c=c: P.op("act", lambda e: e.activation(out=pr[:, c, 1:129], in_=pa, func=AF.Identity),
                                                     reads=[ptb], writes=[pr_tb]))
        P.op("pool", lambda e: e.tensor_copy(out=prev[:], in_=pr[:, :, 128:129]), reads=[pr_tb], writes=[prev_tb])
        P.op("dve", lambda e: e.tensor_tensor(out=dif[:], in0=pr[:, :, 0:128], in1=pr[:, :, 1:129], op=ALU.subtract), reads=[pr_tb], writes=[X_tb])
        P.op("dve", lambda e: e.tensor_tensor(out=dif[:], in0=dif[:], in1=bcast_last(par[:, 0:11], 128), op=ALU.mult), reads=[X_tb, par_tb], writes=[X_tb])
        P.op("dve", lambda e: e.tensor_tensor(out=X[:], in0=dif[:], in1=pr[:, :, 1:129], op=ALU.add), reads=[X_tb, pr_tb], writes=[X_tb])
        ck(4)
        r_, k_, v_ = X[:, 0:3, :], X[:, 3:6, :], X[:, 6:9, :]
        P.op("act", lambda e: e.activation(out=LA[0:64, :], in_=X[0:64, 9, :], func=AF.Tanh), reads=[X_tb], writes=[fm_tb["lw"]])
        P.op("act", lambda e: e.activation(out=LA[64:128, :], in_=X[64:128, 9, :], func=AF.Identity), reads=[X_tb], writes=[fm_tb["lw"]])
        P.op("act", lambda e: e.activation(out=SG[:], in_=X[:, 10, :], func=AF.Sigmoid), reads=[X_tb], writes=[fm_tb["g"]])
        for cc in range(3):
            pa, ptb = q128.next()
            P.op("pe", lambda e, cc=cc, pa=pa: e.matmul(pa, lhsT=lora[0:64, cc * 128:(cc + 1) * 128], rhs=LA[0:64, :], start=True, stop=True),
                 reads=[par_tb, fm_tb["lw"]], writes=[ptb])
            P.op("act", lambda e, cc=cc, pa=pa: e.activation(out=fm["lw"][:, cc, :], in_=pa, func=AF.Sigmoid, bias=par[:, 11 + cc:12 + cc], scale=1.0),
                 reads=[ptb, par_tb], writes=[fm_tb["cum"]])
            pa2, ptb2 = q128.next()
            P.op("pe", lambda e, cc=cc, pa2=pa2: e.matmul(pa2, lhsT=lora[64:128, cc * 128:(cc + 1) * 128], rhs=LA[64:128, :], start=True, stop=True),
                 reads=[par_tb, fm_tb["lw"]], writes=[ptb2])
            P.op("act", lambda e, cc=cc, pa2=pa2: e.activation(out=fm["a"][:, cc, :], in_=pa2, func=AF.Sigmoid, bias=par[:, 14 + cc:15 + cc], scale=1.0),
                 reads=[ptb2, par_tb], writes=[fm_tb["a"]])
            pa3, ptb3 = q128.next()
            P.op("pe", lambda e, cc=cc, pa3=pa3: e.matmul(pa3, lhsT=g2w[:, cc * 128:(cc + 1) * 128], rhs=SG[:], start=True, stop=True),
                 reads=[par_tb, fm_tb["g"]], writes=[ptb3])
            P.op("act", lambda e, cc=cc, pa3=pa3: e.activation(out=fm["g"][:, cc, :], in_=pa3, func=AF.Identity), reads=[ptb3], writes=[fm_tb["bon"]])
        dv = lambda fn, rd, wr: P.op("dve", fn, reads=[fm_tb[n] if isinstance(n, str) else n for n in rd],
                                     writes=[fm_tb[n] if isinstance(n, str) else n for n in wr])
        F = fm
        pcol = lambda c0: bcast_last(par[:, c0:c0 + 3], 128)
        dv(lambda e: e.tensor_scalar(out=F["lw"][:], in0=F["lw"][:], scalar1=DECAY_C, scalar2=None, op0=ALU.mult), ["cum"], ["cum"])
        out_dma(dr["o_g"].rearrange("(c p) t -> p c t", p=128)[:, :, t0:t0 + 128], F["g"][:], fm_tb["bon"])
        dv(lambda e: e.tensor_tensor(out=F["kk"][:], in0=k_, in1=pcol(17), op=ALU.mult), [X_tb, par_tb], ["kk"])
        dv(lambda e: e.tensor_tensor(out=F["t1"][:], in0=F["kk"][:], in1=F["kk"][:], op=ALU.mult), ["kk"], ["t1"])
        for cc in range(3):
            pa, ptb = q128.next()
            P.op("pe", lambda e, cc=cc, pa=pa: e.matmul(pa, lhsT=BO, rhs=F["t1"][:, cc, :], start=True, stop=True), reads=[cst_tb, fm_tb["t1"]], writes=[ptb])
            P.op("act", lambda e, cc=cc, pa=pa: e.activation(out=F["kp"][:, cc, :], in_=pa, func=AF.Sqrt), reads=[ptb], writes=[fm_tb["kp"]])
        dv(lambda e: e.tensor_scalar(out=F["kp"][:], in0=F["kp"][:], scalar1=1e-12, scalar2=None, op0=ALU.max), ["kp"], ["kp"])
        dv(lambda e: e.reciprocal(out=F["kp"][:], in_=F["kp"][:]), ["kp"], ["kp"])
        dv(lambda e: e.tensor_tensor(out=F["kk"][:], in0=F["kk"][:], in1=F["kp"][:], op=ALU.mult), ["kk", "kp"], ["kk"])
        dv(lambda e: e.tensor_tensor(out=F["t1"][:], in0=F["a"][:], in1=pcol(20), op=ALU.mult), ["a", par_tb], ["t1"])
        dv(lambda e: e.tensor_tensor(out=F["t1"][:], in0=F["t1"][:], in1=pcol(23), op=ALU.add), ["t1", par_tb], ["t1"])
        dv(lambda e: e.tensor_tensor(out=F["kp"][:], in0=k_, in1=F["t1"][:], op=ALU.mult), [X_tb, "t1", "kp"], ["kp"])
        dv(lambda e: e.tensor_tensor(out=F["t1"][:], in0=r_, in1=pcol(26), op=ALU.mult), [X_tb, par_tb], ["t1"])
        dv(lambda e: e.tensor_tensor(out=F["t1"][:], in0=F["t1"][:], in1=F["kp"][:], op=ALU.mult), ["t1", "kp"], ["t1"])
        for cc in range(3):
            pa, ptb = q128.next()
            P.op("pe", lambda e, cc=cc, pa=pa: e.matmul(pa, lhsT=BO, rhs=F["t1"][:, cc, :], start=True, stop=True), reads=[cst_tb, fm_tb["t1"]], writes=[ptb])
            P.op("dve", lambda e, cc=cc, pa=pa: e.tensor_tensor(out=F["bon"][:, cc, :], in0=pa, in1=X[:, 6 + cc, :], op=ALU.mult),
                 reads=[ptb, X_tb], writes=[fm_tb["g"]])
        out_dma(dr["o_bon"].rearrange("(c p) t -> p c t", p=128)[:, :, t0:t0 + 128], F["bon"][:], fm_tb["g"])
        dv(lambda e: e.tensor_tensor(out=F["beta"][:], in0=F["a"][:], in1=F["kk"][:], op=ALU.mult), ["a", "kk"], ["beta"])
        for cc in range(3):
            dv(lambda e, cc=cc: e.tensor_tensor_scan(out=F["cum"][:, cc, :], data0=ones[:], data1=F["lw"][:, cc, :], initial=0.0,
                                                     op0=ALU.mult, op1=ALU.add), ["cum", par_tb], ["einc"])
        dv(lambda e: e.tensor_copy(out=gC[:], in_=F["cum"][:, :, 127]), ["einc"], ["einc"])
        dv(lambda e: e.tensor_tensor(out=F["t1"][:], in0=F["cum"][:], in1=F["lw"][:], op=ALU.subtract), ["einc", "t1"], ["t1"])
        ac = lambda fn, rd, wr: P.op("act", fn, reads=[fm_tb[n] for n in rd], writes=[fm_tb[n] for n in wr])
        ac(lambda e: e.activation(out=F["einc"][:], in_=F["cum"][:], func=AF.Exp), ["einc"], ["eexc"])
        ac(lambda e: e.activation(out=F["eexc"][:], in_=F["t1"][:], func=AF.Exp), ["t1"], ["einv"])
        ac(lambda e: e.activation(out=F["einv"][:], in_=F["cum"][:], func=AF.Exp, scale=-1.0), ["einc"], ["eend"])
        for cc in range(3):
            ac(lambda e, cc=cc: e.activation(out=F["eend"][:, cc, :], in_=F["cum"][:, cc, :], func=AF.Exp, scale=-1.0, bias=gC[:, cc:cc + 1]),
               ["einc"], ["at"])
        ac(lambda e: e.activation(out=GCe[:], in_=gC[:], func=AF.Exp), ["einc"], ["at"])
        dv(lambda e: e.scalar_tensor_tensor(out=F["at"][:], in0=F["kk"][:], scalar=-1.0, in1=F["eexc"][:], op0=ALU.mult, op1=ALU.mult),
           ["kk", "einv", "at"], ["bh"])
        dv(lambda e: e.tensor_tensor(out=F["bh"][:], in0=F["beta"][:], in1=F["einv"][:], op=ALU.mult), ["beta", "eend"], ["kh"])
        dv(lambda e: e.tensor_tensor(out=F["kh"][:], in0=F["kp"][:], in1=F["einv"][:], op=ALU.mult), ["kp", "eend"], ["rt"])
        dv(lambda e: e.tensor_tensor(out=F["rt"][:], in0=r_, in1=F["einc"][:], op=ALU.mult), [X_tb, "eexc"], ["bc"])
        dv(lambda e: e.tensor_tensor(out=F["bc"][:], in0=F["beta"][:], in1=F["eend"][:], op=ALU.mult), ["beta", "at"], ["kc"])
        dv(lambda e: e.tensor_tensor(out=F["kc"][:], in0=F["kp"][:], in1=F["eend"][:], op=ALU.mult), ["kp", "at"], ["lw"])
        allf = [fm_tb[n] for n in ("bh", "kh", "rt", "bc", "kc", "lw")]
        ck(5)
        for nm, src, stb in (("at", F["at"], fm_tb["bh"]), ("bc", F["bc"], fm_tb["kc"]), ("kc", F["kc"], fm_tb["lw"]), ("v", None, X_tb)):
            for cc in range(3):
                pa, ptb = q128.next()
                s_ap = X[:, 6 + cc, :] if src is None else src[:, cc, :]
                P.op("pe", lambda e, pa=pa, s_ap=s_ap: e.transpose(out=pa, in_=s_ap, identity=ident), reads=[stb, cst_tb], writes=[ptb])
                P.op("act", lambda e, pa=pa, nm=nm, cc=cc: e.activation(out=tokm[nm][:, cc * 128:(cc + 1) * 128], in_=pa, func=AF.Identity),
                     reads=[ptb], writes=[tok_tb[nm]])
        ck(6)
        def _head(hd, b=b):
            cc, p0 = hd // 2, (hd % 2) * 64
            sl = slice(p0, p0 + 64)
            aT, bT, kT, rT = F["at"][sl, cc, :], F["bh"][sl, cc, :], F["kh"][sl, cc, :], F["rt"][sl, cc, :]
            tcol = slice(hd * 64, hd * 64 + 64)
            MX, MT, MKT, NBT, NKT, DG = (HW[hd % 2][n] for n in ("MX", "MT", "MKT", "NBT", "NKT", "DG"))
            MX_tb, MT_tb, MKT_tb, NBT_tb, NKT_tb, DG_tb = (HW_tb[hd % 2][n] for n in ("MX", "MT", "MKT", "NBT", "NKT", "DG"))

            def mm_mask(lhsT, rhs, mask, dst, dtb):
                pa, ptb = q128.next()
                P.op("pe", lambda e: e.matmul(pa, lhsT=lhsT, rhs=rhs, start=True, stop=True), reads=allf, writes=[ptb])
                P.op("dve", lambda e: e.tensor_tensor(out=dst, in0=pa, in1=mask, op=ALU.mult), reads=[ptb, cst_tb], writes=[dtb])

            mm_mask(aT, bT, LS, MX[:, 0:128], MX_tb)
            mm_mask(bT, aT, US, MT[:], MT_tb)
            mm_mask(kT, aT, US, MKT[:], MKT_tb)
            mm_mask(bT, rT, UI, NBT[:], NBT_tb)
            mm_mask(kT, rT, UI, NKT[:], NKT_tb)
            P.op("pool", lambda e, tcol=tcol: e.tensor_copy(out=MX[:, 128:192], in_=tokm["at"][:, tcol]), reads=[tok_tb["at"]], writes=[MX_tb])
            pa, ptb = q128.next()
            P.op("pe", lambda e, pa=pa, tcol=tcol: e.matmul(pa[:, 0:64], lhsT=MKT[:], rhs=tokm["v"][:, tcol], start=True, stop=True),
                 reads=[MKT_tb, tok_tb["v"]], writes=[ptb])
            P.op("act", lambda e, pa=pa: e.activation(out=MX[:, 192:256], in_=pa[:, 0:64], func=AF.Identity), reads=[ptb], writes=[MX_tb])
            for it in range(7):
                last = it == 6
                ph, phtb = h256.next()
                if not last:
                    P.op("pe", lambda e, ph=ph: e.matmul(ph, lhsT=MT[:], rhs=MX[:], start=True, stop=True), reads=[MT_tb, MX_tb], writes=[phtb])
                    pa, ptb = q128.next()
                    P.op("pe", lambda e, pa=pa: e.matmul(pa, lhsT=MX[:, 0:128], rhs=MT[:], start=True, stop=True), reads=[MT_tb, MX_tb], writes=[ptb])
                    P.op("act", lambda e, ph=ph: e.activation(out=MX[:, 0:128], in_=ph[:, 0:128], func=AF.Identity), reads=[phtb], writes=[MX_tb])
                    P.op("dve", lambda e, ph=ph: e.tensor_tensor(out=MX[:, 128:256], in0=MX[:, 128:256], in1=ph[:, 128:256], op=ALU.add),
                         reads=[phtb, MX_tb], writes=[MX_tb])
                    P.op("act", lambda e, pa=pa: e.activation(out=MT[:], in_=pa, func=AF.Identity), reads=[ptb], writes=[MT_tb])
                else:
                    P.op("pe", lambda e, ph=ph: e.matmul(ph[:, 128:256], lhsT=MT[:], rhs=MX[:, 128:256], start=True, stop=True),
                         reads=[MT_tb, MX_tb], writes=[phtb])
                    P.op("dve", lambda e, ph=ph: e.tensor_tensor(out=MX[:, 128:256], in0=MX[:, 128:256], in1=ph[:, 128:256], op=ALU.add),
                         reads=[phtb, MX_tb], writes=[MX_tb])
            W0, U0 = MX[:, 128:192], MX[:, 192:256]
            P.op("dve", lambda e, p0=p0, cc=cc: e.tensor_scalar(out=DG[:, 0:64], in0=cst[:, 0, p0:p0 + 64], scalar1=GCe[:, cc:cc + 1], scalar2=None, op0=ALU.mult),
                 reads=[cst_tb, fm_tb["at"]], writes=[DG_tb])
            pa, ptb = q128.next()
            P.op("pe", lambda e, pa=pa, tcol=tcol: e.matmul(pa[0:64, 0:64], lhsT=W0, rhs=tokm["bc"][:, tcol], start=True, stop=False),
                 reads=[MX_tb, tok_tb["bc"]], writes=[ptb])
            P.op("pe", lambda e, pa=pa, p0=p0: e.matmul(pa[0:64, 0:64], lhsT=cst[:, 0, p0:p0 + 64], rhs=DG[:, 0:64], start=False, stop=True),
                 reads=[DG_tb, cst_tb], writes=[ptb], acc=True)
            P.op("act", lambda e, pa=pa, hd=hd: e.activation(out=ATb[:, hd, :], in_=pa[0:64, 0:64], func=AF.Identity), reads=[ptb], writes=[AD_tb[hd]])
            pa, ptb = q128.next()
            P.op("pe", lambda e, pa=pa, tcol=tcol: e.matmul(pa[0:64, 0:64], lhsT=tokm["bc"][:, tcol], rhs=U0, start=True, stop=False),
                 reads=[MX_tb, tok_tb["bc"]], writes=[ptb])
            P.op("pe", lambda e, pa=pa, tcol=tcol: e.matmul(pa[0:64, 0:64], lhsT=tokm["kc"][:, tcol], rhs=tokm["v"][:, tcol], start=False, stop=True),
                 reads=[tok_tb["kc"], tok_tb["v"]], writes=[ptb], acc=True)
            P.op("act", lambda e, pa=pa, hd=hd: e.activation(out=Db[:, hd, :], in_=pa[0:64, 0:64], func=AF.Identity), reads=[ptb], writes=[AD_tb[hd]])
            pa, ptb = q128.next()
            P.op("pe", lambda e, pa=pa: e.matmul(pa[0:64, :], lhsT=W0, rhs=NBT[:], start=True, stop=False), reads=[MX_tb, NBT_tb], writes=[ptb])
            P.op("pe", lambda e, pa=pa, p0=p0, cc=cc: e.matmul(pa[0:64, :], lhsT=cst[:, 0, p0:p0 + 64], rhs=F["rt"][:, cc, :], start=False, stop=True),
                 reads=[cst_tb] + allf, writes=[ptb], acc=True)
            oa, otb = ost_s.next()
            P.op("act", lambda e, pa=pa, oa=oa: e.activation(out=oa[0:64, :], in_=pa[0:64, :], func=AF.Identity), reads=[ptb], writes=[otb])
            out_dma(dr["o_YwT"][b, hd], oa[0:64, :], otb)
            pa, ptb = q128.next()
            P.op("pe", lambda e, pa=pa: e.matmul(pa[0:64, :], lhsT=U0, rhs=NBT[:], start=True, stop=False), reads=[MX_tb, NBT_tb], writes=[ptb])
            P.op("pe", lambda e, pa=pa, tcol=tcol: e.matmul(pa[0:64, :], lhsT=tokm["v"][:, tcol], rhs=NKT[:], start=False, stop=True),
                 reads=[tok_tb["v"], NKT_tb], writes=[ptb], acc=True)
            oa, otb = ost_s.next()
            P.op("act", lambda e, pa=pa, oa=oa: e.activation(out=oa[0:64, :], in_=pa[0:64, :], func=AF.Identity), reads=[ptb], writes=[otb])
            out_dma(dr["o_Y0T"][b, hd], oa[0:64, :], otb)
            pa, ptb = q128.next()
            P.op("pe", lambda e, pa=pa, hd=hd: e.matmul(pa[0:64, :], lhsT=ATb[:, hd, :], rhs=PQ[:, hd, :], start=True, stop=True),
                 reads=[AD_tb[hd], PQ_tb[hd]], writes=[ptb])
            pa2, ptb2 = q128.next()
            P.op("pe", lambda e, pa2=pa2, hd=hd: e.matmul(pa2[0:64, 0:64], lhsT=PQ[:, hd, 0:64], rhs=ATb[:, hd, :], start=True, stop=True),
                 reads=[AD_tb[hd], PQ_tb[hd]], writes=[ptb2])
            P.op("act", lambda e, pa=pa, hd=hd: e.activation(out=PQ[:, hd, 0:64], in_=pa[0:64, 0:64], func=AF.Identity), reads=[ptb], writes=[PQ_tb[hd]])
            P.op("dve", lambda e, pa=pa, hd=hd: e.tensor_tensor(out=PQ[:, hd, 64:128], in0=pa[0:64, 64:128], in1=Db[:, hd, :], op=ALU.add),
                 reads=[ptb, AD_tb[hd]], writes=[PQ_tb[hd]])
            P.op("act", lambda e, pa2=pa2, hd=hd: e.activation(out=PTt[:, hd, :], in_=pa2[0:64, 0:64], func=AF.Identity), reads=[ptb2], writes=[PQ_tb[hd]])
        for hd in range(6):
            _head(hd)
        ck(7)
        emit_pq_out(b + 1)

        for m in range(2):
            def ev(pa, ptb, m=m):
                oa, otb = obf_s.next()
                P.op("act", lambda e: e.activation(out=oa[:, 0:128], in_=pa, func=AF.Identity), reads=[ptb], writes=[otb])
                for hh_ in range(2):
                    h = 2 * m + hh_
                    s2 = slice(hh_ * 64, hh_ * 64 + 64)
                    pq, pqtb = q128.next()
                    P.op("pe", lambda e, pq=pq, s2=s2: e.matmul(pq, lhsT=wuk[s2, m, :], rhs=oa[s2, 0:128], start=True, stop=True), reads=[otb, par_tb], writes=[pqtb])
                    ob, obtb = obf_s.next()
                    P.op("act", lambda e, pq=pq, ob=ob: e.activation(out=ob[:, 0:128], in_=pq, func=AF.Identity, scale=0.125), reads=[pqtb], writes=[obtb])
                    out_dma(dr["o_qlat"][:, h, t0:t0 + 128], ob[:, 0:128], obtb)
            projT(C_DQ + m * 128, ev)
        for m in range(3):
            def ev(pa, ptb, m=m):
                oa, otb = obf_s.next()
                P.op("act", lambda e: e.activation(out=oa[:, 0:128], in_=pa, func=AF.Identity), reads=[ptb], writes=[otb])
                out_dma(dr["o_sq"][m * 128:(m + 1) * 128, t0:t0 + 128], oa[:, 0:128], otb)
            projT(C_SQ + m * 128, ev)

        def ev(pa, ptb):
            oa, otb = obf_s.next()
            P.op("act", lambda e: e.activation(out=oa[:, 0:128], in_=pa, func=AF.Identity), reads=[ptb], writes=[otb])
            out_dma(dr["o_sk"][:, t0:t0 + 128], oa[:, 0:128], otb)
        projT(C_SK, ev)
        ck(8)
        for (col0, ncol, off, bank) in ((C_CKV, 452, 0, 0), (C_SV, 128, 452, 1)):
            for kc in range(8):
                P.op("pe", lambda e, kc=kc, col0=col0, ncol=ncol, bank=bank: e.matmul(pbk[bank][:, 0:ncol], lhsT=hT[:, kc, :], rhs=win[:, kc, col0:col0 + ncol],
                                                                                      start=(kc == 0), stop=(kc == 7)),
                     reads=[win_tb, hT_tb], writes=[pT_tb[bank]], acc=True)
            P.op("act", lambda e, ncol=ncol, off=off, bank=bank: e.activation(out=tk[:, off:off + ncol], in_=pbk[bank][:, 0:ncol], func=AF.Identity),
                 reads=[pT_tb[bank]], writes=[tk_tb])
        N = nsc
        nv = lambda fn: P.op("dve", fn, reads=[tk_tb, nsc_tb, par_tb], writes=[nsc_tb])
        nv(lambda e: e.tensor_tensor(out=N["t"][:, 0:128], in0=tk[:, 0:128], in1=tk[:, 0:128], op=ALU.mult))
        nv(lambda e: e.tensor_reduce(out=N["rs"][:, 0:1], in_=N["t"][:, 0:128], axis=AX.X, op=ALU.add))
        P.op("act", lambda e: e.activation(out=N["rs"][:, 0:1], in_=N["rs"][:, 0:1], func=AF.Sqrt, scale=1.0 / 128, bias=eps6[:, 0:1]),
             reads=[nsc_tb, par_tb], writes=[nsc_tb])
        nv(lambda e: e.reciprocal(out=N["rs"][:, 0:1], in_=N["rs"][:, 0:1]))
        nv(lambda e: e.scalar_tensor_tensor(out=N["t"][:, 0:128], in0=tk[:, 0:128], scalar=N["rs"][:, 0:1], in1=bcp[:, 0:128], op0=ALU.mult, op1=ALU.mult))
        oa, otb = obf_s.next()
        P.op("dve", lambda e, oa=oa: e.tensor_copy(out=oa[:, 0:128], in_=N["t"][:, 0:128]), reads=[nsc_tb], writes=[otb])
        out_dma(dr["o_ckv"][t0:t0 + 128, :], oa[:, 0:128], otb)
        pa, ptb = q128.next()
        pab = pa.bitcast(BF16)
        P.op("pe", lambda e, pab=pab, oa=oa: e.transpose(out=pab[:, 0:128], in_=oa[:, 0:128], identity=identb[:]), reads=[otb, cst_tb], writes=[ptb])
        P.op("act", lambda e, pab=pab, oa=oa: e.activation(out=oa[:, 128:256], in_=pab[:, 0:128], func=AF.Identity), reads=[ptb], writes=[otb])
        out_dma(dr["o_ckvT"][:, t0:t0 + 128], oa[:, 128:256], otb)
        nv(lambda e: e.bn_stats(out=N["st"][:], in_=tk[:, 384:448]))
        nv(lambda e: e.bn_aggr(out=N["mv"][:], in_=N["st"][:]))
        P.op("act", lambda e: e.activation(out=N["rs"][:, 1:2], in_=N["mv"][:, 1:2], func=AF.Sqrt, scale=1.0, bias=eps6[:, 1:2]),
             reads=[nsc_tb, par_tb], writes=[nsc_tb])
        nv(lambda e: e.reciprocal(out=N["rs"][:, 1:2], in_=N["rs"][:, 1:2]))
        nv(lambda e: e.tensor_scalar(out=N["t"][:, 128:192], in0=tk[:, 384:448], scalar1=N["mv"][:, 0:1], scalar2=N["rs"][:, 1:2], op0=ALU.subtract, op1=ALU.mult))
        nv(lambda e: e.tensor_tensor(out=N["t"][:, 128:192], in0=N["t"][:, 128:192], in1=bcp[:, 128:192], op=ALU.mult))
        oa, otb = obf_s.next()
        P.op("dve", lambda e, oa=oa: e.tensor_tensor(out=oa[:, 0:64], in0=N["t"][:, 128:192], in1=bcp[:, 192:256], op=ALU.add), reads=[nsc_tb, par_tb], writes=[otb])
        pa, ptb = q128.next()
        pab = pa.bitcast(BF16)
        P.op("pe", lambda e, pab=pab, oa=oa: e.transpose(out=pab[0:64, 0:128], in_=oa[:, 0:64], identity=identb[:]), reads=[otb, cst_tb], writes=[ptb])
        P.op("act", lambda e, pab=pab, oa=oa: e.activation(out=oa[0:64, 128:256], in_=pab[0:64, 0:128], func=AF.Identity), reads=[ptb], writes=[otb])
        out_dma(dr["o_ikT"][:, t0:t0 + 128], oa[0:64, 128:256], otb)
        P.op("act", lambda e: e.activation(out=N["sg"][:], in_=tk[:, 448:452], func=AF.Sign), reads=[tk_tb], writes=[nsc_tb])
        nv(lambda e: e.tensor_tensor(out=N["aiw"][:], in0=tk[:, 448:452], in1=N["sg"][:], op=ALU.mult))
        oa, otb = ost_s.next()
        P.op("dve", lambda e, oa=oa: e.tensor_copy(out=oa[:, 0:4], in_=N["sg"][:]), reads=[nsc_tb], writes=[otb])
        out_dma(dr["o_sgn"][t0:t0 + 128, :], oa[:, 0:4], otb)
        ob, obtb = obf_s.next()
        for h in range(4):
            P.op("dve", lambda e, h=h, ob=ob: e.tensor_scalar(out=ob[:, h * 64:(h + 1) * 64], in0=tk[:, 128 + h * 64:128 + (h + 1) * 64],
                                                              scalar1=N["aiw"][:, h:h + 1], scalar2=1.0 / 16, op0=ALU.mult, op1=ALU.mult),
                 reads=[tk_tb, nsc_tb], writes=[obtb])
        oc, octb = obf_s.next()
        for h in range(4):
            pa, ptb = q128.next()
            pab = pa.bitcast(BF16)
            P.op("pe", lambda e, pab=pab, ob=ob, h=h: e.transpose(out=pab[0:64, 0:128], in_=ob[:, h * 64:(h + 1) * 64], identity=identb[:]),
                 reads=[obtb, cst_tb], writes=[ptb])
            P.op("act", lambda e, pab=pab, oc=oc, h=h: e.activation(out=oc[0:64, h * 128:(h + 1) * 128], in_=pab[0:64, 0:128], func=AF.Identity),
                 reads=[ptb], writes=[octb])
        out_dma(dr["o_iqs"][:, :, t0:t0 + 128], oc[0:64, :].rearrange("p (h t) -> p h t", h=4), octb)
        od, odtb = obf_s.next()
        P.op("dve", lambda e, od=od: e.tensor_copy(out=od[:, 0:128], in_=tk[:, 452:580]), reads=[tk_tb], writes=[odtb])
        out_dma(dr["o_sv"][t0:t0 + 128, :], od[:, 0:128], odtb)


GN_EPS = 64e-5
NITER = 16
TOPK = 256


def emit_k2(P, nc, NBQ, dr, parts=("rwkv", "swa", "dsa")):
    NT = NBQ * 128
    NKT = 8 * NBQ
    seg_of = lambda j: (8 * j) // NBQ
    out_tb = dr["out_tb"]
    pb = [P.psum(f"k2pb{i}", [128, 512], F32) for i in range(8)]
    bank = [P.tb(f"k2bank{i}") for i in range(8)]
    cst = P.sbuf("cst2", [128, 128], F32)
    cst_tb = P.tb("cst2")
    P.op("sp", lambda e: e.dma_start(out=cst[:], in_=dr["ident"]), writes=[cst_tb], dma=True)
    identb = P.sbuf("identb2", [128, 128], BF16)
    onesb = P.sbuf("onesb", [128, 64], BF16)
    o64 = P.sbuf("o64", [64, 64], F32)
    P.op("dve", lambda e: e.tensor_copy(out=identb[:], in_=cst[:]), reads=[cst_tb], writes=[cst_tb])
    P.op("pool", lambda e: e.memset(onesb[:], 1.0), writes=[cst_tb])
    P.op("pool", lambda e: e.memset(o64[:], 1.0 / 64), writes=[cst_tb])
    obuf = [P.sbuf(f"k2o{i}", [64, 512], BF16) for i in range(4)]
    obs = Slots(P, [o[:] for o in obuf])

    def out_mix(head, j, src, stb):
        P.op("sp", lambda e: e.dma_start(out=dr["mixT"][head, :, j * 128:(j + 1) * 128], in_=src), reads=[stb], writes=[out_tb], dma=True)

    if "rwkv" in parts:
      with P.scope():
        rp = P.sbuf("rp", [64, 16], F32)
        rp_tb = P.tb("rp")
        P.op("sp", lambda e: e.dma_start(out=rp[:], in_=dr["rwkv_ln"]), writes=[rp_tb], dma=True)
        epsg = P.sbuf("epsg", [64, 1], F32)
        P.op("pool", lambda e: e.memset(epsg[:], GN_EPS), writes=[rp_tb])
        segP = P.sbuf("segP", [64, 8, 6, 64], F32)
        segQ = P.sbuf("segQ", [64, 8, 6, 64], F32)
        seg_tb = P.tb("seg")
        P.op("sp", lambda e: e.dma_start(out=segP[:], in_=dr["segPT"].rearrange("s h a b -> a s h b")), writes=[seg_tb], dma=True)
        P.op("sp", lambda e: e.dma_start(out=segQ[:], in_=dr["segQ"].rearrange("s h a b -> a s h b")), writes=[seg_tb], dma=True)
        St = P.sbuf("St", [64, 8, 6, 64], F32)
        St_tb = [P.tb(f"St{s}") for s in range(8)]
        P.op("pool", lambda e: e.memset(St[:, 0, :, :], 0.0), writes=[St_tb[0]])
        for s in range(7):
            for hd in range(6):
                P.op("pe", lambda e, s=s, hd=hd: e.matmul(pb[0][0:64, hd * 64:(hd + 1) * 64], lhsT=segP[:, s, hd, :], rhs=St[:, s, hd, :],
                                                          start=True, stop=True), reads=[seg_tb, St_tb[s]], writes=[bank[0]], acc=True)
            P.op("dve", lambda e, s=s: e.tensor_tensor(out=St[:, s + 1, :, :].rearrange("p h v -> p (h v)"), in0=pb[0][0:64, 0:384],
                                                       in1=segQ[:, s, :, :].rearrange("p h v -> p (h v)"), op=ALU.add),
                 reads=[bank[0], seg_tb], writes=[St_tb[s + 1]])
        rin = [{n: P.sbuf(f"r_{n}{i}", [64, 6, w], F32) for n, w in (("yw", 128), ("y0", 128), ("pt", 64), ("q", 64), ("bon", 128), ("g", 128))} for i in range(2)]
        rin_tb = [P.tb(f"rin{i}") for i in range(2)]
        Sb = P.sbuf("Sb", [64, 6, 64], F32)
        Sb_tb = P.tb("Sb")
        yy = P.sbuf("yy", [64, 6, 128], F32)
        cen = P.sbuf("cen", [64, 6, 128], F32)
        sq = P.sbuf("sqr", [64, 6, 128], F32)
        rsd = P.sbuf("rsd", [64, 6, 128], F32)
        ww_tb = P.tb("rwkvwork")
        for j in range(NBQ):
            I = rin[j % 2]
            itb = rin_tb[j % 2]
            for n, src in (("yw", dr["YwT"][j]), ("y0", dr["Y0T"][j]), ("pt", dr["PbT"][j]), ("q", dr["Qb"][j])):
                P.op("sp", lambda e, n=n, src=src, I=I: e.dma_start(out=I[n][:], in_=src.rearrange("h a b -> a h b")), writes=[itb], dma=True)
            P.op("sp", lambda e, I=I, j=j: e.dma_start(out=I["bon"][:], in_=dr["bon"][:, :, j * 128:(j + 1) * 128]), writes=[itb], dma=True)
            P.op("sp", lambda e, I=I, j=j: e.dma_start(out=I["g"][:], in_=dr["g"][:, :, j * 128:(j + 1) * 128]), writes=[itb], dma=True)
            s = seg_of(j)
            for hd in range(6):
                P.op("pe", lambda e, hd=hd, I=I, s=s: e.matmul(pb[0][0:64, hd * 64:(hd + 1) * 64], lhsT=I["pt"][:, hd, :], rhs=St[:, s, hd, :], start=True, stop=True),
                     reads=[itb, St_tb[s]], writes=[bank[0]], acc=True)
            P.op("dve", lambda e, I=I: e.tensor_tensor(out=Sb[:].rearrange("p h v -> p (h v)"), in0=pb[0][0:64, 0:384],
                                                       in1=I["q"][:].rearrange("p h v -> p (h v)"), op=ALU.add), reads=[bank[0], itb], writes=[Sb_tb])
            for half in range(2):
                bk = 1 + half
                for q in range(3):
                    hd = half * 3 + q
                    P.op("pe", lambda e, hd=hd, q=q, bk=bk, I=I: e.matmul(pb[bk][0:64, q * 128:(q + 1) * 128], lhsT=Sb[:, hd, :], rhs=I["yw"][:, hd, :], start=True, stop=True),
                         reads=[Sb_tb, itb], writes=[bank[bk]], acc=True)
                hs = slice(half * 3, half * 3 + 3)
                f3 = lambda t, hs=hs: t[:, hs, :].rearrange("p h t -> p (h t)")
                P.op("dve", lambda e, bk=bk, I=I, f3=f3: e.tensor_tensor(out=f3(yy), in0=pb[bk][0:64, 0:384], in1=f3(I["y0"]), op=ALU.add),
                     reads=[bank[bk], itb], writes=[ww_tb])
                P.op("pe", lambda e, bk=bk, f3=f3: e.matmul(pb[bk][0:64, 0:384], lhsT=o64[:], rhs=f3(yy), start=True, stop=True), reads=[ww_tb, cst_tb], writes=[bank[bk]])
                P.op("dve", lambda e, bk=bk, f3=f3: e.tensor_tensor(out=f3(cen), in0=f3(yy), in1=pb[bk][0:64, 0:384], op=ALU.subtract), reads=[bank[bk], ww_tb], writes=[ww_tb])
                P.op("pool", lambda e, f3=f3: e.tensor_tensor(out=f3(sq), in0=f3(cen), in1=f3(cen), op=ALU.mult), reads=[ww_tb], writes=[ww_tb])
                P.op("pe", lambda e, bk=bk, f3=f3: e.matmul(pb[bk][0:64, 0:384], lhsT=o64[:], rhs=f3(sq), start=True, stop=True), reads=[ww_tb, cst_tb], writes=[bank[bk]])
                P.op("act", lambda e, bk=bk, f3=f3: e.activation(out=f3(rsd), in_=pb[bk][0:64, 0:384], func=AF.Sqrt, bias=epsg[:, 0:1], scale=1.0),
                     reads=[bank[bk], rp_tb], writes=[ww_tb])
                P.op("dve", lambda e, f3=f3: e.reciprocal(out=f3(rsd), in_=f3(rsd)), reads=[ww_tb], writes=[ww_tb])
                P.op("dve", lambda e, f3=f3: e.tensor_tensor(out=f3(cen), in0=f3(cen), in1=f3(rsd), op=ALU.mult), reads=[ww_tb], writes=[ww_tb])
                ob, obtb = obs.next()
                for q in range(3):
                    hd = half * 3 + q
                    P.op("dve", lambda e, hd=hd: e.tensor_scalar(out=cen[:, hd, :], in0=cen[:, hd, :], scalar1=rp[:, hd:hd + 1], scalar2=rp[:, 6 + hd:7 + hd],
                                                                 op0=ALU.mult, op1=ALU.add), reads=[ww_tb, rp_tb], writes=[ww_tb])
                P.op("pool", lambda e, f3=f3, I=I: e.tensor_tensor(out=f3(cen), in0=f3(cen), in1=f3(I["bon"]), op=ALU.add), reads=[ww_tb, itb], writes=[ww_tb])
                P.op("pool", lambda e, f3=f3, I=I, ob=ob: e.tensor_tensor(out=ob[:, 0:384], in0=f3(cen), in1=f3(I["g"]), op=ALU.mult), reads=[ww_tb, itb], writes=[obtb])
                P.op("sp", lambda e, ob=ob, j=j, half=half: e.dma_start(out=dr["mixT"][half * 3:half * 3 + 3, :, j * 128:(j + 1) * 128].rearrange("h p t -> p h t"),
                                                                          in_=ob[:, 0:384].rearrange("p (h t) -> p h t", h=3)), reads=[obtb], writes=[out_tb], dma=True)

    if "swa" in parts:
      with P.scope():
        swb = P.sbuf("swb", [128, 2, 6, 128], F32)
        swbf = P.sbuf("swbf", [128, 6, 128], F32)
        sw_tb = P.tb("swc")
        P.op("sp", lambda e: e.dma_start(out=swb[:], in_=dr["swb"]), writes=[sw_tb], dma=True)
        P.op("sp", lambda e: e.dma_start(out=swbf[:], in_=dr["swb_first"]), writes=[sw_tb], dma=True)
        es = P.sbuf("esink", [64, 6], F32)
        P.op("sp", lambda e: e.dma_start(out=es[:], in_=dr["sink_b"]), writes=[sw_tb], dma=True)
        P.op("act", lambda e: e.activation(out=es[:], in_=es[:], func=AF.Exp), reads=[sw_tb], writes=[sw_tb])
        sin = [{"q": P.sbuf(f"s_q{i}", [64, 6, 128], BF16), "k": P.sbuf(f"s_k{i}", [64, 2, 256], BF16), "v": P.sbuf(f"s_v{i}", [128, 2, 128], BF16)} for i in range(2)]
        sin_tb = [P.tb(f"sin{i}") for i in range(2)]
        stmp = P.sbuf("stmp", [128, 384], F32)
        sE = [P.sbuf(f"sE{i}", [128, 384], BF16) for i in range(2)]
        sE_tb = [P.tb(), P.tb()]
        st_tb = P.tb("stmp")
        srec = P.sbuf("srec", [64, 384], F32)
        for j in range(NBQ):
            I = sin[j % 2]
            itb = sin_tb[j % 2]
            P.op("sp", lambda e, I=I, j=j: e.dma_start(out=I["q"][:], in_=dr["sq"][:, :, j * 128:(j + 1) * 128]), writes=[itb], dma=True)
            P.op("sp", lambda e, I=I, j=j: e.dma_start(out=I["k"][:], in_=dr["sk2"][:, :, j, :]), writes=[itb], dma=True)
            P.op("sp", lambda e, I=I, j=j: e.dma_start(out=I["v"][:], in_=dr["sv2"][j].rearrange("k p c -> p k c")), writes=[itb], dma=True)
            for g in range(2):
                bn, bd = 5, 6
                for kt2 in range(2):
                    bs = 3 + kt2
                    P.op("pe", lambda e, g=g, kt2=kt2, bs=bs, I=I: e.matmul(pb[bs][:, 0:384], lhsT=I["k"][:, g, kt2 * 128:(kt2 + 1) * 128],
                                                                            rhs=I["q"][:, 3 * g:3 * g + 3, :], start=True, stop=True),
                         reads=[itb], writes=[bank[bs]])
                    btab = swbf[:, 3 * g:3 * g + 3, :] if (j == 0 and kt2 == 0) else swb[:, kt2, 3 * g:3 * g + 3, :]
                    P.op("dve", lambda e, bs=bs, btab=btab: e.scalar_tensor_tensor(out=stmp[:].rearrange("p (h t) -> p h t", h=3), in0=pb[bs][:, 0:384].rearrange("p (h t) -> p h t", h=3),
                                                                                  scalar=0.125, in1=btab, op0=ALU.mult, op1=ALU.add),
                         reads=[bank[bs], sw_tb], writes=[st_tb])
                    E, etb = sE[kt2], sE_tb[kt2]
                    P.op("act", lambda e, E=E: e.activation(out=E[:], in_=stmp[:], func=AF.Exp), reads=[st_tb], writes=[etb])
                    P.op("pe", lambda e, E=E, g=g, kt2=kt2, I=I: e.matmul(pb[bn][0:64, 0:384], lhsT=I["v"][:, kt2, g * 64:(g + 1) * 64], rhs=E[:],
                                                                          start=(kt2 == 0), stop=(kt2 == 1)), reads=[etb, itb], writes=[bank[bn]], acc=True)
                    P.op("pe", lambda e, E=E, kt2=kt2: e.matmul(pb[bd][0:64, 0:384], lhsT=onesb[:], rhs=E[:], start=(kt2 == 0), stop=(kt2 == 1)),
                         reads=[etb, cst_tb], writes=[bank[bd]], acc=True)
                for q in range(3):
                    P.op("dve", lambda e, q=q, g=g: e.tensor_scalar(out=srec[:, q * 128:(q + 1) * 128], in0=pb[bd][0:64, q * 128:(q + 1) * 128],
                                                                    scalar1=es[:, 3 * g + q:3 * g + q + 1], scalar2=None, op0=ALU.add),
                         reads=[bank[bd], sw_tb], writes=[st_tb])
                P.op("dve", lambda e: e.reciprocal(out=srec[:], in_=srec[:]), reads=[st_tb], writes=[st_tb])
                ob, obtb = obs.next()
                P.op("dve", lambda e, ob=ob: e.tensor_tensor(out=ob[:, 0:384], in0=pb[bn][0:64, 0:384], in1=srec[:], op=ALU.mult), reads=[bank[bn], st_tb], writes=[obtb])
                P.op("sp", lambda e, ob=ob, j=j, g=g: e.dma_start(out=dr["mixT"][10 + 3 * g:13 + 3 * g, :, j * 128:(j + 1) * 128].rearrange("h p t -> p h t"),
                                                                    in_=ob[:, 0:384].rearrange("p (h t) -> p h t", h=3)), reads=[obtb], writes=[out_tb], dma=True)

    if "dsa" in parts:
      with P.scope():
        ckvT = P.sbuf("ckvT", [128, NKT * 128], BF16)
        ckv = P.sbuf("ckv", [128, NKT, 128], BF16)
        key_tb = P.tb("keys")
        for c in range(0, NKT, 16):
            P.op("sp", lambda e, c=c: e.dma_start(out=ckvT[:, c * 128:(c + 16) * 128], in_=dr["ckvT"][:, c * 128:(c + 16) * 128]), writes=[key_tb], dma=True)
            P.op("sp", lambda e, c=c: e.dma_start(out=ckv[:, c:c + 16, :], in_=dr["ckv"][c * 128:(c + 16) * 128, :].rearrange("(k p) r -> p k r", p=128)),
                 writes=[key_tb], dma=True)
        ikc = [P.sbuf(f"ikc{i}", [64, 512], BF16) for i in range(3)]
        ikc_tb = [P.tb() for _ in range(3)]
        wuv_f = P.sbuf("wuv_f", [128, 4, 64], F32)
        wuv = P.sbuf("wuv", [128, 4, 64], BF16)
        dpar_tb = P.tb("dpar")
        P.op("sp", lambda e: e.dma_start(out=wuv_f[:], in_=dr["wuv"].rearrange("h r d -> r h d")), writes=[dpar_tb], dma=True)
        P.op("dve", lambda e: e.tensor_copy(out=wuv[:], in_=wuv_f[:]), reads=[dpar_tb], writes=[dpar_tb])
        dmask = P.sbuf("dmask", [128, 1024], F32)
        ab = P.sbuf("ab", [128, 4, 128], F32)
        P.op("sp", lambda e: e.dma_start(out=dmask[:], in_=dr["dmask"]), writes=[dpar_tb], dma=True)
        P.op("sp", lambda e: e.dma_start(out=ab[:], in_=dr["ab"]), writes=[dpar_tb], dma=True)
        Isc = P.sbuf("Isc", [128, NKT * 128], F32)
        Isc_tb = P.tb("Isc")
        msk = P.sbuf("msk", [128, NKT * 128], BF16)
        msk_tb = P.tb("msk")
        qin = [{"ql": P.sbuf(f"d_ql{i}", [128, 4, 128], BF16), "iq": P.sbuf(f"d_iq{i}", [64, 4, 128], BF16), "sg": P.sbuf(f"d_sg{i}", [128, 4], F32)} for i in range(2)]
        qin_tb = [P.tb(), P.tb()]
        rl = [P.sbuf(f"rl{i}", [128, 512], F32) for i in range(2)]
        rl_tb = [P.tb(), P.tb()]
        bs_ = {n: P.sbuf("bs_" + n, [128, 1], F32) for n in ("lo", "hi", "mid", "cnt", "ge", "d")}
        bs_tb = P.tb("bisect")
        Eb = [P.sbuf(f"Eb{i}", [128, 512], BF16) for i in range(2)]
        Eb_tb = [[P.tb() for _ in range(4)] for _ in range(2)]
        PTb = [P.sbuf(f"PTb{i}", [128, 512], BF16) for i in range(2)]
        PT_tb = [P.tb(), P.tb()]
        olat = P.sbuf("olat", [128, 512], BF16)
        drec = P.sbuf("drec", [64, 512], F32)
        fin_tb = P.tb("dsafin")
        junk = P.sbuf("junk8", [128, NKT * 128], mybir.dt.uint8)
        junk_tb = P.tb("junk8")
        B = bs_

        def emitA(j):
            nkt = 8 * (j + 1)
            n = nkt * 128
            Q = qin[j % 2]
            qtb = qin_tb[j % 2]
            P.op("sp", lambda e: e.dma_start(out=Q["ql"][:], in_=dr["qlat"][:, :, j * 128:(j + 1) * 128]), writes=[qtb], dma=True)
            P.op("sp", lambda e: e.dma_start(out=Q["iq"][:], in_=dr["iqs"][:, :, j * 128:(j + 1) * 128]), writes=[qtb], dma=True)
            P.op("sp", lambda e: e.dma_start(out=Q["sg"][:], in_=dr["sgn"][j * 128:(j + 1) * 128, :]), writes=[qtb], dma=True)
            for c4 in range(nkt // 4):
                ii = c4 % 3
                P.op("sp", lambda e, c4=c4, ii=ii: e.dma_start(out=ikc[ii][:], in_=dr["ikT"][:, c4 * 512:(c4 + 1) * 512]), writes=[ikc_tb[ii]], dma=True)
                for h in range(4):
                    bk = (c4 * 4 + h) % 2
                    P.op("pe", lambda e, h=h, bk=bk, ii=ii: e.matmul(pb[bk][:], lhsT=Q["iq"][:, h, :], rhs=ikc[ii][:], start=True, stop=True),
                         reads=[qtb, ikc_tb[ii]], writes=[bank[bk]])
                    P.op("act", lambda e, bk=bk: e.activation(out=rl[bk][:], in_=pb[bk][:], func=AF.Relu), reads=[bank[bk]], writes=[rl_tb[bk]])
                    dst = Isc[:, c4 * 512:(c4 + 1) * 512]
                    if h == 0:
                        P.op("dve", lambda e, bk=bk, dst=dst: e.tensor_scalar(out=dst, in0=rl[bk][:], scalar1=Q["sg"][:, 0:1], scalar2=None, op0=ALU.mult),
                             reads=[rl_tb[bk], qtb], writes=[Isc_tb])
                    else:
                        P.op("dve", lambda e, bk=bk, dst=dst, h=h: e.scalar_tensor_tensor(out=dst, in0=rl[bk][:], scalar=Q["sg"][:, h:h + 1], in1=dst,
                                                                                         op0=ALU.mult, op1=ALU.add), reads=[rl_tb[bk], qtb, Isc_tb], writes=[Isc_tb])
            P.op("dve", lambda e: e.tensor_tensor(out=Isc[:, n - 1024:n], in0=Isc[:, n - 1024:n], in1=dmask[:], op=ALU.add), reads=[Isc_tb, dpar_tb], writes=[Isc_tb])
            bv = lambda fn: P.op("dve", fn, reads=[bs_tb, Isc_tb], writes=[bs_tb])
            bv(lambda e: e.memset(B["lo"][:], -64.0))
            bv(lambda e: e.tensor_reduce(out=B["hi"][:], in_=Isc[:, 0:n], axis=AX.X, op=ALU.max))
            bv(lambda e: e.tensor_scalar(out=B["d"][:], in0=B["hi"][:], scalar1=64.0, scalar2=None, op0=ALU.add))

        def bisect_iter(j, it):
            n = 8 * (j + 1) * 128
            ck_ = 0.5 ** (it + 1)
            bv = lambda fn: P.op("dve", fn, reads=[bs_tb, Isc_tb], writes=[bs_tb])
            bv(lambda e: e.scalar_tensor_tensor(out=B["mid"][:], in0=B["d"][:], scalar=ck_, in1=B["lo"][:], op0=ALU.mult, op1=ALU.add))
            P.op("dve", lambda e: e.tensor_scalar(out=junk[:, 0:n], in0=Isc[:, 0:n], scalar1=B["mid"][:, 0:1], scalar2=0.0, op0=ALU.is_ge, op1=ALU.add,
                                                  accum_out=B["cnt"][:]), reads=[bs_tb, Isc_tb], writes=[bs_tb, junk_tb])
            bv(lambda e: e.tensor_scalar(out=B["ge"][:], in0=B["cnt"][:], scalar1=float(TOPK) - 0.5, scalar2=ck_, op0=ALU.is_ge, op1=ALU.mult))
            bv(lambda e: e.scalar_tensor_tensor(out=B["lo"][:], in0=B["ge"][:], scalar=B["d"][:, 0:1], in1=B["lo"][:], op0=ALU.mult, op1=ALU.add))

        def final_mask(j):
            n = 8 * (j + 1) * 128
            P.op("dve", lambda e: e.tensor_scalar(out=msk[:, 0:n], in0=Isc[:, 0:n], scalar1=B["lo"][:, 0:1], scalar2=None, op0=ALU.is_ge),
                 reads=[bs_tb, Isc_tb], writes=[msk_tb])

        def tileB(j, kt):
            nkt = 8 * (j + 1)
            Q = qin[j % 2]
            qtb = qin_tb[j % 2]
            i2 = kt % 2
            dl = nkt - 1 - kt
            pmT = pb[2].bitcast(BF16)
            P.op("pe", lambda e: e.transpose(out=pmT[:, 0:128], in_=msk[:, kt * 128:(kt + 1) * 128], identity=identb[:]),
                 reads=[msk_tb, cst_tb], writes=[bank[2]])
            bS = 3 + i2
            P.op("pe", lambda e: e.matmul(pb[bS][:], lhsT=ckvT[:, kt * 128:(kt + 1) * 128], rhs=Q["ql"][:], start=True, stop=True),
                 reads=[key_tb, qtb], writes=[bank[bS]])
            for h in range(4):
                P.op("act", lambda e, h=h: e.activation(out=Eb[i2][:, h * 128:(h + 1) * 128], in_=pb[bS][:, h * 128:(h + 1) * 128], func=AF.Exp,
                                                        bias=ab[:, h, dl:dl + 1], scale=1.0), reads=[bank[bS], dpar_tb], writes=[Eb_tb[i2][h]])
            P.op("dve", lambda e: e.tensor_tensor(out=PTb[i2][:].rearrange("p (h t) -> p h t", h=4), in0=Eb[i2][:].rearrange("p (h t) -> p h t", h=4),
                                                  in1=pmT[:, 0:128].unsqueeze(1).to_broadcast([128, 4, 128]), op=ALU.mult),
                 reads=Eb_tb[i2] + [bank[2]], writes=[PT_tb[i2]])
            P.op("pe", lambda e: e.matmul(pb[5][:], lhsT=ckv[:, kt, :], rhs=PTb[i2][:], start=(kt == 0), stop=(kt == nkt - 1)),
                 reads=[key_tb, PT_tb[i2]], writes=[bank[5]], acc=True)
            P.op("pe", lambda e: e.matmul(pb[6][0:64, :], lhsT=onesb[:], rhs=PTb[i2][:], start=(kt == 0), stop=(kt == nkt - 1)),
                 reads=[cst_tb, PT_tb[i2]], writes=[bank[6]], acc=True)

        def finB(j):
            P.op("act", lambda e: e.activation(out=olat[:], in_=pb[5][:], func=AF.Identity), reads=[bank[5]], writes=[fin_tb])
            P.op("dve", lambda e: e.reciprocal(out=drec[:], in_=pb[6][0:64, :]), reads=[bank[6]], writes=[fin_tb])
            for h in range(4):
                P.op("pe", lambda e, h=h: e.matmul(pb[7][0:64, h * 128:(h + 1) * 128], lhsT=wuv[:, h, :], rhs=olat[:, h * 128:(h + 1) * 128], start=True, stop=True),
                     reads=[fin_tb, dpar_tb], writes=[bank[7]], acc=True)
            ob, obtb = obs.next()
            P.op("dve", lambda e: e.tensor_tensor(out=ob[:, 0:512], in0=pb[7][0:64, :], in1=drec[:], op=ALU.mult), reads=[bank[7], fin_tb], writes=[obtb])
            P.op("sp", lambda e: e.dma_start(out=dr["mixT"][6:10, :, j * 128:(j + 1) * 128].rearrange("h p t -> p h t"),
                                             in_=ob[:, 0:512].rearrange("p (h t) -> p h t", h=4)), reads=[obtb], writes=[out_tb], dma=True)

        emitA(0)
        for it in range(NITER):
            bisect_iter(0, it)
        final_mask(0)
        for j in range(NBQ):
            tiles = list(range(8 * (j + 1)))
            if j + 1 < NBQ:
                emitA(j + 1)
                per = -(-len(tiles) // NITER)
                for it in range(NITER):
                    bisect_iter(j + 1, it)
                    for kt in tiles[it * per:(it + 1) * per]:
                        tileB(j, kt)
                for kt in tiles[NITER * per:]:
                    tileB(j, kt)
            else:
                for kt in tiles:
                    tileB(j, kt)
            finB(j)
            if j + 1 < NBQ:
                final_mask(j + 1)


def emit_ffn(P, nc, NT, exp_ids, dr, ident, ident_tb):
    PT = min(1024, NT)
    NB = PT // 128
    npass = NT // PT
    NX = len(exp_ids)
    TW = min(512, PT)
    NTT = PT // TW
    BPT = TW // 128
    es = P.es
    wo_tb = P.tb("wo")
    stg = [P.sbuf(f"stg{i}", [128, 2048], F32) for i in range(3)]
    stg_tb = [P.tb(f"stg{i}") for i in range(3)]
    rw = P.sbuf("rw", [128, 8, NE], F32)
    rw_tb = P.tb("rw")
    rbias = P.sbuf("rbias", [128, NE], F32)
    bc = [P.sbuf(f"bc{i}", [128, D], F32) for i in range(3)]
    bc_tb = [P.tb(f"bc{i}") for i in range(3)]
    modT = P.sbuf("modT", [128, 48], F32)
    modT_tb = P.tb("modT")
    eps_t = P.sbuf("eps_t", [128, 1], F32)
    xm = P.sbuf("xm", [128, NB, D], F32)
    xm_tb = [P.tb(f"xm{b}") for b in range(NB)]
    yacc = P.sbuf("yacc", [128, NB, D], F32)
    ya_tb = [P.tb(f"ya{b}") for b in range(NB)]
    h2T = P.sbuf("h2T", [128, 8, PT], BF16)
    h2_tb = [P.tb(f"h2{b}") for b in range(NB)]
    h2f_tb = P.tb("h2f")
    gateT = P.sbuf("gateT", [65, PT], F32)
    gT_tb = [P.tb(f"gT{b}") for b in range(NB)]
    xr = [P.sbuf(f"xr{i}", [128, D], F32) for i in range(2)]
    xr_tb = [P.tb(f"xr{i}") for i in range(2)]
    zt = P.sbuf("zt", [128, D], F32)
    zt_tb = P.tb("zt")
    lnsc = {"st": P.sbuf("ln_st", [128, 2, 6], F32), "mv": P.sbuf("ln_mv", [128, 2], F32),
            "rs": P.sbuf("ln_rs", [128, 1], F32), "xn": P.sbuf("ln_xn", [128, D], F32),
            "tb": P.tb("ln_s"), "xn_tb": P.tb("ln_xn")}
    rt_tb = P.tb("rt")
    wb_tb = [{k: P.tb(f"{k}b{i}") for k in ("w1", "w3", "w2")} for i in range(2)]
    selt_tb = P.tb("selt")
    Gb_tb = P.tb("Gb")
    ssb_tb = [P.tb(f"ssb{i}") for i in range(2)]
    gg_tb = [[P.tb(f"gg{fc}_{tt}") for tt in range(NTT)] for fc in range(2)]
    pb = [P.psum(f"pb{i}", [128, 512], F32) for i in range(8)]
    pb_tb = [P.tb(f"pb{i}") for i in range(8)]

    P.op("pool", lambda e: e.memset(eps_t[:], LN_EPS), writes=[modT_tb])
    P.op("sp", lambda e: e.dma_start(out=modT[:], in_=dr["modT"]), writes=[modT_tb], dma=True)
    P.op("dve", lambda e: e.tensor_scalar(out=modT[:, 32:40], in0=modT[:, 32:40], scalar1=1.0, scalar2=None,
                                          op0=ALU.add), reads=[modT_tb], writes=[modT_tb])
    P.op("sp", lambda e: e.dma_start(out=rw[:], in_=dr["router_w"].rearrange("(kc p) n -> p kc n", p=128)),
         writes=[rw_tb], dma=True)
    P.op("sp", lambda e: e.dma_start(out=rbias[:], in_=dr["rbias_b"]), writes=[rw_tb], dma=True)
    def load_bc(i, src):
        P.op("sp", lambda e: e.dma_start(out=bc[i][:], in_=src), writes=[bc_tb[i]], dma=True)

    def _pass(ps):
        t0 = ps * PT
        load_bc(0, dr["modb"][:, 2 * D:3 * D])
        load_bc(1, dr["lnp"][:, 0, :])
        load_bc(2, dr["lnp"][:, 1, :])
        P.op("pool", lambda e: e.memset(gateT[64:65, :], 1.0), writes=gT_tb)
        _sc1 = P.scope()
        _sc1.__enter__()
        wo = P.sbuf(f"p{ps}_wo", [64, 16, D], BF16)
        mixb = [P.sbuf(f"p{ps}_mixb{i}", [64, 16, 128], BF16) for i in range(2)]
        mixb_tb = [P.tb(f"mixb{i}") for i in range(2)]
        h2f = P.sbuf(f"p{ps}_h2f", [128, 8, 128], F32)
        rt = {k: P.sbuf(f"p{ps}_rt_" + k, [128, n], F32) for k, n in
              [("sc", 64), ("sel", 64), ("eq", 64), ("sel2", 64), ("m1", 8), ("m2", 8), ("grp", 8), ("t8", 8),
               ("gm", 8), ("g4", 8), ("selm", 64), ("em", 64), ("gt", 64), ("den", 1), ("gate", 64)]}
        for c in range(16):
            s_ = c % 3
            P.op("sp", lambda e, c=c, s_=s_: e.dma_start(out=stg[s_][0:64, 0:D], in_=dr["w_out"][c * 64:(c + 1) * 64, :]),
                 writes=[stg_tb[s_]], dma=True)
            P.op("pool", lambda e, c=c, s_=s_: e.tensor_copy(out=wo[:, c, :], in_=stg[s_][0:64, 0:D]),
                 reads=[stg_tb[s_]], writes=[wo_tb])


        for b in range(NB):
            tk = t0 + b * 128
            xi = b % 2
            P.op("sp", lambda e, xi=xi, tk=tk: e.dma_start(out=xr[xi][:], in_=dr["xres"][tk:tk + 128, :]),
                 writes=[xr_tb[xi]], dma=True)
            P.op("sp", lambda e, xi=xi, tk=tk: e.dma_start(out=mixb[xi][:], in_=dr["mixT"][:, :, tk:tk + 128].rearrange("h p t -> p h t")),
                 writes=[mixb_tb[xi]], dma=True)
            for hlf in range(2):
                for c in range(16):
                    P.op("pe", lambda e, c=c, hlf=hlf, xi=xi: e.matmul(
                        pb[hlf][:], lhsT=mixb[xi][:, c, :], rhs=wo[:, c, hlf * 512:(hlf + 1) * 512],
                        start=(c == 0), stop=(c == 15)), reads=[mixb_tb[xi], wo_tb], writes=[pb_tb[hlf]], acc=True)
            for hlf in range(2):
                P.op("dve", lambda e, hlf=hlf: e.tensor_tensor(out=zt[:, hlf * 512:(hlf + 1) * 512], in0=pb[hlf][:],
                                                               in1=bc[0][:, hlf * 512:(hlf + 1) * 512], op=ALU.mult),
                     reads=[pb_tb[hlf], bc_tb[0]], writes=[zt_tb])
            P.op("dve", lambda e, xi=xi: e.scalar_tensor_tensor(out=zt[:], in0=xr[xi][:], scalar=ALPHA, in1=zt[:],
                                                                op0=ALU.mult, op1=ALU.add),
                 reads=[xr_tb[xi], zt_tb], writes=[zt_tb])
            emit_layernorm(P, zt[:], zt_tb, xm[:, b, :], xm_tb[b], bc[1][:], bc[2][:], [bc_tb[1], bc_tb[2]], lnsc, eps_t, "m")
            for kc in range(8):
                bank = 2 + kc // 4
                P.op("pe", lambda e, kc=kc, bank=bank, b=b: e.transpose(
                    out=pb[bank][:, (kc % 4) * 128:(kc % 4 + 1) * 128], in_=xm[:, b, kc * 128:(kc + 1) * 128],
                    identity=ident[:]), reads=[xm_tb[b], ident_tb], writes=[pb_tb[bank]], acc=True)
            for kc in range(8):
                bank = 2 + kc // 4
                src = pb[bank][:, (kc % 4) * 128:(kc % 4 + 1) * 128]
                P.op("act", lambda e, kc=kc, src=src, b=b: e.activation(
                    out=h2T[:, kc, b * 128:(b + 1) * 128], in_=src, func=AF.Identity,
                    bias=modT[:, 24 + kc:25 + kc], scale=modT[:, 32 + kc:33 + kc]),
                    reads=[pb_tb[bank], modT_tb], writes=[h2_tb[b]])
                P.op("act", lambda e, kc=kc, src=src: e.activation(
                    out=h2f[:, kc, :], in_=src, func=AF.Identity,
                    bias=modT[:, 24 + kc:25 + kc], scale=modT[:, 32 + kc:33 + kc]),
                    reads=[pb_tb[bank], modT_tb], writes=[h2f_tb])
            for kc in range(8):
                P.op("pe", lambda e, kc=kc: e.matmul(pb[4][:, 0:NE], lhsT=h2f[:, kc, :], rhs=rw[:, kc, :],
                                                     start=(kc == 0), stop=(kc == 7)),
                     reads=[h2f_tb, rw_tb], writes=[pb_tb[4]], acc=True)
            R = rt
            P.op("act", lambda e: e.activation(out=R["sc"][:], in_=pb[4][:, 0:NE], func=AF.Sigmoid),
                 reads=[pb_tb[4]], writes=[rt_tb])
            dv = lambda fn: P.op("dve", fn, reads=[rt_tb, rw_tb], writes=[rt_tb])
            g3 = lambda t: t[:].rearrange("p (g j) -> p g j", j=8)
            dv(lambda e: e.tensor_tensor(out=R["sel"][:], in0=R["sc"][:], in1=rbias[:], op=ALU.add))
            dv(lambda e: e.tensor_reduce(out=R["m1"][:], in_=g3(R["sel"]), axis=AX.X, op=ALU.max))
            dv(lambda e: e.tensor_tensor(out=g3(R["eq"]), in0=g3(R["sel"]), in1=bcast_last(R["m1"][:], 8), op=ALU.is_equal))
            dv(lambda e: e.scalar_tensor_tensor(out=R["sel2"][:], in0=R["eq"][:], scalar=-4.0, in1=R["sel"][:],
                                                op0=ALU.mult, op1=ALU.add))
            dv(lambda e: e.tensor_reduce(out=R["m2"][:], in_=g3(R["sel2"]), axis=AX.X, op=ALU.max))
            dv(lambda e: e.tensor_tensor(out=R["grp"][:], in0=R["m1"][:], in1=R["m2"][:], op=ALU.add))
            dv(lambda e: e.max(out=R["t8"][:], in_=R["grp"][:]))
            dv(lambda e: e.tensor_scalar(out=R["gm"][:], in0=R["grp"][:], scalar1=R["t8"][:, 3:4], scalar2=None, op0=ALU.is_ge))
            dv(lambda e: e.tensor_scalar(out=R["g4"][:], in0=R["gm"][:], scalar1=4.0, scalar2=-4.0, op0=ALU.mult, op1=ALU.add))
            dv(lambda e: e.tensor_tensor(out=g3(R["selm"]), in0=g3(R["sel"]), in1=bcast_last(R["gm"][:], 8), op=ALU.mult))
            dv(lambda e: e.tensor_tensor(out=g3(R["selm"]), in0=g3(R["selm"]), in1=bcast_last(R["g4"][:], 8), op=ALU.add))
            dv(lambda e: e.max(out=R["t8"][:], in_=R["selm"][:]))
            dv(lambda e: e.tensor_scalar(out=R["em"][:], in0=R["selm"][:], scalar1=R["t8"][:, 7:8], scalar2=None, op0=ALU.is_ge))
            dv(lambda e: e.tensor_tensor(out=R["gt"][:], in0=R["sc"][:], in1=R["em"][:], op=ALU.mult))
            dv(lambda e: e.tensor_reduce(out=R["den"][:], in_=R["gt"][:], axis=AX.X, op=ALU.add))
            dv(lambda e: e.reciprocal(out=R["den"][:], in_=R["den"][:]))
            dv(lambda e: e.tensor_scalar(out=R["gate"][:], in0=R["gt"][:], scalar1=R["den"][:, 0:1], scalar2=2.5,
                                         op0=ALU.mult, op1=ALU.mult))
            P.op("pe", lambda e: e.transpose(out=pb[5][0:64, 0:128], in_=R["gate"][:], identity=ident[:]),
                 reads=[rt_tb, ident_tb], writes=[pb_tb[5]])
            P.op("act", lambda e, b=b: e.activation(out=gateT[0:64, b * 128:(b + 1) * 128], in_=pb[5][0:64, 0:128],
                                                    func=AF.Identity), reads=[pb_tb[5]], writes=[gT_tb[b]])
        _sc1.__exit__(None, None, None)
        _sc2 = P.scope()
        _sc2.__enter__()
        wb = [{"w1": P.sbuf(f"p{ps}_w1b{i}", [128, 8, DE], BF16), "w3": P.sbuf(f"p{ps}_w3b{i}", [128, 8, DE], BF16),
               "w2": P.sbuf(f"p{ps}_w2b{i}", [128, 2, D], BF16)} for i in range(2)]
        selt = P.sbuf(f"p{ps}_selt", [65, 128], F32)
        Gb = P.sbuf(f"p{ps}_Gb", [128, PT], BF16)
        ssb = [P.sbuf(f"p{ps}_ssb{i}", [128, TW], F32) for i in range(2)]
        ggT = P.sbuf(f"p{ps}_ggT", [128, 2, PT], BF16)

        for xi_, eid in enumerate(exp_ids):
            par = xi_ % 2
            W = wb[par]
            Wt = wb_tb[par]
            P.op("sp", lambda e, xi_=xi_: e.dma_start(out=stg[0][:].rearrange("p (kc f) -> p kc f", f=DE),
                                                     in_=dr["ew1"][xi_].rearrange("(kc p) f -> p kc f", p=128)),
                 writes=[stg_tb[0]], dma=True)
            P.op("pool", lambda e, W=W: e.tensor_copy(out=W["w1"][:].rearrange("p kc f -> p (kc f)"), in_=stg[0][:]),
                 reads=[stg_tb[0]], writes=[Wt["w1"]])
            P.op("sp", lambda e, xi_=xi_: e.dma_start(out=stg[1][:].rearrange("p (kc f) -> p kc f", f=DE),
                                                     in_=dr["ew3"][xi_].rearrange("(kc p) f -> p kc f", p=128)),
                 writes=[stg_tb[1]], dma=True)
            P.op("pool", lambda e, W=W: e.tensor_copy(out=W["w3"][:].rearrange("p kc f -> p (kc f)"), in_=stg[1][:]),
                 reads=[stg_tb[1]], writes=[Wt["w3"]])
            P.op("sp", lambda e, xi_=xi_: e.dma_start(out=stg[2][:].rearrange("p (fc d) -> p fc d", d=D),
                                                     in_=dr["ew2"][xi_].rearrange("(fc p) d -> p fc d", p=128)),
                 writes=[stg_tb[2]], dma=True)
            P.op("pool", lambda e, W=W: e.tensor_copy(out=W["w2"][:].rearrange("p fc d -> p (fc d)"), in_=stg[2][:]),
                 reads=[stg_tb[2]], writes=[Wt["w2"]])
            P.op("pool", lambda e, eid=eid: e.tensor_copy(out=selt[:], in_=ident[0:65, eid:eid + 1].to_broadcast([65, 128])),
                 reads=[ident_tb], writes=[selt_tb])
            for tt in range(NTT):
                P.op("pe", lambda e, tt=tt: e.matmul(pb[6][:, 0:TW], lhsT=selt[:], rhs=gateT[:, tt * TW:(tt + 1) * TW],
                                                     start=True, stop=True),
                     reads=[selt_tb] + gT_tb, writes=[pb_tb[6]])
                P.op("act", lambda e, tt=tt: e.activation(out=Gb[:, tt * TW:(tt + 1) * TW], in_=pb[6][:, 0:TW], func=AF.Identity),
                     reads=[pb_tb[6]], writes=[Gb_tb])
            for tt in range(NTT):
                for fc in range(2):
                    i2 = (tt * 2 + fc) % 2
                    b1, b3 = i2, 2 + i2
                    h_tbs = h2_tb[tt * BPT:(tt + 1) * BPT]
                    for kc in range(8):
                        P.op("pe", lambda e, kc=kc, fc=fc, tt=tt, b1=b1, W=W: e.matmul(
                            pb[b1][:, 0:TW], lhsT=W["w1"][:, kc, fc * 128:(fc + 1) * 128], rhs=h2T[:, kc, tt * TW:(tt + 1) * TW],
                            start=(kc == 0), stop=(kc == 7)), reads=[Wt["w1"]] + h_tbs, writes=[pb_tb[b1]], acc=True)
                    for kc in range(8):
                        P.op("pe", lambda e, kc=kc, fc=fc, tt=tt, b3=b3, W=W: e.matmul(
                            pb[b3][:, 0:TW], lhsT=W["w3"][:, kc, fc * 128:(fc + 1) * 128], rhs=h2T[:, kc, tt * TW:(tt + 1) * TW],
                            start=(kc == 0), stop=(kc == 7)), reads=[Wt["w3"]] + h_tbs, writes=[pb_tb[b3]], acc=True)
                    P.op("act", lambda e, i2=i2, b1=b1: e.activation(out=ssb[i2][:], in_=pb[b1][:, 0:TW], func=AF.Silu),
                         reads=[pb_tb[b1]], writes=[ssb_tb[i2]])
                    P.op("dve", lambda e, i2=i2, b3=b3: e.tensor_tensor(out=ssb[i2][:], in0=ssb[i2][:], in1=pb[b3][:, 0:TW], op=ALU.mult),
                         reads=[ssb_tb[i2], pb_tb[b3]], writes=[ssb_tb[i2]])
                    P.op("dve", lambda e, i2=i2, fc=fc, tt=tt: e.tensor_tensor(
                        out=ggT[:, fc, tt * TW:(tt + 1) * TW], in0=ssb[i2][:], in1=Gb[:, tt * TW:(tt + 1) * TW], op=ALU.mult),
                        reads=[ssb_tb[i2], Gb_tb], writes=[gg_tb[fc][tt]])
            for b in range(NB):
                tt = b // BPT
                for dh in range(2):
                    bk = 4 + (b * 2 + dh) % 2
                    for fc in range(2):
                        P.op("pe", lambda e, b=b, dh=dh, fc=fc, bk=bk, W=W: e.matmul(
                            pb[bk][:], lhsT=ggT[:, fc, b * 128:(b + 1) * 128], rhs=W["w2"][:, fc, dh * 512:(dh + 1) * 512],
                            start=(fc == 0), stop=(fc == 1)), reads=[gg_tb[fc][tt], Wt["w2"]], writes=[pb_tb[bk]], acc=True)
                    if xi_ == 0:
                        P.op("dve", lambda e, b=b, dh=dh, bk=bk: e.tensor_copy(out=yacc[:, b, dh * 512:(dh + 1) * 512], in_=pb[bk][:]),
                             reads=[pb_tb[bk]], writes=[ya_tb[b]])
                    else:
                        P.op("dve", lambda e, b=b, dh=dh, bk=bk: e.tensor_tensor(
                            out=yacc[:, b, dh * 512:(dh + 1) * 512], in0=yacc[:, b, dh * 512:(dh + 1) * 512], in1=pb[bk][:], op=ALU.add),
                            reads=[pb_tb[bk], ya_tb[b]], writes=[ya_tb[b]])
        _sc2.__exit__(None, None, None)
        load_bc(0, dr["modb"][:, 5 * D:6 * D])
        load_bc(1, dr["lnp"][:, 2, :])
        load_bc(2, dr["lnp"][:, 3, :])
        for b in range(NB):
            tk = t0 + b * 128
            xi = b % 2
            P.op("dve", lambda e, b=b: e.tensor_tensor(out=zt[:], in0=yacc[:, b, :], in1=bc[0][:], op=ALU.mult),
                 reads=[ya_tb[b], bc_tb[0]], writes=[zt_tb])
            P.op("dve", lambda e, b=b: e.scalar_tensor_tensor(out=zt[:], in0=xm[:, b, :], scalar=ALPHA, in1=zt[:],
                                                              op0=ALU.mult, op1=ALU.add),
                 reads=[xm_tb[b], zt_tb], writes=[zt_tb])
            emit_layernorm(P, zt[:], zt_tb, xr[xi][:], xr_tb[xi], bc[1][:], bc[2][:], [bc_tb[1], bc_tb[2]], lnsc, eps_t, "f")
            P.op("sp", lambda e, xi=xi, tk=tk: e.dma_start(out=dr["out"][tk:tk + 128, :], in_=xr[xi][:]),
                 reads=[xr_tb[xi]], writes=[dr["out_tb"]], dma=True)

    for ps in range(npass):
        _pass(ps)


def emit_k0(P, nc, dr):
    cT = P.sbuf("cT", [128, 8], F32)
    c_tb = P.tb("cT")
    P.op("sp", lambda e: e.dma_start(out=cT[:], in_=dr["cT"]), writes=[c_tb], dma=True)
    P.op("act", lambda e: e.activation(out=cT[:], in_=cT[:], func=AF.Silu), reads=[c_tb], writes=[c_tb])
    wm = [P.sbuf(f"wm{i}", [128, 8, 768], F32) for i in range(2)]
    wm_tb = [P.tb(), P.tb()]
    bm = P.sbuf("bm", [1, 4, 768], F32)
    P.op("sp", lambda e: e.dma_start(out=bm[:], in_=dr["b_mod"].unsqueeze(0)), writes=[c_tb], dma=True)
    ps = [P.psum(f"k0ps{i}", [128, 512], F32) for i in range(2)]
    ps_tb = [P.tb(), P.tb()]
    ot = P.sbuf("k0o", [1, 4, 768], F32)
    o_tb = P.tb("k0o")
    for l in range(4):
        W, wtb = wm[l % 2], wm_tb[l % 2]
        P.op("sp", lambda e, l=l, W=W: e.dma_start(out=W[:], in_=dr["w_mod"][l].rearrange("(kc p) n -> p kc n", p=128)), writes=[wtb], dma=True)
        for hf in range(2):
            for kc in range(8):
                P.op("pe", lambda e, kc=kc, hf=hf, W=W: e.matmul(ps[hf][0:1, 0:384], lhsT=cT[:, kc:kc + 1], rhs=W[:, kc, hf * 384:(hf + 1) * 384],
                                                                 start=(kc == 0), stop=(kc == 7)), reads=[c_tb, wtb], writes=[ps_tb[hf]], acc=True)
            P.op("dve", lambda e, l=l, hf=hf: e.tensor_tensor(out=ot[0:1, l, hf * 384:(hf + 1) * 384], in0=ps[hf][0:1, 0:384],
                                                              in1=bm[0:1, l, hf * 384:(hf + 1) * 384], op=ALU.add), reads=[ps_tb[hf], c_tb], writes=[o_tb])
    P.op("sp", lambda e: e.dma_start(out=dr["mod"].unsqueeze(0), in_=ot[:]), reads=[o_tb], writes=[dr["out_tb"]], dma=True)


D = 1024
def consts128():
    i = np.arange(128)
    ident = np.eye(128, dtype=np.float32)
    bo = (i[:, None] // 64 == i[None, :] // 64).astype(np.float32)
    ls = (i[None, :] < i[:, None]).astype(np.float32)
    us = (i[None, :] > i[:, None]).astype(np.float32)
    ui = (i[None, :] >= i[:, None]).astype(np.float32)
    return np.ascontiguousarray(np.stack([ident, bo, ls, us, ui], 1))

def k1_common(inp, l, mod):
    col = lambda v: np.ascontiguousarray(v.reshape(-1, 128).T)
    par = np.zeros((128, 64), np.float32)
    par[:, 0:11] = col(inp['rwkv_mu'][l])
    par[:, 11:14] = col(inp['rwkv_w0'][l]); par[:, 14:17] = col(inp['rwkv_a0'][l])
    par[:, 17:20] = col(inp['rwkv_k_k'][l]); par[:, 20:23] = col(inp['rwkv_k_a'][l])
    par[:, 26:29] = col(inp['rwkv_r_k'][l].reshape(-1))
    par[:, 29:37] = col(mod[0:D]); par[:, 37:45] = col(mod[D:2 * D])
    bcp = np.zeros((128, 320), np.float32)
    bcp[:, 0:128] = inp['dsa_kv_norm'][l][None]; bcp[:, 128:192] = inp['dsa_ik_g'][l][None]; bcp[:, 192:256] = inp['dsa_ik_b'][l][None]
    lora = np.ascontiguousarray(np.concatenate([inp['rwkv_w2'][l], inp['rwkv_a2'][l]], 0))
    wuk = np.ascontiguousarray(inp['dsa_w_uk'][l].reshape(2, 128, 128).transpose(1, 0, 2))
    return {"par": par, "bcp": bcp, "lora": lora, "g2w": np.ascontiguousarray(inp['rwkv_g2'][l]), "wuk": wuk,
            "cst": consts128(), "w_in": np.ascontiguousarray(inp['w_in'][l])}


def alibi_slopes(n):
    return (2.0 ** (-8.0 * (np.arange(n, dtype=np.float32) + 1.0) / n)).astype(np.float32)

K1_CAT = {"o_bon": 1, "o_g": 1, "o_qlat": 2, "o_iqs": 2, "o_sgn": 0, "o_ckv": 0, "o_ckvT": 1, "o_ikT": 1, "o_sq": 1, "o_sk": 1, "o_sv": 0,
          "o_YwT": 0, "o_Y0T": 0}

def k2_tables(i):
    sl = alibi_slopes(10)
    swa_sl, dsa_sl = sl[:6], sl[6:]
    p = np.arange(128)
    r = np.arange(8)[None, :, None]; pq = p[:, None, None]; pk = p[None, None, :]
    valid = (r < i) | ((r == i) & (pk <= pq))
    dmask = np.where(valid, 0.0, -1e30).astype(np.float32).reshape(128, 1024)
    dl = np.arange(128)[None, None, :]
    ab = (dsa_sl[None, :, None] * (128.0 * (7 - dl - i) + p[:, None, None] - 127.0)).astype(np.float32)
    q = p[None, None, None, :]; pk4 = p[:, None, None, None]; tile = np.arange(2)[None, :, None, None]
    dist = q - pk4 + np.where(tile == 0, 128, 0)
    ok = (dist >= 0) & (dist < 128)
    swb = np.where(ok, -swa_sl[None, None, :, None] * dist, -30000.0).astype(np.float32)
    swb_first = swb[:, 0].copy()
    if i == 0:
        swb_first[:] = -30000.0
    return {"dmask": dmask, "ab": np.ascontiguousarray(ab), "swb": np.ascontiguousarray(swb), "swb_first": np.ascontiguousarray(swb_first),
            "ident": np.eye(128, dtype=np.float32)}


def k2_inputs(K1, inp, l, NBQ):
    G = {n: np.concatenate([K1[c][n] for c in range(8)], ax) for n, ax in K1_CAT.items()}
    PT = np.stack([K1[c]["o_PT"] for c in range(8)]); Qa = np.stack([K1[c]["o_Q"] for c in range(8)])
    T = 8 * NBQ * 128
    bf = G["o_sk"].dtype
    maps = []
    for i in range(8):
        gbs = 8 * np.arange(NBQ) + i
        tok = (gbs[:, None] * 128 + np.arange(128)[None, :]).reshape(-1)
        prev = ((gbs - 1)[:, None] * 128 + np.arange(128)[None, :])
        own = (gbs[:, None] * 128 + np.arange(128)[None, :])
        m = {}
        m["YwT"] = np.ascontiguousarray(G["o_YwT"][gbs]); m["Y0T"] = np.ascontiguousarray(G["o_Y0T"][gbs])
        m["PbT"] = np.ascontiguousarray(PT[gbs // NBQ, gbs % NBQ]); m["Qb"] = np.ascontiguousarray(Qa[gbs // NBQ, gbs % NBQ])
        m["segPT"] = np.ascontiguousarray(PT[:, NBQ]); m["segQ"] = np.ascontiguousarray(Qa[:, NBQ])
        hm = lambda a: np.ascontiguousarray(a.reshape(6, 64, T)[:, :, tok].transpose(1, 0, 2))
        m["bon"] = hm(G["o_bon"]); m["g"] = hm(G["o_g"]); m["sq"] = hm(G["o_sq"])
        m["qlat"] = np.ascontiguousarray(G["o_qlat"][:, :, tok]); m["iqs"] = np.ascontiguousarray(G["o_iqs"][:, :, tok]); m["sgn"] = np.ascontiguousarray(G["o_sgn"][tok])
        sk = G["o_sk"].reshape(2, 64, T); sv = G["o_sv"]
        sk2 = np.zeros((64, 2, NBQ, 256), bf); sv2 = np.zeros((NBQ, 2, 128, 128), bf)
        for j in range(NBQ):
            if gbs[j] > 0:
                sk2[:, :, j, 0:128] = sk[:, :, prev[j]].transpose(1, 0, 2); sv2[j, 0] = sv[prev[j]]
            sk2[:, :, j, 128:256] = sk[:, :, own[j]].transpose(1, 0, 2); sv2[j, 1] = sv[own[j]]
        m["sk2"] = sk2; m["sv2"] = sv2
        m["ckv"] = G["o_ckv"]; m["ckvT"] = G["o_ckvT"]; m["ikT"] = G["o_ikT"]
        m["wuv"] = np.ascontiguousarray(inp['dsa_w_uv'][l])
        m["sink_b"] = np.ascontiguousarray(np.broadcast_to(inp['swa_sinks'][l][None], (64, 6)))
        rl = np.zeros((64, 16), np.float32)
        rl[:, 0:6] = inp['rwkv_ln_g'][l].reshape(6, 64).T; rl[:, 6:12] = inp['rwkv_ln_b'][l].reshape(6, 64).T
        m["rwkv_ln"] = rl
        m.update(k2_tables(i))
        maps.append(m)
    return maps


NPBF = ml_dtypes.bfloat16
NBQ_FULL = 16
_PROGS = {}


def _k1_spec(NBLK):
    NTk = NBLK * 128
    return {"o_YwT": ([NBLK, 6, 64, 128], F32), "o_Y0T": ([NBLK, 6, 64, 128], F32), "o_PT": ([NBLK + 1, 6, 64, 64], F32), "o_Q": ([NBLK + 1, 6, 64, 64], F32),
            "o_bon": ([384, NTk], F32), "o_g": ([384, NTk], F32), "o_qlat": ([128, 4, NTk], BF16), "o_iqs": ([64, 4, NTk], BF16), "o_sgn": ([NTk, 4], F32),
            "o_ckv": ([NTk, 128], BF16), "o_ckvT": ([128, NTk], BF16), "o_ikT": ([64, NTk], BF16), "o_sq": ([384, NTk], BF16), "o_sk": ([128, NTk], BF16),
            "o_sv": ([NTk, 128], BF16)}


def _build_k0():
    nc = bass.Bass("TRN2", target_bir_lowering=False)
    di = lambda n, s: nc.dram_tensor(n, list(s), F32, kind="ExternalInput").ap()
    dr = {"cT": di("cT", [128, 8]), "w_mod": di("w_mod", [4, 1024, 768]), "b_mod": di("b_mod", [4, 768])}
    dr["mod"] = nc.dram_tensor("mod", [4, 768], F32, kind="ExternalOutput").ap()
    with ExitStack() as es:
        P = Prog(nc, es)
        dr["out_tb"] = P.tb("out")
        emit_k0(P, nc, dr)
        P.finish([dr["out_tb"]])
    return nc


def _build_k1(NBLK):
    nc = bass.Bass("TRN2", target_bir_lowering=False)
    NTk = NBLK * 128
    di = lambda n, s: nc.dram_tensor(n, list(s), F32, kind="ExternalInput").ap()
    dr = {"x": di("x", [NTk, D]), "xh": di("xh", [1, D]), "par": di("par", [128, 64]), "bcp": di("bcp", [128, 320]), "lora": di("lora", [128, 384]),
          "g2w": di("g2w", [128, 384]), "wuk": di("wuk", [128, 2, 128]), "cst": di("cst", [128, 5, 128]), "w_in": di("w_in", [D, 2756])}
    for n, (s, dt) in _k1_spec(NBLK).items():
        dr[n] = nc.dram_tensor(n, s, dt, kind="ExternalOutput").ap()
    with ExitStack() as es:
        P = Prog(nc, es)
        dr["out_tb"] = P.tb("out")
        emit_k1(P, nc, NBLK, dr)
        P.finish([dr["out_tb"]])
    return nc


def _k2_in(NBQ):
    return {"YwT": ([NBQ, 6, 64, 128], F32), "Y0T": ([NBQ, 6, 64, 128], F32), "PbT": ([NBQ, 6, 64, 64], F32), "Qb": ([NBQ, 6, 64, 64], F32),
            "segPT": ([8, 6, 64, 64], F32), "segQ": ([8, 6, 64, 64], F32), "bon": ([64, 6, NBQ * 128], F32), "g": ([64, 6, NBQ * 128], F32),
            "sq": ([64, 6, NBQ * 128], BF16), "qlat": ([128, 4, NBQ * 128], BF16), "iqs": ([64, 4, NBQ * 128], BF16), "sgn": ([NBQ * 128, 4], F32),
            "sk2": ([64, 2, NBQ, 256], BF16), "sv2": ([NBQ, 2, 128, 128], BF16), "ckv": ([8 * NBQ * 128, 128], BF16), "ckvT": ([128, 8 * NBQ * 128], BF16),
            "ikT": ([64, 8 * NBQ * 128], BF16), "wuv": ([4, 128, 64], F32), "sink_b": ([64, 6], F32), "rwkv_ln": ([64, 16], F32), "dmask": ([128, 1024], F32),
            "ab": ([128, 4, 128], F32), "swb": ([128, 2, 6, 128], F32), "swb_first": ([128, 6, 128], F32), "ident": ([128, 128], F32)}


def _build_k2(NBQ):
    nc = bass.Bass("TRN2", target_bir_lowering=False)
    dr = {n: nc.dram_tensor(n, s, dt, kind="ExternalInput").ap() for n, (s, dt) in _k2_in(NBQ).items()}
    dr["mixT"] = nc.dram_tensor("mixT", [16, 64, NBQ * 128], BF16, kind="ExternalOutput").ap()
    with ExitStack() as es:
        P = Prog(nc, es)
        dr["out_tb"] = P.tb("out")
        emit_k2(P, nc, NBQ, dr)
        P.finish([dr["out_tb"]])
    return nc


def _build_k3(NT, exp_ids):
    nc = bass.Bass("TRN2", target_bir_lowering=False)
    di = lambda n, s: nc.dram_tensor(n, list(s), F32, kind="ExternalInput").ap()
    NX = len(exp_ids)
    dr = {"xres": di("xres", [NT, D]), "mixT": nc.dram_tensor("mixT", [16, 64, NT], BF16, kind="ExternalInput").ap(),
          "modb": di("modb", [128, 6 * D]), "modT": di("modT", [128, 48]), "lnp": di("lnp", [128, 4, D]), "w_out": di("w_out", [D, D]),
          "router_w": di("router_w", [D, NE]), "rbias_b": di("rbias_b", [128, NE]), "ew1": di("ew1", [NX, D, DE]), "ew3": di("ew3", [NX, D, DE]),
          "ew2": di("ew2", [NX, DE, D]), "ident": di("ident", [128, 128])}
    dr["out"] = nc.dram_tensor("out", [NT, D], F32, kind="ExternalOutput").ap()
    with ExitStack() as es:
        P = Prog(nc, es)
        dr["out_tb"] = P.tb("out")
        ident = P.sbuf("ident_sb", [128, 128], F32)
        ident_tb = P.tb("ident")
        P.op("sp", lambda e: e.dma_start(out=ident[:], in_=dr["ident"]), writes=[ident_tb], dma=True)
        emit_ffn(P, nc, NT, exp_ids, dr, ident, ident_tb)
        P.finish([dr["out_tb"]])
    return nc


def _prog(key, fn):
    if key not in _PROGS:
        _PROGS[key] = fn()
    return _PROGS[key]


def _run(nc, maps):
    res = run_bass_kernel_spmd(nc, maps, core_ids=list(range(8)))
    return [{k: np.asarray(v) for k, v in r.items()} for r in res.results]


def _forward(inp, NBQ=NBQ_FULL, n_layers=4, exp_ids=None):
    NT = NBQ * 128
    T = 8 * NT
    if exp_ids is None:
        exp_ids = list(range(NE)) + [NE]
    inp = {k: np.asarray(v) for k, v in inp.items()}
    cT = np.ascontiguousarray(inp['c'][0].reshape(8, 128).T)
    r0 = _run(_prog("k0", _build_k0), [{"cT": cT, "w_mod": np.ascontiguousarray(inp['w_mod'][:, :, i * 768:(i + 1) * 768]),
                                         "b_mod": np.ascontiguousarray(inp['b_mod'][:, i * 768:(i + 1) * 768])} for i in range(8)])
    mod = np.concatenate([r0[i]["mod"] for i in range(8)], 1)
    x = np.ascontiguousarray(inp['x'][0][:T])
    ident = np.eye(128, dtype=np.float32)
    toks = [((8 * np.arange(NBQ) + i)[:, None] * 128 + np.arange(128)[None, :]).reshape(-1) for i in range(8)]
    for l in range(n_layers):
        common = k1_common(inp, l, mod[l])
        maps = []
        for c in range(8):
            m = dict(common)
            m["par"] = common["par"].copy()
            m["par"][:, 45] = 0.0 if c == 0 else 1.0
            m["x"] = np.ascontiguousarray(x[c * NT:(c + 1) * NT])
            m["xh"] = np.ascontiguousarray(x[c * NT - 1:c * NT]) if c > 0 else np.zeros((1, D), np.float32)
            maps.append(m)
        K1 = _run(_prog(("k1", NBQ), lambda: _build_k1(NBQ)), maps)
        K2 = _run(_prog(("k2", NBQ), lambda: _build_k2(NBQ)), k2_inputs(K1, inp, l, NBQ))
        del K1
        sel = [e for e in exp_ids if e < NE]
        ew1 = np.concatenate([inp['exp_w1'][l][sel], inp['sh_w1'][l][None]], 0)
        ew3 = np.concatenate([inp['exp_w3'][l][sel], inp['sh_w3'][l][None]], 0)
        ew2 = np.concatenate([inp['exp_w2'][l][sel], inp['sh_w2'][l][None]], 0)
        lnp = np.stack([inp['ln_mix_g'][l], inp['ln_mix_b'][l], inp['ln_ffn_g'][l], inp['ln_ffn_b'][l]])
        common3 = {"modb": np.ascontiguousarray(np.broadcast_to(mod[l][None], (128, 6 * D))), "modT": np.ascontiguousarray(mod[l].reshape(48, 128).T),
                   "lnp": np.ascontiguousarray(np.broadcast_to(lnp[None], (128, 4, D))), "w_out": np.ascontiguousarray(inp['w_out'][l]),
                   "router_w": np.ascontiguousarray(inp['router_w'][l]),
                   "rbias_b": np.ascontiguousarray(np.broadcast_to(inp['router_bias'][l][None], (128, NE))), "ew1": ew1, "ew3": ew3, "ew2": ew2, "ident": ident}
        maps3 = [{**common3, "xres": np.ascontiguousarray(x[toks[i]]), "mixT": K2[i]["mixT"]} for i in range(8)]
        K3 = _run(_prog(("k3", NT, tuple(exp_ids)), lambda: _build_k3(NT, exp_ids)), maps3)
        del maps3, ew1, ew3, ew2
        xn = np.empty_like(x)
        for i in range(8):
            xn[toks[i]] = K3[i]["out"]
        x = xn
    return x


def kernel(**inputs):
    x = _forward(inputs)
    return np.ascontiguousarray(x[None].astype(np.float32))
```

```python
import numpy as np
import ml_dtypes


from contextlib import ExitStack
import concourse.bass as bass
import concourse.mybir as mybir
from concourse.bass_utils import run_bass_kernel_spmd

F32 = mybir.dt.float32
BF16 = mybir.dt.bfloat16
AF = mybir.ActivationFunctionType
ALU = mybir.AluOpType
AX = mybir.AxisListType


SKIP_SELF = False


class TB:
    __slots__ = ("name", "w", "r")

    def __init__(self, name="?"):
        self.name = name
        self.w = None
        self.r = {}


class Prog:
    ENGS = ("pe", "dve", "act", "pool", "sp")

    def __init__(self, nc, es, ring=8):
        self.nc = nc
        self.es = es
        self.ops = {e: [] for e in self.ENGS}
        self.cnt = {e: 0 for e in self.ENGS}
        self.waited = {e: {} for e in self.ENGS}
        self.sems = {}
        for e in self.ENGS:
            self.sems["c_" + e] = es.enter_context(nc.semaphore("c_" + e))
        self.ring = {}
        for e in ("sp", "pool", "act"):
            names = [f"d_{e}{i}" for i in range(ring)]
            for n in names:
                self.sems[n] = es.enter_context(nc.semaphore(n))
            self.ring[e] = {"names": names, "uses": [0] * ring, "next": 0}
        self.nbuf = 0

    def tb(self, name=None):
        self.nbuf += 1
        return TB(name or f"b{self.nbuf}")

    def sbuf(self, name, shape, dtype):
        return self.es.enter_context(self.nc.sbuf_tensor("sb_" + name, list(shape), dtype))

    def psum(self, name, shape, dtype=F32):
        return self.es.enter_context(self.nc.psum_tensor("ps_" + name, list(shape), dtype))

    def _need(self, eng, evs):
        need = {}
        for ev in evs:
            if ev is None:
                continue
            k, v = ev
            if need.get(k, 0) < v:
                need[k] = v
        w = self.waited[eng]
        for k, v in need.items():
            if SKIP_SELF and k == "c_" + eng:
                continue
            if w.get(k, 0) < v:
                self.ops[eng].append(("wait", k, v))
                w[k] = v

    def op(self, eng, fn, reads=(), writes=(), dma=False, acc=False):
        evs = []
        for b in reads:
            evs.append(b.w)
        for b in writes:
            if not (acc and eng == "pe" and b.w is not None and b.w[0] == "c_pe"):
                evs.append(b.w)
            for k, v in b.r.items():
                evs.append((k, v))
        if dma:
            rg = self.ring[eng]
            i = rg["next"]
            rg["next"] = (i + 1) % len(rg["names"])
            k = rg["names"][i]
            evs.append((k, 16 * rg["uses"][i]))
            rg["uses"][i] += 1
            ev = (k, 16 * rg["uses"][i])
            inc = 16
        else:
            self.cnt[eng] += 1
            k = "c_" + eng
            ev = (k, self.cnt[eng])
            inc = 1
        self._need(eng, evs)
        self.ops[eng].append(("op", fn, k, inc))
        for b in reads:
            if b.r.get(ev[0], 0) < ev[1]:
                b.r[ev[0]] = ev[1]
        for b in writes:
            b.w = ev
            b.r = {}
        return ev

    def barrier(self):
        evs = [("c_" + x, self.cnt[x]) for x in self.ENGS]
        for e, rg in self.ring.items():
            for n, u in zip(rg["names"], rg["uses"]):
                evs.append((n, 16 * u))
        for e in self.ENGS:
            self._need(e, evs)

    def scope(self):
        prog = self

        class _Scope:
            def __enter__(self_):
                self_.old = prog.es
                self_.st = ExitStack()
                self_.st.__enter__()
                prog.es = self_.st
                return prog

            def __exit__(self_, *a):
                prog.barrier()
                prog.es = self_.old
                return self_.st.__exit__(*a)
        return _Scope()

    def finish(self, out_bufs):
        self._need("sp", [b.w for b in out_bufs])
        nc = self.nc
        sems = self.sems
        ops = self.ops

        def replay(engobj, lst):
            for it in lst:
                if it[0] == "wait":
                    engobj.wait_ge(sems[it[1]], it[2])
                else:
                    ins = it[1](engobj)
                    ins.then_inc(sems[it[2]], it[3])

        with nc.Block() as block:
            @block.tensor
            def _(e):
                replay(e, ops["pe"])

            @block.vector
            def _(e):
                replay(e, ops["dve"])

            @block.scalar
            def _(e):
                replay(e, ops["act"])

            @block.gpsimd
            def _(e):
                replay(e, ops["pool"])

            @block.sync
            def _(e):
                replay(e, ops["sp"])


D = 1024
ALPHA = (2 * 4) ** 0.25
LN_EPS = 1e-5
NE = 64
DE = 256


def bcast_last(ap, n):
    shp = list(ap.shape)
    return ap.unsqueeze(len(shp)).to_broadcast(shp + [n])


class Consts:
    pass


def emit_layernorm(P, z, z_tb, out, out_tb, gb, bb, par_tb, sc, eps_t, tagn):
    st, mv, rs, xn = sc["st"], sc["mv"], sc["rs"], sc["xn"]
    stb = sc["tb"]
    P.op("dve", lambda e: e.bn_stats(out=st[:, 0, :], in_=z[:, 0:512]), reads=[z_tb], writes=[stb])
    P.op("dve", lambda e: e.bn_stats(out=st[:, 1, :], in_=z[:, 512:1024]), reads=[z_tb, stb], writes=[stb])
    P.op("dve", lambda e: e.bn_aggr(out=mv[:], in_=st[:].rearrange("p a b -> p (a b)")), reads=[stb], writes=[stb])
    P.op("act", lambda e: e.activation(out=rs[:], in_=mv[:, 1:2], func=AF.Sqrt, bias=eps_t[:, 0:1], scale=1.0),
         reads=[stb], writes=[stb])
    P.op("dve", lambda e: e.reciprocal(out=rs[:], in_=rs[:]), reads=[stb], writes=[stb])
    P.op("dve", lambda e: e.tensor_scalar(out=xn[:], in0=z, scalar1=mv[:, 0:1], scalar2=rs[:, 0:1],
                                          op0=ALU.subtract, op1=ALU.mult), reads=[z_tb, stb], writes=[sc["xn_tb"]])
    P.op("pool", lambda e: e.tensor_tensor(out=xn[:], in0=xn[:], in1=gb, op=ALU.mult),
         reads=[sc["xn_tb"]] + list(par_tb), writes=[sc["xn_tb"]])
    P.op("pool", lambda e: e.tensor_tensor(out=out, in0=xn[:], in1=bb, op=ALU.add),
         reads=[sc["xn_tb"]] + list(par_tb), writes=[out_tb])


NRW = 1408
C_DQ, C_CKV, C_IQ, C_IK, C_IW = 1408, 1664, 1792, 2048, 2112
C_SQ, C_SK, C_SV = 2116, 2500, 2628
PIN = 2756
DECAY_C = -0.6065306597126334


STOP = 99


class StopEmit(Exception):
    pass


def ck(n):
    if STOP == n:
        raise StopEmit()


class Slots:
    def __init__(self, P, aps, tbs=None):
        self.aps = aps
        self.tbs = tbs if tbs is not None else [P.tb() for _ in aps]
        self.i = 0

    def next(self):
        i = self.i
        self.i = (i + 1) % len(self.aps)
        return self.aps[i], self.tbs[i]


def emit_k1(P, nc, NBLK, dr):
    cst = P.sbuf("cst", [128, 5, 128], F32)
    cst_tb = P.tb("cst")
    P.op("sp", lambda e: e.dma_start(out=cst[:], in_=dr["cst"]), writes=[cst_tb], dma=True)
    ident, BO, LS, US, UI = (cst[:, i, :] for i in range(5))
    identb = P.sbuf("identb", [128, 128], BF16)
    P.op("dve", lambda e: e.tensor_copy(out=identb[:], in_=ident), reads=[cst_tb], writes=[cst_tb])
    par = P.sbuf("par", [128, 64], F32)
    par_tb = P.tb("par")
    P.op("sp", lambda e: e.dma_start(out=par[:], in_=dr["par"]), writes=[par_tb], dma=True)
    P.op("dve", lambda e: e.tensor_scalar(out=par[:, 37:45], in0=par[:, 37:45], scalar1=1.0, scalar2=None, op0=ALU.add),
         reads=[par_tb], writes=[par_tb])
    P.op("dve", lambda e: e.tensor_scalar(out=par[:, 23:26], in0=par[:, 20:23], scalar1=-1.0, scalar2=1.0, op0=ALU.mult, op1=ALU.add),
         reads=[par_tb], writes=[par_tb])
    bcp = P.sbuf("bcp", [128, 320], F32)
    P.op("sp", lambda e: e.dma_start(out=bcp[:], in_=dr["bcp"]), writes=[par_tb], dma=True)
    eps6 = P.sbuf("eps6", [128, 2], F32)
    P.op("pool", lambda e: e.memset(eps6[:, 0:1], 1e-6), writes=[par_tb])
    P.op("pool", lambda e: e.memset(eps6[:, 1:2], 1e-5), writes=[par_tb])
    lora = P.sbuf("lora", [128, 384], F32)
    g2w = P.sbuf("g2w", [128, 384], F32)
    P.op("sp", lambda e: e.dma_start(out=lora[:], in_=dr["lora"]), writes=[par_tb], dma=True)
    P.op("sp", lambda e: e.dma_start(out=g2w[:], in_=dr["g2w"]), writes=[par_tb], dma=True)
    wuk_f = P.sbuf("wuk_f", [128, 2, 128], F32)
    wuk = P.sbuf("wuk", [128, 2, 128], BF16)
    P.op("sp", lambda e: e.dma_start(out=wuk_f[:], in_=dr["wuk"]), writes=[par_tb], dma=True)
    P.op("dve", lambda e: e.tensor_copy(out=wuk[:], in_=wuk_f[:]), reads=[par_tb], writes=[par_tb])
    win = P.sbuf("win", [128, 8, PIN], BF16)
    win_tb = P.tb("win")
    wst = [P.sbuf(f"wst{i}", [128, PIN], F32) for i in range(2)]
    wst_tb = [P.tb() for _ in range(2)]
    for kc in range(8):
        s = kc % 2
        P.op("sp", lambda e, kc=kc, s=s: e.dma_start(out=wst[s][:], in_=dr["w_in"][kc * 128:(kc + 1) * 128, :]),
             writes=[wst_tb[s]], dma=True)
        P.op("pool", lambda e, kc=kc, s=s: e.tensor_copy(out=win[:, kc, :], in_=wst[s][:]), reads=[wst_tb[s]], writes=[win_tb])
    pbk = [P.psum(f"k1pb{i}", [128, 512], F32) for i in range(8)]
    bank_tb = [P.tb(f"bank{i}") for i in range(8)]
    q128 = Slots(P, [pbk[b][:, q * 128:(q + 1) * 128] for q in range(4) for b in (2, 3, 4, 5)],
                 [bank_tb[b] for q in range(4) for b in (2, 3, 4, 5)])
    h256 = Slots(P, [pbk[b][:, q * 256:(q + 1) * 256] for q in range(2) for b in (6, 7)],
                 [bank_tb[b] for q in range(2) for b in (6, 7)])
    pT_tb = [bank_tb[0], bank_tb[1]]
    xb = [P.sbuf(f"xb{i}", [128, D], F32) for i in range(2)]
    xb_tb = [P.tb(), P.tb()]
    hT = P.sbuf("hT", [128, 8, 128], BF16)
    hT_tb = P.tb("hT")
    hh = P.sbuf("hh", [128, 8, 1], BF16)
    pr = P.sbuf("pr", [128, 11, 129], F32)
    pr_tb = P.tb("pr")
    prev = P.sbuf("prevc", [128, 11, 1], F32)
    prev_tb = P.tb("prev")
    X = P.sbuf("X", [128, 11, 128], F32)
    X_tb = P.tb("X")
    dif = P.sbuf("dif", [128, 11, 128], F32)
    fm = {n: P.sbuf("fm_" + n, [128, 3, 128], F32) for n in
          ["lw", "a", "kk", "kp", "t1", "cum", "einc", "eexc", "einv", "eend", "at", "bh", "kh", "rt", "bc", "kc", "g", "bon", "beta"]}
    fa = P.tb("fmall")
    fm_tb = {n: fa for n in fm}
    LA = P.sbuf("LA", [128, 128], F32)
    SG = P.sbuf("SG", [128, 128], F32)
    gC = P.sbuf("gC", [128, 3], F32)
    GCe = P.sbuf("GCe", [128, 3], F32)
    ones = P.sbuf("ones128", [128, 128], F32)
    P.op("pool", lambda e: e.memset(ones[:], 1.0), writes=[par_tb])
    tokm = {n: P.sbuf("tok_" + n, [128, 384], F32) for n in ["at", "bc", "kc", "v"]}
    tok_tb = {n: P.tb("tok_" + n) for n in tokm}
    tk = P.sbuf("tk", [128, 580], F32)
    tk_tb = P.tb("tk")
    HW = [{"MX": P.sbuf(f"MX{i}", [128, 256], F32), "MT": P.sbuf(f"MT{i}", [128, 128], F32), "MKT": P.sbuf(f"MKT{i}", [128, 128], F32),
           "NBT": P.sbuf(f"NBT{i}", [128, 128], F32), "NKT": P.sbuf(f"NKT{i}", [128, 128], F32), "DG": P.sbuf(f"DG{i}", [128, 128], F32)} for i in range(2)]
    HW_tb = [{n: P.tb(f"{n}{i}") for n in ("MX", "MT", "MKT", "NBT", "NKT", "DG")} for i in range(2)]
    ATb = P.sbuf("ATb", [64, 6, 64], F32)
    Db = P.sbuf("Db", [64, 6, 64], F32)
    AD_tb = [P.tb() for _ in range(6)]
    PQ = P.sbuf("PQ", [64, 6, 128], F32)
    PTt = P.sbuf("PTt", [64, 6, 64], F32)
    PQ_tb = [P.tb() for _ in range(6)]
    ost = [P.sbuf(f"ost{i}", [128, 128], F32) for i in range(4)]
    ost_s = Slots(P, [o[:] for o in ost])
    obf = [P.sbuf(f"obf{i}", [128, 512], BF16) for i in range(4)]
    obf_s = Slots(P, [o[:] for o in obf])
    nsc = {"st": P.sbuf("n_st", [128, 6], F32), "mv": P.sbuf("n_mv", [128, 2], F32), "rs": P.sbuf("n_rs", [128, 2], F32),
           "aiw": P.sbuf("n_aiw", [128, 4], F32), "sg": P.sbuf("n_sg", [128, 4], F32), "t": P.sbuf("n_t", [128, 256], F32)}
    nsc_tb = P.tb("nsc")
    out_tb = dr["out_tb"]

    def out_dma(dst, src, rtb):
        P.op("sp", lambda e: e.dma_start(out=dst, in_=src), reads=[rtb], writes=[out_tb], dma=True)

    for hd in range(6):
        P.op("pool", lambda e, hd=hd: e.memset(PQ[:, hd, 64:128], 0.0), writes=[PQ_tb[hd]])
        P.op("pool", lambda e, hd=hd: e.tensor_copy(out=PQ[:, hd, 0:64], in_=ident[0:64, 0:64]), reads=[cst_tb], writes=[PQ_tb[hd]])
        P.op("pool", lambda e, hd=hd: e.tensor_copy(out=PTt[:, hd, :], in_=ident[0:64, 0:64]), reads=[cst_tb], writes=[PQ_tb[hd]])

    def emit_pq_out(b):
        for hd in range(6):
            out_dma(dr["o_PT"][b, hd], PTt[:, hd, :], PQ_tb[hd])
            out_dma(dr["o_Q"][b, hd], PQ[:, hd, 64:128], PQ_tb[hd])

    ck(1)
    P.op("sp", lambda e: e.dma_start(out=xb[1][0:1, :], in_=dr["xh"]), writes=[xb_tb[1]], dma=True)
    for kc in range(8):
        P.op("pe", lambda e, kc=kc: e.transpose(out=pbk[0][:, kc:kc + 1], in_=xb[1][0:1, kc * 128:(kc + 1) * 128],
                                                identity=ident[0:1, 0:1]), reads=[xb_tb[1], cst_tb], writes=[pT_tb[0]], acc=True)
    for kc in range(8):
        P.op("act", lambda e, kc=kc: e.activation(out=hh[:, kc, :], in_=pbk[0][:, kc:kc + 1], func=AF.Identity,
                                                  bias=par[:, 29 + kc:30 + kc], scale=par[:, 37 + kc:38 + kc]),
             reads=[pT_tb[0], par_tb], writes=[hT_tb])
    for c in range(11):
        pa, ptb = q128.next()
        for kc in range(8):
            P.op("pe", lambda e, c=c, kc=kc, pa=pa: e.matmul(pa[:, 0:1], lhsT=win[:, kc, c * 128:(c + 1) * 128], rhs=hh[:, kc, :],
                                                             start=(kc == 0), stop=(kc == 7)), reads=[win_tb, hT_tb], writes=[ptb], acc=True)
        P.op("dve", lambda e, c=c, pa=pa: e.tensor_scalar(out=prev[:, c, :], in0=pa[:, 0:1], scalar1=par[:, 45:46], scalar2=None, op0=ALU.mult),
             reads=[ptb, par_tb], writes=[prev_tb])

    ck(2)
    emit_pq_out(0)
    for b in range(NBLK):
        t0 = b * 128
        xi = b % 2
        P.op("sp", lambda e, xi=xi, t0=t0: e.dma_start(out=xb[xi][:], in_=dr["x"][t0:t0 + 128, :]), writes=[xb_tb[xi]], dma=True)
        for kc in range(8):
            bank = kc // 4
            P.op("pe", lambda e, kc=kc, bank=bank, xi=xi: e.transpose(out=pbk[bank][:, (kc % 4) * 128:(kc % 4 + 1) * 128],
                                                                      in_=xb[xi][:, kc * 128:(kc + 1) * 128], identity=ident),
                 reads=[xb_tb[xi], cst_tb], writes=[pT_tb[bank]], acc=True)
        for kc in range(8):
            bank = kc // 4
            P.op("act", lambda e, kc=kc, bank=bank: e.activation(out=hT[:, kc, :], in_=pbk[bank][:, (kc % 4) * 128:(kc % 4 + 1) * 128],
                                                                 func=AF.Identity, bias=par[:, 29 + kc:30 + kc], scale=par[:, 37 + kc:38 + kc]),
                 reads=[pT_tb[bank], par_tb], writes=[hT_tb])

        ck(3)

        def projT(col0, evac):
            pa, ptb = q128.next()
            for kc in range(8):
                P.op("pe", lambda e, kc=kc, pa=pa: e.matmul(pa, lhsT=win[:, kc, col0:col0 + 128], rhs=hT[:, kc, :],
                                                            start=(kc == 0), stop=(kc == 7)), reads=[win_tb, hT_tb], writes=[ptb], acc=True)
            evac(pa, ptb)

        P.op("pool", lambda e: e.tensor_copy(out=pr[:, :, 0:1], in_=prev[:]), reads=[prev_tb], writes=[pr_tb])
        for c in range(11):
            projT(c * 128, lambda pa, ptb, c=c: P.op("act", lambda e: e.activation(out=pr[:, c, 1:129], in_=pa, func=AF.Identity),
                                                     reads=[ptb], writes=[pr_tb]))
        P.op("pool", lambda e: e.tensor_copy(out=prev[:], in_=pr[:, :, 128:129]), reads=[pr_tb], writes=[prev_tb])
        P.op("dve", lambda e: e.tensor_tensor(out=dif[:], in0=pr[:, :, 0:128], in1=pr[:, :, 1:129], op=ALU.subtract), reads=[pr_tb], writes=[X_tb])
        P.op("dve", lambda e: e.tensor_tensor(out=dif[:], in0=dif[:], in1=bcast_last(par[:, 0:11], 128), op=ALU.mult), reads=[X_tb, par_tb], writes=[X_tb])
        P.op("dve", lambda e: e.tensor_tensor(out=X[:], in0=dif[:], in1=pr[:, :, 1:129], op=ALU.add), reads=[X_tb, pr_tb], writes=[X_tb])
        ck(4)
        r_, k_, v_ = X[:, 0:3, :], X[:, 3:6, :], X[:, 6:9, :]
        P.op("act", lambda e: e.activation(out=LA[0:64, :], in_=X[0:64, 9, :], func=AF.Tanh), reads=[X_tb], writes=[fm_tb["lw"]])
        P.op("act", lambda e: e.activation(out=LA[64:128, :], in_=X[64:128, 9, :], func=AF.Identity), reads=[X_tb], writes=[fm_tb["lw"]])
        P.op("act", lambda e: e.activation(out=SG[:], in_=X[:, 10, :], func=AF.Sigmoid), reads=[X_tb], writes=[fm_tb["g"]])
        for cc in range(3):
            pa, ptb = q128.next()
            P.op("pe", lambda e, cc=cc, pa=pa: e.matmul(pa, lhsT=lora[0:64, cc * 128:(cc + 1) * 128], rhs=LA[0:64, :], start=True, stop=True),
                 reads=[par_tb, fm_tb["lw"]], writes=[ptb])
            P.op("act", lambda e, cc=cc, pa=pa: e.activation(out=fm["lw"][:, cc, :], in_=pa, func=AF.Sigmoid, bias=par[:, 11 + cc:12 + cc], scale=1.0),
                 reads=[ptb, par_tb], writes=[fm_tb["cum"]])
            pa2, ptb2 = q128.next()
            P.op("pe", lambda e, cc=cc, pa2=pa2: e.matmul(pa2, lhsT=lora[64:128, cc * 128:(cc + 1) * 128], rhs=LA[64:128, :], start=True, stop=True),
                 reads=[par_tb, fm_tb["lw"]], writes=[ptb2])
            P.op("act", lambda e, cc=cc, pa2=pa2: e.activation(out=fm["a"][:, cc, :], in_=pa2, func=AF.Sigmoid, bias=par[:, 14 + cc:15 + cc], scale=1.0),
                 reads=[ptb2, par_tb], writes=[fm_tb["a"]])
            pa3, ptb3 = q128.next()
            P.op("pe", lambda e, cc=cc, pa3=pa3: e.matmul(pa3, lhsT=g2w[:, cc * 128:(cc + 1) * 128], rhs=SG[:], start=True, stop=True),
                 reads=[par_tb, fm_tb["g"]], writes=[ptb3])
            P.op("act", lambda e, cc=cc, pa3=pa3: e.activation(out=fm["g"][:, cc, :], in_=pa3, func=AF.Identity), reads=[ptb3], writes=[fm_tb["bon"]])
        dv = lambda fn, rd, wr: P.op("dve", fn, reads=[fm_tb[n] if isinstance(n, str) else n for n in rd],
                                     writes=[fm_tb[n] if isinstance(n, str) else n for n in wr])
        F = fm
        pcol = lambda c0: bcast_last(par[:, c0:c0 + 3], 128)
        dv(lambda e: e.tensor_scalar(out=F["lw"][:], in0=F["lw"][:], scalar1=DECAY_C, scalar2=None, op0=ALU.mult), ["cum"], ["cum"])
        out_dma(dr["o_g"].rearrange("(c p) t -> p c t", p=128)[:, :, t0:t0 + 128], F["g"][:], fm_tb["bon"])
        dv(lambda e: e.tensor_tensor(out=F["kk"][:], in0=k_, in1=pcol(17), op=ALU.mult), [X_tb, par_tb], ["kk"])
        dv(lambda e: e.tensor_tensor(out=F["t1"][:], in0=F["kk"][:], in1=F["kk"][:], op=ALU.mult), ["kk"], ["t1"])
        for cc in range(3):
            pa, ptb = q128.next()
            P.op("pe", lambda e, cc=cc, pa=pa: e.matmul(pa, lhsT=BO, rhs=F["t1"][:, cc, :], start=True, stop=True), reads=[cst_tb, fm_tb["t1"]], writes=[ptb])
            P.op("act", lambda e, cc=cc, pa=pa: e.activation(out=F["kp"][:, cc, :], in_=pa, func=AF.Sqrt), reads=[ptb], writes=[fm_tb["kp"]])
        dv(lambda e: e.tensor_scalar(out=F["kp"][:], in0=F["kp"][:], scalar1=1e-12, scalar2=None, op0=ALU.max), ["kp"], ["kp"])
        dv(lambda e: e.reciprocal(out=F["kp"][:], in_=F["kp"][:]), ["kp"], ["kp"])
        dv(lambda e: e.tensor_tensor(out=F["kk"][:], in0=F["kk"][:], in1=F["kp"][:], op=ALU.mult), ["kk", "kp"], ["kk"])
        dv(lambda e: e.tensor_tensor(out=F["t1"][:], in0=F["a"][:], in1=pcol(20), op=ALU.mult), ["a", par_tb], ["t1"])
        dv(lambda e: e.tensor_tensor(out=F["t1"][:], in0=F["t1"][:], in1=pcol(23), op=ALU.add), ["t1", par_tb], ["t1"])
        dv(lambda e: e.tensor_tensor(out=F["kp"][:], in0=k_, in1=F["t1"][:], op=ALU.mult), [X_tb, "t1", "kp"], ["kp"])
        dv(lambda e: e.tensor_tensor(out=F["t1"][:], in0=r_, in1=pcol(26), op=ALU.mult), [X_tb, par_tb], ["t1"])
        dv(lambda e: e.tensor_tensor(out=F["t1"][:], in0=F["t1"][:], in1=F["kp"][:], op=ALU.mult), ["t1", "kp"], ["t1"])
        for cc in range(3):
            pa, ptb = q128.next()
            P.op("pe", lambda e, cc=cc, pa=pa: e.matmul(pa, lhsT=BO, rhs=F["t1"][:, cc, :], start=True, stop=True), reads=[cst_tb, fm_tb["t1"]], writes=[ptb])
            P.op("dve", lambda e, cc=cc, pa=pa: e.tensor_tensor(out=F["bon"][:, cc, :], in0=pa, in1=X[:, 6 + cc, :], op=ALU.mult),
                 reads=[ptb, X_tb], writes=[fm_tb["g"]])
        out_dma(dr["o_bon"].rearrange("(c p) t -> p c t", p=128)[:, :, t0:t0 + 128], F["bon"][:], fm_tb["g"])
        dv(lambda e: e.tensor_tensor(out=F["beta"][:], in0=F["a"][:], in1=F["kk"][:], op=ALU.mult), ["a", "kk"], ["beta"])
        for cc in range(3):
            dv(lambda e, cc=cc: e.tensor_tensor_scan(out=F["cum"][:, cc, :], data0=ones[:], data1=F["lw"][:, cc, :], initial=0.0,
                                                     op0=ALU.mult, op1=ALU.add), ["cum", par_tb], ["einc"])
        dv(lambda e: e.tensor_copy(out=gC[:], in_=F["cum"][:, :, 127]), ["einc"], ["einc"])
        dv(lambda e: e.tensor_tensor(out=F["t1"][:], in0=F["cum"][:], in1=F["lw"][:], op=ALU.subtract), ["einc", "t1"], ["t1"])
        ac = lambda fn, rd, wr: P.op("act", fn, reads=[fm_tb[n] for n in rd], writes=[fm_tb[n] for n in wr])
        ac(lambda e: e.activation(out=F["einc"][:], in_=F["cum"][:], func=AF.Exp), ["einc"], ["eexc"])
        ac(lambda e: e.activation(out=F["eexc"][:], in_=F["t1"][:], func=AF.Exp), ["t1"], ["einv"])
        ac(lambda e: e.activation(out=F["einv"][:], in_=F["cum"][:], func=AF.Exp, scale=-1.0), ["einc"], ["eend"])
        for cc in range(3):
            ac(lambda e, cc=cc: e.activation(out=F["eend"][:, cc, :], in_=F["cum"][:, cc, :], func=AF.Exp, scale=-1.0, bias=gC[:, cc:cc + 1]),
               ["einc"], ["at"])
        ac(lambda e: e.activation(out=GCe[:], in_=gC[:], func=AF.Exp), ["einc"], ["at"])
        dv(lambda e: e.scalar_tensor_tensor(out=F["at"][:], in0=F["kk"][:], scalar=-1.0, in1=F["eexc"][:], op0=ALU.mult, op1=ALU.mult),
           ["kk", "einv", "at"], ["bh"])
        dv(lambda e: e.tensor_tensor(out=F["bh"][:], in0=F["beta"][:], in1=F["einv"][:], op=ALU.mult), ["beta", "eend"], ["kh"])
        dv(lambda e: e.tensor_tensor(out=F["kh"][:], in0=F["kp"][:], in1=F["einv"][:], op=ALU.mult), ["kp", "eend"], ["rt"])
        dv(lambda e: e.tensor_tensor(out=F["rt"][:], in0=r_, in1=F["einc"][:], op=ALU.mult), [X_tb, "eexc"], ["bc"])
        dv(lambda e: e.tensor_tensor(out=F["bc"][:], in0=F["beta"][:], in1=F["eend"][:], op=ALU.mult), ["beta", "at"], ["kc"])
        dv(lambda e: e.tensor_tensor(out=F["kc"][:], in0=F["kp"][:], in1=F["eend"][:], op=ALU.mult), ["kp", "at"], ["lw"])
        allf = [fm_tb[n] for n in ("bh", "kh", "rt", "bc", "kc", "lw")]
        ck(5)
        for nm, src, stb in (("at", F["at"], fm_tb["bh"]), ("bc", F["bc"], fm_tb["kc"]), ("kc", F["kc"], fm_tb["lw"]), ("v", None, X_tb)):
            for cc in range(3):
                pa, ptb = q128.next()
                s_ap = X[:, 6 + cc, :] if src is None else src[:, cc, :]
                P.op("pe", lambda e, pa=pa, s_ap=s_ap: e.transpose(out=pa, in_=s_ap, identity=ident), reads=[stb, cst_tb], writes=[ptb])
                P.op("act", lambda e, pa=pa, nm=nm, cc=cc: e.activation(out=tokm[nm][:, cc * 128:(cc + 1) * 128], in_=pa, func=AF.Identity),
                     reads=[ptb], writes=[tok_tb[nm]])
        ck(6)
        def _head(hd, b=b):
            cc, p0 = hd // 2, (hd % 2) * 64
            sl = slice(p0, p0 + 64)
            aT, bT, kT, rT = F["at"][sl, cc, :], F["bh"][sl, cc, :], F["kh"][sl, cc, :], F["rt"][sl, cc, :]
            tcol = slice(hd * 64, hd * 64 + 64)
            MX, MT, MKT, NBT, NKT, DG = (HW[hd % 2][n] for n in ("MX", "MT", "MKT", "NBT", "NKT", "DG"))
            MX_tb, MT_tb, MKT_tb, NBT_tb, NKT_tb, DG_tb = (HW_tb[hd % 2][n] for n in ("MX", "MT", "MKT", "NBT", "NKT", "DG"))

            def mm_mask(lhsT, rhs, mask, dst, dtb):
                pa, ptb = q128.next()
                P.op("pe", lambda e: e.matmul(pa, lhsT=lhsT, rhs=rhs, start=True, stop=True), reads=allf, writes=[ptb])
                P.op("dve", lambda e: e.tensor_tensor(out=dst, in0=pa, in1=mask, op=ALU.mult), reads=[ptb, cst_tb], writes=[dtb])

            mm_mask(aT, bT, LS, MX[:, 0:128], MX_tb)
            mm_mask(bT, aT, US, MT[:], MT_tb)
            mm_mask(kT, aT, US, MKT[:], MKT_tb)
            mm_mask(bT, rT, UI, NBT[:], NBT_tb)
            mm_mask(kT, rT, UI, NKT[:], NKT_tb)
            P.op("pool", lambda e, tcol=tcol: e.tensor_copy(out=MX[:, 128:192], in_=tokm["at"][:, tcol]), reads=[tok_tb["at"]], writes=[MX_tb])
            pa, ptb = q128.next()
            P.op("pe", lambda e, pa=pa, tcol=tcol: e.matmul(pa[:, 0:64], lhsT=MKT[:], rhs=tokm["v"][:, tcol], start=True, stop=True),
                 reads=[MKT_tb, tok_tb["v"]], writes=[ptb])
            P.op("act", lambda e, pa=pa: e.activation(out=MX[:, 192:256], in_=pa[:, 0:64], func=AF.Identity), reads=[ptb], writes=[MX_tb])
            for it in range(7):
                last = it == 6
                ph, phtb = h256.next()
                if not last:
                    P.op("pe", lambda e, ph=ph: e.matmul(ph, lhsT=MT[:], rhs=MX[:], start=True, stop=True), reads=[MT_tb, MX_tb], writes=[phtb])
                    pa, ptb = q128.next()
                    P.op("pe", lambda e, pa=pa: e.matmul(pa, lhsT=MX[:, 0:128], rhs=MT[:], start=True, stop=True), reads=[MT_tb, MX_tb], writes=[ptb])
                    P.op("act", lambda e, ph=ph: e.activation(out=MX[:, 0:128], in_=ph[:, 0:128], func=AF.Identity), reads=[phtb], writes=[MX_tb])
                    P.op("dve", lambda e, ph=ph: e.tensor_tensor(out=MX[:, 128:256], in0=MX[:, 128:256], in1=ph[:, 128:256], op=ALU.add),
                         reads=[phtb, MX_tb], writes=[MX_tb])
                    P.op("act", lambda e, pa=pa: e.activation(out=MT[:], in_=pa, func=AF.Identity), reads=[ptb], writes=[MT_tb])
                else:
                    P.op("pe", lambda e, ph=ph: e.matmul(ph[:, 128:256], lhsT=MT[:], rhs=MX[:, 128:256], start=True, stop=True),
                         reads=[MT_tb, MX_tb], writes=[phtb])
                    P.op("dve", lambda e, ph=ph: e.tensor_tensor(out=MX[:, 128:256], in0=MX[:, 128:256], in1=ph[:, 128:256], op=ALU.add),
                         reads=[phtb, MX_tb], writes=[MX_tb])
            W0, U0 = MX[:, 128:192], MX[:, 192:256]
            P.op("dve", lambda e, p0=p0, cc=cc: e.tensor_scalar(out=DG[:, 0:64], in0=cst[:, 0, p0:p0 + 64], scalar1=GCe[:, cc:cc + 1], scalar2=None, op0=ALU.mult),
                 reads=[cst_tb, fm_tb["at"]], writes=[DG_tb])
            pa, ptb = q128.next()
            P.op("pe", lambda e, pa=pa, tcol=tcol: e.matmul(pa[0:64, 0:64], lhsT=W0, rhs=tokm["bc"][:, tcol], start=True, stop=False),
                 reads=[MX_tb, tok_tb["bc"]], writes=[ptb])
            P.op("pe", lambda e, pa=pa, p0=p0: e.matmul(pa[0:64, 0:64], lhsT=cst[:, 0, p0:p0 + 64], rhs=DG[:, 0:64], start=False, stop=True),
                 reads=[DG_tb, cst_tb], writes=[ptb], acc=True)
            P.op("act", lambda e, pa=pa, hd=hd: e.activation(out=ATb[:, hd, :], in_=pa[0:64, 0:64], func=AF.Identity), reads=[ptb], writes=[AD_tb[hd]])
            pa, ptb = q128.next()
            P.op("pe", lambda e, pa=pa, tcol=tcol: e.matmul(pa[0:64, 0:64], lhsT=tokm["bc"][:, tcol], rhs=U0, start=True, stop=False),
                 reads=[MX_tb, tok_tb["bc"]], writes=[ptb])
            P.op("pe", lambda e, pa=pa, tcol=tcol: e.matmul(pa[0:64, 0:64], lhsT=tokm["kc"][:, tcol], rhs=tokm["v"][:, tcol], start=False, stop=True),
                 reads=[tok_tb["kc"], tok_tb["v"]], writes=[ptb], acc=True)
            P.op("act", lambda e, pa=pa, hd=hd: e.activation(out=Db[:, hd, :], in_=pa[0:64, 0:64], func=AF.Identity), reads=[ptb], writes=[AD_tb[hd]])
            pa, ptb = q128.next()
            P.op("pe", lambda e, pa=pa: e.matmul(pa[0:64, :], lhsT=W0, rhs=NBT[:], start=True, stop=False), reads=[MX_tb, NBT_tb], writes=[ptb])
            P.op("pe", lambda e, pa=pa, p0=p0, cc=cc: e.matmul(pa[0:64, :], lhsT=cst[:, 0, p0:p0 + 64], rhs=F["rt"][:, cc, :], start=False, stop=True),
                 reads=[cst_tb] + allf, writes=[ptb], acc=True)
            oa, otb = ost_s.next()
            P.op("act", lambda e, pa=pa, oa=oa: e.activation(out=oa[0:64, :], in_=pa[0:64, :], func=AF.Identity), reads=[ptb], writes=[otb])
            out_dma(dr["o_YwT"][b, hd], oa[0:64, :], otb)
            pa, ptb = q128.next()
            P.op("pe", lambda e, pa=pa: e.matmul(pa[0:64, :], lhsT=U0, rhs=NBT[:], start=True, stop=False), reads=[MX_tb, NBT_tb], writes=[ptb])
            P.op("pe", lambda e, pa=pa, tcol=tcol: e.matmul(pa[0:64, :], lhsT=tokm["v"][:, tcol], rhs=NKT[:], start=False, stop=True),
                 reads=[tok_tb["v"], NKT_tb], writes=[ptb], acc=True)
            oa, otb = ost_s.next()
            P.op("act", lambda e, pa=pa, oa=oa: e.activation(out=oa[0:64, :], in_=pa[0:64, :], func=AF.Identity), reads=[ptb], writes=[otb])
            out_dma(dr["o_Y0T"][b, hd], oa[0:64, :], otb)
            pa, ptb = q128.next()
            P.op("pe", lambda e, pa=pa, hd=hd: e.matmul(pa[0:64, :], lhsT=ATb[:, hd, :], rhs=PQ[:, hd, :], start=True, stop=True),
                 reads=[AD_tb[hd], PQ_tb[hd]], writes=[ptb])
            pa2, ptb2 = q128.next()
            P.op("pe", lambda e, pa2=pa2, hd=hd: e.matmul(pa2[0:64, 0:64], lhsT=PQ[:, hd, 0:64], rhs=ATb[:, hd, :], start=True, stop=True),
                 reads=[AD_tb[hd], PQ_tb[hd]], writes=[ptb2])
            P.op("act", lambda e, pa=pa, hd=hd: e.activation(out=PQ[:, hd, 0:64], in_=pa[0:64, 0:64], func=AF.Identity), reads=[ptb], writes=[PQ_tb[hd]])
            P.op("dve", lambda e, pa=pa, hd=hd: e.tensor_tensor(out=PQ[:, hd, 64:128], in0=pa[0:64, 64:128], in1=Db[:, hd, :], op=ALU.add),
                 reads=[ptb, AD_tb[hd]], writes=[PQ_tb[hd]])
            P.op("act", lambda e, pa2=pa2, hd=hd: e.activation(out=PTt[:, hd, :], in_=pa2[0:64, 0:64], func=AF.Identity), reads=[ptb2], writes=[PQ_tb[hd]])
        for hd in range(6):
            _head(hd)
        ck(7)
        emit_pq_out(b + 1)

        for m in range(2):
            def ev(pa, ptb, m=m):
                oa, otb = obf_s.next()
                P.op("act", lambda e: e.activation(out=oa[:, 0:128], in_=pa, func=AF.Identity), reads=[ptb], writes=[otb])
                for hh_ in range(2):
                    h = 2 * m + hh_
                    s2 = slice(hh_ * 64, hh_ * 64 + 64)
                    pq, pqtb = q128.next()
                    P.op("pe", lambda e, pq=pq, s2=s2: e.matmul(pq, lhsT=wuk[s2, m, :], rhs=oa[s2, 0:128], start=True, stop=True), reads=[otb, par_tb], writes=[pqtb])
                    ob, obtb = obf_s.next()
                    P.op("act", lambda e, pq=pq, ob=ob: e.activation(out=ob[:, 0:128], in_=pq, func=AF.Identity, scale=0.125), reads=[pqtb], writes=[obtb])
                    out_dma(dr["o_qlat"][:, h, t0:t0 + 128], ob[:, 0:128], obtb)
            projT(C_DQ + m * 128, ev)
        for m in range(3):
            def ev(pa, ptb, m=m):
                oa, otb = obf_s.next()
                P.op("act", lambda e: e.activation(out=oa[:, 0:128], in_=pa, func=AF.Identity), reads=[ptb], writes=[otb])
                out_dma(dr["o_sq"][m * 128:(m + 1) * 128, t0:t0 + 128], oa[:, 0:128], otb)
            projT(C_SQ + m * 128, ev)

        def ev(pa, ptb):
            oa, otb = obf_s.next()
            P.op("act", lambda e: e.activation(out=oa[:, 0:128], in_=pa, func=AF.Identity), reads=[ptb], writes=[otb])
            out_dma(dr["o_sk"][:, t0:t0 + 128], oa[:, 0:128], otb)
        projT(C_SK, ev)
        ck(8)
        for (col0, ncol, off, bank) in ((C_CKV, 452, 0, 0), (C_SV, 128, 452, 1)):
            for kc in range(8):
                P.op("pe", lambda e, kc=kc, col0=col0, ncol=ncol, bank=bank: e.matmul(pbk[bank][:, 0:ncol], lhsT=hT[:, kc, :], rhs=win[:, kc, col0:col0 + ncol],
                                                                                      start=(kc == 0), stop=(kc == 7)),
                     reads=[win_tb, hT_tb], writes=[pT_tb[bank]], acc=True)
            P.op("act", lambda e, ncol=ncol, off=off, bank=bank: e.activation(out=tk[:, off:off + ncol], in_=pbk[bank][:, 0:ncol], func=AF.Identity),
                 reads=[pT_tb[bank]], writes=[tk_tb])
        N = nsc
        nv = lambda fn: P.op("dve", fn, reads=[tk_tb, nsc_tb, par_tb], writes=[nsc_tb])
        nv(lambda e: e.tensor_tensor(out=N["t"][:, 0:128], in0=tk[:, 0:128], in1=tk[:, 0:128], op=ALU.mult))
        nv(lambda e: e.tensor_reduce(out=N["rs"][:, 0:1], in_=N["t"][:, 0:128], axis=AX.X, op=ALU.add))
        P.op("act", lambda e: e.activation(out=N["rs"][:, 0:1], in_=N["rs"][:, 0:1], func=AF.Sqrt, scale=1.0 / 128, bias=eps6[:, 0:1]),
             reads=[nsc_tb, par_tb], writes=[nsc_tb])
        nv(lambda e: e.reciprocal(out=N["rs"][:, 0:1], in_=N["rs"][:, 0:1]))
        nv(lambda e: e.scalar_tensor_tensor(out=N["t"][:, 0:128], in0=tk[:, 0:128], scalar=N["rs"][:, 0:1], in1=bcp[:, 0:128], op0=ALU.mult, op1=ALU.mult))
        oa, otb = obf_s.next()
        P.op("dve", lambda e, oa=oa: e.tensor_copy(out=oa[:, 0:128], in_=N["t"][:, 0:128]), reads=[nsc_tb], writes=[otb])
        out_dma(dr["o_ckv"][t0:t0 + 128, :], oa[:, 0:128], otb)
        pa, ptb = q128.next()
        pab = pa.bitcast(BF16)
        P.op("pe", lambda e, pab=pab, oa=oa: e.transpose(out=pab[:, 0:128], in_=oa[:, 0:128], identity=identb[:]), reads=[otb, cst_tb], writes=[ptb])
        P.op("act", lambda e, pab=pab, oa=oa: e.activation(out=oa[:, 128:256], in_=pab[:, 0:128], func=AF.Identity), reads=[ptb], writes=[otb])
        out_dma(dr["o_ckvT"][:, t0:t0 + 128], oa[:, 128:256], otb)
        nv(lambda e: e.bn_stats(out=N["st"][:], in_=tk[:, 384:448]))
        nv(lambda e: e.bn_aggr(out=N["mv"][:], in_=N["st"][:]))
        P.op("act", lambda e: e.activation(out=N["rs"][:, 1:2], in_=N["mv"][:, 1:2], func=AF.Sqrt, scale=1.0, bias=eps6[:, 1:2]),
             reads=[nsc_tb, par_tb], writes=[nsc_tb])
        nv(lambda e: e.reciprocal(out=N["rs"][:, 1:2], in_=N["rs"][:, 1:2]))
        nv(lambda e: e.tensor_scalar(out=N["t"][:, 128:192], in0=tk[:, 384:448], scalar1=N["mv"][:, 0:1], scalar2=N["rs"][:, 1:2], op0=ALU.subtract, op1=ALU.mult))
        nv(lambda e: e.tensor_tensor(out=N["t"][:, 128:192], in0=N["t"][:, 128:192], in1=bcp[:, 128:192], op=ALU.mult))
        oa, otb = obf_s.next()
        P.op("dve", lambda e, oa=oa: e.tensor_tensor(out=oa[:, 0:64], in0=N["t"][:, 128:192], in1=bcp[:, 192:256], op=ALU.add), reads=[nsc_tb, par_tb], writes=[otb])
        pa, ptb = q128.next()
        pab = pa.bitcast(BF16)
        P.op("pe", lambda e, pab=pab, oa=oa: e.transpose(out=pab[0:64, 0:128], in_=oa[:, 0:64], identity=identb[:]), reads=[otb, cst_tb], writes=[ptb])
        P.op("act", lambda e, pab=pab, oa=oa: e.activation(out=oa[0:64, 128:256], in_=pab[0:64, 0:128], func=AF.Identity), reads=[ptb], writes=[otb])
        out_dma(dr["o_ikT"][:, t0:t0 + 128], oa[0:64, 128:256], otb)
        P.op("act", lambda e: e.activation(out=N["sg"][:], in_=tk[:, 448:452], func=AF.Sign), reads=[tk_tb], writes=[nsc_tb])
        nv(lambda e: e.tensor_tensor(out=N["aiw"][:], in0=tk[:, 448:452], in1=N["sg"][:], op=ALU.mult))
        oa, otb = ost_s.next()
        P.op("dve", lambda e, oa=oa: e.tensor_copy(out=oa[:, 0:4], in_=N["sg"][:]), reads=[nsc_tb], writes=[otb])
        out_dma(dr["o_sgn"][t0:t0 + 128, :], oa[:, 0:4], otb)
        ob, obtb = obf_s.next()
        for h in range(4):
            P.op("dve", lambda e, h=h, ob=ob: e.tensor_scalar(out=ob[:, h * 64:(h + 1) * 64], in0=tk[:, 128 + h * 64:128 + (h + 1) * 64],
                                                              scalar1=N["aiw"][:, h:h + 1], scalar2=1.0 / 16, op0=ALU.mult, op1=ALU.mult),
                 reads=[tk_tb, nsc_tb], writes=[obtb])
        oc, octb = obf_s.next()
        for h in range(4):
            pa, ptb = q128.next()
            pab = pa.bitcast(BF16)
            P.op("pe", lambda e, pab=pab, ob=ob, h=h: e.transpose(out=pab[0:64, 0:128], in_=ob[:, h * 64:(h + 1) * 64], identity=identb[:]),
                 reads=[obtb, cst_tb], writes=[ptb])
            P.op("act", lambda e, pab=pab, oc=oc, h=h: e.activation(out=oc[0:64, h * 128:(h + 1) * 128], in_=pab[0:64, 0:128], func=AF.Identity),
                 reads=[ptb], writes=[octb])
        out_dma(dr["o_iqs"][:, :, t0:t0 + 128], oc[0:64, :].rearrange("p (h t) -> p h t", h=4), octb)
        od, odtb = obf_s.next()
        P.op("dve", lambda e, od=od: e.tensor_copy(out=od[:, 0:128], in_=tk[:, 452:580]), reads=[tk_tb], writes=[odtb])
        out_dma(dr["o_sv"][t0:t0 + 128, :], od[:, 0:128], odtb)


GN_EPS = 64e-5
NITER = 14
TOPK = 256


def emit_k2(P, nc, NBQ, dr, parts=("rwkv", "swa", "dsa")):
    NT = NBQ * 128
    NKT = 8 * NBQ
    seg_of = lambda j: (8 * j) // NBQ
    out_tb = dr["out_tb"]
    pb = [P.psum(f"k2pb{i}", [128, 512], F32) for i in range(8)]
    bank = [P.tb(f"k2bank{i}") for i in range(8)]
    cst = P.sbuf("cst2", [128, 128], F32)
    cst_tb = P.tb("cst2")
    P.op("sp", lambda e: e.dma_start(out=cst[:], in_=dr["ident"]), writes=[cst_tb], dma=True)
    identb = P.sbuf("identb2", [128, 128], BF16)
    onesb = P.sbuf("onesb", [128, 64], BF16)
    o64 = P.sbuf("o64", [64, 64], F32)
    P.op("dve", lambda e: e.tensor_copy(out=identb[:], in_=cst[:]), reads=[cst_tb], writes=[cst_tb])
    P.op("pool", lambda e: e.memset(onesb[:], 1.0), writes=[cst_tb])
    P.op("pool", lambda e: e.memset(o64[:], 1.0 / 64), writes=[cst_tb])
    obuf = [P.sbuf(f"k2o{i}", [64, 512], BF16) for i in range(4)]
    obs = Slots(P, [o[:] for o in obuf])

    def out_mix(head, j, src, stb):
        P.op("sp", lambda e: e.dma_start(out=dr["mixT"][head, :, j * 128:(j + 1) * 128], in_=src), reads=[stb], writes=[out_tb], dma=True)

    if "rwkv" in parts:
      with P.scope():
        rp = P.sbuf("rp", [64, 16], F32)
        rp_tb = P.tb("rp")
        P.op("sp", lambda e: e.dma_start(out=rp[:], in_=dr["rwkv_ln"]), writes=[rp_tb], dma=True)
        epsg = P.sbuf("epsg", [64, 1], F32)
        P.op("pool", lambda e: e.memset(epsg[:], GN_EPS), writes=[rp_tb])
        segP = P.sbuf("segP", [64, 8, 6, 64], F32)
        segQ = P.sbuf("segQ", [64, 8, 6, 64], F32)
        seg_tb = P.tb("seg")
        P.op("sp", lambda e: e.dma_start(out=segP[:], in_=dr["segPT"].rearrange("s h a b -> a s h b")), writes=[seg_tb], dma=True)
        P.op("sp", lambda e: e.dma_start(out=segQ[:], in_=dr["segQ"].rearrange("s h a b -> a s h b")), writes=[seg_tb], dma=True)
        St = P.sbuf("St", [64, 8, 6, 64], F32)
        St_tb = [P.tb(f"St{s}") for s in range(8)]
        P.op("pool", lambda e: e.memset(St[:, 0, :, :], 0.0), writes=[St_tb[0]])
        for s in range(7):
            for hd in range(6):
                P.op("pe", lambda e, s=s, hd=hd: e.matmul(pb[0][0:64, hd * 64:(hd + 1) * 64], lhsT=segP[:, s, hd, :], rhs=St[:, s, hd, :],
                                                          start=True, stop=True), reads=[seg_tb, St_tb[s]], writes=[bank[0]], acc=True)
            P.op("dve", lambda e, s=s: e.tensor_tensor(out=St[:, s + 1, :, :].rearrange("p h v -> p (h v)"), in0=pb[0][0:64, 0:384],
                                                       in1=segQ[:, s, :, :].rearrange("p h v -> p (h v)"), op=ALU.add),
                 reads=[bank[0], seg_tb], writes=[St_tb[s + 1]])
        rin = [{n: P.sbuf(f"r_{n}{i}", [64, 6, w], F32) for n, w in (("yw", 128), ("y0", 128), ("pt", 64), ("q", 64), ("bon", 128), ("g", 128))} for i in range(2)]
        rin_tb = [P.tb(f"rin{i}") for i in range(2)]
        Sb = P.sbuf("Sb", [64, 6, 64], F32)
        Sb_tb = P.tb("Sb")
        yy = P.sbuf("yy", [64, 6, 128], F32)
        cen = P.sbuf("cen", [64, 6, 128], F32)
        sq = P.sbuf("sqr", [64, 6, 128], F32)
        rsd = P.sbuf("rsd", [64, 6, 128], F32)
        ww_tb = P.tb("rwkvwork")
        for j in range(NBQ):
            I = rin[j % 2]
            itb = rin_tb[j % 2]
            for n, src in (("yw", dr["YwT"][j]), ("y0", dr["Y0T"][j]), ("pt", dr["PbT"][j]), ("q", dr["Qb"][j])):
                P.op("sp", lambda e, n=n, src=src, I=I: e.dma_start(out=I[n][:], in_=src.rearrange("h a b -> a h b")), writes=[itb], dma=True)
            P.op("sp", lambda e, I=I, j=j: e.dma_start(out=I["bon"][:], in_=dr["bon"][:, :, j * 128:(j + 1) * 128]), writes=[itb], dma=True)
            P.op("sp", lambda e, I=I, j=j: e.dma_start(out=I["g"][:], in_=dr["g"][:, :, j * 128:(j + 1) * 128]), writes=[itb], dma=True)
            s = seg_of(j)
            for hd in range(6):
                P.op("pe", lambda e, hd=hd, I=I, s=s: e.matmul(pb[0][0:64, hd * 64:(hd + 1) * 64], lhsT=I["pt"][:, hd, :], rhs=St[:, s, hd, :], start=True, stop=True),
                     reads=[itb, St_tb[s]], writes=[bank[0]], acc=True)
            P.op("dve", lambda e, I=I: e.tensor_tensor(out=Sb[:].rearrange("p h v -> p (h v)"), in0=pb[0][0:64, 0:384],
                                                       in1=I["q"][:].rearrange("p h v -> p (h v)"), op=ALU.add), reads=[bank[0], itb], writes=[Sb_tb])
            for half in range(2):
                bk = 1 + half
                for q in range(3):
                    hd = half * 3 + q
                    P.op("pe", lambda e, hd=hd, q=q, bk=bk, I=I: e.matmul(pb[bk][0:64, q * 128:(q + 1) * 128], lhsT=Sb[:, hd, :], rhs=I["yw"][:, hd, :], start=True, stop=True),
                         reads=[Sb_tb, itb], writes=[bank[bk]], acc=True)
                hs = slice(half * 3, half * 3 + 3)
                f3 = lambda t, hs=hs: t[:, hs, :].rearrange("p h t -> p (h t)")
                P.op("dve", lambda e, bk=bk, I=I, f3=f3: e.tensor_tensor(out=f3(yy), in0=pb[bk][0:64, 0:384], in1=f3(I["y0"]), op=ALU.add),
                     reads=[bank[bk], itb], writes=[ww_tb])
                P.op("pe", lambda e, bk=bk, f3=f3: e.matmul(pb[bk][0:64, 0:384], lhsT=o64[:], rhs=f3(yy), start=True, stop=True), reads=[ww_tb, cst_tb], writes=[bank[bk]])
                P.op("dve", lambda e, bk=bk, f3=f3: e.tensor_tensor(out=f3(cen), in0=f3(yy), in1=pb[bk][0:64, 0:384], op=ALU.subtract), reads=[bank[bk], ww_tb], writes=[ww_tb])
                P.op("pool", lambda e, f3=f3: e.tensor_tensor(out=f3(sq), in0=f3(cen), in1=f3(cen), op=ALU.mult), reads=[ww_tb], writes=[ww_tb])
                P.op("pe", lambda e, bk=bk, f3=f3: e.matmul(pb[bk][0:64, 0:384], lhsT=o64[:], rhs=f3(sq), start=True, stop=True), reads=[ww_tb, cst_tb], writes=[bank[bk]])
                P.op("act", lambda e, bk=bk, f3=f3: e.activation(out=f3(rsd), in_=pb[bk][0:64, 0:384], func=AF.Sqrt, bias=epsg[:, 0:1], scale=1.0),
                     reads=[bank[bk], rp_tb], writes=[ww_tb])
                P.op("dve", lambda e, f3=f3: e.reciprocal(out=f3(rsd), in_=f3(rsd)), reads=[ww_tb], writes=[ww_tb])
                P.op("dve", lambda e, f3=f3: e.tensor_tensor(out=f3(cen), in0=f3(cen), in1=f3(rsd), op=ALU.mult), reads=[ww_tb], writes=[ww_tb])
                ob, obtb = obs.next()
                for q in range(3):
                    hd = half * 3 + q
                    P.op("dve", lambda e, hd=hd: e.tensor_scalar(out=cen[:, hd, :], in0=cen[:, hd, :], scalar1=rp[:, hd:hd + 1], scalar2=rp[:, 6 + hd:7 + hd],
                                                                 op0=ALU.mult, op1=ALU.add), reads=[ww_tb, rp_tb], writes=[ww_tb])
                P.op("pool", lambda e, f3=f3, I=I: e.tensor_tensor(out=f3(cen), in0=f3(cen), in1=f3(I["bon"]), op=ALU.add), reads=[ww_tb, itb], writes=[ww_tb])
                P.op("pool", lambda e, f3=f3, I=I, ob=ob: e.tensor_tensor(out=ob[:, 0:384], in0=f3(cen), in1=f3(I["g"]), op=ALU.mult), reads=[ww_tb, itb], writes=[obtb])
                P.op("sp", lambda e, ob=ob, j=j, half=half: e.dma_start(out=dr["mixT"][half * 3:half * 3 + 3, :, j * 128:(j + 1) * 128].rearrange("h p t -> p h t"),
                                                                          in_=ob[:, 0:384].rearrange("p (h t) -> p h t", h=3)), reads=[obtb], writes=[out_tb], dma=True)

    if "swa" in parts:
      with P.scope():
        swb = P.sbuf("swb", [128, 2, 6, 128], F32)
        swbf = P.sbuf("swbf", [128, 6, 128], F32)
        sw_tb = P.tb("swc")
        P.op("sp", lambda e: e.dma_start(out=swb[:], in_=dr["swb"]), writes=[sw_tb], dma=True)
        P.op("sp", lambda e: e.dma_start(out=swbf[:], in_=dr["swb_first"]), writes=[sw_tb], dma=True)
        es = P.sbuf("esink", [64, 6], F32)
        P.op("sp", lambda e: e.dma_start(out=es[:], in_=dr["sink_b"]), writes=[sw_tb], dma=True)
        P.op("act", lambda e: e.activation(out=es[:], in_=es[:], func=AF.Exp), reads=[sw_tb], writes=[sw_tb])
        sin = [{"q": P.sbuf(f"s_q{i}", [64, 6, 128], BF16), "k": P.sbuf(f"s_k{i}", [64, 2, 256], BF16), "v": P.sbuf(f"s_v{i}", [128, 2, 128], BF16)} for i in range(2)]
        sin_tb = [P.tb(f"sin{i}") for i in range(2)]
        stmp = P.sbuf("stmp", [128, 384], F32)
        sE = [P.sbuf(f"sE{i}", [128, 384], BF16) for i in range(2)]
        sE_tb = [P.tb(), P.tb()]
        st_tb = P.tb("stmp")
        srec = P.sbuf("srec", [64, 384], F32)
        for j in range(NBQ):
            I = sin[j % 2]
            itb = sin_tb[j % 2]
            P.op("sp", lambda e, I=I, j=j: e.dma_start(out=I["q"][:], in_=dr["sq"][:, :, j * 128:(j + 1) * 128]), writes=[itb], dma=True)
            P.op("sp", lambda e, I=I, j=j: e.dma_start(out=I["k"][:], in_=dr["sk2"][:, :, j, :]), writes=[itb], dma=True)
            P.op("sp", lambda e, I=I, j=j: e.dma_start(out=I["v"][:], in_=dr["sv2"][j].rearrange("k p c -> p k c")), writes=[itb], dma=True)
            for g in range(2):
                bn, bd = 5, 6
                for kt2 in range(2):
                    bs = 3 + kt2
                    P.op("pe", lambda e, g=g, kt2=kt2, bs=bs, I=I: e.matmul(pb[bs][:, 0:384], lhsT=I["k"][:, g, kt2 * 128:(kt2 + 1) * 128],
                                                                            rhs=I["q"][:, 3 * g:3 * g + 3, :], start=True, stop=True),
                         reads=[itb], writes=[bank[bs]])
                    btab = swbf[:, 3 * g:3 * g + 3, :] if (j == 0 and kt2 == 0) else swb[:, kt2, 3 * g:3 * g + 3, :]
                    P.op("dve", lambda e, bs=bs, btab=btab: e.scalar_tensor_tensor(out=stmp[:].rearrange("p (h t) -> p h t", h=3), in0=pb[bs][:, 0:384].rearrange("p (h t) -> p h t", h=3),
                                                                                  scalar=0.125, in1=btab, op0=ALU.mult, op1=ALU.add),
                         reads=[bank[bs], sw_tb], writes=[st_tb])
                    E, etb = sE[kt2], sE_tb[kt2]
                    P.op("act", lambda e, E=E: e.activation(out=E[:], in_=stmp[:], func=AF.Exp), reads=[st_tb], writes=[etb])
                    P.op("pe", lambda e, E=E, g=g, kt2=kt2, I=I: e.matmul(pb[bn][0:64, 0:384], lhsT=I["v"][:, kt2, g * 64:(g + 1) * 64], rhs=E[:],
                                                                          start=(kt2 == 0), stop=(kt2 == 1)), reads=[etb, itb], writes=[bank[bn]], acc=True)
                    P.op("pe", lambda e, E=E, kt2=kt2: e.matmul(pb[bd][0:64, 0:384], lhsT=onesb[:], rhs=E[:], start=(kt2 == 0), stop=(kt2 == 1)),
                         reads=[etb, cst_tb], writes=[bank[bd]], acc=True)
                for q in range(3):
                    P.op("dve", lambda e, q=q, g=g: e.tensor_scalar(out=srec[:, q * 128:(q + 1) * 128], in0=pb[bd][0:64, q * 128:(q + 1) * 128],
                                                                    scalar1=es[:, 3 * g + q:3 * g + q + 1], scalar2=None, op0=ALU.add),
                         reads=[bank[bd], sw_tb], writes=[st_tb])
                P.op("dve", lambda e: e.reciprocal(out=srec[:], in_=srec[:]), reads=[st_tb], writes=[st_tb])
                ob, obtb = obs.next()
                P.op("dve", lambda e, ob=ob: e.tensor_tensor(out=ob[:, 0:384], in0=pb[bn][0:64, 0:384], in1=srec[:], op=ALU.mult), reads=[bank[bn], st_tb], writes=[obtb])
                P.op("sp", lambda e, ob=ob, j=j, g=g: e.dma_start(out=dr["mixT"][10 + 3 * g:13 + 3 * g, :, j * 128:(j + 1) * 128].rearrange("h p t -> p h t"),
                                                                    in_=ob[:, 0:384].rearrange("p (h t) -> p h t", h=3)), reads=[obtb], writes=[out_tb], dma=True)

    if "dsa" in parts:
      with P.scope():
        ckvT = P.sbuf("ckvT", [128, NKT * 128], BF16)
        ckv = P.sbuf("ckv", [128, NKT, 128], BF16)
        key_tb = P.tb("keys")
        for c in range(0, NKT, 16):
            P.op("sp", lambda e, c=c: e.dma_start(out=ckvT[:, c * 128:(c + 16) * 128], in_=dr["ckvT"][:, c * 128:(c + 16) * 128]), writes=[key_tb], dma=True)
            P.op("sp", lambda e, c=c: e.dma_start(out=ckv[:, c:c + 16, :], in_=dr["ckv"][c * 128:(c + 16) * 128, :].rearrange("(k p) r -> p k r", p=128)),
                 writes=[key_tb], dma=True)
        ikc = [P.sbuf(f"ikc{i}", [64, 512], BF16) for i in range(3)]
        ikc_tb = [P.tb() for _ in range(3)]
        wuv_f = P.sbuf("wuv_f", [128, 4, 64], F32)
        wuv = P.sbuf("wuv", [128, 4, 64], BF16)
        dpar_tb = P.tb("dpar")
        P.op("sp", lambda e: e.dma_start(out=wuv_f[:], in_=dr["wuv"].rearrange("h r d -> r h d")), writes=[dpar_tb], dma=True)
        P.op("dve", lambda e: e.tensor_copy(out=wuv[:], in_=wuv_f[:]), reads=[dpar_tb], writes=[dpar_tb])
        dmask = P.sbuf("dmask", [128, 1024], F32)
        ab = P.sbuf("ab", [128, 4, 128], F32)
        P.op("sp", lambda e: e.dma_start(out=dmask[:], in_=dr["dmask"]), writes=[dpar_tb], dma=True)
        P.op("sp", lambda e: e.dma_start(out=ab[:], in_=dr["ab"]), writes=[dpar_tb], dma=True)
        Isc = P.sbuf("Isc", [128, NKT * 128], F32)
        Isc_tb = P.tb("Isc")
        msk = P.sbuf("msk", [128, NKT * 128], BF16)
        msk_tb = P.tb("msk")
        qin = [{"ql": P.sbuf(f"d_ql{i}", [128, 4, 128], BF16), "iq": P.sbuf(f"d_iq{i}", [64, 4, 128], BF16), "sg": P.sbuf(f"d_sg{i}", [128, 4], F32)} for i in range(2)]
        qin_tb = [P.tb(), P.tb()]
        rl = [P.sbuf(f"rl{i}", [128, 512], F32) for i in range(2)]
        rl_tb = [P.tb(), P.tb()]
        bs_ = {n: P.sbuf("bs_" + n, [128, 1], F32) for n in ("lo", "hi", "mid", "cnt", "ge", "d")}
        bs_tb = P.tb("bisect")
        Eb = [P.sbuf(f"Eb{i}", [128, 512], BF16) for i in range(2)]
        Eb_tb = [[P.tb() for _ in range(4)] for _ in range(2)]
        PTb = [P.sbuf(f"PTb{i}", [128, 512], BF16) for i in range(2)]
        PT_tb = [P.tb(), P.tb()]
        olat = P.sbuf("olat", [128, 512], BF16)
        drec = P.sbuf("drec", [64, 512], F32)
        fin_tb = P.tb("dsafin")
        junk = P.sbuf("junk8", [128, NKT * 128], mybir.dt.uint8)
        junk_tb = P.tb("junk8")
        B = bs_

        def emitA(j):
            nkt = 8 * (j + 1)
            n = nkt * 128
            Q = qin[j % 2]
            qtb = qin_tb[j % 2]
            P.op("sp", lambda e: e.dma_start(out=Q["ql"][:], in_=dr["qlat"][:, :, j * 128:(j + 1) * 128]), writes=[qtb], dma=True)
            P.op("sp", lambda e: e.dma_start(out=Q["iq"][:], in_=dr["iqs"][:, :, j * 128:(j + 1) * 128]), writes=[qtb], dma=True)
            P.op("sp", lambda e: e.dma_start(out=Q["sg"][:], in_=dr["sgn"][j * 128:(j + 1) * 128, :]), writes=[qtb], dma=True)
            for c4 in range(nkt // 4):
                ii = c4 % 3
                P.op("sp", lambda e, c4=c4, ii=ii: e.dma_start(out=ikc[ii][:], in_=dr["ikT"][:, c4 * 512:(c4 + 1) * 512]), writes=[ikc_tb[ii]], dma=True)
                for h in range(4):
                    bk = (c4 * 4 + h) % 2
                    P.op("pe", lambda e, h=h, bk=bk, ii=ii: e.matmul(pb[bk][:], lhsT=Q["iq"][:, h, :], rhs=ikc[ii][:], start=True, stop=True),
                         reads=[qtb, ikc_tb[ii]], writes=[bank[bk]])
                    P.op("act", lambda e, bk=bk: e.activation(out=rl[bk][:], in_=pb[bk][:], func=AF.Relu), reads=[bank[bk]], writes=[rl_tb[bk]])
                    dst = Isc[:, c4 * 512:(c4 + 1) * 512]
                    if h == 0:
                        P.op("dve", lambda e, bk=bk, dst=dst: e.tensor_scalar(out=dst, in0=rl[bk][:], scalar1=Q["sg"][:, 0:1], scalar2=None, op0=ALU.mult),
                             reads=[rl_tb[bk], qtb], writes=[Isc_tb])
                    else:
                        P.op("dve", lambda e, bk=bk, dst=dst, h=h: e.scalar_tensor_tensor(out=dst, in0=rl[bk][:], scalar=Q["sg"][:, h:h + 1], in1=dst,
                                                                                         op0=ALU.mult, op1=ALU.add), reads=[rl_tb[bk], qtb, Isc_tb], writes=[Isc_tb])
            P.op("dve", lambda e: e.tensor_tensor(out=Isc[:, n - 1024:n], in0=Isc[:, n - 1024:n], in1=dmask[:], op=ALU.add), reads=[Isc_tb, dpar_tb], writes=[Isc_tb])
            bv = lambda fn: P.op("dve", fn, reads=[bs_tb, Isc_tb], writes=[bs_tb])
            bv(lambda e: e.memset(B["lo"][:], -64.0))
            bv(lambda e: e.tensor_reduce(out=B["hi"][:], in_=Isc[:, 0:n], axis=AX.X, op=ALU.max))
            bv(lambda e: e.tensor_scalar(out=B["d"][:], in0=B["hi"][:], scalar1=64.0, scalar2=None, op0=ALU.add))

        def bisect_iter(j, it):
            n = 8 * (j + 1) * 128
            ck_ = 0.5 ** (it + 1)
            bv = lambda fn: P.op("dve", fn, reads=[bs_tb, Isc_tb], writes=[bs_tb])
            bv(lambda e: e.scalar_tensor_tensor(out=B["mid"][:], in0=B["d"][:], scalar=ck_, in1=B["lo"][:], op0=ALU.mult, op1=ALU.add))
            P.op("dve", lambda e: e.tensor_scalar(out=junk[:, 0:n], in0=Isc[:, 0:n], scalar1=B["mid"][:, 0:1], scalar2=0.0, op0=ALU.is_ge, op1=ALU.add,
                                                  accum_out=B["cnt"][:]), reads=[bs_tb, Isc_tb], writes=[bs_tb, junk_tb])
            bv(lambda e: e.tensor_scalar(out=B["ge"][:], in0=B["cnt"][:], scalar1=float(TOPK) - 0.5, scalar2=ck_, op0=ALU.is_ge, op1=ALU.mult))
            bv(lambda e: e.scalar_tensor_tensor(out=B["lo"][:], in0=B["ge"][:], scalar=B["d"][:, 0:1], in1=B["lo"][:], op0=ALU.mult, op1=ALU.add))

        def final_mask(j):
            n = 8 * (j + 1) * 128
            P.op("dve", lambda e: e.tensor_scalar(out=msk[:, 0:n], in0=Isc[:, 0:n], scalar1=B["lo"][:, 0:1], scalar2=None, op0=ALU.is_ge),
                 reads=[bs_tb, Isc_tb], writes=[msk_tb])

        def tileB(j, kt):
            nkt = 8 * (j + 1)
            Q = qin[j % 2]
            qtb = qin_tb[j % 2]
            i2 = kt % 2
            dl = nkt - 1 - kt
            pmT = pb[2].bitcast(BF16)
            P.op("pe", lambda e: e.transpose(out=pmT[:, 0:128], in_=msk[:, kt * 128:(kt + 1) * 128], identity=identb[:]),
                 reads=[msk_tb, cst_tb], writes=[bank[2]])
            bS = 3 + i2
            P.op("pe", lambda e: e.matmul(pb[bS][:], lhsT=ckvT[:, kt * 128:(kt + 1) * 128], rhs=Q["ql"][:], start=True, stop=True),
                 reads=[key_tb, qtb], writes=[bank[bS]])
            for h in range(4):
                P.op("act", lambda e, h=h: e.activation(out=Eb[i2][:, h * 128:(h + 1) * 128], in_=pb[bS][:, h * 128:(h + 1) * 128], func=AF.Exp,
                                                        bias=ab[:, h, dl:dl + 1], scale=1.0), reads=[bank[bS], dpar_tb], writes=[Eb_tb[i2][h]])
            P.op("dve", lambda e: e.tensor_tensor(out=PTb[i2][:].rearrange("p (h t) -> p h t", h=4), in0=Eb[i2][:].rearrange("p (h t) -> p h t", h=4),
                                                  in1=pmT[:, 0:128].unsqueeze(1).to_broadcast([128, 4, 128]), op=ALU.mult),
                 reads=Eb_tb[i2] + [bank[2]], writes=[PT_tb[i2]])
            P.op("pe", lambda e: e.matmul(pb[5][:], lhsT=ckv[:, kt, :], rhs=PTb[i2][:], start=(kt == 0), stop=(kt == nkt - 1)),
                 reads=[key_tb, PT_tb[i2]], writes=[bank[5]], acc=True)
            P.op("pe", lambda e: e.matmul(pb[6][0:64, :], lhsT=onesb[:], rhs=PTb[i2][:], start=(kt == 0), stop=(kt == nkt - 1)),
                 reads=[cst_tb, PT_tb[i2]], writes=[bank[6]], acc=True)

        def finB(j):
            P.op("act", lambda e: e.activation(out=olat[:], in_=pb[5][:], func=AF.Identity), reads=[bank[5]], writes=[fin_tb])
            P.op("dve", lambda e: e.reciprocal(out=drec[:], in_=pb[6][0:64, :]), reads=[bank[6]], writes=[fin_tb])
            for h in range(4):
                P.op("pe", lambda e, h=h: e.matmul(pb[7][0:64, h * 128:(h + 1) * 128], lhsT=wuv[:, h, :], rhs=olat[:, h * 128:(h + 1) * 128], start=True, stop=True),
                     reads=[fin_tb, dpar_tb], writes=[bank[7]], acc=True)
            ob, obtb = obs.next()
            P.op("dve", lambda e: e.tensor_tensor(out=ob[:, 0:512], in0=pb[7][0:64, :], in1=drec[:], op=ALU.mult), reads=[bank[7], fin_tb], writes=[obtb])
            P.op("sp", lambda e: e.dma_start(out=dr["mixT"][6:10, :, j * 128:(j + 1) * 128].rearrange("h p t -> p h t"),
                                             in_=ob[:, 0:512].rearrange("p (h t) -> p h t", h=4)), reads=[obtb], writes=[out_tb], dma=True)

        emitA(0)
        for it in range(NITER):
            bisect_iter(0, it)
        final_mask(0)
        for j in range(NBQ):
            tiles = list(range(8 * (j + 1)))
            if j + 1 < NBQ:
                emitA(j + 1)
                per = -(-len(tiles) // NITER)
                for it in range(NITER):
                    bisect_iter(j + 1, it)
                    for kt in tiles[it * per:(it + 1) * per]:
                        tileB(j, kt)
                for kt in tiles[NITER * per:]:
                    tileB(j, kt)
            else:
                for kt in tiles:
                    tileB(j, kt)
            finB(j)
            if j + 1 < NBQ:
                final_mask(j + 1)


def emit_ffn(P, nc, NT, exp_ids, dr, ident, ident_tb):
    PT = min(1024, NT)
    NB = PT // 128
    npass = NT // PT
    NX = len(exp_ids)
    TW = min(512, PT)
    NTT = PT // TW
    BPT = TW // 128
    es = P.es
    wo_tb = P.tb("wo")
    stg = [P.sbuf(f"stg{i}", [128, 2048], F32) for i in range(3)]
    stg_tb = [P.tb(f"stg{i}") for i in range(3)]
    rw = P.sbuf("rw", [128, 8, NE], F32)
    rw_tb = P.tb("rw")
    rbias = P.sbuf("rbias", [128, NE], F32)
    bc = [P.sbuf(f"bc{i}", [128, D], F32) for i in range(3)]
    bc_tb = [P.tb(f"bc{i}") for i in range(3)]
    modT = P.sbuf("modT", [128, 48], F32)
    modT_tb = P.tb("modT")
    eps_t = P.sbuf("eps_t", [128, 1], F32)
    xm = P.sbuf("xm", [128, NB, D], F32)
    xm_tb = [P.tb(f"xm{b}") for b in range(NB)]
    yacc = P.sbuf("yacc", [128, NB, D], F32)
    ya_tb = [P.tb(f"ya{b}") for b in range(NB)]
    h2T = P.sbuf("h2T", [128, 8, PT], BF16)
    h2_tb = [P.tb(f"h2{b}") for b in range(NB)]
    h2f_tb = P.tb("h2f")
    gateT = P.sbuf("gateT", [65, PT], BF16)
    gT_tb = [P.tb(f"gT{b}") for b in range(NB)]
    xr = [P.sbuf(f"xr{i}", [128, D], F32) for i in range(2)]
    xr_tb = [P.tb(f"xr{i}") for i in range(2)]
    zt = P.sbuf("zt", [128, D], F32)
    zt_tb = P.tb("zt")
    lnsc = {"st": P.sbuf("ln_st", [128, 2, 6], F32), "mv": P.sbuf("ln_mv", [128, 2], F32),
            "rs": P.sbuf("ln_rs", [128, 1], F32), "xn": P.sbuf("ln_xn", [128, D], F32),
            "tb": P.tb("ln_s"), "xn_tb": P.tb("ln_xn")}
    rt_tb = P.tb("rt")
    wb_tb = [{k: P.tb(f"{k}b{i}") for k in ("w1", "w3", "w2")} for i in range(2)]
    selt_tb = P.tb("selt")
    Gb_tb = P.tb("Gb")
    ssb_tb = [P.tb(f"ssb{i}") for i in range(2)]
    gg_tb = [[P.tb(f"gg{fc}_{tt}") for tt in range(NTT)] for fc in range(2)]
    pb = [P.psum(f"pb{i}", [128, 512], F32) for i in range(8)]
    pb_tb = [P.tb(f"pb{i}") for i in range(8)]

    P.op("pool", lambda e: e.memset(eps_t[:], LN_EPS), writes=[modT_tb])
    P.op("sp", lambda e: e.dma_start(out=modT[:], in_=dr["modT"]), writes=[modT_tb], dma=True)
    P.op("dve", lambda e: e.tensor_scalar(out=modT[:, 32:40], in0=modT[:, 32:40], scalar1=1.0, scalar2=None,
                                          op0=ALU.add), reads=[modT_tb], writes=[modT_tb])
    P.op("sp", lambda e: e.dma_start(out=rw[:], in_=dr["router_w"].rearrange("(kc p) n -> p kc n", p=128)),
         writes=[rw_tb], dma=True)
    P.op("sp", lambda e: e.dma_start(out=rbias[:], in_=dr["rbias_b"]), writes=[rw_tb], dma=True)
    def load_bc(i, src):
        P.op("sp", lambda e: e.dma_start(out=bc[i][:], in_=src), writes=[bc_tb[i]], dma=True)

    def _pass(ps):
        t0 = ps * PT
        load_bc(0, dr["modb"][:, 2 * D:3 * D])
        load_bc(1, dr["lnp"][:, 0, :])
        load_bc(2, dr["lnp"][:, 1, :])
        P.op("pool", lambda e: e.memset(gateT[64:65, :], 1.0), writes=gT_tb)
        _sc1 = P.scope()
        _sc1.__enter__()
        wo = P.sbuf(f"p{ps}_wo", [64, 16, D], BF16)
        mixb = [P.sbuf(f"p{ps}_mixb{i}", [64, 16, 128], BF16) for i in range(2)]
        mixb_tb = [P.tb(f"mixb{i}") for i in range(2)]
        h2f = P.sbuf(f"p{ps}_h2f", [128, 8, 128], F32)
        rt = {k: P.sbuf(f"p{ps}_rt_" + k, [128, n], F32) for k, n in
              [("sc", 64), ("sel", 64), ("eq", 64), ("sel2", 64), ("m1", 8), ("m2", 8), ("grp", 8), ("t8", 8),
               ("gm", 8), ("g4", 8), ("selm", 64), ("em", 64), ("gt", 64), ("den", 1), ("gate", 64)]}
        for c in range(16):
            s_ = c % 3
            P.op("sp", lambda e, c=c, s_=s_: e.dma_start(out=stg[s_][0:64, 0:D], in_=dr["w_out"][c * 64:(c + 1) * 64, :]),
                 writes=[stg_tb[s_]], dma=True)
            P.op("pool", lambda e, c=c, s_=s_: e.tensor_copy(out=wo[:, c, :], in_=stg[s_][0:64, 0:D]),
                 reads=[stg_tb[s_]], writes=[wo_tb])


        for b in range(NB):
            tk = t0 + b * 128
            xi = b % 2
            P.op("sp", lambda e, xi=xi, tk=tk: e.dma_start(out=xr[xi][:], in_=dr["xres"][tk:tk + 128, :]),
                 writes=[xr_tb[xi]], dma=True)
            P.op("sp", lambda e, xi=xi, tk=tk: e.dma_start(out=mixb[xi][:], in_=dr["mixT"][:, :, tk:tk + 128].rearrange("h p t -> p h t")),
                 writes=[mixb_tb[xi]], dma=True)
            for hlf in range(2):
                for c in range(16):
                    P.op("pe", lambda e, c=c, hlf=hlf, xi=xi: e.matmul(
                        pb[hlf][:], lhsT=mixb[xi][:, c, :], rhs=wo[:, c, hlf * 512:(hlf + 1) * 512],
                        start=(c == 0), stop=(c == 15)), reads=[mixb_tb[xi], wo_tb], writes=[pb_tb[hlf]], acc=True)
            for hlf in range(2):
                P.op("dve", lambda e, hlf=hlf: e.tensor_tensor(out=zt[:, hlf * 512:(hlf + 1) * 512], in0=pb[hlf][:],
                                                               in1=bc[0][:, hlf * 512:(hlf + 1) * 512], op=ALU.mult),
                     reads=[pb_tb[hlf], bc_tb[0]], writes=[zt_tb])
            P.op("dve", lambda e, xi=xi: e.scalar_tensor_tensor(out=zt[:], in0=xr[xi][:], scalar=ALPHA, in1=zt[:],
                                                                op0=ALU.mult, op1=ALU.add),
                 reads=[xr_tb[xi], zt_tb], writes=[zt_tb])
            emit_layernorm(P, zt[:], zt_tb, xm[:, b, :], xm_tb[b], bc[1][:], bc[2][:], [bc_tb[1], bc_tb[2]], lnsc, eps_t, "m")
            for kc in range(8):
                bank = 2 + kc // 4
                P.op("pe", lambda e, kc=kc, bank=bank, b=b: e.transpose(
                    out=pb[bank][:, (kc % 4) * 128:(kc % 4 + 1) * 128], in_=xm[:, b, kc * 128:(kc + 1) * 128],
                    identity=ident[:]), reads=[xm_tb[b], ident_tb], writes=[pb_tb[bank]], acc=True)
            for kc in range(8):
                bank = 2 + kc // 4
                src = pb[bank][:, (kc % 4) * 128:(kc % 4 + 1) * 128]
                P.op("act", lambda e, kc=kc, src=src, b=b: e.activation(
                    out=h2T[:, kc, b * 128:(b + 1) * 128], in_=src, func=AF.Identity,
                    bias=modT[:, 24 + kc:25 + kc], scale=modT[:, 32 + kc:33 + kc]),
                    reads=[pb_tb[bank], modT_tb], writes=[h2_tb[b]])
                P.op("act", lambda e, kc=kc, src=src: e.activation(
                    out=h2f[:, kc, :], in_=src, func=AF.Identity,
                    bias=modT[:, 24 + kc:25 + kc], scale=modT[:, 32 + kc:33 + kc]),
                    reads=[pb_tb[bank], modT_tb], writes=[h2f_tb])
            for kc in range(8):
                P.op("pe", lambda e, kc=kc: e.matmul(pb[4][:, 0:NE], lhsT=h2f[:, kc, :], rhs=rw[:, kc, :],
                                                     start=(kc == 0), stop=(kc == 7)),
                     reads=[h2f_tb, rw_tb], writes=[pb_tb[4]], acc=True)
            R = rt
            P.op("act", lambda e: e.activation(out=R["sc"][:], in_=pb[4][:, 0:NE], func=AF.Sigmoid),
                 reads=[pb_tb[4]], writes=[rt_tb])
            dv = lambda fn: P.op("dve", fn, reads=[rt_tb, rw_tb], writes=[rt_tb])
            g3 = lambda t: t[:].rearrange("p (g j) -> p g j", j=8)
            dv(lambda e: e.tensor_tensor(out=R["sel"][:], in0=R["sc"][:], in1=rbias[:], op=ALU.add))
            dv(lambda e: e.tensor_reduce(out=R["m1"][:], in_=g3(R["sel"]), axis=AX.X, op=ALU.max))
            dv(lambda e: e.tensor_tensor(out=g3(R["eq"]), in0=g3(R["sel"]), in1=bcast_last(R["m1"][:], 8), op=ALU.is_equal))
            dv(lambda e: e.scalar_tensor_tensor(out=R["sel2"][:], in0=R["eq"][:], scalar=-4.0, in1=R["sel"][:],
                                                op0=ALU.mult, op1=ALU.add))
            dv(lambda e: e.tensor_reduce(out=R["m2"][:], in_=g3(R["sel2"]), axis=AX.X, op=ALU.max))
            dv(lambda e: e.tensor_tensor(out=R["grp"][:], in0=R["m1"][:], in1=R["m2"][:], op=ALU.add))
            dv(lambda e: e.max(out=R["t8"][:], in_=R["grp"][:]))
            dv(lambda e: e.tensor_scalar(out=R["gm"][:], in0=R["grp"][:], scalar1=R["t8"][:, 3:4], scalar2=None, op0=ALU.is_ge))
            dv(lambda e: e.tensor_scalar(out=R["g4"][:], in0=R["gm"][:], scalar1=4.0, scalar2=-4.0, op0=ALU.mult, op1=ALU.add))
            dv(lambda e: e.tensor_tensor(out=g3(R["selm"]), in0=g3(R["sel"]), in1=bcast_last(R["gm"][:], 8), op=ALU.mult))
            dv(lambda e: e.tensor_tensor(out=g3(R["selm"]), in0=g3(R["selm"]), in1=bcast_last(R["g4"][:], 8), op=ALU.add))
            dv(lambda e: e.max(out=R["t8"][:], in_=R["selm"][:]))
            dv(lambda e: e.tensor_scalar(out=R["em"][:], in0=R["selm"][:], scalar1=R["t8"][:, 7:8], scalar2=None, op0=ALU.is_ge))
            dv(lambda e: e.tensor_tensor(out=R["gt"][:], in0=R["sc"][:], in1=R["em"][:], op=ALU.mult))
            dv(lambda e: e.tensor_reduce(out=R["den"][:], in_=R["gt"][:], axis=AX.X, op=ALU.add))
            dv(lambda e: e.reciprocal(out=R["den"][:], in_=R["den"][:]))
            dv(lambda e: e.tensor_scalar(out=R["gate"][:], in0=R["gt"][:], scalar1=R["den"][:, 0:1], scalar2=2.5,
                                         op0=ALU.mult, op1=ALU.mult))
            P.op("pe", lambda e: e.transpose(out=pb[5][0:64, 0:128], in_=R["gate"][:], identity=ident[:]),
                 reads=[rt_tb, ident_tb], writes=[pb_tb[5]])
            P.op("act", lambda e, b=b: e.activation(out=gateT[0:64, b * 128:(b + 1) * 128], in_=pb[5][0:64, 0:128],
                                                    func=AF.Identity), reads=[pb_tb[5]], writes=[gT_tb[b]])
        _sc1.__exit__(None, None, None)
        _sc2 = P.scope()
        _sc2.__enter__()
        wb = [{"w1": P.sbuf(f"p{ps}_w1b{i}", [128, 8, DE], BF16), "w3": P.sbuf(f"p{ps}_w3b{i}", [128, 8, DE], BF16),
               "w2": P.sbuf(f"p{ps}_w2b{i}", [128, 2, D], BF16)} for i in range(2)]
        selt = P.sbuf(f"p{ps}_selt", [65, 128], BF16)
        Gb = P.sbuf(f"p{ps}_Gb", [128, PT], BF16)
        ssb = [P.sbuf(f"p{ps}_ssb{i}", [128, TW], F32) for i in range(2)]
        ggT = P.sbuf(f"p{ps}_ggT", [128, 2, PT], BF16)

        for xi_, eid in enumerate(exp_ids):
            par = xi_ % 2
            W = wb[par]
            Wt = wb_tb[par]
            P.op("sp", lambda e, xi_=xi_: e.dma_start(out=stg[0][:].rearrange("p (kc f) -> p kc f", f=DE),
                                                     in_=dr["ew1"][xi_].rearrange("(kc p) f -> p kc f", p=128)),
                 writes=[stg_tb[0]], dma=True)
            P.op("pool", lambda e, W=W: e.tensor_copy(out=W["w1"][:].rearrange("p kc f -> p (kc f)"), in_=stg[0][:]),
                 reads=[stg_tb[0]], writes=[Wt["w1"]])
            P.op("sp", lambda e, xi_=xi_: e.dma_start(out=stg[1][:].rearrange("p (kc f) -> p kc f", f=DE),
                                                     in_=dr["ew3"][xi_].rearrange("(kc p) f -> p kc f", p=128)),
                 writes=[stg_tb[1]], dma=True)
            P.op("pool", lambda e, W=W: e.tensor_copy(out=W["w3"][:].rearrange("p kc f -> p (kc f)"), in_=stg[1][:]),
                 reads=[stg_tb[1]], writes=[Wt["w3"]])
            P.op("sp", lambda e, xi_=xi_: e.dma_start(out=stg[2][:].rearrange("p (fc d) -> p fc d", d=D),
                                                     in_=dr["ew2"][xi_].rearrange("(fc p) d -> p fc d", p=128)),
                 writes=[stg_tb[2]], dma=True)
            P.op("pool", lambda e, W=W: e.tensor_copy(out=W["w2"][:].rearrange("p fc d -> p (fc d)"), in_=stg[2][:]),
                 reads=[stg_tb[2]], writes=[Wt["w2"]])
            P.op("pool", lambda e, eid=eid: e.tensor_copy(out=selt[:], in_=ident[0:65, eid:eid + 1].to_broadcast([65, 128])),
                 reads=[ident_tb], writes=[selt_tb])
            for tt in range(NTT):
                P.op("pe", lambda e, tt=tt: e.matmul(pb[6][:, 0:TW], lhsT=selt[:], rhs=gateT[:, tt * TW:(tt + 1) * TW],
                                                     start=True, stop=True),
                     reads=[selt_tb] + gT_tb, writes=[pb_tb[6]])
                P.op("act", lambda e, tt=tt: e.activation(out=Gb[:, tt * TW:(tt + 1) * TW], in_=pb[6][:, 0:TW], func=AF.Identity),
                     reads=[pb_tb[6]], writes=[Gb_tb])
            for tt in range(NTT):
                for fc in range(2):
                    i2 = (tt * 2 + fc) % 2
                    b1, b3 = i2, 2 + i2
                    h_tbs = h2_tb[tt * BPT:(tt + 1) * BPT]
                    for kc in range(8):
                        P.op("pe", lambda e, kc=kc, fc=fc, tt=tt, b1=b1, W=W: e.matmul(
                            pb[b1][:, 0:TW], lhsT=W["w1"][:, kc, fc * 128:(fc + 1) * 128], rhs=h2T[:, kc, tt * TW:(tt + 1) * TW],
                            start=(kc == 0), stop=(kc == 7)), reads=[Wt["w1"]] + h_tbs, writes=[pb_tb[b1]], acc=True)
                    for kc in range(8):
                        P.op("pe", lambda e, kc=kc, fc=fc, tt=tt, b3=b3, W=W: e.matmul(
                            pb[b3][:, 0:TW], lhsT=W["w3"][:, kc, fc * 128:(fc + 1) * 128], rhs=h2T[:, kc, tt * TW:(tt + 1) * TW],
                            start=(kc == 0), stop=(kc == 7)), reads=[Wt["w3"]] + h_tbs, writes=[pb_tb[b3]], acc=True)
                    P.op("act", lambda e, i2=i2, b1=b1: e.activation(out=ssb[i2][:], in_=pb[b1][:, 0:TW], func=AF.Silu),
                         reads=[pb_tb[b1]], writes=[ssb_tb[i2]])
                    P.op("dve", lambda e, i2=i2, b3=b3: e.tensor_tensor(out=ssb[i2][:], in0=ssb[i2][:], in1=pb[b3][:, 0:TW], op=ALU.mult),
                         reads=[ssb_tb[i2], pb_tb[b3]], writes=[ssb_tb[i2]])
                    P.op("dve", lambda e, i2=i2, fc=fc, tt=tt: e.tensor_tensor(
                        out=ggT[:, fc, tt * TW:(tt + 1) * TW], in0=ssb[i2][:], in1=Gb[:, tt * TW:(tt + 1) * TW], op=ALU.mult),
                        reads=[ssb_tb[i2], Gb_tb], writes=[gg_tb[fc][tt]])
            for b in range(NB):
                tt = b // BPT
                for dh in range(2):
                    bk = 4 + (b * 2 + dh) % 2
                    for fc in range(2):
                        P.op("pe", lambda e, b=b, dh=dh, fc=fc, bk=bk, W=W: e.matmul(
                            pb[bk][:], lhsT=ggT[:, fc, b * 128:(b + 1) * 128], rhs=W["w2"][:, fc, dh * 512:(dh + 1) * 512],
                            start=(fc == 0), stop=(fc == 1)), reads=[gg_tb[fc][tt], Wt["w2"]], writes=[pb_tb[bk]], acc=True)
                    if xi_ == 0:
                        P.op("dve", lambda e, b=b, dh=dh, bk=bk: e.tensor_copy(out=yacc[:, b, dh * 512:(dh + 1) * 512], in_=pb[bk][:]),
                             reads=[pb_tb[bk]], writes=[ya_tb[b]])
                    else:
                        P.op("dve", lambda e, b=b, dh=dh, bk=bk: e.tensor_tensor(
                            out=yacc[:, b, dh * 512:(dh + 1) * 512], in0=yacc[:, b, dh * 512:(dh + 1) * 512], in1=pb[bk][:], op=ALU.add),
                            reads=[pb_tb[bk], ya_tb[b]], writes=[ya_tb[b]])
        _sc2.__exit__(None, None, None)
        load_bc(0, dr["modb"][:, 5 * D:6 * D])
        load_bc(1, dr["lnp"][:, 2, :])
        load_bc(2, dr["lnp"][:, 3, :])
        for b in range(NB):
            tk = t0 + b * 128
            xi = b % 2
            P.op("dve", lambda e, b=b: e.tensor_tensor(out=zt[:], in0=yacc[:, b, :], in1=bc[0][:], op=ALU.mult),
                 reads=[ya_tb[b], bc_tb[0]], writes=[zt_tb])
            P.op("dve", lambda e, b=b: e.scalar_tensor_tensor(out=zt[:], in0=xm[:, b, :], scalar=ALPHA, in1=zt[:],
                                                              op0=ALU.mult, op1=ALU.add),
                 reads=[xm_tb[b], zt_tb], writes=[zt_tb])
            emit_layernorm(P, zt[:], zt_tb, xr[xi][:], xr_tb[xi], bc[1][:], bc[2][:], [bc_tb[1], bc_tb[2]], lnsc, eps_t, "f")
            P.op("sp", lambda e, xi=xi, tk=tk: e.dma_start(out=dr["out"][tk:tk + 128, :], in_=xr[xi][:]),
                 reads=[xr_tb[xi]], writes=[dr["out_tb"]], dma=True)

    for ps in range(npass):
        _pass(ps)


def emit_k0(P, nc, dr):
    cT = P.sbuf("cT", [128, 8], F32)
    c_tb = P.tb("cT")
    P.op("sp", lambda e: e.dma_start(out=cT[:], in_=dr["cT"]), writes=[c_tb], dma=True)
    P.op("act", lambda e: e.activation(out=cT[:], in_=cT[:], func=AF.Silu), reads=[c_tb], writes=[c_tb])
    wm = [P.sbuf(f"wm{i}", [128, 8, 768], F32) for i in range(2)]
    wm_tb = [P.tb(), P.tb()]
    bm = P.sbuf("bm", [1, 4, 768], F32)
    P.op("sp", lambda e: e.dma_start(out=bm[:], in_=dr["b_mod"].unsqueeze(0)), writes=[c_tb], dma=True)
    ps = [P.psum(f"k0ps{i}", [128, 512], F32) for i in range(2)]
    ps_tb = [P.tb(), P.tb()]
    ot = P.sbuf("k0o", [1, 4, 768], F32)
    o_tb = P.tb("k0o")
    for l in range(4):
        W, wtb = wm[l % 2], wm_tb[l % 2]
        P.op("sp", lambda e, l=l, W=W: e.dma_start(out=W[:], in_=dr["w_mod"][l].rearrange("(kc p) n -> p kc n", p=128)), writes=[wtb], dma=True)
        for hf in range(2):
            for kc in range(8):
                P.op("pe", lambda e, kc=kc, hf=hf, W=W: e.matmul(ps[hf][0:1, 0:384], lhsT=cT[:, kc:kc + 1], rhs=W[:, kc, hf * 384:(hf + 1) * 384],
                                                                 start=(kc == 0), stop=(kc == 7)), reads=[c_tb, wtb], writes=[ps_tb[hf]], acc=True)
            P.op("dve", lambda e, l=l, hf=hf: e.tensor_tensor(out=ot[0:1, l, hf * 384:(hf + 1) * 384], in0=ps[hf][0:1, 0:384],
                                                              in1=bm[0:1, l, hf * 384:(hf + 1) * 384], op=ALU.add), reads=[ps_tb[hf], c_tb], writes=[o_tb])
    P.op("sp", lambda e: e.dma_start(out=dr["mod"].unsqueeze(0), in_=ot[:]), reads=[o_tb], writes=[dr["out_tb"]], dma=True)


D = 1024
def consts128():
    i = np.arange(128)
    ident = np.eye(128, dtype=np.float32)
    bo = (i[:, None] // 64 == i[None, :] // 64).astype(np.float32)
    ls = (i[None, :] < i[:, None]).astype(np.float32)
    us = (i[None, :] > i[:, None]).astype(np.float32)
    ui = (i[None, :] >= i[:, None]).astype(np.float32)
    return np.ascontiguousarray(np.stack([ident, bo, ls, us, ui], 1))

def k1_common(inp, l, mod):
    col = lambda v: np.ascontiguousarray(v.reshape(-1, 128).T)
    par = np.zeros((128, 64), np.float32)
    par[:, 0:11] = col(inp['rwkv_mu'][l])
    par[:, 11:14] = col(inp['rwkv_w0'][l]); par[:, 14:17] = col(inp['rwkv_a0'][l])
    par[:, 17:20] = col(inp['rwkv_k_k'][l]); par[:, 20:23] = col(inp['rwkv_k_a'][l])
    par[:, 26:29] = col(inp['rwkv_r_k'][l].reshape(-1))
    par[:, 29:37] = col(mod[0:D]); par[:, 37:45] = col(mod[D:2 * D])
    bcp = np.zeros((128, 320), np.float32)
    bcp[:, 0:128] = inp['dsa_kv_norm'][l][None]; bcp[:, 128:192] = inp['dsa_ik_g'][l][None]; bcp[:, 192:256] = inp['dsa_ik_b'][l][None]
    lora = np.ascontiguousarray(np.concatenate([inp['rwkv_w2'][l], inp['rwkv_a2'][l]], 0))
    wuk = np.ascontiguousarray(inp['dsa_w_uk'][l].reshape(2, 128, 128).transpose(1, 0, 2))
    return {"par": par, "bcp": bcp, "lora": lora, "g2w": np.ascontiguousarray(inp['rwkv_g2'][l]), "wuk": wuk,
            "cst": consts128(), "w_in": np.ascontiguousarray(inp['w_in'][l])}


def alibi_slopes(n):
    return (2.0 ** (-8.0 * (np.arange(n, dtype=np.float32) + 1.0) / n)).astype(np.float32)

K1_CAT = {"o_bon": 1, "o_g": 1, "o_qlat": 2, "o_iqs": 2, "o_sgn": 0, "o_ckv": 0, "o_ckvT": 1, "o_ikT": 1, "o_sq": 1, "o_sk": 1, "o_sv": 0,
          "o_YwT": 0, "o_Y0T": 0}

def k2_tables(i):
    sl = alibi_slopes(10)
    swa_sl, dsa_sl = sl[:6], sl[6:]
    p = np.arange(128)
    r = np.arange(8)[None, :, None]; pq = p[:, None, None]; pk = p[None, None, :]
    valid = (r < i) | ((r == i) & (pk <= pq))
    dmask = np.where(valid, 0.0, -1e30).astype(np.float32).reshape(128, 1024)
    dl = np.arange(128)[None, None, :]
    ab = (dsa_sl[None, :, None] * (128.0 * (7 - dl - i) + p[:, None, None] - 127.0)).astype(np.float32)
    q = p[None, None, None, :]; pk4 = p[:, None, None, None]; tile = np.arange(2)[None, :, None, None]
    dist = q - pk4 + np.where(tile == 0, 128, 0)
    ok = (dist >= 0) & (dist < 128)
    swb = np.where(ok, -swa_sl[None, None, :, None] * dist, -30000.0).astype(np.float32)
    swb_first = swb[:, 0].copy()
    if i == 0:
        swb_first[:] = -30000.0
    return {"dmask": dmask, "ab": np.ascontiguousarray(ab), "swb": np.ascontiguousarray(swb), "swb_first": np.ascontiguousarray(swb_first),
            "ident": np.eye(128, dtype=np.float32)}


def k2_inputs(K1, inp, l, NBQ):
    G = {n: np.concatenate([K1[c][n] for c in range(8)], ax) for n, ax in K1_CAT.items()}
    PT = np.stack([K1[c]["o_PT"] for c in range(8)]); Qa = np.stack([K1[c]["o_Q"] for c in range(8)])
    T = 8 * NBQ * 128
    bf = G["o_sk"].dtype
    maps = []
    for i in range(8):
        gbs = 8 * np.arange(NBQ) + i
        tok = (gbs[:, None] * 128 + np.arange(128)[None, :]).reshape(-1)
        prev = ((gbs - 1)[:, None] * 128 + np.arange(128)[None, :])
        own = (gbs[:, None] * 128 + np.arange(128)[None, :])
        m = {}
        m["YwT"] = np.ascontiguousarray(G["o_YwT"][gbs]); m["Y0T"] = np.ascontiguousarray(G["o_Y0T"][gbs])
        m["PbT"] = np.ascontiguousarray(PT[gbs // NBQ, gbs % NBQ]); m["Qb"] = np.ascontiguousarray(Qa[gbs // NBQ, gbs % NBQ])
        m["segPT"] = np.ascontiguousarray(PT[:, NBQ]); m["segQ"] = np.ascontiguousarray(Qa[:, NBQ])
        hm = lambda a: np.ascontiguousarray(a.reshape(6, 64, T)[:, :, tok].transpose(1, 0, 2))
        m["bon"] = hm(G["o_bon"]); m["g"] = hm(G["o_g"]); m["sq"] = hm(G["o_sq"])
        m["qlat"] = np.ascontiguousarray(G["o_qlat"][:, :, tok]); m["iqs"] = np.ascontiguousarray(G["o_iqs"][:, :, tok]); m["sgn"] = np.ascontiguousarray(G["o_sgn"][tok])
        sk = G["o_sk"].reshape(2, 64, T); sv = G["o_sv"]
        sk2 = np.zeros((64, 2, NBQ, 256), bf); sv2 = np.zeros((NBQ, 2, 128, 128), bf)
        for j in range(NBQ):
            if gbs[j] > 0:
                sk2[:, :, j, 0:128] = sk[:, :, prev[j]].transpose(1, 0, 2); sv2[j, 0] = sv[prev[j]]
            sk2[:, :, j, 128:256] = sk[:, :, own[j]].transpose(1, 0, 2); sv2[j, 1] = sv[own[j]]
        m["sk2"] = sk2; m["sv2"] = sv2
        m["ckv"] = G["o_ckv"]; m["ckvT"] = G["o_ckvT"]; m["ikT"] = G["o_ikT"]
        m["wuv"] = np.ascontiguousarray(inp['dsa_w_uv'][l])
        m["sink_b"] = np.ascontiguousarray(np.broadcast_to(inp['swa_sinks'][l][None], (64, 6)))
        rl = np.zeros((64, 16), np.float32)
        rl[:, 0:6] = inp['rwkv_ln_g'][l].reshape(6, 64).T; rl[:, 6:12] = inp['rwkv_ln_b'][l].reshape(6, 64).T
        m["rwkv_ln"] = rl
        m.update(k2_tables(i))
        maps.append(m)
    return maps


NPBF = ml_dtypes.bfloat16
NBQ_FULL = 16
_PROGS = {}


def _k1_spec(NBLK):
    NTk = NBLK * 128
    return {"o_YwT": ([NBLK, 6, 64, 128], F32), "o_Y0T": ([NBLK, 6, 64, 128], F32), "o_PT": ([NBLK + 1, 6, 64, 64], F32), "o_Q": ([NBLK + 1, 6, 64, 64], F32),
            "o_bon": ([384, NTk], F32), "o_g": ([384, NTk], F32), "o_qlat": ([128, 4, NTk], BF16), "o_iqs": ([64, 4, NTk], BF16), "o_sgn": ([NTk, 4], F32),
            "o_ckv": ([NTk, 128], BF16), "o_ckvT": ([128, NTk], BF16), "o_ikT": ([64, NTk], BF16), "o_sq": ([384, NTk], BF16), "o_sk": ([128, NTk], BF16),
            "o_sv": ([NTk, 128], BF16)}


def _build_k0():
    nc = bass.Bass("TRN2", target_bir_lowering=False)
    di = lambda n, s: nc.dram_tensor(n, list(s), F32, kind="ExternalInput").ap()
    dr = {"cT": di("cT", [128, 8]), "w_mod": di("w_mod", [4, 1024, 768]), "b_mod": di("b_mod", [4, 768])}
    dr["mod"] = nc.dram_tensor("mod", [4, 768], F32, kind="ExternalOutput").ap()
    with ExitStack() as es:
        P = Prog(nc, es)
        dr["out_tb"] = P.tb("out")
        emit_k0(P, nc, dr)
        P.finish([dr["out_tb"]])
    return nc


def _build_k1(NBLK):
    nc = bass.Bass("TRN2", target_bir_lowering=False)
    NTk = NBLK * 128
    di = lambda n, s: nc.dram_tensor(n, list(s), F32, kind="ExternalInput").ap()
    dr = {"x": di("x", [NTk, D]), "xh": di("xh", [1, D]), "par": di("par", [128, 64]), "bcp": di("bcp", [128, 320]), "lora": di("lora", [128, 384]),
          "g2w": di("g2w", [128, 384]), "wuk": di("wuk", [128, 2, 128]), "cst": di("cst", [128, 5, 128]), "w_in": di("w_in", [D, 2756])}
    for n, (s, dt) in _k1_spec(NBLK).items():
        dr[n] = nc.dram_tensor(n, s, dt, kind="ExternalOutput").ap()
    with ExitStack() as es:
        P = Prog(nc, es)
        dr["out_tb"] = P.tb("out")
        emit_k1(P, nc, NBLK, dr)
        P.finish([dr["out_tb"]])
    return nc


def _k2_in(NBQ):
    return {"YwT": ([NBQ, 6, 64, 128], F32), "Y0T": ([NBQ, 6, 64, 128], F32), "PbT": ([NBQ, 6, 64, 64], F32), "Qb": ([NBQ, 6, 64, 64], F32),
            "segPT": ([8, 6, 64, 64], F32), "segQ": ([8, 6, 64, 64], F32), "bon": ([64, 6, NBQ * 128], F32), "g": ([64, 6, NBQ * 128], F32),
            "sq": ([64, 6, NBQ * 128], BF16), "qlat": ([128, 4, NBQ * 128], BF16), "iqs": ([64, 4, NBQ * 128], BF16), "sgn": ([NBQ * 128, 4], F32),
            "sk2": ([64, 2, NBQ, 256], BF16), "sv2": ([NBQ, 2, 128, 128], BF16), "ckv": ([8 * NBQ * 128, 128], BF16), "ckvT": ([128, 8 * NBQ * 128], BF16),
            "ikT": ([64, 8 * NBQ * 128], BF16), "wuv": ([4, 128, 64], F32), "sink_b": ([64, 6], F32), "rwkv_ln": ([64, 16], F32), "dmask": ([128, 1024], F32),
            "ab": ([128, 4, 128], F32), "swb": ([128, 2, 6, 128], F32), "swb_first": ([128, 6, 128], F32), "ident": ([128, 128], F32)}


def _build_k2(NBQ):
    nc = bass.Bass("TRN2", target_bir_lowering=False)
    dr = {n: nc.dram_tensor(n, s, dt, kind="ExternalInput").ap() for n, (s, dt) in _k2_in(NBQ).items()}
    dr["mixT"] = nc.dram_tensor("mixT", [16, 64, NBQ * 128], BF16, kind="ExternalOutput").ap()
    with ExitStack() as es:
        P = Prog(nc, es)
        dr["out_tb"] = P.tb("out")
        emit_k2(P, nc, NBQ, dr)
        P.finish([dr["out_tb"]])
    return nc


def _build_k3(NT, exp_ids):
    nc = bass.Bass("TRN2", target_bir_lowering=False)
    di = lambda n, s: nc.dram_tensor(n, list(s), F32, kind="ExternalInput").ap()
    NX = len(exp_ids)
    dr = {"xres": di("xres", [NT, D]), "mixT": nc.dram_tensor("mixT", [16, 64, NT], BF16, kind="ExternalInput").ap(),
          "modb": di("modb", [128, 6 * D]), "modT": di("modT", [128, 48]), "lnp": di("lnp", [128, 4, D]), "w_out": di("w_out", [D, D]),
          "router_w": di("router_w", [D, NE]), "rbias_b": di("rbias_b", [128, NE]), "ew1": di("ew1", [NX, D, DE]), "ew3": di("ew3", [NX, D, DE]),
          "ew2": di("ew2", [NX, DE, D]), "ident": di("ident", [128, 128])}
    dr["out"] = nc.dram_tensor("out", [NT, D], F32, kind="ExternalOutput").ap()
    with ExitStack() as es:
        P = Prog(nc, es)
        dr["out_tb"] = P.tb("out")
        ident = P.sbuf("ident_sb", [128, 128], F32)
        ident_tb = P.tb("ident")
        P.op("sp", lambda e: e.dma_start(out=ident[:], in_=dr["ident"]), writes=[ident_tb], dma=True)
        emit_ffn(P, nc, NT, exp_ids, dr, ident, ident_tb)
        P.finish([dr["out_tb"]])
    return nc


def _prog(key, fn):
    if key not in _PROGS:
        _PROGS[key] = fn()
    return _PROGS[key]


def _run(nc, maps):
    res = run_bass_kernel_spmd(nc, maps, core_ids=list(range(8)))
    return [{k: np.asarray(v) for k, v in r.items()} for r in res.results]


def _forward(inp, NBQ=NBQ_FULL, n_layers=4, exp_ids=None):
    NT = NBQ * 128
    T = 8 * NT
    if exp_ids is None:
        exp_ids = list(range(NE)) + [NE]
    inp = {k: np.asarray(v) for k, v in inp.items()}
    cT = np.ascontiguousarray(inp['c'][0].reshape(8, 128).T)
    r0 = _run(_prog("k0", _build_k0), [{"cT": cT, "w_mod": np.ascontiguousarray(inp['w_mod'][:, :, i * 768:(i + 1) * 768]),
                                         "b_mod": np.ascontiguousarray(inp['b_mod'][:, i * 768:(i + 1) * 768])} for i in range(8)])
    mod = np.concatenate([r0[i]["mod"] for i in range(8)], 1)
    x = np.ascontiguousarray(inp['x'][0][:T])
    ident = np.eye(128, dtype=np.float32)
    toks = [((8 * np.arange(NBQ) + i)[:, None] * 128 + np.arange(128)[None, :]).reshape(-1) for i in range(8)]
    for l in range(n_layers):
        common = k1_common(inp, l, mod[l])
        maps = []
        for c in range(8):
            m = dict(common)
            m["par"] = common["par"].copy()
            m["par"][:, 45] = 0.0 if c == 0 else 1.0
            m["x"] = np.ascontiguousarray(x[c * NT:(c + 1) * NT])
            m["xh"] = np.ascontiguousarray(x[c * NT - 1:c * NT]) if c > 0 else np.zeros((1, D), np.float32)
            maps.append(m)
        K1 = _run(_prog(("k1", NBQ), lambda: _build_k1(NBQ)), maps)
        K2 = _run(_prog(("k2", NBQ), lambda: _build_k2(NBQ)), k2_inputs(K1, inp, l, NBQ))
        del K1
        sel = [e for e in exp_ids if e < NE]
        ew1 = np.concatenate([inp['exp_w1'][l][sel], inp['sh_w1'][l][None]], 0)
        ew3 = np.concatenate([inp['exp_w3'][l][sel], inp['sh_w3'][l][None]], 0)
        ew2 = np.concatenate([inp['exp_w2'][l][sel], inp['sh_w2'][l][None]], 0)
        lnp = np.stack([inp['ln_mix_g'][l], inp['ln_mix_b'][l], inp['ln_ffn_g'][l], inp['ln_ffn_b'][l]])
        common3 = {"modb": np.ascontiguousarray(np.broadcast_to(mod[l][None], (128, 6 * D))), "modT": np.ascontiguousarray(mod[l].reshape(48, 128).T),
                   "lnp": np.ascontiguousarray(np.broadcast_to(lnp[None], (128, 4, D))), "w_out": np.ascontiguousarray(inp['w_out'][l]),
                   "router_w": np.ascontiguousarray(inp['router_w'][l]),
                   "rbias_b": np.ascontiguousarray(np.broadcast_to(inp['router_bias'][l][None], (128, NE))), "ew1": ew1, "ew3": ew3, "ew2": ew2, "ident": ident}
        maps3 = [{**common3, "xres": np.ascontiguousarray(x[toks[i]]), "mixT": K2[i]["mixT"]} for i in range(8)]
        K3 = _run(_prog(("k3", NT, tuple(exp_ids)), lambda: _build_k3(NT, exp_ids)), maps3)
        del maps3, ew1, ew3, ew2
        xn = np.empty_like(x)
        for i in range(8):
            xn[toks[i]] = K3[i]["out"]
        x = xn
    return x


def kernel(**inputs):
    x = _forward(inputs)
    return np.ascontiguousarray(x[None].astype(np.float32))
```

```python
import numpy as np
import ml_dtypes


from contextlib import ExitStack
import concourse.bass as bass
import concourse.mybir as mybir
from concourse.bass_utils import run_bass_kernel_spmd

F32 = mybir.dt.float32
BF16 = mybir.dt.bfloat16
AF = mybir.ActivationFunctionType
ALU = mybir.AluOpType
AX = mybir.AxisListType


SKIP_SELF = False


class TB:
    __slots__ = ("name", "w", "r")

    def __init__(self, name="?"):
        self.name = name
        self.w = None
        self.r = {}


class Prog:
    ENGS = ("pe", "dve", "act", "pool", "sp")

    def __init__(self, nc, es, ring=8):
        self.nc = nc
        self.es = es
        self.ops = {e: [] for e in self.ENGS}
        self.cnt = {e: 0 for e in self.ENGS}
        self.waited = {e: {} for e in self.ENGS}
        self.sems = {}
        for e in self.ENGS:
            self.sems["c_" + e] = es.enter_context(nc.semaphore("c_" + e))
        self.ring = {}
        for e in ("sp", "pool", "act"):
            names = [f"d_{e}{i}" for i in range(ring)]
            for n in names:
                self.sems[n] = es.enter_context(nc.semaphore(n))
            self.ring[e] = {"names": names, "uses": [0] * ring, "next": 0}
        self.nbuf = 0

    def tb(self, name=None):
        self.nbuf += 1
        return TB(name or f"b{self.nbuf}")

    def sbuf(self, name, shape, dtype):
        return self.es.enter_context(self.nc.sbuf_tensor("sb_" + name, list(shape), dtype))

    def psum(self, name, shape, dtype=F32):
        return self.es.enter_context(self.nc.psum_tensor("ps_" + name, list(shape), dtype))

    def _need(self, eng, evs):
        need = {}
        for ev in evs:
            if ev is None:
                continue
            k, v = ev
            if need.get(k, 0) < v:
                need[k] = v
        w = self.waited[eng]
        for k, v in need.items():
            if SKIP_SELF and k == "c_" + eng:
                continue
            if w.get(k, 0) < v:
                self.ops[eng].append(("wait", k, v))
                w[k] = v

    def op(self, eng, fn, reads=(), writes=(), dma=False, acc=False):
        evs = []
        for b in reads:
            evs.append(b.w)
        for b in writes:
            if not (acc and eng == "pe" and b.w is not None and b.w[0] == "c_pe"):
                evs.append(b.w)
            for k, v in b.r.items():
                evs.append((k, v))
        if dma:
            rg = self.ring[eng]
            i = rg["next"]
            rg["next"] = (i + 1) % len(rg["names"])
            k = rg["names"][i]
            evs.append((k, 16 * rg["uses"][i]))
            rg["uses"][i] += 1
            ev = (k, 16 * rg["uses"][i])
            inc = 16
        else:
            self.cnt[eng] += 1
            k = "c_" + eng
            ev = (k, self.cnt[eng])
            inc = 1
        self._need(eng, evs)
        self.ops[eng].append(("op", fn, k, inc))
        for b in reads:
            if b.r.get(ev[0], 0) < ev[1]:
                b.r[ev[0]] = ev[1]
        for b in writes:
            b.w = ev
            b.r = {}
        return ev

    def barrier(self):
        evs = [("c_" + x, self.cnt[x]) for x in self.ENGS]
        for e, rg in self.ring.items():
            for n, u in zip(rg["names"], rg["uses"]):
                evs.append((n, 16 * u))
        for e in self.ENGS:
            self._need(e, evs)

    def scope(self):
        prog = self

        class _Scope:
            def __enter__(self_):
                self_.old = prog.es
                self_.st = ExitStack()
                self_.st.__enter__()
                prog.es = self_.st
                return prog

            def __exit__(self_, *a):
                prog.barrier()
                prog.es = self_.old
                return self_.st.__exit__(*a)
        return _Scope()

    def finish(self, out_bufs):
        self._need("sp", [b.w for b in out_bufs])
        nc = self.nc
        sems = self.sems
        ops = self.ops

        def replay(engobj, lst):
            for it in lst:
                if it[0] == "wait":
                    engobj.wait_ge(sems[it[1]], it[2])
                else:
                    ins = it[1](engobj)
                    ins.then_inc(sems[it[2]], it[3])

        with nc.Block() as block:
            @block.tensor
            def _(e):
                replay(e, ops["pe"])

            @block.vector
            def _(e):
                replay(e, ops["dve"])

            @block.scalar
            def _(e):
                replay(e, ops["act"])

            @block.gpsimd
            def _(e):
                replay(e, ops["pool"])

            @block.sync
            def _(e):
                replay(e, ops["sp"])


D = 1024
ALPHA = (2 * 4) ** 0.25
LN_EPS = 1e-5
NE = 64
DE = 256


def bcast_last(ap, n):
    shp = list(ap.shape)
    return ap.unsqueeze(len(shp)).to_broadcast(shp + [n])


class Consts:
    pass


def emit_layernorm(P, z, z_tb, out, out_tb, gb, bb, par_tb, sc, eps_t, tagn):
    st, mv, rs, xn = sc["st"], sc["mv"], sc["rs"], sc["xn"]
    stb = sc["tb"]
    P.op("dve", lambda e: e.bn_stats(out=st[:, 0, :], in_=z[:, 0:512]), reads=[z_tb], writes=[stb])
    P.op("dve", lambda e: e.bn_stats(out=st[:, 1, :], in_=z[:, 512:1024]), reads=[z_tb, stb], writes=[stb])
    P.op("dve", lambda e: e.bn_aggr(out=mv[:], in_=st[:].rearrange("p a b -> p (a b)")), reads=[stb], writes=[stb])
    P.op("act", lambda e: e.activation(out=rs[:], in_=mv[:, 1:2], func=AF.Sqrt, bias=eps_t[:, 0:1], scale=1.0),
         reads=[stb], writes=[stb])
    P.op("dve", lambda e: e.reciprocal(out=rs[:], in_=rs[:]), reads=[stb], writes=[stb])
    P.op("dve", lambda e: e.tensor_scalar(out=xn[:], in0=z, scalar1=mv[:, 0:1], scalar2=rs[:, 0:1],
                                          op0=ALU.subtract, op1=ALU.mult), reads=[z_tb, stb], writes=[sc["xn_tb"]])
    P.op("pool", lambda e: e.tensor_tensor(out=xn[:], in0=xn[:], in1=gb, op=ALU.mult),
         reads=[sc["xn_tb"]] + list(par_tb), writes=[sc["xn_tb"]])
    P.op("pool", lambda e: e.tensor_tensor(out=out, in0=xn[:], in1=bb, op=ALU.add),
         reads=[sc["xn_tb"]] + list(par_tb), writes=[out_tb])


NRW = 1408
C_DQ, C_CKV, C_IQ, C_IK, C_IW = 1408, 1664, 1792, 2048, 2112
C_SQ, C_SK, C_SV = 2116, 2500, 2628
PIN = 2756
DECAY_C = -0.6065306597126334


STOP = 99


class StopEmit(Exception):
    pass


def ck(n):
    if STOP == n:
        raise StopEmit()


class Slots:
    def __init__(self, P, aps, tbs=None):
        self.aps = aps
        self.tbs = tbs if tbs is not None else [P.tb() for _ in aps]
        self.i = 0

    def next(self):
        i = self.i
        self.i = (i + 1) % len(self.aps)
        return self.aps[i], self.tbs[i]


def emit_k1(P, nc, NBLK, dr):
    cst = P.sbuf("cst", [128, 5, 128], F32)
    cst_tb = P.tb("cst")
    P.op("sp", lambda e: e.dma_start(out=cst[:], in_=dr["cst"]), writes=[cst_tb], dma=True)
    ident, BO, LS, US, UI = (cst[:, i, :] for i in range(5))
    identb = P.sbuf("identb", [128, 128], BF16)
    P.op("dve", lambda e: e.tensor_copy(out=identb[:], in_=ident), reads=[cst_tb], writes=[cst_tb])
    par = P.sbuf("par", [128, 64], F32)
    par_tb = P.tb("par")
    P.op("sp", lambda e: e.dma_start(out=par[:], in_=dr["par"]), writes=[par_tb], dma=True)
    P.op("dve", lambda e: e.tensor_scalar(out=par[:, 37:45], in0=par[:, 37:45], scalar1=1.0, scalar2=None, op0=ALU.add),
         reads=[par_tb], writes=[par_tb])
    P.op("dve", lambda e: e.tensor_scalar(out=par[:, 23:26], in0=par[:, 20:23], scalar1=-1.0, scalar2=1.0, op0=ALU.mult, op1=ALU.add),
         reads=[par_tb], writes=[par_tb])
    bcp = P.sbuf("bcp", [128, 320], F32)
    P.op("sp", lambda e: e.dma_start(out=bcp[:], in_=dr["bcp"]), writes=[par_tb], dma=True)
    eps6 = P.sbuf("eps6", [128, 2], F32)
    P.op("pool", lambda e: e.memset(eps6[:, 0:1], 1e-6), writes=[par_tb])
    P.op("pool", lambda e: e.memset(eps6[:, 1:2], 1e-5), writes=[par_tb])
    lora = P.sbuf("lora", [128, 384], F32)
    g2w = P.sbuf("g2w", [128, 384], F32)
    P.op("sp", lambda e: e.dma_start(out=lora[:], in_=dr["lora"]), writes=[par_tb], dma=True)
    P.op("sp", lambda e: e.dma_start(out=g2w[:], in_=dr["g2w"]), writes=[par_tb], dma=True)
    wuk_f = P.sbuf("wuk_f", [128, 2, 128], F32)
    wuk = P.sbuf("wuk", [128, 2, 128], BF16)
    P.op("sp", lambda e: e.dma_start(out=wuk_f[:], in_=dr["wuk"]), writes=[par_tb], dma=True)
    P.op("dve", lambda e: e.tensor_copy(out=wuk[:], in_=wuk_f[:]), reads=[par_tb], writes=[par_tb])
    win = P.sbuf("win", [128, 8, PIN], BF16)
    win_tb = P.tb("win")
    wst = [P.sbuf(f"wst{i}", [128, PIN], F32) for i in range(2)]
    wst_tb = [P.tb() for _ in range(2)]
    for kc in range(8):
        s = kc % 2
        P.op("sp", lambda e, kc=kc, s=s: e.dma_start(out=wst[s][:], in_=dr["w_in"][kc * 128:(kc + 1) * 128, :]),
             writes=[wst_tb[s]], dma=True)
        P.op("pool", lambda e, kc=kc, s=s: e.tensor_copy(out=win[:, kc, :], in_=wst[s][:]), reads=[wst_tb[s]], writes=[win_tb])
    pbk = [P.psum(f"k1pb{i}", [128, 512], F32) for i in range(8)]
    bank_tb = [P.tb(f"bank{i}") for i in range(8)]
    q128 = Slots(P, [pbk[b][:, q * 128:(q + 1) * 128] for q in range(4) for b in (2, 3, 4, 5)],
                 [bank_tb[b] for q in range(4) for b in (2, 3, 4, 5)])
    h256 = Slots(P, [pbk[b][:, q * 256:(q + 1) * 256] for q in range(2) for b in (6, 7)],
                 [bank_tb[b] for q in range(2) for b in (6, 7)])
    pT_tb = [bank_tb[0], bank_tb[1]]
    xb = [P.sbuf(f"xb{i}", [128, D], F32) for i in range(2)]
    xb_tb = [P.tb(), P.tb()]
    hT = P.sbuf("hT", [128, 8, 128], BF16)
    hT_tb = P.tb("hT")
    hh = P.sbuf("hh", [128, 8, 1], BF16)
    pr = P.sbuf("pr", [128, 11, 129], F32)
    pr_tb = P.tb("pr")
    prev = P.sbuf("prevc", [128, 11, 1], F32)
    prev_tb = P.tb("prev")
    X = P.sbuf("X", [128, 11, 128], F32)
    X_tb = P.tb("X")
    dif = P.sbuf("dif", [128, 11, 128], F32)
    fm = {n: P.sbuf("fm_" + n, [128, 3, 128], F32) for n in
          ["lw", "a", "kk", "kp", "t1", "cum", "einc", "eexc", "einv", "eend", "at", "bh", "kh", "rt", "bc", "kc", "g", "bon", "beta"]}
    fa = P.tb("fmall")
    fm_tb = {n: fa for n in fm}
    LA = P.sbuf("LA", [128, 128], F32)
    SG = P.sbuf("SG", [128, 128], F32)
    gC = P.sbuf("gC", [128, 3], F32)
    GCe = P.sbuf("GCe", [128, 3], F32)
    ones = P.sbuf("ones128", [128, 128], F32)
    P.op("pool", lambda e: e.memset(ones[:], 1.0), writes=[par_tb])
    tokm = {n: P.sbuf("tok_" + n, [128, 384], F32) for n in ["at", "bc", "kc", "v"]}
    tok_tb = {n: P.tb("tok_" + n) for n in tokm}
    tk = P.sbuf("tk", [128, 580], F32)
    tk_tb = P.tb("tk")
    HW = [{"MX": P.sbuf(f"MX{i}", [128, 256], F32), "MT": P.sbuf(f"MT{i}", [128, 128], F32), "MKT": P.sbuf(f"MKT{i}", [128, 128], F32),
           "NBT": P.sbuf(f"NBT{i}", [128, 128], F32), "NKT": P.sbuf(f"NKT{i}", [128, 128], F32), "DG": P.sbuf(f"DG{i}", [128, 128], F32)} for i in range(2)]
    HW_tb = [{n: P.tb(f"{n}{i}") for n in ("MX", "MT", "MKT", "NBT", "NKT", "DG")} for i in range(2)]
    ATb = P.sbuf("ATb", [64, 6, 64], F32)
    Db = P.sbuf("Db", [64, 6, 64], F32)
    AD_tb = [P.tb() for _ in range(6)]
    PQ = P.sbuf("PQ", [64, 6, 128], F32)
    PTt = P.sbuf("PTt", [64, 6, 64], F32)
    PQ_tb = [P.tb() for _ in range(6)]
    ost = [P.sbuf(f"ost{i}", [128, 128], F32) for i in range(4)]
    ost_s = Slots(P, [o[:] for o in ost])
    obf = [P.sbuf(f"obf{i}", [128, 512], BF16) for i in range(4)]
    obf_s = Slots(P, [o[:] for o in obf])
    nsc = {"st": P.sbuf("n_st", [128, 6], F32), "mv": P.sbuf("n_mv", [128, 2], F32), "rs": P.sbuf("n_rs", [128, 2], F32),
           "aiw": P.sbuf("n_aiw", [128, 4], F32), "sg": P.sbuf("n_sg", [128, 4], F32), "t": P.sbuf("n_t", [128, 256], F32)}
    nsc_tb = P.tb("nsc")
    out_tb = dr["out_tb"]

    def out_dma(dst, src, rtb):
        P.op("sp", lambda e: e.dma_start(out=dst, in_=src), reads=[rtb], writes=[out_tb], dma=True)

    for hd in range(6):
        P.op("pool", lambda e, hd=hd: e.memset(PQ[:, hd, 64:128], 0.0), writes=[PQ_tb[hd]])
        P.op("pool", lambda e, hd=hd: e.tensor_copy(out=PQ[:, hd, 0:64], in_=ident[0:64, 0:64]), reads=[cst_tb], writes=[PQ_tb[hd]])
        P.op("pool", lambda e, hd=hd: e.tensor_copy(out=PTt[:, hd, :], in_=ident[0:64, 0:64]), reads=[cst_tb], writes=[PQ_tb[hd]])

    def emit_pq_out(b):
        for hd in range(6):
            out_dma(dr["o_PT"][b, hd], PTt[:, hd, :], PQ_tb[hd])
            out_dma(dr["o_Q"][b, hd], PQ[:, hd, 64:128], PQ_tb[hd])

    ck(1)
    P.op("sp", lambda e: e.dma_start(out=xb[1][0:1, :], in_=dr["xh"]), writes=[xb_tb[1]], dma=True)
    for kc in range(8):
        P.op("pe", lambda e, kc=kc: e.transpose(out=pbk[0][:, kc:kc + 1], in_=xb[1][0:1, kc * 128:(kc + 1) * 128],
                                                identity=ident[0:1, 0:1]), reads=[xb_tb[1], cst_tb], writes=[pT_tb[0]], acc=True)
    for kc in range(8):
        P.op("act", lambda e, kc=kc: e.activation(out=hh[:, kc, :], in_=pbk[0][:, kc:kc + 1], func=AF.Identity,
                                                  bias=par[:, 29 + kc:30 + kc], scale=par[:, 37 + kc:38 + kc]),
             reads=[pT_tb[0], par_tb], writes=[hT_tb])
    for c in range(11):
        pa, ptb = q128.next()
        for kc in range(8):
            P.op("pe", lambda e, c=c, kc=kc, pa=pa: e.matmul(pa[:, 0:1], lhsT=win[:, kc, c * 128:(c + 1) * 128], rhs=hh[:, kc, :],
                                                             start=(kc == 0), stop=(kc == 7)), reads=[win_tb, hT_tb], writes=[ptb], acc=True)
        P.op("dve", lambda e, c=c, pa=pa: e.tensor_scalar(out=prev[:, c, :], in0=pa[:, 0:1], scalar1=par[:, 45:46], scalar2=None, op0=ALU.mult),
             reads=[ptb, par_tb], writes=[prev_tb])

    ck(2)
    emit_pq_out(0)
    for b in range(NBLK):
        t0 = b * 128
        xi = b % 2
        P.op("sp", lambda e, xi=xi, t0=t0: e.dma_start(out=xb[xi][:], in_=dr["x"][t0:t0 + 128, :]), writes=[xb_tb[xi]], dma=True)
        for kc in range(8):
            bank = kc // 4
            P.op("pe", lambda e, kc=kc, bank=bank, xi=xi: e.transpose(out=pbk[bank][:, (kc % 4) * 128:(kc % 4 + 1) * 128],
                                                                      in_=xb[xi][:, kc * 128:(kc + 1) * 128], identity=ident),
                 reads=[xb_tb[xi], cst_tb], writes=[pT_tb[bank]], acc=True)
        for kc in range(8):
            bank = kc // 4
            P.op("act", lambda e, kc=kc, bank=bank: e.activation(out=hT[:, kc, :], in_=pbk[bank][:, (kc % 4) * 128:(kc % 4 + 1) * 128],
                                                                 func=AF.Identity, bias=par[:, 29 + kc:30 + kc], scale=par[:, 37 + kc:38 + kc]),
                 reads=[pT_tb[bank], par_tb], writes=[hT_tb])

        ck(3)

        def projT(col0, evac):
            pa, ptb = q128.next()
            for kc in range(8):
                P.op("pe", lambda e, kc=kc, pa=pa: e.matmul(pa, lhsT=win[:, kc, col0:col0 + 128], rhs=hT[:, kc, :],
                                                            start=(kc == 0), stop=(kc == 7)), reads=[win_tb, hT_tb], writes=[ptb], acc=True)
            evac(pa, ptb)

        P.op("pool", lambda e: e.tensor_copy(out=pr[:, :, 0:1], in_=prev[:]), reads=[prev_tb], writes=[pr_tb])
        for c in range(11):
            projT(c * 128, lambda pa, ptb, c=c: P.op("act", lambda e: e.activation(out=pr[:, c, 1:129], in_=pa, func=AF.Identity),
                                                     reads=[ptb], writes=[pr_tb]))
        P.op("pool", lambda e: e.tensor_copy(out=prev[:], in_=pr[:, :, 128:129]), reads=[pr_tb], writes=[prev_tb])
        P.op("dve", lambda e: e.tensor_tensor(out=dif[:], in0=pr[:, :, 0:128], in1=pr[:, :, 1:129], op=ALU.subtract), reads=[pr_tb], writes=[X_tb])
        P.op("dve", lambda e: e.tensor_tensor(out=dif[:], in0=dif[:], in1=bcast_last(par[:, 0:11], 128), op=ALU.mult), reads=[X_tb, par_tb], writes=[X_tb])
        P.op("dve", lambda e: e.tensor_tensor(out=X[:], in0=dif[:], in1=pr[:, :, 1:129], op=ALU.add), reads=[X_tb, pr_tb], writes=[X_tb])
        ck(4)
        r_, k_, v_ = X[:, 0:3, :], X[:, 3:6, :], X[:, 6:9, :]
        P.op("act", lambda e: e.activation(out=LA[0:64, :], in_=X[0:64, 9, :], func=AF.Tanh), reads=[X_tb], writes=[fm_tb["lw"]])
        P.op("act", lambda e: e.activation(out=LA[64:128, :], in_=X[64:128, 9, :], func=AF.Identity), reads=[X_tb], writes=[fm_tb["lw"]])
        P.op("act", lambda e: e.activation(out=SG[:], in_=X[:, 10, :], func=AF.Sigmoid), reads=[X_tb], writes=[fm_tb["g"]])
        for cc in range(3):
            pa, ptb = q128.next()
            P.op("pe", lambda e, cc=cc, pa=pa: e.matmul(pa, lhsT=lora[0:64, cc * 128:(cc + 1) * 128], rhs=LA[0:64, :], start=True, stop=True),
                 reads=[par_tb, fm_tb["lw"]], writes=[ptb])
            P.op("act", lambda e, cc=cc, pa=pa: e.activation(out=fm["lw"][:, cc, :], in_=pa, func=AF.Sigmoid, bias=par[:, 11 + cc:12 + cc], scale=1.0),
                 reads=[ptb, par_tb], writes=[fm_tb["cum"]])
            pa2, ptb2 = q128.next()
            P.op("pe", lambda e, cc=cc, pa2=pa2: e.matmul(pa2, lhsT=lora[64:128, cc * 128:(cc + 1) * 128], rhs=LA[64:128, :], start=True, stop=True),
                 reads=[par_tb, fm_tb["lw"]], writes=[ptb2])
            P.op("act", lambda e, cc=cc, pa2=pa2: e.activation(out=fm["a"][:, cc, :], in_=pa2, func=AF.Sigmoid, bias=par[:, 14 + cc:15 + cc], scale=1.0),
                 reads=[ptb2, par_tb], writes=[fm_tb["a"]])
            pa3, ptb3 = q128.next()
            P.op("pe", lambda e, cc=cc, pa3=pa3: e.matmul(pa3, lhsT=g2w[:, cc * 128:(cc + 1) * 128], rhs=SG[:], start=True, stop=True),
                 reads=[par_tb, fm_tb["g"]], writes=[ptb3])
            P.op("act", lambda e, cc=cc, pa3=pa3: e.activation(out=fm["g"][:, cc, :], in_=pa3, func=AF.Identity), reads=[ptb3], writes=[fm_tb["bon"]])
        dv = lambda fn, rd, wr: P.op("dve", fn, reads=[fm_tb[n] if isinstance(n, str) else n for n in rd],
                                     writes=[fm_tb[n] if isinstance(n, str) else n for n in wr])
        F = fm
        pcol = lambda c0: bcast_last(par[:, c0:c0 + 3], 128)
        dv(lambda e: e.tensor_scalar(out=F["lw"][:], in0=F["lw"][:], scalar1=DECAY_C, scalar2=None, op0=ALU.mult), ["cum"], ["cum"])
        out_dma(dr["o_g"].rearrange("(c p) t -> p c t", p=128)[:, :, t0:t0 + 128], F["g"][:], fm_tb["bon"])
        dv(lambda e: e.tensor_tensor(out=F["kk"][:], in0=k_, in1=pcol(17), op=ALU.mult), [X_tb, par_tb], ["kk"])
        dv(lambda e: e.tensor_tensor(out=F["t1"][:], in0=F["kk"][:], in1=F["kk"][:], op=ALU.mult), ["kk"], ["t1"])
        for cc in range(3):
            pa, ptb = q128.next()
            P.op("pe", lambda e, cc=cc, pa=pa: e.matmul(pa, lhsT=BO, rhs=F["t1"][:, cc, :], start=True, stop=True), reads=[cst_tb, fm_tb["t1"]], writes=[ptb])
            P.op("act", lambda e, cc=cc, pa=pa: e.activation(out=F["kp"][:, cc, :], in_=pa, func=AF.Sqrt), reads=[ptb], writes=[fm_tb["kp"]])
        dv(lambda e: e.tensor_scalar(out=F["kp"][:], in0=F["kp"][:], scalar1=1e-12, scalar2=None, op0=ALU.max), ["kp"], ["kp"])
        dv(lambda e: e.reciprocal(out=F["kp"][:], in_=F["kp"][:]), ["kp"], ["kp"])
        dv(lambda e: e.tensor_tensor(out=F["kk"][:], in0=F["kk"][:], in1=F["kp"][:], op=ALU.mult), ["kk", "kp"], ["kk"])
        dv(lambda e: e.tensor_tensor(out=F["t1"][:], in0=F["a"][:], in1=pcol(20), op=ALU.mult), ["a", par_tb], ["t1"])
        dv(lambda e: e.tensor_tensor(out=F["t1"][:], in0=F["t1"][:], in1=pcol(23), op=ALU.add), ["t1", par_tb], ["t1"])
        dv(lambda e: e.tensor_tensor(out=F["kp"][:], in0=k_, in1=F["t1"][:], op=ALU.mult), [X_tb, "t1", "kp"], ["kp"])
        dv(lambda e: e.tensor_tensor(out=F["t1"][:], in0=r_, in1=pcol(26), op=ALU.mult), [X_tb, par_tb], ["t1"])
        dv(lambda e: e.tensor_tensor(out=F["t1"][:], in0=F["t1"][:], in1=F["kp"][:], op=ALU.mult), ["t1", "kp"], ["t1"])
        for cc in range(3):
            pa, ptb = q128.next()
            P.op("pe", lambda e, cc=cc, pa=pa: e.matmul(pa, lhsT=BO, rhs=F["t1"][:, cc, :], start=True, stop=True), reads=[cst_tb, fm_tb["t1"]], writes=[ptb])
            P.op("dve", lambda e, cc=cc, pa=pa: e.tensor_tensor(out=F["bon"][:, cc, :], in0=pa, in1=X[:, 6 + cc, :], op=ALU.mult),
                 reads=[ptb, X_tb], writes=[fm_tb["g"]])
        out_dma(dr["o_bon"].rearrange("(c p) t -> p c t", p=128)[:, :, t0:t0 + 128], F["bon"][:], fm_tb["g"])
        dv(lambda e: e.tensor_tensor(out=F["beta"][:], in0=F["a"][:], in1=F["kk"][:], op=ALU.mult), ["a", "kk"], ["beta"])
        for cc in range(3):
            dv(lambda e, cc=cc: e.tensor_tensor_scan(out=F["cum"][:, cc, :], data0=ones[:], data1=F["lw"][:, cc, :], initial=0.0,
                                                     op0=ALU.mult, op1=ALU.add), ["cum", par_tb], ["einc"])
        dv(lambda e: e.tensor_copy(out=gC[:], in_=F["cum"][:, :, 127]), ["einc"], ["einc"])
        dv(lambda e: e.tensor_tensor(out=F["t1"][:], in0=F["cum"][:], in1=F["lw"][:], op=ALU.subtract), ["einc", "t1"], ["t1"])
        ac = lambda fn, rd, wr: P.op("act", fn, reads=[fm_tb[n] for n in rd], writes=[fm_tb[n] for n in wr])
        ac(lambda e: e.activation(out=F["einc"][:], in_=F["cum"][:], func=AF.Exp), ["einc"], ["eexc"])
        ac(lambda e: e.activation(out=F["eexc"][:], in_=F["t1"][:], func=AF.Exp), ["t1"], ["einv"])
        ac(lambda e: e.activation(out=F["einv"][:], in_=F["cum"][:], func=AF.Exp, scale=-1.0), ["einc"], ["eend"])
        for cc in range(3):
            ac(lambda e, cc=cc: e.activation(out=F["eend"][:, cc, :], in_=F["cum"][:, cc, :], func=AF.Exp, scale=-1.0, bias=gC[:, cc:cc + 1]),
               ["einc"], ["at"])
        ac(lambda e: e.activation(out=GCe[:], in_=gC[:], func=AF.Exp), ["einc"], ["at"])
        dv(lambda e: e.scalar_tensor_tensor(out=F["at"][:], in0=F["kk"][:], scalar=-1.0, in1=F["eexc"][:], op0=ALU.mult, op1=ALU.mult),
           ["kk", "einv", "at"], ["bh"])
        dv(lambda e: e.tensor_tensor(out=F["bh"][:], in0=F["beta"][:], in1=F["einv"][:], op=ALU.mult), ["beta", "eend"], ["kh"])
        dv(lambda e: e.tensor_tensor(out=F["kh"][:], in0=F["kp"][:], in1=F["einv"][:], op=ALU.mult), ["kp", "eend"], ["rt"])
        dv(lambda e: e.tensor_tensor(out=F["rt"][:], in0=r_, in1=F["einc"][:], op=ALU.mult), [X_tb, "eexc"], ["bc"])
        dv(lambda e: e.tensor_tensor(out=F["bc"][:], in0=F["beta"][:], in1=F["eend"][:], op=ALU.mult), ["beta", "at"], ["kc"])
        dv(lambda e: e.tensor_tensor(out=F["kc"][:], in0=F["kp"][:], in1=F["eend"][:], op=ALU.mult), ["kp", "at"], ["lw"])
        allf = [fm_tb[n] for n in ("bh", "kh", "rt", "bc", "kc", "lw")]
        ck(5)
        for nm, src, stb in (("at", F["at"], fm_tb["bh"]), ("bc", F["bc"], fm_tb["kc"]), ("kc", F["kc"], fm_tb["lw"]), ("v", None, X_tb)):
            for cc in range(3):
                pa, ptb = q128.next()
                s_ap = X[:, 6 + cc, :] if src is None else src[:, cc, :]
                P.op("pe", lambda e, pa=pa, s_ap=s_ap: e.transpose(out=pa, in_=s_ap, identity=ident), reads=[stb, cst_tb], writes=[ptb])
                P.op("act", lambda e, pa=pa, nm=nm, cc=cc: e.activation(out=tokm[nm][:, cc * 128:(cc + 1) * 128], in_=pa, func=AF.Identity),
                     reads=[ptb], writes=[tok_tb[nm]])
        ck(6)
        def _head(hd, b=b):
            cc, p0 = hd // 2, (hd % 2) * 64
            sl = slice(p0, p0 + 64)
            aT, bT, kT, rT = F["at"][sl, cc, :], F["bh"][sl, cc, :], F["kh"][sl, cc, :], F["rt"][sl, cc, :]
            tcol = slice(hd * 64, hd * 64 + 64)
            MX, MT, MKT, NBT, NKT, DG = (HW[hd % 2][n] for n in ("MX", "MT", "MKT", "NBT", "NKT", "DG"))
            MX_tb, MT_tb, MKT_tb, NBT_tb, NKT_tb, DG_tb = (HW_tb[hd % 2][n] for n in ("MX", "MT", "MKT", "NBT", "NKT", "DG"))

            def mm_mask(lhsT, rhs, mask, dst, dtb):
                pa, ptb = q128.next()
                P.op("pe", lambda e: e.matmul(pa, lhsT=lhsT, rhs=rhs, start=True, stop=True), reads=allf, writes=[ptb])
                P.op("dve", lambda e: e.tensor_tensor(out=dst, in0=pa, in1=mask, op=ALU.mult), reads=[ptb, cst_tb], writes=[dtb])

            mm_mask(aT, bT, LS, MX[:, 0:128], MX_tb)
            mm_mask(bT, aT, US, MT[:], MT_tb)
            mm_mask(kT, aT, US, MKT[:], MKT_tb)
            mm_mask(bT, rT, UI, NBT[:], NBT_tb)
            mm_mask(kT, rT, UI, NKT[:], NKT_tb)
            P.op("pool", lambda e, tcol=tcol: e.tensor_copy(out=MX[:, 128:192], in_=tokm["at"][:, tcol]), reads=[tok_tb["at"]], writes=[MX_tb])
            pa, ptb = q128.next()
            P.op("pe", lambda e, pa=pa, tcol=tcol: e.matmul(pa[:, 0:64], lhsT=MKT[:], rhs=tokm["v"][:, tcol], start=True, stop=True),
                 reads=[MKT_tb, tok_tb["v"]], writes=[ptb])
            P.op("act", lambda e, pa=pa: e.activation(out=MX[:, 192:256], in_=pa[:, 0:64], func=AF.Identity), reads=[ptb], writes=[MX_tb])
            for it in range(7):
                last = it == 6
                ph, phtb = h256.next()
                if not last:
                    P.op("pe", lambda e, ph=ph: e.matmul(ph, lhsT=MT[:], rhs=MX[:], start=True, stop=True), reads=[MT_tb, MX_tb], writes=[phtb])
                    pa, ptb = q128.next()
                    P.op("pe", lambda e, pa=pa: e.matmul(pa, lhsT=MX[:, 0:128], rhs=MT[:], start=True, stop=True), reads=[MT_tb, MX_tb], writes=[ptb])
                    P.op("act", lambda e, ph=ph: e.activation(out=MX[:, 0:128], in_=ph[:, 0:128], func=AF.Identity), reads=[phtb], writes=[MX_tb])
                    P.op("dve", lambda e, ph=ph: e.tensor_tensor(out=MX[:, 128:256], in0=MX[:, 128:256], in1=ph[:, 128:256], op=ALU.add),
                         reads=[phtb, MX_tb], writes=[MX_tb])
                    P.op("act", lambda e, pa=pa: e.activation(out=MT[:], in_=pa, func=AF.Identity), reads=[ptb], writes=[MT_tb])
                else:
                    P.op("pe", lambda e, ph=ph: e.matmul(ph[:, 128:256], lhsT=MT[:], rhs=MX[:, 128:256], start=True, stop=True),
                         reads=[MT_tb, MX_tb], writes=[phtb])
                    P.op("dve", lambda e, ph=ph: e.tensor_tensor(out=MX[:, 128:256], in0=MX[:, 128:256], in1=ph[:, 128:256], op=ALU.add),
                         reads=[phtb, MX_tb], writes=[MX_tb])
            W0, U0 = MX[:, 128:192], MX[:, 192:256]
            P.op("dve", lambda e, p0=p0, cc=cc: e.tensor_scalar(out=DG[:, 0:64], in0=cst[:, 0, p0:p0 + 64], scalar1=GCe[:, cc:cc + 1], scalar2=None, op0=ALU.mult),
                 reads=[cst_tb, fm_tb["at"]], writes=[DG_tb])
            pa, ptb = q128.next()
            P.op("pe", lambda e, pa=pa, tcol=tcol: e.matmul(pa[0:64, 0:64], lhsT=W0, rhs=tokm["bc"][:, tcol], start=True, stop=False),
                 reads=[MX_tb, tok_tb["bc"]], writes=[ptb])
            P.op("pe", lambda e, pa=pa, p0=p0: e.matmul(pa[0:64, 0:64], lhsT=cst[:, 0, p0:p0 + 64], rhs=DG[:, 0:64], start=False, stop=True),
                 reads=[DG_tb, cst_tb], writes=[ptb], acc=True)
            P.op("act", lambda e, pa=pa, hd=hd: e.activation(out=ATb[:, hd, :], in_=pa[0:64, 0:64], func=AF.Identity), reads=[ptb], writes=[AD_tb[hd]])
            pa, ptb = q128.next()
            P.op("pe", lambda e, pa=pa, tcol=tcol: e.matmul(pa[0:64, 0:64], lhsT=tokm["bc"][:, tcol], rhs=U0, start=True, stop=False),
                 reads=[MX_tb, tok_tb["bc"]], writes=[ptb])
            P.op("pe", lambda e, pa=pa, tcol=tcol: e.matmul(pa[0:64, 0:64], lhsT=tokm["kc"][:, tcol], rhs=tokm["v"][:, tcol], start=False, stop=True),
                 reads=[tok_tb["kc"], tok_tb["v"]], writes=[ptb], acc=True)
            P.op("act", lambda e, pa=pa, hd=hd: e.activation(out=Db[:, hd, :], in_=pa[0:64, 0:64], func=AF.Identity), reads=[ptb], writes=[AD_tb[hd]])
            pa, ptb = q128.next()
            P.op("pe", lambda e, pa=pa: e.matmul(pa[0:64, :], lhsT=W0, rhs=NBT[:], start=True, stop=False), reads=[MX_tb, NBT_tb], writes=[ptb])
            P.op("pe", lambda e, pa=pa, p0=p0, cc=cc: e.matmul(pa[0:64, :], lhsT=cst[:, 0, p0:p0 + 64], rhs=F["rt"][:, cc, :], start=False, stop=True),
                 reads=[cst_tb] + allf, writes=[ptb], acc=True)
            oa, otb = ost_s.next()
            P.op("act", lambda e, pa=pa, oa=oa: e.activation(out=oa[0:64, :], in_=pa[0:64, :], func=AF.Identity), reads=[ptb], writes=[otb])
            out_dma(dr["o_YwT"][b, hd], oa[0:64, :], otb)
            pa, ptb = q128.next()
            P.op("pe", lambda e, pa=pa: e.matmul(pa[0:64, :], lhsT=U0, rhs=NBT[:], start=True, stop=False), reads=[MX_tb, NBT_tb], writes=[ptb])
            P.op("pe", lambda e, pa=pa, tcol=tcol: e.matmul(pa[0:64, :], lhsT=tokm["v"][:, tcol], rhs=NKT[:], start=False, stop=True),
                 reads=[tok_tb["v"], NKT_tb], writes=[ptb], acc=True)
            oa, otb = ost_s.next()
            P.op("act", lambda e, pa=pa, oa=oa: e.activation(out=oa[0:64, :], in_=pa[0:64, :], func=AF.Identity), reads=[ptb], writes=[otb])
            out_dma(dr["o_Y0T"][b, hd], oa[0:64, :], otb)
            pa, ptb = q128.next()
            P.op("pe", lambda e, pa=pa, hd=hd: e.matmul(pa[0:64, :], lhsT=ATb[:, hd, :], rhs=PQ[:, hd, :], start=True, stop=True),
                 reads=[AD_tb[hd], PQ_tb[hd]], writes=[ptb])
            pa2, ptb2 = q128.next()
            P.op("pe", lambda e, pa2=pa2, hd=hd: e.matmul(pa2[0:64, 0:64], lhsT=PQ[:, hd, 0:64], rhs=ATb[:, hd, :], start=True, stop=True),
                 reads=[AD_tb[hd], PQ_tb[hd]], writes=[ptb2])
            P.op("act", lambda e, pa=pa, hd=hd: e.activation(out=PQ[:, hd, 0:64], in_=pa[0:64, 0:64], func=AF.Identity), reads=[ptb], writes=[PQ_tb[hd]])
            P.op("dve", lambda e, pa=pa, hd=hd: e.tensor_tensor(out=PQ[:, hd, 64:128], in0=pa[0:64, 64:128], in1=Db[:, hd, :], op=ALU.add),
                 reads=[ptb, AD_tb[hd]], writes=[PQ_tb[hd]])
            P.op("act", lambda e, pa2=pa2, hd=hd: e.activation(out=PTt[:, hd, :], in_=pa2[0:64, 0:64], func=AF.Identity), reads=[ptb2], writes=[PQ_tb[hd]])
        for hd in range(6):
            _head(hd)
        ck(7)
        emit_pq_out(b + 1)

        for m in range(2):
            def ev(pa, ptb, m=m):
                oa, otb = obf_s.next()
                P.op("act", lambda e: e.activation(out=oa[:, 0:128], in_=pa, func=AF.Identity), reads=[ptb], writes=[otb])
                for hh_ in range(2):
                    h = 2 * m + hh_
                    s2 = slice(hh_ * 64, hh_ * 64 + 64)
                    pq, pqtb = q128.next()
                    P.op("pe", lambda e, pq=pq, s2=s2: e.matmul(pq, lhsT=wuk[s2, m, :], rhs=oa[s2, 0:128], start=True, stop=True), reads=[otb, par_tb], writes=[pqtb])
                    ob, obtb = obf_s.next()
                    P.op("act", lambda e, pq=pq, ob=ob: e.activation(out=ob[:, 0:128], in_=pq, func=AF.Identity, scale=0.125), reads=[pqtb], writes=[obtb])
                    out_dma(dr["o_qlat"][:, h, t0:t0 + 128], ob[:, 0:128], obtb)
            projT(C_DQ + m * 128, ev)
        for m in range(3):
            def ev(pa, ptb, m=m):
                oa, otb = obf_s.next()
                P.op("act", lambda e: e.activation(out=oa[:, 0:128], in_=pa, func=AF.Identity), reads=[ptb], writes=[otb])
                out_dma(dr["o_sq"][m * 128:(m + 1) * 128, t0:t0 + 128], oa[:, 0:128], otb)
            projT(C_SQ + m * 128, ev)

        def ev(pa, ptb):
            oa, otb = obf_s.next()
            P.op("act", lambda e: e.activation(out=oa[:, 0:128], in_=pa, func=AF.Identity), reads=[ptb], writes=[otb])
            out_dma(dr["o_sk"][:, t0:t0 + 128], oa[:, 0:128], otb)
        projT(C_SK, ev)
        ck(8)
        for (col0, ncol, off, bank) in ((C_CKV, 452, 0, 0), (C_SV, 128, 452, 1)):
            for kc in range(8):
                P.op("pe", lambda e, kc=kc, col0=col0, ncol=ncol, bank=bank: e.matmul(pbk[bank][:, 0:ncol], lhsT=hT[:, kc, :], rhs=win[:, kc, col0:col0 + ncol],
                                                                                      start=(kc == 0), stop=(kc == 7)),
                     reads=[win_tb, hT_tb], writes=[pT_tb[bank]], acc=True)
            P.op("act", lambda e, ncol=ncol, off=off, bank=bank: e.activation(out=tk[:, off:off + ncol], in_=pbk[bank][:, 0:ncol], func=AF.Identity),
                 reads=[pT_tb[bank]], writes=[tk_tb])
        N = nsc
        nv = lambda fn: P.op("dve", fn, reads=[tk_tb, nsc_tb, par_tb], writes=[nsc_tb])
        nv(lambda e: e.tensor_tensor(out=N["t"][:, 0:128], in0=tk[:, 0:128], in1=tk[:, 0:128], op=ALU.mult))
        nv(lambda e: e.tensor_reduce(out=N["rs"][:, 0:1], in_=N["t"][:, 0:128], axis=AX.X, op=ALU.add))
        P.op("act", lambda e: e.activation(out=N["rs"][:, 0:1], in_=N["rs"][:, 0:1], func=AF.Sqrt, scale=1.0 / 128, bias=eps6[:, 0:1]),
             reads=[nsc_tb, par_tb], writes=[nsc_tb])
        nv(lambda e: e.reciprocal(out=N["rs"][:, 0:1], in_=N["rs"][:, 0:1]))
        nv(lambda e: e.scalar_tensor_tensor(out=N["t"][:, 0:128], in0=tk[:, 0:128], scalar=N["rs"][:, 0:1], in1=bcp[:, 0:128], op0=ALU.mult, op1=ALU.mult))
        oa, otb = obf_s.next()
        P.op("dve", lambda e, oa=oa: e.tensor_copy(out=oa[:, 0:128], in_=N["t"][:, 0:128]), reads=[nsc_tb], writes=[otb])
        out_dma(dr["o_ckv"][t0:t0 + 128, :], oa[:, 0:128], otb)
        pa, ptb = q128.next()
        pab = pa.bitcast(BF16)
        P.op("pe", lambda e, pab=pab, oa=oa: e.transpose(out=pab[:, 0:128], in_=oa[:, 0:128], identity=identb[:]), reads=[otb, cst_tb], writes=[ptb])
        P.op("act", lambda e, pab=pab, oa=oa: e.activation(out=oa[:, 128:256], in_=pab[:, 0:128], func=AF.Identity), reads=[ptb], writes=[otb])
        out_dma(dr["o_ckvT"][:, t0:t0 + 128], oa[:, 128:256], otb)
        nv(lambda e: e.bn_stats(out=N["st"][:], in_=tk[:, 384:448]))
        nv(lambda e: e.bn_aggr(out=N["mv"][:], in_=N["st"][:]))
        P.op("act", lambda e: e.activation(out=N["rs"][:, 1:2], in_=N["mv"][:, 1:2], func=AF.Sqrt, scale=1.0, bias=eps6[:, 1:2]),
             reads=[nsc_tb, par_tb], writes=[nsc_tb])
        nv(lambda e: e.reciprocal(out=N["rs"][:, 1:2], in_=N["rs"][:, 1:2]))
        nv(lambda e: e.tensor_scalar(out=N["t"][:, 128:192], in0=tk[:, 384:448], scalar1=N["mv"][:, 0:1], scalar2=N["rs"][:, 1:2], op0=ALU.subtract, op1=ALU.mult))
        nv(lambda e: e.tensor_tensor(out=N["t"][:, 128:192], in0=N["t"][:, 128:192], in1=bcp[:, 128:192], op=ALU.mult))
        oa, otb = obf_s.next()
        P.op("dve", lambda e, oa=oa: e.tensor_tensor(out=oa[:, 0:64], in0=N["t"][:, 128:192], in1=bcp[:, 192:256], op=ALU.add), reads=[nsc_tb, par_tb], writes=[otb])
        pa, ptb = q128.next()
        pab = pa.bitcast(BF16)
        P.op("pe", lambda e, pab=pab, oa=oa: e.transpose(out=pab[0:64, 0:128], in_=oa[:, 0:64], identity=identb[:]), reads=[otb, cst_tb], writes=[ptb])
        P.op("act", lambda e, pab=pab, oa=oa: e.activation(out=oa[0:64, 128:256], in_=pab[0:64, 0:128], func=AF.Identity), reads=[ptb], writes=[otb])
        out_dma(dr["o_ikT"][:, t0:t0 + 128], oa[0:64, 128:256], otb)
        P.op("act", lambda e: e.activation(out=N["sg"][:], in_=tk[:, 448:452], func=AF.Sign), reads=[tk_tb], writes=[nsc_tb])
        nv(lambda e: e.tensor_tensor(out=N["aiw"][:], in0=tk[:, 448:452], in1=N["sg"][:], op=ALU.mult))
        oa, otb = ost_s.next()
        P.op("dve", lambda e, oa=oa: e.tensor_copy(out=oa[:, 0:4], in_=N["sg"][:]), reads=[nsc_tb], writes=[otb])
        out_dma(dr["o_sgn"][t0:t0 + 128, :], oa[:, 0:4], otb)
        ob, obtb = obf_s.next()
        for h in range(4):
            P.op("dve", lambda e, h=h, ob=ob: e.tensor_scalar(out=ob[:, h * 64:(h + 1) * 64], in0=tk[:, 128 + h * 64:128 + (h + 1) * 64],
                                                              scalar1=N["aiw"][:, h:h + 1], scalar2=1.0 / 16, op0=ALU.mult, op1=ALU.mult),
                 reads=[tk_tb, nsc_tb], writes=[obtb])
        oc, octb = obf_s.next()
        for h in range(4):
            pa, ptb = q128.next()
            pab = pa.bitcast(BF16)
            P.op("pe", lambda e, pab=pab, ob=ob, h=h: e.transpose(out=pab[0:64, 0:128], in_=ob[:, h * 64:(h + 1) * 64], identity=identb[:]),
                 reads=[obtb, cst_tb], writes=[ptb])
            P.op("act", lambda e, pab=pab, oc=oc, h=h: e.activation(out=oc[0:64, h * 128:(h + 1) * 128], in_=pab[0:64, 0:128], func=AF.Identity),
                 reads=[ptb], writes=[octb])
        out_dma(dr["o_iqs"][:, :, t0:t0 + 128], oc[0:64, :].rearrange("p (h t) -> p h t", h=4), octb)
        od, odtb = obf_s.next()
        P.op("dve", lambda e, od=od: e.tensor_copy(out=od[:, 0:128], in_=tk[:, 452:580]), reads=[tk_tb], writes=[odtb])
        out_dma(dr["o_sv"][t0:t0 + 128, :], od[:, 0:128], odtb)


GN_EPS = 64e-5
NITER = 14
TOPK = 256


def emit_k2(P, nc, NBQ, dr, parts=("rwkv", "swa", "dsa")):
    NT = NBQ * 128
    NKT = 8 * NBQ
    seg_of = lambda j: (8 * j) // NBQ
    out_tb = dr["out_tb"]
    pb = [P.psum(f"k2pb{i}", [128, 512], F32) for i in range(8)]
    bank = [P.tb(f"k2bank{i}") for i in range(8)]
    cst = P.sbuf("cst2", [128, 128], F32)
    cst_tb = P.tb("cst2")
    P.op("sp", lambda e: e.dma_start(out=cst[:], in_=dr["ident"]), writes=[cst_tb], dma=True)
    identb = P.sbuf("identb2", [128, 128], BF16)
    onesb = P.sbuf("onesb", [128, 64], BF16)
    o64 = P.sbuf("o64", [64, 64], F32)
    P.op("dve", lambda e: e.tensor_copy(out=identb[:], in_=cst[:]), reads=[cst_tb], writes=[cst_tb])
    P.op("pool", lambda e: e.memset(onesb[:], 1.0), writes=[cst_tb])
    P.op("pool", lambda e: e.memset(o64[:], 1.0 / 64), writes=[cst_tb])
    obuf = [P.sbuf(f"k2o{i}", [64, 512], BF16) for i in range(4)]
    obs = Slots(P, [o[:] for o in obuf])

    def out_mix(head, j, src, stb):
        P.op("sp", lambda e: e.dma_start(out=dr["mixT"][head, :, j * 128:(j + 1) * 128], in_=src), reads=[stb], writes=[out_tb], dma=True)

    if "rwkv" in parts:
      with P.scope():
        rp = P.sbuf("rp", [64, 16], F32)
        rp_tb = P.tb("rp")
        P.op("sp", lambda e: e.dma_start(out=rp[:], in_=dr["rwkv_ln"]), writes=[rp_tb], dma=True)
        epsg = P.sbuf("epsg", [64, 1], F32)
        P.op("pool", lambda e: e.memset(epsg[:], GN_EPS), writes=[rp_tb])
        segP = P.sbuf("segP", [64, 8, 6, 64], F32)
        segQ = P.sbuf("segQ", [64, 8, 6, 64], F32)
        seg_tb = P.tb("seg")
        P.op("sp", lambda e: e.dma_start(out=segP[:], in_=dr["segPT"].rearrange("s h a b -> a s h b")), writes=[seg_tb], dma=True)
        P.op("sp", lambda e: e.dma_start(out=segQ[:], in_=dr["segQ"].rearrange("s h a b -> a s h b")), writes=[seg_tb], dma=True)
        St = P.sbuf("St", [64, 8, 6, 64], F32)
        St_tb = [P.tb(f"St{s}") for s in range(8)]
        P.op("pool", lambda e: e.memset(St[:, 0, :, :], 0.0), writes=[St_tb[0]])
        for s in range(7):
            for hd in range(6):
                P.op("pe", lambda e, s=s, hd=hd: e.matmul(pb[0][0:64, hd * 64:(hd + 1) * 64], lhsT=segP[:, s, hd, :], rhs=St[:, s, hd, :],
                                                          start=True, stop=True), reads=[seg_tb, St_tb[s]], writes=[bank[0]], acc=True)
            P.op("dve", lambda e, s=s: e.tensor_tensor(out=St[:, s + 1, :, :].rearrange("p h v -> p (h v)"), in0=pb[0][0:64, 0:384],
                                                       in1=segQ[:, s, :, :].rearrange("p h v -> p (h v)"), op=ALU.add),
                 reads=[bank[0], seg_tb], writes=[St_tb[s + 1]])
        rin = [{n: P.sbuf(f"r_{n}{i}", [64, 6, w], F32) for n, w in (("yw", 128), ("y0", 128), ("pt", 64), ("q", 64), ("bon", 128), ("g", 128))} for i in range(2)]
        rin_tb = [P.tb(f"rin{i}") for i in range(2)]
        Sb = P.sbuf("Sb", [64, 6, 64], F32)
        Sb_tb = P.tb("Sb")
        yy = P.sbuf("yy", [64, 6, 128], F32)
        cen = P.sbuf("cen", [64, 6, 128], F32)
        sq = P.sbuf("sqr", [64, 6, 128], F32)
        rsd = P.sbuf("rsd", [64, 6, 128], F32)
        ww_tb = P.tb("rwkvwork")
        for j in range(NBQ):
            I = rin[j % 2]
            itb = rin_tb[j % 2]
            for n, src in (("yw", dr["YwT"][j]), ("y0", dr["Y0T"][j]), ("pt", dr["PbT"][j]), ("q", dr["Qb"][j])):
                P.op("sp", lambda e, n=n, src=src, I=I: e.dma_start(out=I[n][:], in_=src.rearrange("h a b -> a h b")), writes=[itb], dma=True)
            P.op("sp", lambda e, I=I, j=j: e.dma_start(out=I["bon"][:], in_=dr["bon"][:, :, j * 128:(j + 1) * 128]), writes=[itb], dma=True)
            P.op("sp", lambda e, I=I, j=j: e.dma_start(out=I["g"][:], in_=dr["g"][:, :, j * 128:(j + 1) * 128]), writes=[itb], dma=True)
            s = seg_of(j)
            for hd in range(6):
                P.op("pe", lambda e, hd=hd, I=I, s=s: e.matmul(pb[0][0:64, hd * 64:(hd + 1) * 64], lhsT=I["pt"][:, hd, :], rhs=St[:, s, hd, :], start=True, stop=True),
                     reads=[itb, St_tb[s]], writes=[bank[0]], acc=True)
            P.op("dve", lambda e, I=I: e.tensor_tensor(out=Sb[:].rearrange("p h v -> p (h v)"), in0=pb[0][0:64, 0:384],
                                                       in1=I["q"][:].rearrange("p h v -> p (h v)"), op=ALU.add), reads=[bank[0], itb], writes=[Sb_tb])
            for half in range(2):
                bk = 1 + half
                for q in range(3):
                    hd = half * 3 + q
                    P.op("pe", lambda e, hd=hd, q=q, bk=bk, I=I: e.matmul(pb[bk][0:64, q * 128:(q + 1) * 128], lhsT=Sb[:, hd, :], rhs=I["yw"][:, hd, :], start=True, stop=True),
                         reads=[Sb_tb, itb], writes=[bank[bk]], acc=True)
                hs = slice(half * 3, half * 3 + 3)
                f3 = lambda t, hs=hs: t[:, hs, :].rearrange("p h t -> p (h t)")
                P.op("dve", lambda e, bk=bk, I=I, f3=f3: e.tensor_tensor(out=f3(yy), in0=pb[bk][0:64, 0:384], in1=f3(I["y0"]), op=ALU.add),
                     reads=[bank[bk], itb], writes=[ww_tb])
                P.op("pe", lambda e, bk=bk, f3=f3: e.matmul(pb[bk][0:64, 0:384], lhsT=o64[:], rhs=f3(yy), start=True, stop=True), reads=[ww_tb, cst_tb], writes=[bank[bk]])
                P.op("dve", lambda e, bk=bk, f3=f3: e.tensor_tensor(out=f3(cen), in0=f3(yy), in1=pb[bk][0:64, 0:384], op=ALU.subtract), reads=[bank[bk], ww_tb], writes=[ww_tb])
                P.op("pool", lambda e, f3=f3: e.tensor_tensor(out=f3(sq), in0=f3(cen), in1=f3(cen), op=ALU.mult), reads=[ww_tb], writes=[ww_tb])
                P.op("pe", lambda e, bk=bk, f3=f3: e.matmul(pb[bk][0:64, 0:384], lhsT=o64[:], rhs=f3(sq), start=True, stop=True), reads=[ww_tb, cst_tb], writes=[bank[bk]])
                P.op("act", lambda e, bk=bk, f3=f3: e.activation(out=f3(rsd), in_=pb[bk][0:64, 0:384], func=AF.Sqrt, bias=epsg[:, 0:1], scale=1.0),
                     reads=[bank[bk], rp_tb], writes=[ww_tb])
                P.op("dve", lambda e, f3=f3: e.reciprocal(out=f3(rsd), in_=f3(rsd)), reads=[ww_tb], writes=[ww_tb])
                P.op("dve", lambda e, f3=f3: e.tensor_tensor(out=f3(cen), in0=f3(cen), in1=f3(rsd), op=ALU.mult), reads=[ww_tb], writes=[ww_tb])
                ob, obtb = obs.next()
                for q in range(3):
                    hd = half * 3 + q
                    P.op("dve", lambda e, hd=hd: e.tensor_scalar(out=cen[:, hd, :], in0=cen[:, hd, :], scalar1=rp[:, hd:hd + 1], scalar2=rp[:, 6 + hd:7 + hd],
                                                                 op0=ALU.mult, op1=ALU.add), reads=[ww_tb, rp_tb], writes=[ww_tb])
                P.op("pool", lambda e, f3=f3, I=I: e.tensor_tensor(out=f3(cen), in0=f3(cen), in1=f3(I["bon"]), op=ALU.add), reads=[ww_tb, itb], writes=[ww_tb])
                P.op("pool", lambda e, f3=f3, I=I, ob=ob: e.tensor_tensor(out=ob[:, 0:384], in0=f3(cen), in1=f3(I["g"]), op=ALU.mult), reads=[ww_tb, itb], writes=[obtb])
                P.op("sp", lambda e, ob=ob, j=j, half=half: e.dma_start(out=dr["mixT"][half * 3:half * 3 + 3, :, j * 128:(j + 1) * 128].rearrange("h p t -> p h t"),
                                                                          in_=ob[:, 0:384].rearrange("p (h t) -> p h t", h=3)), reads=[obtb], writes=[out_tb], dma=True)

    if "swa" in parts:
      with P.scope():
        swb = P.sbuf("swb", [128, 2, 6, 128], F32)
        swbf = P.sbuf("swbf", [128, 6, 128], F32)
        sw_tb = P.tb("swc")
        P.op("sp", lambda e: e.dma_start(out=swb[:], in_=dr["swb"]), writes=[sw_tb], dma=True)
        P.op("sp", lambda e: e.dma_start(out=swbf[:], in_=dr["swb_first"]), writes=[sw_tb], dma=True)
        es = P.sbuf("esink", [64, 6], F32)
        P.op("sp", lambda e: e.dma_start(out=es[:], in_=dr["sink_b"]), writes=[sw_tb], dma=True)
        P.op("act", lambda e: e.activation(out=es[:], in_=es[:], func=AF.Exp), reads=[sw_tb], writes=[sw_tb])
        sin = [{"q": P.sbuf(f"s_q{i}", [64, 6, 128], BF16), "k": P.sbuf(f"s_k{i}", [64, 2, 256], BF16), "v": P.sbuf(f"s_v{i}", [128, 2, 128], BF16)} for i in range(2)]
        sin_tb = [P.tb(f"sin{i}") for i in range(2)]
        stmp = P.sbuf("stmp", [128, 384], F32)
        sE = [P.sbuf(f"sE{i}", [128, 384], BF16) for i in range(2)]
        sE_tb = [P.tb(), P.tb()]
        st_tb = P.tb("stmp")
        srec = P.sbuf("srec", [64, 384], F32)
        for j in range(NBQ):
            I = sin[j % 2]
            itb = sin_tb[j % 2]
            P.op("sp", lambda e, I=I, j=j: e.dma_start(out=I["q"][:], in_=dr["sq"][:, :, j * 128:(j + 1) * 128]), writes=[itb], dma=True)
            P.op("sp", lambda e, I=I, j=j: e.dma_start(out=I["k"][:], in_=dr["sk2"][:, :, j, :]), writes=[itb], dma=True)
            P.op("sp", lambda e, I=I, j=j: e.dma_start(out=I["v"][:], in_=dr["sv2"][j].rearrange("k p c -> p k c")), writes=[itb], dma=True)
            for g in range(2):
                bn, bd = 5, 6
                for kt2 in range(2):
                    bs = 3 + kt2
                    P.op("pe", lambda e, g=g, kt2=kt2, bs=bs, I=I: e.matmul(pb[bs][:, 0:384], lhsT=I["k"][:, g, kt2 * 128:(kt2 + 1) * 128],
                                                                            rhs=I["q"][:, 3 * g:3 * g + 3, :], start=True, stop=True),
                         reads=[itb], writes=[bank[bs]])
                    btab = swbf[:, 3 * g:3 * g + 3, :] if (j == 0 and kt2 == 0) else swb[:, kt2, 3 * g:3 * g + 3, :]
                    P.op("dve", lambda e, bs=bs, btab=btab: e.scalar_tensor_tensor(out=stmp[:].rearrange("p (h t) -> p h t", h=3), in0=pb[bs][:, 0:384].rearrange("p (h t) -> p h t", h=3),
                                                                                  scalar=0.125, in1=btab, op0=ALU.mult, op1=ALU.add),
                         reads=[bank[bs], sw_tb], writes=[st_tb])
                    E, etb = sE[kt2], sE_tb[kt2]
                    P.op("act", lambda e, E=E: e.activation(out=E[:], in_=stmp[:], func=AF.Exp), reads=[st_tb], writes=[etb])
                    P.op("pe", lambda e, E=E, g=g, kt2=kt2, I=I: e.matmul(pb[bn][0:64, 0:384], lhsT=I["v"][:, kt2, g * 64:(g + 1) * 64], rhs=E[:],
                                                                          start=(kt2 == 0), stop=(kt2 == 1)), reads=[etb, itb], writes=[bank[bn]], acc=True)
                    P.op("pe", lambda e, E=E, kt2=kt2: e.matmul(pb[bd][0:64, 0:384], lhsT=onesb[:], rhs=E[:], start=(kt2 == 0), stop=(kt2 == 1)),
                         reads=[etb, cst_tb], writes=[bank[bd]], acc=True)
                for q in range(3):
                    P.op("dve", lambda e, q=q, g=g: e.tensor_scalar(out=srec[:, q * 128:(q + 1) * 128], in0=pb[bd][0:64, q * 128:(q + 1) * 128],
                                                                    scalar1=es[:, 3 * g + q:3 * g + q + 1], scalar2=None, op0=ALU.add),
                         reads=[bank[bd], sw_tb], writes=[st_tb])
                P.op("dve", lambda e: e.reciprocal(out=srec[:], in_=srec[:]), reads=[st_tb], writes=[st_tb])
                ob, obtb = obs.next()
                P.op("dve", lambda e, ob=ob: e.tensor_tensor(out=ob[:, 0:384], in0=pb[bn][0:64, 0:384], in1=srec[:], op=ALU.mult), reads=[bank[bn], st_tb], writes=[obtb])
                P.op("sp", lambda e, ob=ob, j=j, g=g: e.dma_start(out=dr["mixT"][10 + 3 * g:13 + 3 * g, :, j * 128:(j + 1) * 128].rearrange("h p t -> p h t"),
                                                                    in_=ob[:, 0:384].rearrange("p (h t) -> p h t", h=3)), reads=[obtb], writes=[out_tb], dma=True)

    if "dsa" in parts:
      with P.scope():
        ckvT = P.sbuf("ckvT", [128, NKT * 128], BF16)
        ckv = P.sbuf("ckv", [128, NKT, 128], BF16)
        key_tb = P.tb("keys")
        for c in range(0, NKT, 16):
            P.op("sp", lambda e, c=c: e.dma_start(out=ckvT[:, c * 128:(c + 16) * 128], in_=dr["ckvT"][:, c * 128:(c + 16) * 128]), writes=[key_tb], dma=True)
            P.op("sp", lambda e, c=c: e.dma_start(out=ckv[:, c:c + 16, :], in_=dr["ckv"][c * 128:(c + 16) * 128, :].rearrange("(k p) r -> p k r", p=128)),
                 writes=[key_tb], dma=True)
        ikc = [P.sbuf(f"ikc{i}", [64, 512], BF16) for i in range(3)]
        ikc_tb = [P.tb() for _ in range(3)]
        wuv_f = P.sbuf("wuv_f", [128, 4, 64], F32)
        wuv = P.sbuf("wuv", [128, 4, 64], BF16)
        dpar_tb = P.tb("dpar")
        P.op("sp", lambda e: e.dma_start(out=wuv_f[:], in_=dr["wuv"].rearrange("h r d -> r h d")), writes=[dpar_tb], dma=True)
        P.op("dve", lambda e: e.tensor_copy(out=wuv[:], in_=wuv_f[:]), reads=[dpar_tb], writes=[dpar_tb])
        dmask = P.sbuf("dmask", [128, 1024], F32)
        ab = P.sbuf("ab", [128, 4, 128], F32)
        P.op("sp", lambda e: e.dma_start(out=dmask[:], in_=dr["dmask"]), writes=[dpar_tb], dma=True)
        P.op("sp", lambda e: e.dma_start(out=ab[:], in_=dr["ab"]), writes=[dpar_tb], dma=True)
        Isc = P.sbuf("Isc", [128, NKT * 128], F32)
        Isc_tb = P.tb("Isc")
        msk = P.sbuf("msk", [128, NKT * 128], BF16)
        msk_tb = P.tb("msk")
        qin = [{"ql": P.sbuf(f"d_ql{i}", [128, 4, 128], BF16), "iq": P.sbuf(f"d_iq{i}", [64, 4, 128], BF16), "sg": P.sbuf(f"d_sg{i}", [128, 4], F32)} for i in range(2)]
        qin_tb = [P.tb(), P.tb()]
        rl = [P.sbuf(f"rl{i}", [128, 512], F32) for i in range(2)]
        rl_tb = [P.tb(), P.tb()]
        bs_ = {n: P.sbuf("bs_" + n, [128, 1], F32) for n in ("lo", "hi", "mid", "cnt", "ge", "d")}
        bs_tb = P.tb("bisect")
        Eb = [P.sbuf(f"Eb{i}", [128, 512], BF16) for i in range(2)]
        Eb_tb = [[P.tb() for _ in range(4)] for _ in range(2)]
        PTb = [P.sbuf(f"PTb{i}", [128, 512], BF16) for i in range(2)]
        PT_tb = [P.tb(), P.tb()]
        mT = [P.sbuf(f"mT{i}", [128, 128], BF16) for i in range(2)]
        mT_tb = [P.tb(), P.tb()]
        olat = P.sbuf("olat", [128, 512], BF16)
        drec = P.sbuf("drec", [64, 512], F32)
        fin_tb = P.tb("dsafin")
        junk = P.sbuf("junk8", [128, NKT * 128], mybir.dt.uint8)
        junk_tb = P.tb("junk8")
        B = bs_

        def emitA(j):
            nkt = 8 * (j + 1)
            n = nkt * 128
            Q = qin[j % 2]
            qtb = qin_tb[j % 2]
            P.op("sp", lambda e: e.dma_start(out=Q["ql"][:], in_=dr["qlat"][:, :, j * 128:(j + 1) * 128]), writes=[qtb], dma=True)
            P.op("sp", lambda e: e.dma_start(out=Q["iq"][:], in_=dr["iqs"][:, :, j * 128:(j + 1) * 128]), writes=[qtb], dma=True)
            P.op("sp", lambda e: e.dma_start(out=Q["sg"][:], in_=dr["sgn"][j * 128:(j + 1) * 128, :]), writes=[qtb], dma=True)
            for c4 in range(nkt // 4):
                ii = c4 % 3
                P.op("sp", lambda e, c4=c4, ii=ii: e.dma_start(out=ikc[ii][:], in_=dr["ikT"][:, c4 * 512:(c4 + 1) * 512]), writes=[ikc_tb[ii]], dma=True)
                for h in range(4):
                    bk = (c4 * 4 + h) % 2
                    P.op("pe", lambda e, h=h, bk=bk, ii=ii: e.matmul(pb[bk][:], lhsT=Q["iq"][:, h, :], rhs=ikc[ii][:], start=True, stop=True),
                         reads=[qtb, ikc_tb[ii]], writes=[bank[bk]])
                    P.op("act", lambda e, bk=bk: e.activation(out=rl[bk][:], in_=pb[bk][:], func=AF.Relu), reads=[bank[bk]], writes=[rl_tb[bk]])
                    dst = Isc[:, c4 * 512:(c4 + 1) * 512]
                    if h == 0:
                        P.op("dve", lambda e, bk=bk, dst=dst: e.tensor_scalar(out=dst, in0=rl[bk][:], scalar1=Q["sg"][:, 0:1], scalar2=None, op0=ALU.mult),
                             reads=[rl_tb[bk], qtb], writes=[Isc_tb])
                    else:
                        P.op("dve", lambda e, bk=bk, dst=dst, h=h: e.scalar_tensor_tensor(out=dst, in0=rl[bk][:], scalar=Q["sg"][:, h:h + 1], in1=dst,
                                                                                         op0=ALU.mult, op1=ALU.add), reads=[rl_tb[bk], qtb, Isc_tb], writes=[Isc_tb])
            P.op("dve", lambda e: e.tensor_tensor(out=Isc[:, n - 1024:n], in0=Isc[:, n - 1024:n], in1=dmask[:], op=ALU.add), reads=[Isc_tb, dpar_tb], writes=[Isc_tb])
            bv = lambda fn: P.op("dve", fn, reads=[bs_tb, Isc_tb], writes=[bs_tb])
            bv(lambda e: e.memset(B["lo"][:], -64.0))
            bv(lambda e: e.tensor_reduce(out=B["hi"][:], in_=Isc[:, 0:n], axis=AX.X, op=ALU.max))
            bv(lambda e: e.tensor_scalar(out=B["d"][:], in0=B["hi"][:], scalar1=64.0, scalar2=None, op0=ALU.add))

        def bisect_iter(j, it):
            n = 8 * (j + 1) * 128
            ck_ = 0.5 ** (it + 1)
            bv = lambda fn: P.op("dve", fn, reads=[bs_tb, Isc_tb], writes=[bs_tb])
            bv(lambda e: e.scalar_tensor_tensor(out=B["mid"][:], in0=B["d"][:], scalar=ck_, in1=B["lo"][:], op0=ALU.mult, op1=ALU.add))
            P.op("dve", lambda e: e.tensor_scalar(out=junk[:, 0:n], in0=Isc[:, 0:n], scalar1=B["mid"][:, 0:1], scalar2=0.0, op0=ALU.is_ge, op1=ALU.add,
                                                  accum_out=B["cnt"][:]), reads=[bs_tb, Isc_tb], writes=[bs_tb, junk_tb])
            bv(lambda e: e.tensor_scalar(out=B["ge"][:], in0=B["cnt"][:], scalar1=float(TOPK) - 0.5, scalar2=ck_, op0=ALU.is_ge, op1=ALU.mult))
            bv(lambda e: e.scalar_tensor_tensor(out=B["lo"][:], in0=B["ge"][:], scalar=B["d"][:, 0:1], in1=B["lo"][:], op0=ALU.mult, op1=ALU.add))

        def final_mask(j):
            n = 8 * (j + 1) * 128
            P.op("dve", lambda e: e.tensor_scalar(out=msk[:, 0:n], in0=Isc[:, 0:n], scalar1=B["lo"][:, 0:1], scalar2=None, op0=ALU.is_ge),
                 reads=[bs_tb, Isc_tb], writes=[msk_tb])

        def tileB(j, kt):
            nkt = 8 * (j + 1)
            Q = qin[j % 2]
            qtb = qin_tb[j % 2]
            i2 = kt % 2
            dl = nkt - 1 - kt
            pmT = pb[2].bitcast(BF16)
            P.op("pe", lambda e: e.transpose(out=pmT[:, 0:128], in_=msk[:, kt * 128:(kt + 1) * 128], identity=identb[:]),
                 reads=[msk_tb, cst_tb], writes=[bank[2]])
            bS = 3 + i2
            P.op("pe", lambda e: e.matmul(pb[bS][:], lhsT=ckvT[:, kt * 128:(kt + 1) * 128], rhs=Q["ql"][:], start=True, stop=True),
                 reads=[key_tb, qtb], writes=[bank[bS]])
            for h in range(4):
                P.op("act", lambda e, h=h: e.activation(out=Eb[i2][:, h * 128:(h + 1) * 128], in_=pb[bS][:, h * 128:(h + 1) * 128], func=AF.Exp,
                                                        bias=ab[:, h, dl:dl + 1], scale=1.0), reads=[bank[bS], dpar_tb], writes=[Eb_tb[i2][h]])
            P.op("act", lambda e: e.activation(out=mT[i2][:], in_=pmT[:, 0:128], func=AF.Identity), reads=[bank[2]], writes=[mT_tb[i2]])
            P.op("pool", lambda e: e.tensor_tensor(out=PTb[i2][:].rearrange("p (h t) -> p h t", h=4), in0=Eb[i2][:].rearrange("p (h t) -> p h t", h=4),
                                                   in1=mT[i2][:].unsqueeze(1).to_broadcast([128, 4, 128]), op=ALU.mult),
                 reads=Eb_tb[i2] + [mT_tb[i2]], writes=[PT_tb[i2]])
            P.op("pe", lambda e: e.matmul(pb[5][:], lhsT=ckv[:, kt, :], rhs=PTb[i2][:], start=(kt == 0), stop=(kt == nkt - 1)),
                 reads=[key_tb, PT_tb[i2]], writes=[bank[5]], acc=True)
            P.op("pe", lambda e: e.matmul(pb[6][0:64, :], lhsT=onesb[:], rhs=PTb[i2][:], start=(kt == 0), stop=(kt == nkt - 1)),
                 reads=[cst_tb, PT_tb[i2]], writes=[bank[6]], acc=True)

        def finB(j):
            P.op("act", lambda e: e.activation(out=olat[:], in_=pb[5][:], func=AF.Identity), reads=[bank[5]], writes=[fin_tb])
            P.op("dve", lambda e: e.reciprocal(out=drec[:], in_=pb[6][0:64, :]), reads=[bank[6]], writes=[fin_tb])
            for h in range(4):
                P.op("pe", lambda e, h=h: e.matmul(pb[7][0:64, h * 128:(h + 1) * 128], lhsT=wuv[:, h, :], rhs=olat[:, h * 128:(h + 1) * 128], start=True, stop=True),
                     reads=[fin_tb, dpar_tb], writes=[bank[7]], acc=True)
            ob, obtb = obs.next()
            P.op("dve", lambda e: e.tensor_tensor(out=ob[:, 0:512], in0=pb[7][0:64, :], in1=drec[:], op=ALU.mult), reads=[bank[7], fin_tb], writes=[obtb])
            P.op("sp", lambda e: e.dma_start(out=dr["mixT"][6:10, :, j * 128:(j + 1) * 128].rearrange("h p t -> p h t"),
                                             in_=ob[:, 0:512].rearrange("p (h t) -> p h t", h=4)), reads=[obtb], writes=[out_tb], dma=True)

        emitA(0)
        for it in range(NITER):
            bisect_iter(0, it)
        final_mask(0)
        for j in range(NBQ):
            tiles = list(range(8 * (j + 1)))
            if j + 1 < NBQ:
                emitA(j + 1)
                per = -(-len(tiles) // NITER)
                for it in range(NITER):
                    bisect_iter(j + 1, it)
                    for kt in tiles[it * per:(it + 1) * per]:
                        tileB(j, kt)
                for kt in tiles[NITER * per:]:
                    tileB(j, kt)
            else:
                for kt in tiles:
                    tileB(j, kt)
            finB(j)
            if j + 1 < NBQ:
                final_mask(j + 1)


def emit_ffn(P, nc, NT, exp_ids, dr, ident, ident_tb):
    PT = min(1024, NT)
    NB = PT // 128
    npass = NT // PT
    NX = len(exp_ids)
    TW = min(512, PT)
    NTT = PT // TW
    BPT = TW // 128
    es = P.es
    wo_tb = P.tb("wo")
    stg = [P.sbuf(f"stg{i}", [128, 2048], F32) for i in range(3)]
    stg_tb = [P.tb(f"stg{i}") for i in range(3)]
    rw = P.sbuf("rw", [128, 8, NE], F32)
    rw_tb = P.tb("rw")
    rbias = P.sbuf("rbias", [128, NE], F32)
    bc = [P.sbuf(f"bc{i}", [128, D], F32) for i in range(3)]
    bc_tb = [P.tb(f"bc{i}") for i in range(3)]
    modT = P.sbuf("modT", [128, 48], F32)
    modT_tb = P.tb("modT")
    eps_t = P.sbuf("eps_t", [128, 1], F32)
    xm = P.sbuf("xm", [128, NB, D], F32)
    xm_tb = [P.tb(f"xm{b}") for b in range(NB)]
    yacc = P.sbuf("yacc", [128, NB, D], F32)
    ya_tb = [P.tb(f"ya{b}") for b in range(NB)]
    h2T = P.sbuf("h2T", [128, 8, PT], BF16)
    h2_tb = [P.tb(f"h2{b}") for b in range(NB)]
    h2f_tb = P.tb("h2f")
    gateT = P.sbuf("gateT", [65, PT], BF16)
    gT_tb = [P.tb(f"gT{b}") for b in range(NB)]
    xr = [P.sbuf(f"xr{i}", [128, D], F32) for i in range(2)]
    xr_tb = [P.tb(f"xr{i}") for i in range(2)]
    zt = P.sbuf("zt", [128, D], F32)
    zt_tb = P.tb("zt")
    lnsc = {"st": P.sbuf("ln_st", [128, 2, 6], F32), "mv": P.sbuf("ln_mv", [128, 2], F32),
            "rs": P.sbuf("ln_rs", [128, 1], F32), "xn": P.sbuf("ln_xn", [128, D], F32),
            "tb": P.tb("ln_s"), "xn_tb": P.tb("ln_xn")}
    rt_tb = P.tb("rt")
    wb_tb = [{k: P.tb(f"{k}b{i}") for k in ("w1", "w3", "w2")} for i in range(2)]
    selt_tb = P.tb("selt")
    Gb_tb = P.tb("Gb")
    ssb_tb = [P.tb(f"ssb{i}") for i in range(2)]
    gg_tb = [[P.tb(f"gg{fc}_{tt}") for tt in range(NTT)] for fc in range(2)]
    pb = [P.psum(f"pb{i}", [128, 512], F32) for i in range(8)]
    pb_tb = [P.tb(f"pb{i}") for i in range(8)]

    P.op("pool", lambda e: e.memset(eps_t[:], LN_EPS), writes=[modT_tb])
    P.op("sp", lambda e: e.dma_start(out=modT[:], in_=dr["modT"]), writes=[modT_tb], dma=True)
    P.op("dve", lambda e: e.tensor_scalar(out=modT[:, 32:40], in0=modT[:, 32:40], scalar1=1.0, scalar2=None,
                                          op0=ALU.add), reads=[modT_tb], writes=[modT_tb])
    P.op("sp", lambda e: e.dma_start(out=rw[:], in_=dr["router_w"].rearrange("(kc p) n -> p kc n", p=128)),
         writes=[rw_tb], dma=True)
    P.op("sp", lambda e: e.dma_start(out=rbias[:], in_=dr["rbias_b"]), writes=[rw_tb], dma=True)
    def load_bc(i, src):
        P.op("sp", lambda e: e.dma_start(out=bc[i][:], in_=src), writes=[bc_tb[i]], dma=True)

    def _pass(ps):
        t0 = ps * PT
        load_bc(0, dr["modb"][:, 2 * D:3 * D])
        load_bc(1, dr["lnp"][:, 0, :])
        load_bc(2, dr["lnp"][:, 1, :])
        P.op("pool", lambda e: e.memset(gateT[64:65, :], 1.0), writes=gT_tb)
        _sc1 = P.scope()
        _sc1.__enter__()
        wo = P.sbuf(f"p{ps}_wo", [64, 16, D], BF16)
        mixb = [P.sbuf(f"p{ps}_mixb{i}", [64, 16, 128], BF16) for i in range(2)]
        mixb_tb = [P.tb(f"mixb{i}") for i in range(2)]
        h2f = P.sbuf(f"p{ps}_h2f", [128, 8, 128], F32)
        rt = {k: P.sbuf(f"p{ps}_rt_" + k, [128, n], F32) for k, n in
              [("sc", 64), ("sel", 64), ("eq", 64), ("sel2", 64), ("m1", 8), ("m2", 8), ("grp", 8), ("t8", 8),
               ("gm", 8), ("g4", 8), ("selm", 64), ("em", 64), ("gt", 64), ("den", 1), ("gate", 64)]}
        for c in range(16):
            s_ = c % 3
            P.op("sp", lambda e, c=c, s_=s_: e.dma_start(out=stg[s_][0:64, 0:D], in_=dr["w_out"][c * 64:(c + 1) * 64, :]),
                 writes=[stg_tb[s_]], dma=True)
            P.op("pool", lambda e, c=c, s_=s_: e.tensor_copy(out=wo[:, c, :], in_=stg[s_][0:64, 0:D]),
                 reads=[stg_tb[s_]], writes=[wo_tb])


        for b in range(NB):
            tk = t0 + b * 128
            xi = b % 2
            P.op("sp", lambda e, xi=xi, tk=tk: e.dma_start(out=xr[xi][:], in_=dr["xres"][tk:tk + 128, :]),
                 writes=[xr_tb[xi]], dma=True)
            P.op("sp", lambda e, xi=xi, tk=tk: e.dma_start(out=mixb[xi][:], in_=dr["mixT"][:, :, tk:tk + 128].rearrange("h p t -> p h t")),
                 writes=[mixb_tb[xi]], dma=True)
            for hlf in range(2):
                for c in range(16):
                    P.op("pe", lambda e, c=c, hlf=hlf, xi=xi: e.matmul(
                        pb[hlf][:], lhsT=mixb[xi][:, c, :], rhs=wo[:, c, hlf * 512:(hlf + 1) * 512],
                        start=(c == 0), stop=(c == 15)), reads=[mixb_tb[xi], wo_tb], writes=[pb_tb[hlf]], acc=True)
            for hlf in range(2):
                P.op("dve", lambda e, hlf=hlf: e.tensor_tensor(out=zt[:, hlf * 512:(hlf + 1) * 512], in0=pb[hlf][:],
                                                               in1=bc[0][:, hlf * 512:(hlf + 1) * 512], op=ALU.mult),
                     reads=[pb_tb[hlf], bc_tb[0]], writes=[zt_tb])
            P.op("dve", lambda e, xi=xi: e.scalar_tensor_tensor(out=zt[:], in0=xr[xi][:], scalar=ALPHA, in1=zt[:],
                                                                op0=ALU.mult, op1=ALU.add),
                 reads=[xr_tb[xi], zt_tb], writes=[zt_tb])
            emit_layernorm(P, zt[:], zt_tb, xm[:, b, :], xm_tb[b], bc[1][:], bc[2][:], [bc_tb[1], bc_tb[2]], lnsc, eps_t, "m")
            for kc in range(8):
                bank = 2 + kc // 4
                P.op("pe", lambda e, kc=kc, bank=bank, b=b: e.transpose(
                    out=pb[bank][:, (kc % 4) * 128:(kc % 4 + 1) * 128], in_=xm[:, b, kc * 128:(kc + 1) * 128],
                    identity=ident[:]), reads=[xm_tb[b], ident_tb], writes=[pb_tb[bank]], acc=True)
            for kc in range(8):
                bank = 2 + kc // 4
                src = pb[bank][:, (kc % 4) * 128:(kc % 4 + 1) * 128]
                P.op("act", lambda e, kc=kc, src=src, b=b: e.activation(
                    out=h2T[:, kc, b * 128:(b + 1) * 128], in_=src, func=AF.Identity,
                    bias=modT[:, 24 + kc:25 + kc], scale=modT[:, 32 + kc:33 + kc]),
                    reads=[pb_tb[bank], modT_tb], writes=[h2_tb[b]])
                P.op("act", lambda e, kc=kc, src=src: e.activation(
                    out=h2f[:, kc, :], in_=src, func=AF.Identity,
                    bias=modT[:, 24 + kc:25 + kc], scale=modT[:, 32 + kc:33 + kc]),
                    reads=[pb_tb[bank], modT_tb], writes=[h2f_tb])
            for kc in range(8):
                P.op("pe", lambda e, kc=kc: e.matmul(pb[4][:, 0:NE], lhsT=h2f[:, kc, :], rhs=rw[:, kc, :],
                                                     start=(kc == 0), stop=(kc == 7)),
                     reads=[h2f_tb, rw_tb], writes=[pb_tb[4]], acc=True)
            R = rt
            P.op("act", lambda e: e.activation(out=R["sc"][:], in_=pb[4][:, 0:NE], func=AF.Sigmoid),
                 reads=[pb_tb[4]], writes=[rt_tb])
            dv = lambda fn: P.op("dve", fn, reads=[rt_tb, rw_tb], writes=[rt_tb])
            g3 = lambda t: t[:].rearrange("p (g j) -> p g j", j=8)
            dv(lambda e: e.tensor_tensor(out=R["sel"][:], in0=R["sc"][:], in1=rbias[:], op=ALU.add))
            dv(lambda e: e.tensor_reduce(out=R["m1"][:], in_=g3(R["sel"]), axis=AX.X, op=ALU.max))
            dv(lambda e: e.tensor_tensor(out=g3(R["eq"]), in0=g3(R["sel"]), in1=bcast_last(R["m1"][:], 8), op=ALU.is_equal))
            dv(lambda e: e.scalar_tensor_tensor(out=R["sel2"][:], in0=R["eq"][:], scalar=-4.0, in1=R["sel"][:],
                                                op0=ALU.mult, op1=ALU.add))
            dv(lambda e: e.tensor_reduce(out=R["m2"][:], in_=g3(R["sel2"]), axis=AX.X, op=ALU.max))
            dv(lambda e: e.tensor_tensor(out=R["grp"][:], in0=R["m1"][:], in1=R["m2"][:], op=ALU.add))
            dv(lambda e: e.max(out=R["t8"][:], in_=R["grp"][:]))
            dv(lambda e: e.tensor_scalar(out=R["gm"][:], in0=R["grp"][:], scalar1=R["t8"][:, 3:4], scalar2=None, op0=ALU.is_ge))
            dv(lambda e: e.tensor_scalar(out=R["g4"][:], in0=R["gm"][:], scalar1=4.0, scalar2=-4.0, op0=ALU.mult, op1=ALU.add))
            dv(lambda e: e.tensor_tensor(out=g3(R["selm"]), in0=g3(R["sel"]), in1=bcast_last(R["gm"][:], 8), op=ALU.mult))
            dv(lambda e: e.tensor_tensor(out=g3(R["selm"]), in0=g3(R["selm"]), in1=bcast_last(R["g4"][:], 8), op=ALU.add))
            dv(lambda e: e.max(out=R["t8"][:], in_=R["selm"][:]))
            dv(lambda e: e.tensor_scalar(out=R["em"][:], in0=R["selm"][:], scalar1=R["t8"][:, 7:8], scalar2=None, op0=ALU.is_ge))
            dv(lambda e: e.tensor_tensor(out=R["gt"][:], in0=R["sc"][:], in1=R["em"][:], op=ALU.mult))
            dv(lambda e: e.tensor_reduce(out=R["den"][:], in_=R["gt"][:], axis=AX.X, op=ALU.add))
            dv(lambda e: e.reciprocal(out=R["den"][:], in_=R["den"][:]))
            dv(lambda e: e.tensor_scalar(out=R["gate"][:], in0=R["gt"][:], scalar1=R["den"][:, 0:1], scalar2=2.5,
                                         op0=ALU.mult, op1=ALU.mult))
            P.op("pe", lambda e: e.transpose(out=pb[5][0:64, 0:128], in_=R["gate"][:], identity=ident[:]),
                 reads=[rt_tb, ident_tb], writes=[pb_tb[5]])
            P.op("act", lambda e, b=b: e.activation(out=gateT[0:64, b * 128:(b + 1) * 128], in_=pb[5][0:64, 0:128],
                                                    func=AF.Identity), reads=[pb_tb[5]], writes=[gT_tb[b]])
        _sc1.__exit__(None, None, None)
        _sc2 = P.scope()
        _sc2.__enter__()
        wb = [{"w1": P.sbuf(f"p{ps}_w1b{i}", [128, 8, DE], BF16), "w3": P.sbuf(f"p{ps}_w3b{i}", [128, 8, DE], BF16),
               "w2": P.sbuf(f"p{ps}_w2b{i}", [128, 2, D], BF16)} for i in range(2)]
        selt = P.sbuf(f"p{ps}_selt", [65, 128], BF16)
        Gb = P.sbuf(f"p{ps}_Gb", [128, PT], BF16)
        ssb = [P.sbuf(f"p{ps}_ssb{i}", [128, TW], F32) for i in range(2)]
        ggT = P.sbuf(f"p{ps}_ggT", [128, 2, PT], BF16)

        for xi_, eid in enumerate(exp_ids):
            par = xi_ % 2
            W = wb[par]
            Wt = wb_tb[par]
            P.op("sp", lambda e, xi_=xi_: e.dma_start(out=stg[0][:].rearrange("p (kc f) -> p kc f", f=DE),
                                                     in_=dr["ew1"][xi_].rearrange("(kc p) f -> p kc f", p=128)),
                 writes=[stg_tb[0]], dma=True)
            P.op("pool", lambda e, W=W: e.tensor_copy(out=W["w1"][:].rearrange("p kc f -> p (kc f)"), in_=stg[0][:]),
                 reads=[stg_tb[0]], writes=[Wt["w1"]])
            P.op("sp", lambda e, xi_=xi_: e.dma_start(out=stg[1][:].rearrange("p (kc f) -> p kc f", f=DE),
                                                     in_=dr["ew3"][xi_].rearrange("(kc p) f -> p kc f", p=128)),
                 writes=[stg_tb[1]], dma=True)
            P.op("pool", lambda e, W=W: e.tensor_copy(out=W["w3"][:].rearrange("p kc f -> p (kc f)"), in_=stg[1][:]),
                 reads=[stg_tb[1]], writes=[Wt["w3"]])
            P.op("sp", lambda e, xi_=xi_: e.dma_start(out=stg[2][:].rearrange("p (fc d) -> p fc d", d=D),
                                                     in_=dr["ew2"][xi_].rearrange("(fc p) d -> p fc d", p=128)),
                 writes=[stg_tb[2]], dma=True)
            P.op("pool", lambda e, W=W: e.tensor_copy(out=W["w2"][:].rearrange("p fc d -> p (fc d)"), in_=stg[2][:]),
                 reads=[stg_tb[2]], writes=[Wt["w2"]])
            P.op("pool", lambda e, eid=eid: e.tensor_copy(out=selt[:], in_=ident[0:65, eid:eid + 1].to_broadcast([65, 128])),
                 reads=[ident_tb], writes=[selt_tb])
            for tt in range(NTT):
                P.op("pe", lambda e, tt=tt: e.matmul(pb[6][:, 0:TW], lhsT=selt[:], rhs=gateT[:, tt * TW:(tt + 1) * TW],
                                                     start=True, stop=True),
                     reads=[selt_tb] + gT_tb, writes=[pb_tb[6]])
                P.op("act", lambda e, tt=tt: e.activation(out=Gb[:, tt * TW:(tt + 1) * TW], in_=pb[6][:, 0:TW], func=AF.Identity),
                     reads=[pb_tb[6]], writes=[Gb_tb])
            for tt in range(NTT):
                for fc in range(2):
                    i2 = (tt * 2 + fc) % 2
                    b1, b3 = i2, 2 + i2
                    h_tbs = h2_tb[tt * BPT:(tt + 1) * BPT]
                    for kc in range(8):
                        P.op("pe", lambda e, kc=kc, fc=fc, tt=tt, b1=b1, W=W: e.matmul(
                            pb[b1][:, 0:TW], lhsT=W["w1"][:, kc, fc * 128:(fc + 1) * 128], rhs=h2T[:, kc, tt * TW:(tt + 1) * TW],
                            start=(kc == 0), stop=(kc == 7)), reads=[Wt["w1"]] + h_tbs, writes=[pb_tb[b1]], acc=True)
                    for kc in range(8):
                        P.op("pe", lambda e, kc=kc, fc=fc, tt=tt, b3=b3, W=W: e.matmul(
                            pb[b3][:, 0:TW], lhsT=W["w3"][:, kc, fc * 128:(fc + 1) * 128], rhs=h2T[:, kc, tt * TW:(tt + 1) * TW],
                            start=(kc == 0), stop=(kc == 7)), reads=[Wt["w3"]] + h_tbs, writes=[pb_tb[b3]], acc=True)
                    P.op("act", lambda e, i2=i2, b1=b1: e.activation(out=ssb[i2][:], in_=pb[b1][:, 0:TW], func=AF.Silu),
                         reads=[pb_tb[b1]], writes=[ssb_tb[i2]])
                    P.op("dve", lambda e, i2=i2, b3=b3: e.tensor_tensor(out=ssb[i2][:], in0=ssb[i2][:], in1=pb[b3][:, 0:TW], op=ALU.mult),
                         reads=[ssb_tb[i2], pb_tb[b3]], writes=[ssb_tb[i2]])
                    P.op("dve", lambda e, i2=i2, fc=fc, tt=tt: e.tensor_tensor(
                        out=ggT[:, fc, tt * TW:(tt + 1) * TW], in0=ssb[i2][:], in1=Gb[:, tt * TW:(tt + 1) * TW], op=ALU.mult),
                        reads=[ssb_tb[i2], Gb_tb], writes=[gg_tb[fc][tt]])
            for b in range(NB):
                tt = b // BPT
                for dh in range(2):
                    bk = 4 + (b * 2 + dh) % 2
                    for fc in range(2):
                        P.op("pe", lambda e, b=b, dh=dh, fc=fc, bk=bk, W=W: e.matmul(
                            pb[bk][:], lhsT=ggT[:, fc, b * 128:(b + 1) * 128], rhs=W["w2"][:, fc, dh * 512:(dh + 1) * 512],
                            start=(fc == 0), stop=(fc == 1)), reads=[gg_tb[fc][tt], Wt["w2"]], writes=[pb_tb[bk]], acc=True)
                    if xi_ == 0:
                        P.op("dve", lambda e, b=b, dh=dh, bk=bk: e.tensor_copy(out=yacc[:, b, dh * 512:(dh + 1) * 512], in_=pb[bk][:]),
                             reads=[pb_tb[bk]], writes=[ya_tb[b]])
                    else:
                        P.op("dve", lambda e, b=b, dh=dh, bk=bk: e.tensor_tensor(
                            out=yacc[:, b, dh * 512:(dh + 1) * 512], in0=yacc[:, b, dh * 512:(dh + 1) * 512], in1=pb[bk][:], op=ALU.add),
                            reads=[pb_tb[bk], ya_tb[b]], writes=[ya_tb[b]])
        _sc2.__exit__(None, None, None)
        load_bc(0, dr["modb"][:, 5 * D:6 * D])
        load_bc(1, dr["lnp"][:, 2, :])
        load_bc(2, dr["lnp"][:, 3, :])
        for b in range(NB):
            tk = t0 + b * 128
            xi = b % 2
            P.op("dve", lambda e, b=b: e.tensor_tensor(out=zt[:], in0=yacc[:, b, :], in1=bc[0][:], op=ALU.mult),
                 reads=[ya_tb[b], bc_tb[0]], writes=[zt_tb])
            P.op("dve", lambda e, b=b: e.scalar_tensor_tensor(out=zt[:], in0=xm[:, b, :], scalar=ALPHA, in1=zt[:],
                                                              op0=ALU.mult, op1=ALU.add),
                 reads=[xm_tb[b], zt_tb], writes=[zt_tb])
            emit_layernorm(P, zt[:], zt_tb, xr[xi][:], xr_tb[xi], bc[1][:], bc[2][:], [bc_tb[1], bc_tb[2]], lnsc, eps_t, "f")
            P.op("sp", lambda e, xi=xi, tk=tk: e.dma_start(out=dr["out"][tk:tk + 128, :], in_=xr[xi][:]),
                 reads=[xr_tb[xi]], writes=[dr["out_tb"]], dma=True)

    for ps in range(npass):
        _pass(ps)


def emit_k0(P, nc, dr):
    cT = P.sbuf("cT", [128, 8], F32)
    c_tb = P.tb("cT")
    P.op("sp", lambda e: e.dma_start(out=cT[:], in_=dr["cT"]), writes=[c_tb], dma=True)
    P.op("act", lambda e: e.activation(out=cT[:], in_=cT[:], func=AF.Silu), reads=[c_tb], writes=[c_tb])
    wm = [P.sbuf(f"wm{i}", [128, 8, 768], F32) for i in range(2)]
    wm_tb = [P.tb(), P.tb()]
    bm = P.sbuf("bm", [1, 4, 768], F32)
    P.op("sp", lambda e: e.dma_start(out=bm[:], in_=dr["b_mod"].unsqueeze(0)), writes=[c_tb], dma=True)
    ps = [P.psum(f"k0ps{i}", [128, 512], F32) for i in range(2)]
    ps_tb = [P.tb(), P.tb()]
    ot = P.sbuf("k0o", [1, 4, 768], F32)
    o_tb = P.tb("k0o")
    for l in range(4):
        W, wtb = wm[l % 2], wm_tb[l % 2]
        P.op("sp", lambda e, l=l, W=W: e.dma_start(out=W[:], in_=dr["w_mod"][l].rearrange("(kc p) n -> p kc n", p=128)), writes=[wtb], dma=True)
        for hf in range(2):
            for kc in range(8):
                P.op("pe", lambda e, kc=kc, hf=hf, W=W: e.matmul(ps[hf][0:1, 0:384], lhsT=cT[:, kc:kc + 1], rhs=W[:, kc, hf * 384:(hf + 1) * 384],
                                                                 start=(kc == 0), stop=(kc == 7)), reads=[c_tb, wtb], writes=[ps_tb[hf]], acc=True)
            P.op("dve", lambda e, l=l, hf=hf: e.tensor_tensor(out=ot[0:1, l, hf * 384:(hf + 1) * 384], in0=ps[hf][0:1, 0:384],
                                                              in1=bm[0:1, l, hf * 384:(hf + 1) * 384], op=ALU.add), reads=[ps_tb[hf], c_tb], writes=[o_tb])
    P.op("sp", lambda e: e.dma_start(out=dr["mod"].unsqueeze(0), in_=ot[:]), reads=[o_tb], writes=[dr["out_tb"]], dma=True)


D = 1024
def consts128():
    i = np.arange(128)
    ident = np.eye(128, dtype=np.float32)
    bo = (i[:, None] // 64 == i[None, :] // 64).astype(np.float32)
    ls = (i[None, :] < i[:, None]).astype(np.float32)
    us = (i[None, :] > i[:, None]).astype(np.float32)
    ui = (i[None, :] >= i[:, None]).astype(np.float32)
    return np.ascontiguousarray(np.stack([ident, bo, ls, us, ui], 1))

def k1_common(inp, l, mod):
    col = lambda v: np.ascontiguousarray(v.reshape(-1, 128).T)
    par = np.zeros((128, 64), np.float32)
    par[:, 0:11] = col(inp['rwkv_mu'][l])
    par[:, 11:14] = col(inp['rwkv_w0'][l]); par[:, 14:17] = col(inp['rwkv_a0'][l])
    par[:, 17:20] = col(inp['rwkv_k_k'][l]); par[:, 20:23] = col(inp['rwkv_k_a'][l])
    par[:, 26:29] = col(inp['rwkv_r_k'][l].reshape(-1))
    par[:, 29:37] = col(mod[0:D]); par[:, 37:45] = col(mod[D:2 * D])
    bcp = np.zeros((128, 320), np.float32)
    bcp[:, 0:128] = inp['dsa_kv_norm'][l][None]; bcp[:, 128:192] = inp['dsa_ik_g'][l][None]; bcp[:, 192:256] = inp['dsa_ik_b'][l][None]
    lora = np.ascontiguousarray(np.concatenate([inp['rwkv_w2'][l], inp['rwkv_a2'][l]], 0))
    wuk = np.ascontiguousarray(inp['dsa_w_uk'][l].reshape(2, 128, 128).transpose(1, 0, 2))
    return {"par": par, "bcp": bcp, "lora": lora, "g2w": np.ascontiguousarray(inp['rwkv_g2'][l]), "wuk": wuk,
            "cst": consts128(), "w_in": np.ascontiguousarray(inp['w_in'][l])}


def alibi_slopes(n):
    return (2.0 ** (-8.0 * (np.arange(n, dtype=np.float32) + 1.0) / n)).astype(np.float32)

K1_CAT = {"o_bon": 1, "o_g": 1, "o_qlat": 2, "o_iqs": 2, "o_sgn": 0, "o_ckv": 0, "o_ckvT": 1, "o_ikT": 1, "o_sq": 1, "o_sk": 1, "o_sv": 0,
          "o_YwT": 0, "o_Y0T": 0}

def k2_tables(i):
    sl = alibi_slopes(10)
    swa_sl, dsa_sl = sl[:6], sl[6:]
    p = np.arange(128)
    r = np.arange(8)[None, :, None]; pq = p[:, None, None]; pk = p[None, None, :]
    valid = (r < i) | ((r == i) & (pk <= pq))
    dmask = np.where(valid, 0.0, -1e30).astype(np.float32).reshape(128, 1024)
    dl = np.arange(128)[None, None, :]
    ab = (dsa_sl[None, :, None] * (128.0 * (7 - dl - i) + p[:, None, None] - 127.0)).astype(np.float32)
    q = p[None, None, None, :]; pk4 = p[:, None, None, None]; tile = np.arange(2)[None, :, None, None]
    dist = q - pk4 + np.where(tile == 0, 128, 0)
    ok = (dist >= 0) & (dist < 128)
    swb = np.where(ok, -swa_sl[None, None, :, None] * dist, -30000.0).astype(np.float32)
    swb_first = swb[:, 0].copy()
    if i == 0:
        swb_first[:] = -30000.0
    return {"dmask": dmask, "ab": np.ascontiguousarray(ab), "swb": np.ascontiguousarray(swb), "swb_first": np.ascontiguousarray(swb_first),
            "ident": np.eye(128, dtype=np.float32)}


def k2_inputs(K1, inp, l, NBQ):
    G = {n: np.concatenate([K1[c][n] for c in range(8)], ax) for n, ax in K1_CAT.items()}
    PT = np.stack([K1[c]["o_PT"] for c in range(8)]); Qa = np.stack([K1[c]["o_Q"] for c in range(8)])
    T = 8 * NBQ * 128
    bf = G["o_sk"].dtype
    maps = []
    for i in range(8):
        gbs = 8 * np.arange(NBQ) + i
        tok = (gbs[:, None] * 128 + np.arange(128)[None, :]).reshape(-1)
        prev = ((gbs - 1)[:, None] * 128 + np.arange(128)[None, :])
        own = (gbs[:, None] * 128 + np.arange(128)[None, :])
        m = {}
        m["YwT"] = np.ascontiguousarray(G["o_YwT"][gbs]); m["Y0T"] = np.ascontiguousarray(G["o_Y0T"][gbs])
        m["PbT"] = np.ascontiguousarray(PT[gbs // NBQ, gbs % NBQ]); m["Qb"] = np.ascontiguousarray(Qa[gbs // NBQ, gbs % NBQ])
        m["segPT"] = np.ascontiguousarray(PT[:, NBQ]); m["segQ"] = np.ascontiguousarray(Qa[:, NBQ])
        hm = lambda a: np.ascontiguousarray(a.reshape(6, 64, T)[:, :, tok].transpose(1, 0, 2))
        m["bon"] = hm(G["o_bon"]); m["g"] = hm(G["o_g"]); m["sq"] = hm(G["o_sq"])
        m["qlat"] = np.ascontiguousarray(G["o_qlat"][:, :, tok]); m["iqs"] = np.ascontiguousarray(G["o_iqs"][:, :, tok]); m["sgn"] = np.ascontiguousarray(G["o_sgn"][tok])
        sk = G["o_sk"].reshape(2, 64, T); sv = G["o_sv"]
        sk2 = np.zeros((64, 2, NBQ, 256), bf); sv2 = np.zeros((NBQ, 2, 128, 128), bf)
        for j in range(NBQ):
            if gbs[j] > 0:
                sk2[:, :, j, 0:128] = sk[:, :, prev[j]].transpose(1, 0, 2); sv2[j, 0] = sv[prev[j]]
            sk2[:, :, j, 128:256] = sk[:, :, own[j]].transpose(1, 0, 2); sv2[j, 1] = sv[own[j]]
        m["sk2"] = sk2; m["sv2"] = sv2
        m["ckv"] = G["o_ckv"]; m["ckvT"] = G["o_ckvT"]; m["ikT"] = G["o_ikT"]
        m["wuv"] = np.ascontiguousarray(inp['dsa_w_uv'][l])
        m["sink_b"] = np.ascontiguousarray(np.broadcast_to(inp['swa_sinks'][l][None], (64, 6)))
        rl = np.zeros((64, 16), np.float32)
        rl[:, 0:6] = inp['rwkv_ln_g'][l].reshape(6, 64).T; rl[:, 6:12] = inp['rwkv_ln_b'][l].reshape(6, 64).T
        m["rwkv_ln"] = rl
        m.update(k2_tables(i))
        maps.append(m)
    return maps


NPBF = ml_dtypes.bfloat16
NBQ_FULL = 16
_PROGS = {}


def _k1_spec(NBLK):
    NTk = NBLK * 128
    return {"o_YwT": ([NBLK, 6, 64, 128], F32), "o_Y0T": ([NBLK, 6, 64, 128], F32), "o_PT": ([NBLK + 1, 6, 64, 64], F32), "o_Q": ([NBLK + 1, 6, 64, 64], F32),
            "o_bon": ([384, NTk], F32), "o_g": ([384, NTk], F32), "o_qlat": ([128, 4, NTk], BF16), "o_iqs": ([64, 4, NTk], BF16), "o_sgn": ([NTk, 4], F32),
            "o_ckv": ([NTk, 128], BF16), "o_ckvT": ([128, NTk], BF16), "o_ikT": ([64, NTk], BF16), "o_sq": ([384, NTk], BF16), "o_sk": ([128, NTk], BF16),
            "o_sv": ([NTk, 128], BF16)}


def _build_k0():
    nc = bass.Bass("TRN2", target_bir_lowering=False)
    di = lambda n, s: nc.dram_tensor(n, list(s), F32, kind="ExternalInput").ap()
    dr = {"cT": di("cT", [128, 8]), "w_mod": di("w_mod", [4, 1024, 768]), "b_mod": di("b_mod", [4, 768])}
    dr["mod"] = nc.dram_tensor("mod", [4, 768], F32, kind="ExternalOutput").ap()
    with ExitStack() as es:
        P = Prog(nc, es)
        dr["out_tb"] = P.tb("out")
        emit_k0(P, nc, dr)
        P.finish([dr["out_tb"]])
    return nc


def _build_k1(NBLK):
    nc = bass.Bass("TRN2", target_bir_lowering=False)
    NTk = NBLK * 128
    di = lambda n, s: nc.dram_tensor(n, list(s), F32, kind="ExternalInput").ap()
    dr = {"x": di("x", [NTk, D]), "xh": di("xh", [1, D]), "par": di("par", [128, 64]), "bcp": di("bcp", [128, 320]), "lora": di("lora", [128, 384]),
          "g2w": di("g2w", [128, 384]), "wuk": di("wuk", [128, 2, 128]), "cst": di("cst", [128, 5, 128]), "w_in": di("w_in", [D, 2756])}
    for n, (s, dt) in _k1_spec(NBLK).items():
        dr[n] = nc.dram_tensor(n, s, dt, kind="ExternalOutput").ap()
    with ExitStack() as es:
        P = Prog(nc, es)
        dr["out_tb"] = P.tb("out")
        emit_k1(P, nc, NBLK, dr)
        P.finish([dr["out_tb"]])
    return nc


def _k2_in(NBQ):
    return {"YwT": ([NBQ, 6, 64, 128], F32), "Y0T": ([NBQ, 6, 64, 128], F32), "PbT": ([NBQ, 6, 64, 64], F32), "Qb": ([NBQ, 6, 64, 64], F32),
            "segPT": ([8, 6, 64, 64], F32), "segQ": ([8, 6, 64, 64], F32), "bon": ([64, 6, NBQ * 128], F32), "g": ([64, 6, NBQ * 128], F32),
            "sq": ([64, 6, NBQ * 128], BF16), "qlat": ([128, 4, NBQ * 128], BF16), "iqs": ([64, 4, NBQ * 128], BF16), "sgn": ([NBQ * 128, 4], F32),
            "sk2": ([64, 2, NBQ, 256], BF16), "sv2": ([NBQ, 2, 128, 128], BF16), "ckv": ([8 * NBQ * 128, 128], BF16), "ckvT": ([128, 8 * NBQ * 128], BF16),
            "ikT": ([64, 8 * NBQ * 128], BF16), "wuv": ([4, 128, 64], F32), "sink_b": ([64, 6], F32), "rwkv_ln": ([64, 16], F32), "dmask": ([128, 1024], F32),
            "ab": ([128, 4, 128], F32), "swb": ([128, 2, 6, 128], F32), "swb_first": ([128, 6, 128], F32), "ident": ([128, 128], F32)}


def _build_k2(NBQ):
    nc = bass.Bass("TRN2", target_bir_lowering=False)
    dr = {n: nc.dram_tensor(n, s, dt, kind="ExternalInput").ap() for n, (s, dt) in _k2_in(NBQ).items()}
    dr["mixT"] = nc.dram_tensor("mixT", [16, 64, NBQ * 128], BF16, kind="ExternalOutput").ap()
    with ExitStack() as es:
        P = Prog(nc, es)
        dr["out_tb"] = P.tb("out")
        emit_k2(P, nc, NBQ, dr)
        P.finish([dr["out_tb"]])
    return nc


def _build_k3(NT, exp_ids):
    nc = bass.Bass("TRN2", target_bir_lowering=False)
    di = lambda n, s: nc.dram_tensor(n, list(s), F32, kind="ExternalInput").ap()
    NX = len(exp_ids)
    dr = {"xres": di("xres", [NT, D]), "mixT": nc.dram_tensor("mixT", [16, 64, NT], BF16, kind="ExternalInput").ap(),
          "modb": di("modb", [128, 6 * D]), "modT": di("modT", [128, 48]), "lnp": di("lnp", [128, 4, D]), "w_out": di("w_out", [D, D]),
          "router_w": di("router_w", [D, NE]), "rbias_b": di("rbias_b", [128, NE]), "ew1": di("ew1", [NX, D, DE]), "ew3": di("ew3", [NX, D, DE]),
          "ew2": di("ew2", [NX, DE, D]), "ident": di("ident", [128, 128])}
    dr["out"] = nc.dram_tensor("out", [NT, D], F32, kind="ExternalOutput").ap()
    with ExitStack() as es:
        P = Prog(nc, es)
        dr["out_tb"] = P.tb("out")
        ident = P.sbuf("ident_sb", [128, 128], F32)
        ident_tb = P.tb("ident")
        P.op("sp", lambda e: e.dma_start(out=ident[:], in_=dr["ident"]), writes=[ident_tb], dma=True)
        emit_ffn(P, nc, NT, exp_ids, dr, ident, ident_tb)
        P.finish([dr["out_tb"]])
    return nc


def _prog(key, fn):
    if key not in _PROGS:
        _PROGS[key] = fn()
    return _PROGS[key]


def _run(nc, maps):
    res = run_bass_kernel_spmd(nc, maps, core_ids=list(range(8)))
    return [{k: np.asarray(v) for k, v in r.items()} for r in res.results]


def _forward(inp, NBQ=NBQ_FULL, n_layers=4, exp_ids=None):
    NT = NBQ * 128
    T = 8 * NT
    if exp_ids is None:
        exp_ids = list(range(NE)) + [NE]
    inp = {k: np.asarray(v) for k, v in inp.items()}
    cT = np.ascontiguousarray(inp['c'][0].reshape(8, 128).T)
    r0 = _run(_prog("k0", _build_k0), [{"cT": cT, "w_mod": np.ascontiguousarray(inp['w_mod'][:, :, i * 768:(i + 1) * 768]),
                                         "b_mod": np.ascontiguousarray(inp['b_mod'][:, i * 768:(i + 1) * 768])} for i in range(8)])
    mod = np.concatenate([r0[i]["mod"] for i in range(8)], 1)
    x = np.ascontiguousarray(inp['x'][0][:T])
    ident = np.eye(128, dtype=np.float32)
    toks = [((8 * np.arange(NBQ) + i)[:, None] * 128 + np.arange(128)[None, :]).reshape(-1) for i in range(8)]
    for l in range(n_layers):
        common = k1_common(inp, l, mod[l])
        maps = []
        for c in range(8):
            m = dict(common)
            m["par"] = common["par"].copy()
            m["par"][:, 45] = 0.0 if c == 0 else 1.0
            m["x"] = np.ascontiguousarray(x[c * NT:(c + 1) * NT])
            m["xh"] = np.ascontiguousarray(x[c * NT - 1:c * NT]) if c > 0 else np.zeros((1, D), np.float32)
            maps.append(m)
        K1 = _run(_prog(("k1", NBQ), lambda: _build_k1(NBQ)), maps)
        K2 = _run(_prog(("k2", NBQ), lambda: _build_k2(NBQ)), k2_inputs(K1, inp, l, NBQ))
        del K1
        sel = [e for e in exp_ids if e < NE]
        ew1 = np.concatenate([inp['exp_w1'][l][sel], inp['sh_w1'][l][None]], 0)
        ew3 = np.concatenate([inp['exp_w3'][l][sel], inp['sh_w3'][l][None]], 0)
        ew2 = np.concatenate([inp['exp_w2'][l][sel], inp['sh_w2'][l][None]], 0)
        lnp = np.stack([inp['ln_mix_g'][l], inp['ln_mix_b'][l], inp['ln_ffn_g'][l], inp['ln_ffn_b'][l]])
        common3 = {"modb": np.ascontiguousarray(np.broadcast_to(mod[l][None], (128, 6 * D))), "modT": np.ascontiguousarray(mod[l].reshape(48, 128).T),
                   "lnp": np.ascontiguousarray(np.broadcast_to(lnp[None], (128, 4, D))), "w_out": np.ascontiguousarray(inp['w_out'][l]),
                   "router_w": np.ascontiguousarray(inp['router_w'][l]),
                   "rbias_b": np.ascontiguousarray(np.broadcast_to(inp['router_bias'][l][None], (128, NE))), "ew1": ew1, "ew3": ew3, "ew2": ew2, "ident": ident}
        maps3 = [{**common3, "xres": np.ascontiguousarray(x[toks[i]]), "mixT": K2[i]["mixT"]} for i in range(8)]
        K3 = _run(_prog(("k3", NT, tuple(exp_ids)), lambda: _build_k3(NT, exp_ids)), maps3)
        del maps3, ew1, ew3, ew2
        xn = np.empty_like(x)
        for i in range(8):
            xn[toks[i]] = K3[i]["out"]
        x = xn
    return x


def kernel(**inputs):
    x = _forward(inputs)
    return np.ascontiguousarray(x[None].astype(np.float32))
```
